# Optimizing a Trainium2 kernel written in Bass

```python
import math
import jax
import jax.numpy as jnp
from jax import lax
import numpy as np


D_MODEL = 1024
BATCH = 8
SEQ = 4096
DEPTH = 2

HEAD_DIM = 64
D_MIX = D_MODEL
D_SSM = D_MIX // 4
D_ATTN = D_MIX // 2
D_CONV = D_MIX - D_SSM - D_ATTN
SSM_GROUP = 16
N_SSM_GROUPS = D_SSM // SSM_GROUP
SSM_STATE = 64
DT_MIN = 1e-3
DT_MAX = 1e-1
N_ATTN_HEADS = D_ATTN // HEAD_DIM
Q_BLOCK = 128
CONV_WIDTH = 3
N_CONV_HEADS = D_CONV // HEAD_DIM
N_OUT_HEADS = D_MIX // HEAD_DIM
D_IN = D_SSM + 3 * D_ATTN + N_ATTN_HEADS + 3 * D_CONV
N_EXPERTS = 16
N_GROUPS = 4
EXPERTS_PER_GROUP = N_EXPERTS // N_GROUPS
TOP_K = 2
D_EXPERT = D_MODEL // 2
EPS = 1e-6

kernel_name = "hymba_s5_fox_shortconv_grouped_moe"


def rms_norm(x, g):
    xf = x.astype(jnp.float32)
    y = xf * lax.rsqrt(jnp.mean(xf * xf, axis=-1, keepdims=True) + EPS)
    return (y * g.astype(jnp.float32)).astype(x.dtype)


def modulate(h, shift, scale):
    return h * (1 + scale[:, None, :]) + shift[:, None, :]


def s5_mixer(u, lam_re, lam_im, log_dt, b_re, b_im, c_re, c_im, d_skip, glu_w, glu_b):
    f32 = jnp.float32
    bsz, seq, _ = u.shape
    uf = u.astype(f32).reshape(bsz, seq, N_SSM_GROUPS, SSM_GROUP)
    lr = jnp.minimum(lam_re.astype(f32), -1e-4)
    li = lam_im.astype(f32)
    dt = jnp.exp(log_dt.astype(f32))[:, None]
    mag = jnp.exp(lr * dt)
    ab_re = mag * jnp.cos(li * dt)
    ab_im = mag * jnp.sin(li * dt)
    den = lr * lr + li * li
    num_re = ab_re - 1.0
    num_im = ab_im
    z_re = (num_re * lr + num_im * li) / den
    z_im = (num_im * lr - num_re * li) / den
    br = b_re.astype(f32)
    bi = b_im.astype(f32)
    bb_re = z_re[..., None] * br - z_im[..., None] * bi
    bb_im = z_re[..., None] * bi + z_im[..., None] * br
    bu_re = jnp.einsum('gph,bsgh->bsgp', bb_re, uf)
    bu_im = jnp.einsum('gph,bsgh->bsgp', bb_im, uf)
    a_re = jnp.broadcast_to(ab_re, bu_re.shape)
    a_im = jnp.broadcast_to(ab_im, bu_im.shape)

    def combine(e1, e2):
        a1r, a1i, b1r, b1i = e1
        a2r, a2i, b2r, b2i = e2
        return (a2r * a1r - a2i * a1i,
                a2r * a1i + a2i * a1r,
                a2r * b1r - a2i * b1i + b2r,
                a2r * b1i + a2i * b1r + b2i)

    _, _, x_re, x_im = lax.associative_scan(combine, (a_re, a_im, bu_re, bu_im), axis=1)
    y = (jnp.einsum('ghp,bsgp->bsgh', c_re.astype(f32), x_re)
         - jnp.einsum('ghp,bsgp->bsgh', c_im.astype(f32), x_im))
    y = y.reshape(bsz, seq, D_SSM) + d_skip.astype(f32) * uf.reshape(bsz, seq, D_SSM)
    y = jax.nn.gelu(y)
    y = y * jax.nn.sigmoid(y @ glu_w.astype(f32) + glu_b.astype(f32))
    return y.astype(u.dtype)


def fox_attention(q, k, v, f_logit, q_g, k_g):
    f32 = jnp.float32
    bsz, seq, n_heads, hd = q.shape
    q = rms_norm(q, q_g)
    k = rms_norm(k, k_g)
    log_f = jax.nn.log_sigmoid(f_logit.astype(f32))
    cum = jnp.cumsum(log_f, axis=1).transpose(0, 2, 1)
    n_blocks = seq // Q_BLOCK
    qb = q.reshape(bsz, n_blocks, Q_BLOCK, n_heads, hd).transpose(1, 0, 3, 2, 4)
    cqb = cum.reshape(bsz, n_heads, n_blocks, Q_BLOCK).transpose(2, 0, 1, 3)
    key_pos = jnp.arange(seq)
    scale = HEAD_DIM ** -0.5

    def block(args):
        q_blk, cq_blk, blk = args
        logits = jnp.einsum('bhqd,bkhd->bhqk', q_blk, k, preferred_element_type=f32) * scale
        logits = logits + cq_blk[..., None] - cum[:, :, None, :]
        q_pos = blk * Q_BLOCK + jnp.arange(Q_BLOCK)
        mask = key_pos[None, :] <= q_pos[:, None]
        logits = jnp.where(mask[None, None], logits, -jnp.inf)
        p = jax.nn.softmax(logits, axis=-1)
        return jnp.einsum('bhqk,bkhd->bqhd', p.astype(v.dtype), v)

    out = lax.map(block, (qb, cqb, jnp.arange(n_blocks)))
    return out.transpose(1, 0, 2, 3, 4).reshape(bsz, seq, n_heads * hd)


def short_conv(h, b_gate, c_gate, conv_w):
    z = c_gate * h
    zp = jnp.pad(z, ((0, 0), (CONV_WIDTH - 1, 0), (0, 0)))
    y = lax.conv_general_dilated(zp, conv_w[:, None, :].astype(z.dtype), window_strides=(1,),
                                 padding='VALID', dimension_numbers=('NWC', 'WIO', 'NWC'),
                                 feature_group_count=D_CONV)
    return b_gate * y


def mixer_layer(h, w_in, forget_b, lam_re, lam_im, log_dt, b_re, b_im, c_re, c_im, d_skip,
                glu_w, glu_b, q_g, k_g, conv_w, out_norm_g, w_out):
    bsz, seq, _ = h.shape
    proj = h @ w_in
    sizes = (D_SSM, D_ATTN, D_ATTN, D_ATTN, N_ATTN_HEADS, D_CONV, D_CONV, D_CONV)
    offs = [int(o) for o in np.cumsum(sizes)[:-1]]
    u, q, k, v, f_logit, hc, bg, cg = jnp.split(proj, offs, axis=-1)
    y_ssm = s5_mixer(u, lam_re, lam_im, log_dt, b_re, b_im, c_re, c_im, d_skip, glu_w, glu_b)
    heads = lambda t: t.reshape(bsz, seq, N_ATTN_HEADS, HEAD_DIM)
    y_attn = fox_attention(heads(q), heads(k), heads(v), f_logit + forget_b, q_g, k_g)
    y_conv = short_conv(hc, bg, cg, conv_w)
    y = jnp.concatenate([y_ssm, y_attn.astype(h.dtype), y_conv], axis=-1)
    y = rms_norm(y.reshape(bsz, seq, N_OUT_HEADS, HEAD_DIM),
                 out_norm_g.reshape(N_OUT_HEADS, HEAD_DIM)).reshape(bsz, seq, D_MIX)
    return y @ w_out


def moe_layer(h, w_router, router_bias, w_gate, w_up, w_down):
    f32 = jnp.float32
    bsz, seq, d = h.shape
    t = h.reshape(-1, d)
    affinity = jax.nn.sigmoid(jnp.dot(t, w_router, preferred_element_type=f32))
    sel = affinity + router_bias.astype(f32)
    group_score = lax.top_k(sel.reshape(-1, N_GROUPS, EXPERTS_PER_GROUP), TOP_K)[0].sum(-1)
    best_group = jnp.argmax(group_score, axis=-1)
    in_group = (jnp.arange(N_EXPERTS) // EXPERTS_PER_GROUP)[None, :] == best_group[:, None]
    _, idx = lax.top_k(jnp.where(in_group, sel, -jnp.inf), TOP_K)
    wts = jnp.take_along_axis(affinity, idx, axis=-1)
    wts = wts / jnp.sum(wts, axis=-1, keepdims=True)
    combine = jnp.sum(jax.nn.one_hot(idx, N_EXPERTS, dtype=f32) * wts[..., None], axis=1)
    out = jnp.zeros(t.shape, f32)
    for e in range(N_EXPERTS):
        hid = jax.nn.silu(t @ w_gate[e]) * (t @ w_up[e])
        out = out + combine[:, e:e + 1] * (hid @ w_down[e])
    return out.reshape(bsz, seq, d).astype(h.dtype)


def setup_inputs(seed: int = 0) -> dict:
    key = jax.random.key(seed)
    ks = jax.random.split(key, 32)
    f32 = jnp.float32

    def nrm(k, shape, s):
        return s * jax.random.normal(k, shape, f32)

    gshape = (DEPTH, N_SSM_GROUPS, SSM_STATE)
    n_idx = jnp.arange(SSM_STATE, dtype=f32)
    return {
        'x': nrm(ks[0], (BATCH, SEQ, D_MODEL), 1.0),
        'c': nrm(ks[1], (BATCH, D_MODEL), 1.0),
        'ada_w': nrm(ks[2], (DEPTH, D_MODEL, 6 * D_MODEL), 0.5 * D_MODEL ** -0.5),
        'ada_b': nrm(ks[3], (DEPTH, 6 * D_MODEL), 0.02),
        'norm1_g': 1.0 + nrm(ks[4], (DEPTH, D_MODEL), 0.02),
        'w_in': nrm(ks[5], (DEPTH, D_MODEL, D_IN), D_MODEL ** -0.5),
        'forget_b': 2.0 + nrm(ks[6], (DEPTH, N_ATTN_HEADS), 0.1),
        'lam_re': -0.5 + nrm(ks[7], gshape, 0.01),
        'lam_im': math.pi * n_idx + nrm(ks[8], gshape, 0.01),
        'log_dt': jax.random.uniform(ks[9], (DEPTH, N_SSM_GROUPS), f32, math.log(DT_MIN), math.log(DT_MAX)),
        'ssm_b_re': nrm(ks[10], (DEPTH, N_SSM_GROUPS, SSM_STATE, SSM_GROUP), (2 * SSM_GROUP) ** -0.5),
        'ssm_b_im': nrm(ks[11], (DEPTH, N_SSM_GROUPS, SSM_STATE, SSM_GROUP), (2 * SSM_GROUP) ** -0.5),
        'ssm_c_re': nrm(ks[12], (DEPTH, N_SSM_GROUPS, SSM_GROUP, SSM_STATE), (2 * SSM_STATE) ** -0.5),
        'ssm_c_im': nrm(ks[13], (DEPTH, N_SSM_GROUPS, SSM_GROUP, SSM_STATE), (2 * SSM_STATE) ** -0.5),
        'ssm_d': nrm(ks[14], (DEPTH, D_SSM), 1.0),
        'glu_w': nrm(ks[15], (DEPTH, D_SSM, D_SSM), D_SSM ** -0.5),
        'glu_b': nrm(ks[16], (DEPTH, D_SSM), 0.02),
        'q_norm_g': 1.0 + nrm(ks[17], (DEPTH, HEAD_DIM), 0.02),
        'k_norm_g': 1.0 + nrm(ks[18], (DEPTH, HEAD_DIM), 0.02),
        'conv_w': nrm(ks[19], (DEPTH, CONV_WIDTH, D_CONV), CONV_WIDTH ** -0.5),
        'out_norm_g': 1.0 + nrm(ks[20], (DEPTH, D_MIX), 0.02),
        'w_out': nrm(ks[21], (DEPTH, D_MIX, D_MODEL), D_MIX ** -0.5),
        'norm2_g': 1.0 + nrm(ks[22], (DEPTH, D_MODEL), 0.02),
        'w_router': nrm(ks[23], (D_MODEL, N_EXPERTS), D_MODEL ** -0.5),
        'router_bias': nrm(ks[24], (N_EXPERTS,), 0.01),
        'w_gate': nrm(ks[25], (DEPTH, N_EXPERTS, D_MODEL, D_EXPERT), D_MODEL ** -0.5),
        'w_up': nrm(ks[26], (DEPTH, N_EXPERTS, D_MODEL, D_EXPERT), D_MODEL ** -0.5),
        'w_down': nrm(ks[27], (DEPTH, N_EXPERTS, D_EXPERT, D_MODEL), D_EXPERT ** -0.5),
    }


def reference(x, c, ada_w, ada_b, norm1_g, w_in, forget_b, lam_re, lam_im, log_dt,
              ssm_b_re, ssm_b_im, ssm_c_re, ssm_c_im, ssm_d, glu_w, glu_b, q_norm_g, k_norm_g,
              conv_w, out_norm_g, w_out, norm2_g, w_router, router_bias, w_gate, w_up, w_down):
    c_act = jax.nn.silu(c)
    for l in range(DEPTH):
        mod = c_act @ ada_w[l] + ada_b[l]
        sh1, sc1, g1, sh2, sc2, g2 = jnp.split(mod, 6, axis=-1)
        h = modulate(rms_norm(x, norm1_g[l]), sh1, sc1)
        y = mixer_layer(h, w_in[l], forget_b[l], lam_re[l], lam_im[l], log_dt[l],
                        ssm_b_re[l], ssm_b_im[l], ssm_c_re[l], ssm_c_im[l], ssm_d[l],
                        glu_w[l], glu_b[l], q_norm_g[l], k_norm_g[l], conv_w[l],
                        out_norm_g[l], w_out[l])
        x = x + g1[:, None, :] * y
        h = modulate(rms_norm(x, norm2_g[l]), sh2, sc2)
        x = x + g2[:, None, :] * moe_layer(h, w_router, router_bias, w_gate[l], w_up[l], w_down[l])
    return x
```

```python
import numpy as np
from contextlib import ExitStack
import concourse.bass as bass
import concourse.mybir as mybir
from concourse.bass_utils import run_bass_kernel_spmd

F32 = mybir.dt.float32
BF16 = mybir.dt.bfloat16
I32 = mybir.dt.int32
AF = mybir.ActivationFunctionType
ALU = mybir.AluOpType

S = 4096
D = 1024
TT = 512
NT = S // TT
DIN = 2568
NE = 16
DE = 512
EPS = 1e-6
TWO_PI = 6.283185307179586
import os as _os
NOCONV = bool(_os.environ.get('NOCONV'))
POOLENG = _os.environ.get('POOLENG', 'pool')
OFF_U, OFF_Q, OFF_K, OFF_V, OFF_F, OFF_HC, OFF_BG, OFF_CG = 0, 256, 768, 1280, 1792, 1800, 2056, 2312


class Buf:
    __slots__ = ("name", "w", "r")

    def __init__(self, name=""):
        self.name = name
        self.w = {}
        self.r = {}


class Fw:
    ENG = ("pe", "act", "dve", "pool", "sp")

    def __init__(self, nc, ndma=20):
        self.nc = nc
        self.eng = dict(pe=nc.tensor, act=nc.scalar, dve=nc.vector, pool=nc.gpsimd, sp=nc.sync)
        self.sem = {e: nc.alloc_semaphore("sem_" + e) for e in self.ENG}
        self.cnt = {e: 0 for e in self.ENG}
        self.known = {e: {} for e in self.ENG}
        self.dq = {}
        for q in ("sp", "pool"):
            self.dq[q] = dict(sems=[nc.alloc_semaphore(f"dq_{q}_{i}") for i in range(ndma)],
                              uses=[0] * ndma, nxt=0)
        self.allsems = {}
        self.nwaits = 0
        self.bg_on = False
        self.bg_sems = {s_.num for s_ in self.dq["pool"]["sems"]}

    def _wait(self, e, sem, val):
        k = self.known[e]
        if k.get(sem.num, 0) >= val:
            return
        self.eng[e].wait_ge(sem, val)
        self.nwaits += 1
        k[sem.num] = val

    def _deps(self, e, reads, writes):
        need = {}
        mysem = self.sem[e].num

        def add(tok, same_ok):
            sem, val = tok
            if same_ok and sem.num == mysem and e == "pe":
                return
            if need.get(sem.num, (None, 0))[1] < val:
                need[sem.num] = (sem, val)

        for b in reads:
            for tok in b.w.values():
                add(tok, False)
        for b in writes:
            for tok in b.w.values():
                add(tok, True)
            for tok in b.r.values():
                add(tok, True)
        for sem, val in need.values():
            self._wait(e, sem, val)

    def op(self, e, fn, reads=(), writes=()):
        self._deps(e, reads, writes)
        ins = fn(self.eng[e])
        self.cnt[e] += 1
        sem = self.sem[e]
        ins.then_inc(sem, 1)
        tok = (sem, self.cnt[e])
        for b in reads:
            b.r[sem.num] = tok
        for b in writes:
            b.w = {sem.num: tok}
            b.r = {}
        self.allsems[sem.num] = tok
        return tok

    def dma(self, q, out, in_, reads=(), writes=()):
        d = self.dq[q]
        i = d["nxt"]
        d["nxt"] = (i + 1) % len(d["sems"])
        sem = d["sems"][i]
        if d["uses"][i] > 0:
            self._wait(q, sem, 16 * d["uses"][i])
        self._deps(q, reads, writes)
        ins = self.eng[q].dma_start(out=out, in_=in_)
        d["uses"][i] += 1
        tok = (sem, 16 * d["uses"][i])
        ins.then_inc(sem, 16)
        for b in reads:
            b.r[sem.num] = tok
        for b in writes:
            b.w = {sem.num: tok}
            b.r = {}
        self.allsems[sem.num] = tok
        return tok

    def dma_ind(self, out, out_off, in_, in_off, reads=(), writes=()):
        q = "pool"
        d = self.dq[q]
        i = d["nxt"]
        d["nxt"] = (i + 1) % len(d["sems"])
        sem = d["sems"][i]
        if d["uses"][i] > 0:
            self._wait(q, sem, 16 * d["uses"][i])
        self._deps(q, reads, writes)
        ins = self.eng[q].indirect_dma_start(out=out, out_offset=out_off, in_=in_, in_offset=in_off)
        d["uses"][i] += 1
        tok = (sem, 16 * d["uses"][i])
        ins.then_inc(sem, 16)
        for b in reads:
            b.r[sem.num] = tok
        for b in writes:
            b.w = {sem.num: tok}
            b.r = {}
        self.allsems[sem.num] = tok
        return tok

    def barrier(self):
        for e in self.ENG:
            for sem, val in list(self.allsems.values()):
                if sem.num == self.sem[e].num:
                    continue
                if self.bg_on and sem.num in self.bg_sems:
                    continue
                self._wait(e, sem, val)


def build_program(nlayers=2, debug=False, stop_after=None):
    nc = bass.Bass("TRN2", target_bir_lowering=False)
    fw = Fw(nc)
    dbg = {}

    def din(name, shape, dt=F32):
        return nc.dram_tensor(name, list(shape), dt, kind="ExternalInput").ap()

    def dscr(name, shape, dt=F32):
        if debug:
            return nc.dram_tensor(name, list(shape), dt, kind="ExternalOutput").ap()
        return nc.dram_tensor(name, list(shape), dt).ap()

    x_in = din("x", [S, D])
    c_fm = din("c_fm", [128, 8])
    ada_w = din("ada_w", [2, D, 6 * D])
    ada_b_fm = din("ada_b_fm", [2, 128, 48])
    n1g = din("n1g", [2, 128, 8])
    n2g = din("n2g", [2, 128, 8])
    w_in = din("w_in", [2, D, DIN])
    fb = din("fb", [2, 8, 1])
    lam_re = din("lam_re", [2, 128, 8])
    lam_im = din("lam_im", [2, 128, 8])
    log_dt = din("log_dt", [2, 128, 8])
    sb_re = din("sb_re", [2, 128, 8, 16])
    sb_im = din("sb_im", [2, 128, 8, 16])
    sc_re = din("sc_re", [2, 128, 8, 16])
    sc_im = din("sc_im", [2, 128, 8, 16])
    ssm_d = din("ssm_d", [2, 128, 2])
    glu_w = din("glu_w", [2, 256, 256])
    glu_b = din("glu_b", [2, 128, 2])
    qg = din("qg", [2, 128, 1])
    kg = din("kg", [2, 128, 1])
    conv_w = din("conv_w", [2, 128, 2, 3])
    og_fm = din("og_fm", [2, 128, 8])
    og_at = din("og_at", [2, 64, 8])
    w_out = din("w_out", [2, D, D])
    w_router = din("w_router", [D, NE])
    rbias = din("rbias", [128, NE])
    w_gate = din("w_gate", [2, NE, D, DE])
    w_up = din("w_up", [2, NE, D, DE])
    w_down = din("w_down", [2, NE, DE, D])
    ident_in = din("ident", [128, 128])
    e127_in = din("e127", [128, 128])
    blk64_in = din("blk64", [128, 128])
    tri_in = din("tri", [128, 128])
    iota_in = din("iota", [128, TT + 1])
    sel_in = din("sel16", [16, NE, 128])
    out_d = nc.dram_tensor("out", [S, D], F32, kind="ExternalOutput").ap()

    xT_d = dscr("xT_d", [8, 128, S])
    wg_d = dscr("wg_d", [2 * NE, 128, 8 * DE], BF16)
    wu_d = dscr("wu_d", [2 * NE, 128, 8 * DE], BF16)
    wd_d = dscr("wd_d", [2 * NE, 128, 4 * D], BF16)
    uT_d = dscr("uT_d", [2, 128, S])
    qT_d = dscr("qT_d", [8, 65, S], BF16)
    kT_d = dscr("kT_d", [8, 65, S], BF16)
    yh_d = dscr("yh_d", [4, 128, S], BF16)
    ya_d = dscr("ya_d", [8, 64, S], BF16)
    h2_d = dscr("h2_d", [S, D])
    Xs_d = dscr("Xs_d", [80 * 128, D])
    Ys_d = dscr("Ys_d", [80 * 128, D])
    if debug:
        for nm, shp, dt in (("dbg_h", [8, 128, S], BF16), ("dbg_cum", [8, S], F32), ("dbg_mod", [128, 48], F32),
                            ("dbg_v", [128, 32, 8, 65], BF16), ("dbg_ssmpre", [2, 128, S], F32),
                            ("dbg_comb", [S, NE], F32), ("dbg_xmid", [8, 128, S], F32)):
            dbg[nm] = nc.dram_tensor(nm, shp, dt, kind="ExternalOutput").ap()

    es = ExitStack()

    uid = [0]

    def sb(name, shape, dt=F32, stack=None):
        uid[0] += 1
        return (stack or es).enter_context(nc.sbuf_tensor(f"s{uid[0]}_{name}", list(shape), dt))

    PS = [es.enter_context(nc.psum_tensor(f"ps{i}", [128, 512], F32)) for i in range(8)]
    PSB = [Buf(f"ps{i}") for i in range(8)]

    ident = sb("ident", [128, 128]); b_ident = Buf()
    e127 = sb("e127", [128, 128]); b_e127 = Buf()
    tri_f = sb("tri_f", [128, 128]); b_tri = Buf()
    blk_f = sb("blk_f", [128, 128]); blk = sb("blk", [128, 128], BF16); b_blk = Buf()
    ones_b = sb("ones_b", [128, 128], BF16); b_ones = Buf()
    ones_f = sb("ones_f", [128, 128]); b_onesf = Buf()
    iota = sb("iota", [128, TT + 1]); b_iota = Buf()
    sel16_f = sb("sel16_f", [16, NE, 128]); b_sel = Buf()
    cfm = sb("cfm", [128, 8]); b_cfm = Buf()
    cact = sb("cact", [128, 8]); b_cact = Buf()
    rb_sb = sb("rb_sb", [128, NE]); b_rb = Buf()
    wr_sb = sb("wr_sb", [128, 8, NE]); b_wr = Buf()

    fw.dma("sp", ident[:], ident_in, writes=[b_ident])
    fw.dma("sp", e127[:], e127_in, writes=[b_e127])
    fw.dma("sp", tri_f[:], tri_in, writes=[b_tri])
    fw.dma("sp", blk_f[:], blk64_in, writes=[b_blk])
    fw.dma("sp", iota[:], iota_in, writes=[b_iota])
    fw.dma("sp", sel16_f[:], sel_in, writes=[b_sel])
    fw.dma("sp", cfm[:], c_fm, writes=[b_cfm])
    fw.dma("sp", rb_sb[:], rbias, writes=[b_rb])
    fw.dma("sp", wr_sb[:], w_router.rearrange("(kc p) n -> p kc n", p=128), writes=[b_wr])
    fw.op("dve", lambda e: e.tensor_copy(out=blk[:], in_=blk_f[:]), reads=[b_blk], writes=[b_blk])
    fw.op("dve", lambda e: e.memset(ones_b[:], 1.0), writes=[b_ones])
    fw.op("dve", lambda e: e.memset(ones_f[:], 1.0), writes=[b_onesf])
    fw.op("act", lambda e: e.activation(out=cact[:], in_=cfm[:], func=AF.Silu), reads=[b_cfm], writes=[b_cact])

    def OP(e, fn, reads=(), writes=()):
        return fw.op(e, fn, reads, writes)

    def act(out, in_, func, reads, writes, scale=1.0, bias=0.0):
        return fw.op("act", lambda e: e.activation(out=out, in_=in_, func=func, bias=bias, scale=scale), reads, writes)

    def tt(out, in0, in1, op, reads, writes, eng="dve"):
        return fw.op(eng, lambda e: e.tensor_tensor(out=out, in0=in0, in1=in1, op=op), reads, writes)

    def ts(out, in0, s1, s2, op0, op1, reads, writes, eng="dve"):
        if op1 is None:
            return fw.op(eng, lambda e: e.tensor_scalar(out=out, in0=in0, scalar1=s1, scalar2=None, op0=op0), reads, writes)
        return fw.op(eng, lambda e: e.tensor_scalar(out=out, in0=in0, scalar1=s1, scalar2=s2, op0=op0, op1=op1), reads, writes)

    def stt(out, in0, scalar, in1, op0, op1, reads, writes):
        return fw.op("dve", lambda e: e.scalar_tensor_tensor(out=out, in0=in0, scalar=scalar, in1=in1, op0=op0, op1=op1),
                     reads, writes)

    def cp(out, in_, reads, writes, eng="dve"):
        if eng == "act":
            return fw.op("act", lambda e: e.copy(out=out, in_=in_), reads, writes)
        return fw.op(eng, lambda e: e.tensor_copy(out=out, in_=in_), reads, writes)

    ntri = sb("ntri", [128, 128], BF16); ident_b = sb("ident_b", [128, 128], BF16); b_ntri = Buf()
    tt(tri_f[:], tri_f[:], ident[:], ALU.subtract, [b_tri, b_ident], [b_tri])
    ts(ntri[:], tri_f[:], -30000.0, None, ALU.mult, None, [b_tri], [b_ntri])
    cp(ident_b[:], ident[:], [b_ident], [b_ntri])
    eps_c = sb("eps_c", [128, 1]); b_eps = Buf()
    OP("dve", lambda e: e.memset(eps_c[:], EPS), writes=[b_eps])

    hn_sq = [sb(f"hn_sq{i}", [128, TT], BF16) for i in range(2)]; hn_sqb = [Buf() for _ in range(2)]
    hn_rt = [sb(f"hn_rt{i}", [128, TT]) for i in range(2)]; hn_rtb = [Buf() for _ in range(2)]
    hn_ctr = [0]
    HN_PS = 7

    def hnorm(src, srcb, g_ap, gb, out, outb, P=128, n=TT):
        i = hn_ctr[0] % 2
        hn_ctr[0] += 1
        act(hn_sq[i][0:P, 0:n], src, AF.Square, [srcb], [hn_sqb[i]])
        OP("pe", lambda e: e.matmul(PS[HN_PS][0:P, 0:n], blk[0:P, 0:P], hn_sq[i][0:P, 0:n], start=True, stop=True),
           [hn_sqb[i], b_blk], [PSB[HN_PS]])
        act(hn_rt[i][0:P, 0:n], PS[HN_PS][0:P, 0:n], AF.Ln, [PSB[HN_PS], b_eps], [hn_rtb[i]], scale=1.0 / 64, bias=eps_c[0:P, :])
        act(hn_rt[i][0:P, 0:n], hn_rt[i][0:P, 0:n], AF.Exp, [hn_rtb[i]], [hn_rtb[i]], scale=-0.5)
        stt(out, src, g_ap, hn_rt[i][0:P, 0:n], ALU.mult, ALU.mult, [srcb, gb, hn_rtb[i]], [outb])

    with ExitStack() as st:
        xin = [sb(f"xin{i}", [128, D], F32, st) for i in range(4)]
        xinb = [Buf() for _ in range(4)]
        xo = [sb(f"xo{i}", [128, 8, 128], F32, st) for i in range(4)]
        xob = [Buf() for _ in range(4)]
        for tk in range(S // 128):
            i = tk % 4
            fw.dma("sp", xin[i][:], x_in[tk * 128:(tk + 1) * 128, :], writes=[xinb[i]])
            for half in range(2):
                pb = (2 * tk + half) % 8

                def f(e, i=i, half=half, pb=pb):
                    for c4 in range(4):
                        c = half * 4 + c4
                        ins = e.transpose(PS[pb][:, c4 * 128:(c4 + 1) * 128], xin[i][:, c * 128:(c + 1) * 128], ident[:])
                    return ins
                OP("pe", f, [xinb[i], b_ident], [PSB[pb]])
                cp(xo[i][:, half * 4:(half + 1) * 4, :], PS[pb][:].rearrange("p (c t) -> p c t", c=4),
                   [PSB[pb]], [xob[i]], eng=("act" if half == 0 else "dve"))
            fw.dma("sp", xT_d[:, :, tk * 128:(tk + 1) * 128].rearrange("c p t -> p c t"), xo[i][:], reads=[xob[i]])
    fw.barrier()

    def norm_mod(st_, xt, xtb, A, B, ABb, hb, hbb, xn, xnb, sqb, sqbb, rt, rtb, psn):
        act(sqb[:], xt[:], AF.Square, [xtb], [sqbb])

        def f(e):
            for c in range(8):
                ins = e.matmul(PS[psn][:, :], ones_b[:], sqb[:, c, :], start=(c == 0), stop=(c == 7))
            return ins
        OP("pe", f, [sqbb, b_ones], [PSB[psn]])
        act(rt[:], PS[psn][:, :], AF.Ln, [PSB[psn], b_eps], [rtb], scale=1.0 / D, bias=eps_c[:])
        act(rt[:], rt[:], AF.Exp, [rtb], [rtb], scale=-0.5)
        tt(xn[:], xt[:], rt[:].unsqueeze(1).to_broadcast([128, 8, TT]), ALU.mult, [xtb, rtb], [xnb])
        for c in range(8):
            if c % 2 == 0:
                ts(xn[:, c, :], xn[:, c, :], A[:, c:c + 1], B[:, c:c + 1], ALU.mult, ALU.add, [xnb, ABb], [xnb])
            else:
                act(xn[:, c, :], xn[:, c, :], AF.Identity, [xnb, ABb], [xnb], scale=A[:, c:c + 1], bias=B[:, c:c + 1])
        cp(hb[:, 0:4, :], xn[:, 0:4, :], [xnb], [hbb], eng="dve")
        cp(hb[:, 4:8, :], xn[:, 4:8, :], [xnb], [hbb], eng="act")

    for l in range(nlayers if stop_after != "0" else 0):
        last = (l == nlayers - 1)
        with ExitStack() as lay:
            mod = sb("mod", [128, 48], F32, lay); b_mod = Buf()
            A1 = sb("A1", [128, 8], F32, lay); A2 = sb("A2", [128, 8], F32, lay)
            n1g_sb = sb("n1g_sb", [128, 8], F32, lay); n2g_sb = sb("n2g_sb", [128, 8], F32, lay); b_ng = Buf()
            adab = sb("adab", [128, 48], F32, lay); b_adab = Buf()
            cact2 = sb("cact2", [128, 8, 2], F32, lay); b_cact2 = Buf()
            fw.dma("sp", n1g_sb[:], n1g[l], writes=[b_ng])
            fw.dma("sp", n2g_sb[:], n2g[l], writes=[b_ng])
            fw.dma("sp", adab[:], ada_b_fm[l], writes=[b_adab])
            cp(cact2[:], cact[:].unsqueeze(2).to_broadcast([128, 8, 2]), [b_cact], [b_cact2])
            with ExitStack() as st:
                adw = [sb(f"adw{i}", [128, 8, D], F32, st) for i in range(2)]
                adwb = [Buf() for _ in range(2)]
                for j in range(6):
                    i = j % 2
                    fw.dma("sp", adw[i][:], ada_w[l, :, j * D:(j + 1) * D].rearrange("(kc p) n -> p kc n", p=128),
                           writes=[adwb[i]])
                    for c in range(8):
                        def f(e, i=i, j=j, c=c):
                            for kc in range(8):
                                col = 2 * (j * 8 + c)
                                ins = e.matmul(PS[0][:, col:col + 2], adw[i][:, kc, c * 128:(c + 1) * 128], cact2[:, kc, :],
                                               start=(kc == 0), stop=(kc == 7))
                            return ins
                        OP("pe", f, [adwb[i], b_cact2], [PSB[0]])
                tt(mod[:], PS[0][:, 0:96].rearrange("p (n two) -> p n two", two=2)[:, :, 0], adab[:], ALU.add,
                   [PSB[0], b_adab], [b_mod])
                stt(A1[:], mod[:, 8:16], 1.0, n1g_sb[:], ALU.add, ALU.mult, [b_mod, b_ng], [b_mod])
                stt(A2[:], mod[:, 32:40], 1.0, n2g_sb[:], ALU.add, ALU.mult, [b_mod, b_ng], [b_mod])
                if debug and l == 0:
                    fw.dma("sp", dbg["dbg_mod"], mod[:], reads=[b_mod])
                fw.barrier()
            B1 = mod[:, 0:8]; G1 = mod[:, 16:24]; B2 = mod[:, 24:32]; G2 = mod[:, 40:48]

            with ExitStack() as mix:
                V_sb = sb("V_sb", [128, 32, 8, 65], BF16, mix); b_V = Buf()
                cum = sb("cum", [8, S], F32, mix); b_cum = Buf()
                og_fm_sb = sb("og_fm_sb", [128, 8], F32, mix); og_at_sb = sb("og_at_sb", [64, 8], F32, mix); b_og = Buf()
                fw.dma("sp", og_fm_sb[:], og_fm[l], writes=[b_og])
                fw.dma("sp", og_at_sb[:], og_at[l], writes=[b_og])
                OP("dve", lambda e: e.memset(V_sb[:, :, :, 64:65], 1.0), writes=[b_V])
                fw.barrier()
                with ExitStack() as st:
                    win = sb("win", [128, 8, DIN], BF16, st); b_win = Buf()
                    for hh in range(2):
                        fw.dma("pool", win[:, :, hh * 1284:(hh + 1) * 1284],
                               w_in[l, :, hh * 1284:(hh + 1) * 1284].rearrange("(kc p) n -> p kc n", p=128), writes=[Buf()])
                    fw.barrier()
                    fbs = sb("fbs", [8, 1], F32, st); b_fb = Buf()
                    qgs = sb("qgs", [128, 1], F32, st); kgs = sb("kgs", [128, 1], F32, st); b_qk = Buf()
                    cws = sb("cws", [128, 2, 3], F32, st); b_cw = Buf()
                    fw.dma("sp", fbs[:], fb[l], writes=[b_fb])
                    fw.dma("sp", qgs[:], qg[l], writes=[b_qk])
                    fw.dma("sp", kgs[:], kg[l], writes=[b_qk])
                    fw.dma("sp", cws[:], conv_w[l], writes=[b_cw])
                    ts(fbs[:], fbs[:], -1.0, None, ALU.mult, None, [b_fb], [b_fb])
                    ts(qgs[:], qgs[:], 0.125, None, ALU.mult, None, [b_qk], [b_qk])
                    xt = [sb(f"xt{i}", [128, 8, TT], F32, st) for i in range(2)]; xtb = [Buf() for _ in range(2)]
                    sqb = sb("sqb", [128, 8, TT], BF16, st); sqbb = Buf()
                    rt = sb("rt", [128, TT], F32, st); rtb = Buf()
                    xn = sb("xn", [128, 8, TT], F32, st); xnb = Buf()
                    hb = [sb(f"hb{i}", [128, 8, TT], BF16, st) for i in range(2)]; hbb = [Buf() for _ in range(2)]
                    ev = [sb(f"ev{i}", [128, TT], F32, st) for i in range(4)]; evb = [Buf() for _ in range(4)]
                    evo = [sb(f"evo{i}", [128, TT], BF16, st) for i in range(4)]; evob = [Buf() for _ in range(4)]
                    zt = [[sb(f"zt{cc}{i}", [128, TT + 2], F32, st) for i in range(2)] for cc in range(2)]
                    ztb = [[Buf() for _ in range(2)] for _ in range(2)]
                    cy = sb("cy", [128, TT], F32, st); cyb = Buf()
                    fe = sb("fe", [8, TT], F32, st); feb = Buf()
                    evc = [0]
                    gen = [0]
                    for cc in range(2):
                        OP("dve", lambda e, cc=cc: e.memset(zt[cc][1][:, TT:TT + 2], 0.0), writes=[ztb[cc][1]])

                    def proj(ps, off, M, hbt, hbtb):
                        def f(e):
                            for kc in range(8):
                                ins = e.matmul(PS[ps][0:M, :], win[:, kc, off:off + M], hbt[:, kc, :], start=(kc == 0), stop=(kc == 7))
                            return ins
                        OP("pe", f, [hbtb], [PSB[ps]])

                    for it in range(NT):
                        t0 = it * TT
                        i = it % 2
                        fw.dma("sp", xt[i][:], xT_d[:, :, t0:t0 + TT].rearrange("c p t -> p c t"), writes=[xtb[i]])
                        norm_mod(st, xt[i], xtb[i], A1, B1, b_mod, hb[i], hbb[i], xn, xnb, sqb, sqbb, rt, rtb, 0)
                        if debug and l == 0:
                            fw.dma("sp", dbg["dbg_h"][:, :, t0:t0 + TT].rearrange("c p t -> p c t"), hb[i][:], reads=[hbb[i]])
                        for c in range(2):
                            ps = 1 + gen[0] % 2; gen[0] += 1
                            proj(ps, OFF_U + c * 128, 128, hb[i], hbb[i])
                            k = evc[0] % 4; evc[0] += 1
                            cp(ev[k][:], PS[ps][:, :], [PSB[ps]], [evb[k]], eng="act")
                            fw.dma("sp", uT_d[c, :, t0:t0 + TT], ev[k][:], reads=[evb[k]])
                        for (off, gsb, dst) in ((OFF_Q, qgs, qT_d), (OFF_K, kgs, kT_d)):
                            for c in range(4):
                                ps = 1 + gen[0] % 2; gen[0] += 1
                                proj(ps, off + c * 128, 128, hb[i], hbb[i])
                                k = evc[0] % 4; evc[0] += 1
                                cp(ev[k][:], PS[ps][:, :], [PSB[ps]], [evb[k]], eng="act")
                                hnorm(ev[k][:], evb[k], gsb[:, 0:1], b_qk, evo[k][:], evob[k])
                                fw.dma("sp", dst[2 * c, 0:64, t0:t0 + TT], evo[k][0:64, :], reads=[evob[k]])
                                fw.dma("sp", dst[2 * c + 1, 0:64, t0:t0 + TT], evo[k][64:128, :], reads=[evob[k]])
                        ps = 1 + gen[0] % 2; gen[0] += 1
                        proj(ps, OFF_F, 8, hb[i], hbb[i])
                        act(fe[:], PS[ps][0:8, :], AF.Exp, [PSB[ps], b_fb], [feb], scale=-1.0, bias=fbs[:])
                        act(fe[:], fe[:], AF.Ln, [feb], [feb], bias=1.0)
                        init = 0.0 if it == 0 else cum[:, t0 - 1:t0]
                        OP("dve", lambda e, t0=t0, init=init: e.tensor_tensor_scan(
                            out=cum[:, t0:t0 + TT], data0=ones_f[0:8, 0:1].to_broadcast([8, TT]), data1=fe[:], initial=init,
                            op0=ALU.mult, op1=ALU.subtract), [feb, b_onesf, b_cum], [b_cum])
                        for sub in range(4):
                            def f(e, sub=sub, i=i):
                                for kc in range(8):
                                    ins = e.matmul(PS[3][:, :], hb[i][:, kc, sub * 128:(sub + 1) * 128], win[:, kc, OFF_V:OFF_V + 512],
                                                   start=(kc == 0), stop=(kc == 7))
                                return ins
                            OP("pe", f, [hbb[i]], [PSB[3]])
                            cp(V_sb[:, 4 * it + sub, :, 0:64], PS[3][:, :].rearrange("p (h d) -> p h d", h=8), [PSB[3]], [b_V],
                               eng=("act" if sub % 2 else "dve"))
                        for cc in range(2):
                            proj(4, OFF_HC + cc * 128, 128, hb[i], hbb[i])
                            proj(5, OFF_CG + cc * 128, 128, hb[i], hbb[i])
                            proj(6, OFF_BG + cc * 128, 128, hb[i], hbb[i])
                            k = evc[0] % 4; evc[0] += 1
                            z, zb = zt[cc][i], ztb[cc][i]
                            zp, zpb = zt[cc][1 - i], ztb[cc][1 - i]
                            cp(ev[k][:], PS[5][:, :], [PSB[5]], [evb[k]], eng="act")
                            cp(z[:, 0:2], zp[:, TT:TT + 2], [zpb], [zb])
                            tt(z[:, 2:TT + 2], PS[4][:, :], ev[k][:], ALU.mult, [PSB[4], evb[k]], [zb])
                            ts(cy[:], z[:, 2:TT + 2], cws[:, cc, 2:3], None, ALU.mult, None, [zb, b_cw], [cyb])
                            stt(cy[:], z[:, 1:TT + 1], cws[:, cc, 1:2], cy[:], ALU.mult, ALU.add, [zb, b_cw, cyb], [cyb])
                            stt(cy[:], z[:, 0:TT], cws[:, cc, 0:1], cy[:], ALU.mult, ALU.add, [zb, b_cw, cyb], [cyb])
                            tt(ev[k][:], PS[6][:, :], cy[:], ALU.mult, [PSB[6], cyb], [evb[k]])
                            hnorm(ev[k][:], evb[k], og_fm_sb[:, 6 + cc:7 + cc], b_og, evo[k][:], evob[k])
                            fw.dma("sp", yh_d[2 + cc, :, t0:t0 + TT], evo[k][:], reads=[evob[k]])
                    if debug and l == 0:
                        fw.dma("sp", dbg["dbg_cum"], cum[:], reads=[b_cum])
                        fw.dma("sp", dbg["dbg_v"], V_sb[:], reads=[b_V])
                    fw.barrier()
                if stop_after == "A":
                    break
                with ExitStack() as st:
                    def t8(name):
                        return sb(name, [128, 8], F32, st)
                    lre, lim, ldt = t8("lre"), t8("lim"), t8("ldt"); b_p = Buf()
                    fw.dma("sp", lre[:], lam_re[l], writes=[b_p])
                    fw.dma("sp", lim[:], lam_im[l], writes=[b_p])
                    fw.dma("sp", ldt[:], log_dt[l], writes=[b_p])
                    bre = sb("bre", [128, 8, 16], F32, st); bim = sb("bim", [128, 8, 16], F32, st)
                    cre = sb("cre", [128, 8, 16], F32, st); cim = sb("cim", [128, 8, 16], F32, st); b_bc = Buf()
                    fw.dma("sp", bre[:], sb_re[l], writes=[b_bc]); fw.dma("sp", bim[:], sb_im[l], writes=[b_bc])
                    fw.dma("sp", cre[:], sc_re[l], writes=[b_bc]); fw.dma("sp", cim[:], sc_im[l], writes=[b_bc])
                    dsk = sb("dsk", [128, 2], F32, st); glb = sb("glb", [128, 2], F32, st); b_dg = Buf()
                    fw.dma("sp", dsk[:], ssm_d[l], writes=[b_dg]); fw.dma("sp", glb[:], glu_b[l], writes=[b_dg])
                    gluw = sb("gluw", [128, 2, 256], BF16, st); b_gw = Buf()
                    fw.dma("pool", gluw[:], glu_w[l].rearrange("(kc p) n -> p kc n", p=128), writes=[b_gw])
                    r_sb, th = t8("r_sb"), t8("th")
                    dtv, a_, cs, sn, t1_, t2_, zre, zim = t8("dtv"), t8("a_"), t8("cs"), t8("sn"), t8("t1_"), t8("t2_"), t8("zre"), t8("zim")
                    ti = sb("ti", [128, 8], I32, st)
                    C1 = 6.28125
                    C2 = TWO_PI - C1

                    def sincos(out, ang, shape, tmpf, tmpi, bq, shift):
                        ts(tmpf, ang, 1.0 / TWO_PI, shift / TWO_PI, ALU.mult, ALU.add, [bq], [bq])
                        cp(tmpi, tmpf, [bq], [bq])
                        cp(tmpf, tmpi, [bq], [bq])
                        if shift != 0.0:
                            ts(out, ang, shift, None, ALU.add, None, [bq], [bq])
                            stt(out, tmpf, -C1, out, ALU.mult, ALU.add, [bq], [bq])
                        else:
                            stt(out, tmpf, -C1, ang, ALU.mult, ALU.add, [bq], [bq])
                        stt(out, tmpf, -C2, out, ALU.mult, ALU.add, [bq], [bq])
                        ts(out, out, 3.1415925, -3.1415925, ALU.min, ALU.max, [bq], [bq])
                        act(out, out, AF.Sin, [bq], [bq])

                    ts(lre[:], lre[:], -1e-4, None, ALU.min, None, [b_p], [b_p])
                    act(dtv[:], ldt[:], AF.Exp, [b_p], [b_p])
                    tt(a_[:], lre[:], dtv[:], ALU.mult, [b_p], [b_p])
                    act(r_sb[:], a_[:], AF.Exp, [b_p], [b_p])
                    tt(th[:], lim[:], dtv[:], ALU.mult, [b_p], [b_p])
                    sincos(sn[:], th[:], None, t1_[:], ti[:], b_p, 0.0)
                    sincos(cs[:], th[:], None, t1_[:], ti[:], b_p, 1.5707963267948966)
                    tt(cs[:], cs[:], r_sb[:], ALU.mult, [b_p], [b_p])
                    tt(sn[:], sn[:], r_sb[:], ALU.mult, [b_p], [b_p])
                    ts(cs[:], cs[:], -1.0, None, ALU.add, None, [b_p], [b_p])
                    tt(t1_[:], lre[:], lre[:], ALU.mult, [b_p], [b_p])
                    tt(t2_[:], lim[:], lim[:], ALU.mult, [b_p], [b_p])
                    tt(t1_[:], t1_[:], t2_[:], ALU.add, [b_p], [b_p])
                    OP("dve", lambda e: e.reciprocal(out=t1_[:], in_=t1_[:]), [b_p], [b_p])
                    tt(zre[:], cs[:], lre[:], ALU.mult, [b_p], [b_p])
                    tt(t2_[:], sn[:], lim[:], ALU.mult, [b_p], [b_p])
                    tt(zre[:], zre[:], t2_[:], ALU.add, [b_p], [b_p])
                    tt(zre[:], zre[:], t1_[:], ALU.mult, [b_p], [b_p])
                    tt(zim[:], sn[:], lre[:], ALU.mult, [b_p], [b_p])
                    tt(t2_[:], cs[:], lim[:], ALU.mult, [b_p], [b_p])
                    tt(zim[:], zim[:], t2_[:], ALU.subtract, [b_p], [b_p])
                    tt(zim[:], zim[:], t1_[:], ALU.mult, [b_p], [b_p])
                    bbr = sb("bbr", [128, 8, 16], F32, st); bbi = sb("bbi", [128, 8, 16], F32, st); tb = sb("tb", [128, 8, 16], F32, st)
                    zre_b = zre[:].unsqueeze(2).to_broadcast([128, 8, 16]); zim_b = zim[:].unsqueeze(2).to_broadcast([128, 8, 16])
                    tt(bbr[:], bre[:], zre_b, ALU.mult, [b_p, b_bc], [b_bc])
                    tt(tb[:], bim[:], zim_b, ALU.mult, [b_p, b_bc], [b_bc])
                    tt(bbr[:], bbr[:], tb[:], ALU.subtract, [b_bc], [b_bc])
                    tt(bbi[:], bim[:], zre_b, ALU.mult, [b_p, b_bc], [b_bc])
                    tt(tb[:], bre[:], zim_b, ALU.mult, [b_p, b_bc], [b_bc])
                    tt(bbi[:], bbi[:], tb[:], ALU.add, [b_bc], [b_bc])
                    WT = []
                    for nm, src in (("re", bbr), ("im", bbi)):
                        w1 = sb("w1" + nm, [128, 8, 2, 16], F32, st); bw1 = Buf()
                        OP("dve", lambda e, w1=w1: e.memset(w1[:], 0.0), writes=[bw1])
                        cp(w1[0:64, :, 0, :], src[0:64], [b_bc], [bw1])
                        cp(w1[64:128, :, 1, :], src[64:128], [b_bc], [bw1])
                        wt = sb("wt" + nm, [128, 2, 128], BF16, st); bwt = Buf()
                        w1v = w1[:].rearrange("p g a c -> p (g a c)")
                        for ch in range(2):
                            OP("pe", lambda e, ch=ch, w1v=w1v: e.transpose(PS[0][:, 0:128], w1v[:, ch * 128:(ch + 1) * 128], ident[:]),
                               [bw1, b_ident], [PSB[0]])
                            cp(wt[:, ch, :], PS[0][:, 0:128], [PSB[0]], [bwt])
                        WT.append((wt, bwt))
                    CT = []
                    for nm, src, sgn in (("re", cre, 1.0), ("im", cim, -1.0)):
                        ct = sb("ct" + nm, [128, 8, 2, 16], BF16, st); bct = Buf()
                        OP("dve", lambda e, ct=ct: e.memset(ct[:], 0.0), writes=[bct])
                        ts(ct[0:64, :, 0, :], src[0:64], sgn, None, ALU.mult, None, [b_bc], [bct])
                        ts(ct[64:128, :, 1, :], src[64:128], sgn, None, ALU.mult, None, [b_bc], [bct])
                        CT.append((ct, bct))
                    cosT = sb("cosT", [128, 8, TT + 1], F32, st); sinT = sb("sinT", [128, 8, TT + 1], F32, st); b_tab = Buf()
                    with ExitStack() as st2:
                        ang = sb("ang", [128, 8, TT + 1], F32, st2); tf = sb("tf", [128, 8, TT + 1], F32, st2)
                        tii = sb("tii", [128, 8, TT + 1], I32, st2); b_ang = Buf()
                        for gp in range(8):
                            ts(ang[:, gp, :], iota[:], th[:, gp:gp + 1], None, ALU.mult, None, [b_iota, b_p], [b_ang])
                        sincos(sinT[:], ang[:], None, tf[:], tii[:], b_ang, 0.0)
                        sincos(cosT[:], ang[:], None, tf[:], tii[:], b_ang, 1.5707963267948966)
                        fw.barrier()
                    uf = [sb(f"uf{i}", [128, 2, TT], F32, st) for i in range(2)]; ufb = [Buf() for _ in range(2)]
                    ub = [sb(f"ub{i}", [128, 2, TT], BF16, st) for i in range(2)]; ubb = [Buf() for _ in range(2)]
                    ta = [sb(f"ta{i}", [128, TT], F32, st) for i in range(4)]; tab_ = [Buf() for _ in range(4)]
                    wre = [sb(f"wre{i}", [128, TT], F32, st) for i in range(2)]; wim = [sb(f"wim{i}", [128, TT], F32, st) for i in range(2)]
                    wb_ = [Buf() for _ in range(2)]
                    zr = [sb(f"zr{i}", [128, TT], BF16, st) for i in range(2)]; zi = [sb(f"zi{i}", [128, TT], BF16, st) for i in range(2)]
                    zb_ = [Buf() for _ in range(2)]
                    ini = sb("ini", [128, 8, 2], F32, st); b_ini = [Buf() for _ in range(8)]
                    tiny = sb("tiny", [128, 2], F32, st)
                    yp = sb("yp", [128, 2, TT], F32, st); ypb = [Buf() for _ in range(2)]
                    yg = sb("yg", [128, 2, TT], F32, st); ygb_f = [Buf() for _ in range(2)]
                    ygb = sb("ygb", [128, 2, TT], BF16, st); ygbb = Buf()
                    g1t = sb("g1t", [128, TT], F32, st); g1b = Buf(); g2t = sb("g2t", [128, TT], F32, st); g2b = Buf()
                    yo = [sb(f"yo{i}", [128, TT], F32, st) for i in range(2)]; yob = [Buf() for _ in range(2)]
                    yob16 = [sb(f"yob16{i}", [128, TT], BF16, st) for i in range(2)]; yob16b = [Buf() for _ in range(2)]
                    OP("dve", lambda e: e.memset(ini[:], 0.0), writes=b_ini)
                    k = 0
                    def gen_B():
                        k = 0
                        pend = []

                        def run_due(force=False):
                            keep = []
                            for item in list(pend):
                                item[0] -= 1
                                if force or item[0] <= 0:
                                    nxt = item[1]()
                                    while force and nxt is not None:
                                        nxt = nxt()
                                    if nxt is not None:
                                        keep.append([1, nxt])
                                else:
                                    keep.append(item)
                            pend[:] = keep
                        for it in range(NT):
                            t0 = it * TT
                            i = it % 2
                            fw.dma("sp", uf[i][:], uT_d[:, :, t0:t0 + TT].rearrange("c p t -> p c t"), writes=[ufb[i]])
                            cp(ub[i][:], uf[i][:], [ufb[i]], [ubb[i]], eng="act")
                            for gp in range(8):
                                ch, j = gp // 4, gp % 4
                                pa, pb = 0, 1
                                for (pp, (wt, bwt)) in ((pa, WT[0]), (pb, WT[1])):
                                    OP("pe", lambda e, pp=pp, wt=wt, ch=ch, j=j, i=i: e.matmul(
                                        PS[pp][:, :], wt[32 * j:32 * j + 32, ch, :], ub[i][32 * j:32 * j + 32, ch, :],
                                        start=True, stop=True, tile_position=(32 * j, 0)), [bwt, ubb[i]], [PSB[pp]])
                                run_due()
                                cT = cosT[:, gp, 0:TT]; sT = sinT[:, gp, 0:TT]
                                kk = k % 2; k += 1
                                tt(ta[0][:], PS[pa][:, :], cT, ALU.mult, [PSB[pa], b_tab], [tab_[0]])
                                tt(ta[1][:], PS[pb][:, :], sT, ALU.mult, [PSB[pb], b_tab], [tab_[1]])
                                tt(ta[0][:], ta[0][:], ta[1][:], ALU.add, [tab_[0], tab_[1]], [tab_[0]])
                                tt(ta[2][:], PS[pb][:, :], cT, ALU.mult, [PSB[pb], b_tab], [tab_[2]])
                                tt(ta[3][:], PS[pa][:, :], sT, ALU.mult, [PSB[pa], b_tab], [tab_[3]])
                                tt(ta[2][:], ta[2][:], ta[3][:], ALU.subtract, [tab_[2], tab_[3]], [tab_[2]])
                                rb = r_sb[:, gp:gp + 1].to_broadcast([128, TT])
                                OP("dve", lambda e, kk=kk, rb=rb, gp=gp: e.tensor_tensor_scan(
                                    out=wre[kk][:], data0=rb, data1=ta[0][:], initial=ini[:, gp, 0:1], op0=ALU.mult, op1=ALU.add),
                                    [tab_[0], b_p, b_ini[gp]], [wb_[kk]])
                                OP("dve", lambda e, kk=kk, rb=rb, gp=gp: e.tensor_tensor_scan(
                                    out=wim[kk][:], data0=rb, data1=ta[2][:], initial=ini[:, gp, 1:2], op0=ALU.mult, op1=ALU.add),
                                    [tab_[2], b_p, b_ini[gp]], [wb_[kk]])
                                tt(ta[0][:], wre[kk][:], cT, ALU.mult, [wb_[kk], b_tab], [tab_[0]])
                                tt(ta[1][:], wim[kk][:], sT, ALU.mult, [wb_[kk], b_tab], [tab_[1]])
                                tt(zr[kk][:], ta[0][:], ta[1][:], ALU.subtract, [tab_[0], tab_[1]], [zb_[kk]])
                                tt(ta[2][:], wre[kk][:], sT, ALU.mult, [wb_[kk], b_tab], [tab_[2]])
                                tt(ta[3][:], wim[kk][:], cT, ALU.mult, [wb_[kk], b_tab], [tab_[3]])
                                tt(zi[kk][:], ta[2][:], ta[3][:], ALU.add, [tab_[2], tab_[3]], [zb_[kk]])
                                cL = cosT[:, gp, TT:TT + 1]; sL = sinT[:, gp, TT:TT + 1]
                                ts(tiny[:, 0:1], wim[kk][:, TT - 1:TT], sL, None, ALU.mult, None, [wb_[kk], b_tab], [b_ini[gp]])
                                ts(tiny[:, 1:2], wim[kk][:, TT - 1:TT], cL, None, ALU.mult, None, [wb_[kk], b_tab], [b_ini[gp]])
                                stt(ini[:, gp, 0:1], wre[kk][:, TT - 1:TT], cL, tiny[:, 0:1], ALU.mult, ALU.subtract, [wb_[kk], b_tab, b_ini[gp]], [b_ini[gp]])
                                stt(ini[:, gp, 1:2], wre[kk][:, TT - 1:TT], sL, tiny[:, 1:2], ALU.mult, ALU.add, [wb_[kk], b_tab, b_ini[gp]], [b_ini[gp]])
                                py = 2

                                def tail(gp=gp, j=j, kk=kk, py=py, ch=ch, i=i, t0=t0):
                                  def f(e):
                                    e.matmul(PS[py][32 * j:32 * j + 32, :], CT[0][0][:, gp, :, :].rearrange("p a c -> p (a c)"), zr[kk][:],
                                             start=True, stop=False, tile_position=(0, 32 * j))
                                    return e.matmul(PS[py][32 * j:32 * j + 32, :], CT[1][0][:, gp, :, :].rearrange("p a c -> p (a c)"), zi[kk][:],
                                                    start=False, stop=True, tile_position=(0, 32 * j))
                                  OP("pe", f, [zb_[kk], CT[0][1], CT[1][1]], [PSB[py]])
                                  if j == 3:
                                    stt(yp[:, ch, :], uf[i][:, ch, :], dsk[:, ch:ch + 1], PS[py][:, :], ALU.mult, ALU.add,
                                        [ufb[i], b_dg, PSB[py]], [ypb[ch]])
                                    if debug and l == 0:
                                        fw.dma("sp", dbg["dbg_ssmpre"][ch, :, t0:t0 + TT], yp[:, ch, :], reads=[ypb[ch]])
                                    tt(g1t[:], yp[:, ch, :], yp[:, ch, :], ALU.mult, [ypb[ch]], [g1b])
                                    ts(g1t[:], g1t[:], 0.044715, 1.0, ALU.mult, ALU.add, [g1b], [g1b])
                                    tt(g1t[:], g1t[:], yp[:, ch, :], ALU.mult, [g1b, ypb[ch]], [g1b])

                                    def tailB():
                                        act(g1t[:], g1t[:], AF.Sigmoid, [g1b], [g1b], scale=1.5957691216057308)

                                        def tailC():
                                            tt(yg[:, ch, :], yp[:, ch, :], g1t[:], ALU.mult, [g1b, ypb[ch]], [ygb_f[ch]])
                                            cp(ygb[:, ch, :], yg[:, ch, :], [ygb_f[ch]], [ygbb])
                                            return None
                                        return tailC
                                    return tailB
                                  return None
                                pend.append([1, tail])
                                yield
                            def glu_block(t0=t0):
                                for mc in range(2):
                                    def f(e, mc=mc):
                                        e.matmul(PS[7][:, :], gluw[:, 0, mc * 128:(mc + 1) * 128], ygb[:, 0, :], start=True, stop=False)
                                        return e.matmul(PS[7][:, :], gluw[:, 1, mc * 128:(mc + 1) * 128], ygb[:, 1, :], start=False, stop=True)
                                    OP("pe", f, [ygbb, b_gw], [PSB[7]])
                                    act(g2t[:], PS[7][:, :], AF.Sigmoid, [PSB[7], b_dg], [g2b], bias=glb[:, mc:mc + 1])
                                    tt(yo[mc][:], yg[:, mc, :], g2t[:], ALU.mult, [g2b, ygb_f[mc]], [yob[mc]])
                                    hnorm(yo[mc][:], yob[mc], og_fm_sb[:, mc:mc + 1], b_og, yob16[mc][:], yob16b[mc])
                                    fw.dma("sp", yh_d[mc, :, t0:t0 + TT], yob16[mc][:], reads=[yob16b[mc]])
                                return None
                            pend.append([4, glu_block])
                            yield
                        run_due(force=True)
                        yield
                    ckT = sb("ckT", [128, 32, 8], F32, st); cref = sb("cref", [128, 32, 8], F32, st); b_ck = Buf()
                    st3 = ExitStack()
                    ce = sb("ce", [8, 32], F32, st3); dq = sb("dq", [8, 8, 4], F32, st3); b_ce = Buf()
                    dqrow = sb("dqrow", [8, 32, 128], BF16, st3); onesrow = sb("onesrow", [8, S], BF16, st3); b_row = Buf()
                    cp(ce[:], cum[:].rearrange("h (s j) -> h s j", j=128)[:, :, 127], [b_cum], [b_ce])
                    cev = ce[:].rearrange("h (q s) -> h q s", s=4)
                    tt(dq[:], cev, cev[:, :, 3:4].to_broadcast([8, 8, 4]), ALU.subtract, [b_ce], [b_ce])
                    cp(dqrow[:], dq[:].rearrange("h q s -> h (q s)").unsqueeze(2).to_broadcast([8, 32, 128]), [b_ce], [b_row])
                    OP("dve", lambda e: e.memset(onesrow[:], 1.0), writes=[b_row])
                    fw.dma("sp", qT_d[:, 64, :], dqrow[:].rearrange("h s j -> h (s j)"), reads=[b_row])
                    fw.dma("sp", kT_d[:, 64, :], onesrow[:], reads=[b_row])

                    def f(e):
                        for kt in range(32):
                            ins = e.transpose(PS[0][:, kt * 8:(kt + 1) * 8], cum[0:8, kt * 128:(kt + 1) * 128], ident[0:8, 0:8])
                        return ins
                    OP("pe", f, [b_cum, b_ident], [PSB[0]])
                    cp(ckT[:].rearrange("p k h -> p (k h)"), PS[0][:, 0:256], [PSB[0]], [b_ck])
                    OP("pe", lambda e: e.matmul(PS[1][:, 0:256], e127[:], ckT[:].rearrange("p k h -> p (k h)"), start=True, stop=True),
                       [b_ck, b_e127], [PSB[1]])
                    cp(cref[:].rearrange("p k h -> p (k h)"), PS[1][:, 0:256], [PSB[1]], [b_ck])
                    fw.barrier()
                    st3.close()
                    cv = [sb(f"cv{i}", [128, 2048], BF16, st) for i in range(2)]; cvb = [Buf() for _ in range(2)]; cvk = [0]
                    qa = [sb("qa0", [65, S], BF16, st)]; ka = [sb("ka0", [65, S], BF16, st)]
                    qab = [Buf()]; kab = [Buf()]
                    NP = 6
                    pT = [sb(f"pT{i}", [128, TT], BF16, st) for i in range(NP)]; pTb = [Buf() for _ in range(NP)]
                    biasT = [sb(f"biasT{i}", [128, 32], F32, st) for i in range(2)]; biasb = [Buf() for _ in range(2)]
                    osb = [sb(f"osb{i}", [65, TT], F32, st) for i in range(2)]; osbb = [Buf() for _ in range(2)]
                    yat = [sb(f"yat{i}", [64, TT], F32, st) for i in range(2)]; yatb = [Buf() for _ in range(2)]
                    yab = [sb(f"yab{i}", [64, TT], BF16, st) for i in range(2)] ; yabb = [Buf() for _ in range(2)]
                    def gen_C():
                        blkctr = 0
                        pend2 = pend3 = None
                        for h in range(8):
                            hi = 0
                            fw.dma("sp", qa[hi][:], qT_d[h], writes=[qab[hi]])
                            fw.dma("sp", ka[hi][:], kT_d[h], writes=[kab[hi]])
                            for qt in range(8):
                                nkt = 4 * qt + 4
                                bi = (h * 8 + qt) % 2
                                oi = bi
                                po = 6
                                ts(biasT[bi][:, 0:nkt], ckT[:, 0:nkt, h], cref[:, 4 * qt + 3, h:h + 1], -1.0, ALU.subtract, ALU.mult,
                                   [b_ck], [biasb[bi]])

                                SL = (3, 4, 5)
                                LA = 2

                                def s_mm(kt):
                                    slot = SL[(blkctr + kt) % 3]
                                    m = kt - 4 * qt
                                    c0 = 128 * m if m > 0 else 0
                                    def f(e):
                                        ins = e.matmul(PS[slot][:, c0:TT], ka[hi][:, kt * 128:(kt + 1) * 128],
                                                       qa[hi][:, qt * TT + c0:(qt + 1) * TT], start=True, stop=(m < 0))
                                        if m >= 0:
                                            ins = e.matmul(PS[slot][:, c0:c0 + 128], ntri[:], ident_b[:], start=False, stop=True)
                                        return ins
                                    OP("pe", f, [kab[hi], qab[hi], b_ntri], [PSB[slot]])
                                for kt in range(min(LA, nkt)):
                                    s_mm(kt)
                                for kt in range(nkt):
                                    slot = SL[(blkctr + kt) % 3]
                                    if kt + LA < nkt:
                                        s_mm(kt + LA)
                                    m = kt - 4 * qt
                                    c0 = 128 * m if m > 0 else 0
                                    pi = (blkctr + kt) % NP
                                    act(pT[pi][:, c0:TT], PS[slot][:, c0:TT], AF.Exp, [PSB[slot], biasb[bi]], [pTb[pi]],
                                        bias=biasT[bi][:, kt:kt + 1])
                                    OP("pe", lambda e, kt=kt, c0=c0, pi=pi: e.matmul(
                                        PS[po][0:65, c0:TT], V_sb[:, kt, h, :], pT[pi][:, c0:TT], start=(kt == 0), stop=(kt == nkt - 1)),
                                        [pTb[pi], b_V], [PSB[po]])
                                    if kt % 8 == 7 and kt + 1 < nkt:
                                        yield 8
                                blkctr += nkt
                                cp(osb[oi][:], PS[po][0:65, :], [PSB[po]], [osbb[oi]], eng="act")
                                OP("dve", lambda e, oi=oi: e.reciprocal(out=osb[oi][64:65, :], in_=osb[oi][64:65, :]), [osbb[oi]], [osbb[oi]])

                                def phase2(oi=oi, h=h, qt=qt):
                                    OP("pe", lambda e: e.matmul(PS[7][0:64, :], ones_f[64:65, 0:64], osb[oi][64:65, :], start=True, stop=True),
                                       [osbb[oi], b_onesf], [PSB[7]])
                                    tt(yat[oi][:], osb[oi][0:64, :], PS[7][0:64, :], ALU.mult, [osbb[oi], PSB[7]], [yatb[oi]])

                                    def phase3():
                                        hnorm(yat[oi][:], yatb[oi], og_at_sb[:, h:h + 1], b_og, yab[oi][:], yabb[oi], P=64)
                                        fw.dma("sp", ya_d[h, :, qt * TT:(qt + 1) * TT], yab[oi][:], reads=[yabb[oi]])
                                    return phase3
                                if pend3 is not None:
                                    pend3()
                                pend3 = pend2() if pend2 is not None else None
                                pend2 = phase2
                                yield ((nkt - 1) % 8) + 1
                        if pend3 is not None:
                            pend3()
                        if pend2 is not None:
                            pend2()()
                    if l == 0:
                        for l2 in range(nlayers):
                            for e_ in range(NE):
                                for (src, dst, pat) in ((w_gate, wg_d, 8), (w_up, wu_d, 8), (w_down, wd_d, 4)):
                                    for hf in range(2):
                                        i = cvk[0] % 2
                                        cvk[0] += 1
                                        kcs = pat // 2
                                        srcap = src[l2, e_].rearrange("(kc p) n -> p kc n", p=128)[:, hf * kcs:(hf + 1) * kcs, :]
                                        dstv = cv[i][:].rearrange("p (kc n) -> p kc n", kc=kcs)
                                        fw.dma("pool", dstv, srcap, writes=[cvb[i]])
                                        fw.dma("pool", dst[l2 * NE + e_][:, hf * 2048:(hf + 1) * 2048], cv[i][:], reads=[cvb[i]])
                    gB, gC = gen_B(), gen_C()
                    aliveB = aliveC = True
                    cdone, bdone = 0, 0
                    CTOT, BTOT = 8 * sum(4 * q_ + 4 for q_ in range(8)), NT * 9
                    while aliveB or aliveC:
                        if aliveC:
                            try:
                                cdone += next(gC)
                            except StopIteration:
                                aliveC = False
                        while aliveB and (not aliveC or bdone * CTOT <= cdone * BTOT):
                            try:
                                next(gB)
                                bdone += 1
                            except StopIteration:
                                aliveB = False
                    fw.barrier()
            if stop_after == "C":
                break
            NSLOT = 80
            RS = 128
            SUB = RS // 128
            BIG = 1.0e4
            with ExitStack() as dl:
                msk_all = sb("msk_all", [128, 32, 16], F32, dl); eq1_all = sb("eq1_all", [128, 32, 16], F32, dl)
                comb_all = sb("comb_all", [128, 32, 16], F32, dl); b_all = Buf()
                r1i = sb("r1i", [128, 32], I32, dl); r2i = sb("r2i", [128, 32], I32, dl)
                w1s = sb("w1s", [128, 32], F32, dl); w2s = sb("w2s", [128, 32], F32, dl); b_rw = Buf()
                widx = sb("widx", [128, NSLOT], I32, dl); b_slot = Buf()
                with ExitStack() as st:
                    maskT = sb("maskT", [16, S], F32, dl); b_mT = Buf()
                    woa = sb("woa", [128, 4, D], BF16, st); wob = sb("wob", [64, 8, D], BF16, st)
                    fw.dma("pool", woa[:, 0:2, :], w_out[l, 0:256, :].rearrange("(kc p) n -> p kc n", p=128), writes=[Buf()])
                    fw.dma("pool", woa[:, 2:4, :], w_out[l, 768:1024, :].rearrange("(kc p) n -> p kc n", p=128), writes=[Buf()])
                    fw.dma("pool", wob[:], w_out[l, 256:768, :].rearrange("(h p) n -> p h n", p=64), writes=[Buf()])
                    fw.barrier()
                    zrow = sb("zrow", [128, 2048], F32, st); b_z = Buf()
                    OP("dve", lambda e: e.memset(zrow[:], 0.0), writes=[b_z])
                    for c_ in range(NSLOT * RS // 256):
                        fw.dma("pool", Xs_d[c_ * 256:(c_ + 1) * 256, :].rearrange("(p two) n -> p (two n)", two=2), zrow[:], reads=[b_z])
                    xt = sb("xtD", [128, 8, TT], F32, st); xtb = Buf()
                    ys = sb("ys", [128, 4, TT], BF16, st); ysb = Buf()
                    yatt = sb("yatt", [64, 8, TT], BF16, st); yattb = Buf()
                    sqb = sb("sqbD", [128, 8, TT], BF16, st); sqbb = Buf()
                    rt = sb("rtD", [128, TT], F32, st); rtb = Buf()
                    h2f = sb("h2f", [128, 8, TT], F32, st); h2fb = Buf()
                    h2 = sb("h2", [128, 8, TT], BF16, st); h2b = Buf()
                    htok = [sb(f"htok{i}", [128, D], F32, st) for i in range(2)]; htokb = [Buf() for _ in range(2)]
                    aff = sb("aff", [128, 4, 16], F32, st); selv = sb("selv", [128, 4, 16], F32, st); rtmp = sb("rtmp", [128, 4, 16], F32, st)
                    m1 = sb("m1", [128, 16], F32, st); m2 = sb("m2", [128, 16], F32, st); gm = sb("gm", [128, 4], F32, st)
                    b_r = Buf()
                    for it in range(NT):
                        t0 = it * TT
                        msk = msk_all[:, 4 * it:4 * it + 4, :]; comb = comb_all[:, 4 * it:4 * it + 4, :]; eq1 = eq1_all[:, 4 * it:4 * it + 4, :]
                        fw.dma("sp", xt[:], xT_d[:, :, t0:t0 + TT].rearrange("c p t -> p c t"), writes=[xtb])
                        fw.dma("sp", ys[:], yh_d[:, :, t0:t0 + TT].rearrange("c p t -> p c t"), writes=[ysb])
                        fw.dma("sp", yatt[:], ya_d[:, :, t0:t0 + TT].rearrange("h p t -> p h t"), writes=[yattb])
                        for mc in range(8):
                            ps = 4 + mc % 2

                            def f(e, mc=mc, ps=ps):
                                for kc in range(4):
                                    e.matmul(PS[ps][:, :], woa[:, kc, mc * 128:(mc + 1) * 128], ys[:, kc, :], start=(kc == 0), stop=False)
                                for hh in range(8):
                                    ins = e.matmul(PS[ps][:, :], wob[:, hh, mc * 128:(mc + 1) * 128], yatt[:, hh, :], start=False, stop=(hh == 7))
                                return ins
                            OP("pe", f, [ysb, yattb], [PSB[ps]])
                            stt(xt[:, mc, :], PS[ps][:, :], G1[:, mc:mc + 1], xt[:, mc, :], ALU.mult, ALU.add, [PSB[ps], b_mod, xtb], [xtb])
                        if debug and l == 0:
                            fw.dma("sp", dbg["dbg_xmid"][:, :, t0:t0 + TT].rearrange("c p t -> p c t"), xt[:], reads=[xtb])
                        fw.dma("sp", xT_d[:, :, t0:t0 + TT].rearrange("c p t -> p c t"), xt[:], reads=[xtb])
                        norm_mod(st, xt, xtb, A2, B2, b_mod, h2, h2b, h2f, h2fb, sqb, sqbb, rt, rtb, 7)
                        for sub in range(4):
                            hi_ = sub % 2
                            for half in range(2):
                                ps = half

                                def f(e, sub=sub, half=half, ps=ps):
                                    for c4 in range(4):
                                        c = half * 4 + c4
                                        ins = e.transpose(PS[ps][:, c4 * 128:(c4 + 1) * 128], h2f[:, c, sub * 128:(sub + 1) * 128], ident[:])
                                    return ins
                                OP("pe", f, [h2fb, b_ident], [PSB[ps]])
                                cp(htok[hi_][:, half * 512:(half + 1) * 512], PS[ps][:, :], [PSB[ps]], [htokb[hi_]],
                                   eng=("act" if half == 0 else "dve"))
                            fw.dma("sp", h2_d[t0 + sub * 128:t0 + (sub + 1) * 128, :], htok[hi_][:], reads=[htokb[hi_]])
                        for sub in range(4):
                            def f(e, sub=sub):
                                for kc in range(8):
                                    ins = e.matmul(PS[6][:, sub * 16:(sub + 1) * 16], h2f[:, kc, sub * 128:(sub + 1) * 128], wr_sb[:, kc, :],
                                                   start=(kc == 0), stop=(kc == 7))
                                return ins
                            OP("pe", f, [h2fb, b_wr], [PSB[6]])
                        act(aff[:].rearrange("p s e -> p (s e)"), PS[6][:, 0:64], AF.Sigmoid, [PSB[6]], [b_r])
                        tt(selv[:], aff[:], rb_sb[:].unsqueeze(1).to_broadcast([128, 4, 16]), ALU.add, [b_r, b_rb], [b_r])
                        s44 = selv[:].rearrange("p s (g e) -> p (s g) e", e=4)
                        r44 = rtmp[:].rearrange("p s (g e) -> p (s g) e", e=4)
                        RD = lambda o, i_, op: OP("dve", lambda e: e.tensor_reduce(out=o, in_=i_, axis=mybir.AxisListType.X, op=op), [b_r, b_all], [b_r, b_all])
                        RD(m1[:], s44, ALU.max)
                        tt(r44, s44, m1[:].unsqueeze(2).to_broadcast([128, 16, 4]), ALU.is_equal, [b_r], [b_r])
                        stt(r44, r44, -BIG, s44, ALU.mult, ALU.add, [b_r], [b_r])
                        RD(m2[:], r44, ALU.max)
                        tt(m1[:], m1[:], m2[:], ALU.add, [b_r], [b_r])
                        gs = m1[:].rearrange("p (s g) -> p s g", g=4)
                        RD(gm[:], gs, ALU.max)
                        m2v = m2[:].rearrange("p (s g) -> p s g", g=4)
                        tt(m2v, gs, gm[:].unsqueeze(2).to_broadcast([128, 4, 4]), ALU.is_equal, [b_r], [b_r])
                        ts(m2[:], m2[:], BIG, -BIG, ALU.mult, ALU.add, [b_r], [b_r])
                        tt(r44, s44, m2[:].unsqueeze(2).to_broadcast([128, 16, 4]), ALU.add, [b_r], [b_r])
                        RD(gm[:], rtmp[:], ALU.max)
                        tt(eq1, rtmp[:], gm[:].unsqueeze(2).to_broadcast([128, 4, 16]), ALU.is_equal, [b_r, b_all], [b_r, b_all])
                        stt(msk, eq1, -BIG, rtmp[:], ALU.mult, ALU.add, [b_r, b_all], [b_r, b_all])
                        RD(gm[:], msk, ALU.max)
                        tt(msk, rtmp[:], gm[:].unsqueeze(2).to_broadcast([128, 4, 16]), ALU.is_ge, [b_r, b_all], [b_r, b_all])
                        tt(comb, aff[:], msk, ALU.mult, [b_r, b_all], [b_r, b_all])
                        RD(gm[:], comb, ALU.add)
                        OP("dve", lambda e: e.reciprocal(out=gm[:], in_=gm[:]), [b_r], [b_r])
                        tt(comb, comb, gm[:].unsqueeze(2).to_broadcast([128, 4, 16]), ALU.mult, [b_r, b_all], [b_r, b_all])
                        if debug and l == 0:
                            fw.dma("sp", dbg["dbg_comb"][t0:t0 + TT, :].rearrange("(s p) e -> p s e", p=128), comb, reads=[b_all])

                        def f(e, it=it):
                            for sub in range(4):
                                ins = e.transpose(PS[6][0:16, sub * 128:(sub + 1) * 128], msk_all[:, 4 * it + sub, :], ident[:])
                            return ins
                        OP("pe", f, [b_all, b_ident], [PSB[6]])
                        cp(maskT[:, t0:t0 + TT], PS[6][0:16, :], [PSB[6]], [b_mT])
                    fw.barrier()
                with ExitStack() as st:
                    inc = sb("inc", [16, S], F32, st); b_s = Buf()
                    cntf = sb("cntf", [16, 2], F32, st); slf = sb("slf", [16, 2], F32, st); offf = sb("offf", [16, 1], F32, st)
                    endf = sb("endf", [16, 1], F32, st); cnti = sb("cnti", [16, 2], I32, st)
                    cmpt = sb("cmpt", [16, NSLOT], F32, st); sef = sb("sef", [128, NSLOT], F32, st); pidx = sb("pidx", [128, 1], F32, st); pit = sb("pit", [128, 128], F32, st)
                    pos_all = sb("pos_all", [128, 32, 16], F32, st); tmp3 = sb("tmp3", [128, 32, 16], F32, st)
                    rf = sb("rf", [128, 32], F32, st)
                    OP("dve", lambda e: e.tensor_tensor_scan(out=inc[:], data0=ones_f[0:16, 0:1].to_broadcast([16, S]), data1=maskT[:],
                                                             initial=0.0, op0=ALU.mult, op1=ALU.add), [b_mT, b_onesf], [b_s])
                    ts(cntf[:], inc[:, S - 1:S].to_broadcast([16, 2]), 1.0 / RS, (RS - 1.0) / RS - (RS - 1.0) / (2 * RS), ALU.mult, ALU.add, [b_s], [b_s])
                    cp(cnti[:], cntf[:], [b_s], [b_s])
                    cp(slf[:], cnti[:], [b_s], [b_s])
                    OP("pe", lambda e: e.matmul(PS[0][0:16, 0:2], tri_f[0:16, 0:16], slf[:], start=True, stop=True), [b_s, b_tri], [PSB[0]])
                    cp(offf[:], PS[0][0:16, 0:1], [PSB[0]], [b_s])
                    tt(endf[:], offf[:], slf[:, 0:1], ALU.add, [b_s], [b_s])
                    ts(offf[:], offf[:], float(RS), None, ALU.mult, None, [b_s], [b_s])
                    tt(inc[:], inc[:], maskT[:], ALU.subtract, [b_s, b_mT], [b_s])
                    ts(inc[:], inc[:], offf[:, 0:1], None, ALU.add, None, [b_s], [b_s])

                    def f(e):
                        for tk in range(32):
                            ins = e.transpose(PS[1][:, tk * 16:(tk + 1) * 16], inc[:, tk * 128:(tk + 1) * 128], ident[0:16, 0:16])
                        return ins
                    OP("pe", f, [b_s, b_ident], [PSB[1]])
                    cp(pos_all[:].rearrange("p k e -> p (k e)"), PS[1][:, :], [PSB[1]], [b_s])
                    RD2 = lambda o, i_: OP("dve", lambda e: e.tensor_reduce(out=o, in_=i_, axis=mybir.AxisListType.X, op=ALU.add), [b_s, b_all], [b_s, b_rw])
                    tt(tmp3[:], eq1_all[:], pos_all[:], ALU.mult, [b_s, b_all], [b_s])
                    RD2(rf[:], tmp3[:])
                    cp(r1i[:], rf[:], [b_s], [b_rw])
                    tt(tmp3[:], eq1_all[:], comb_all[:], ALU.mult, [b_s, b_all], [b_s])
                    RD2(w1s[:], tmp3[:])
                    tt(eq1_all[:], msk_all[:], eq1_all[:], ALU.subtract, [b_all], [b_all])
                    tt(tmp3[:], eq1_all[:], pos_all[:], ALU.mult, [b_s, b_all], [b_s])
                    RD2(rf[:], tmp3[:])
                    cp(r2i[:], rf[:], [b_s], [b_rw])
                    tt(tmp3[:], eq1_all[:], comb_all[:], ALU.mult, [b_s, b_all], [b_s])
                    RD2(w2s[:], tmp3[:])
                    ts(cmpt[:], iota[0:16, 0:NSLOT], endf[:, 0:1], None, ALU.is_ge, None, [b_iota, b_s], [b_s])
                    OP("pe", lambda e: e.matmul(PS[2][:, 0:NSLOT], ones_f[0:16, :], cmpt[:], start=True, stop=True), [b_s, b_onesf], [PSB[2]])
                    ts(sef[:], PS[2][:, 0:NSLOT], 15.0, float(l * NE), ALU.min, ALU.add, [PSB[2]], [b_s])
                    tt(pit[:], ident[:], iota[:, 0:128], ALU.mult, [b_ident, b_iota], [b_s])
                    OP("dve", lambda e: e.tensor_reduce(out=pidx[:], in_=pit[:], axis=mybir.AxisListType.X, op=ALU.add), [b_s], [b_s])
                    stt(sef[:], sef[:], 128.0, pidx[:, 0:1].to_broadcast([128, NSLOT]), ALU.mult, ALU.add, [b_s], [b_s])
                    cp(widx[:], sef[:], [b_s], [b_slot])
                    fw.barrier()
                with ExitStack() as st:
                    hrow = [sb(f"hrow{i}", [128, D], F32, st) for i in range(3)]; hrowb = [Buf() for _ in range(3)]
                    for tk in range(32):
                        i = tk % 3
                        fw.dma("sp", hrow[i][:], h2_d[tk * 128:(tk + 1) * 128, :], writes=[hrowb[i]])
                        for ri in (r1i, r2i):
                            fw.dma_ind(Xs_d[:, :], bass.IndirectOffsetOnAxis(ap=ri[:, tk:tk + 1], axis=0), hrow[i][:], None,
                                       reads=[hrowb[i], b_rw])
                    fw.barrier()
                with ExitStack() as st:
                    wg = [sb(f"wg{i}", [128, 8, DE], BF16, st) for i in range(2)]; wgb = [Buf() for _ in range(2)]
                    wu = [sb(f"wu{i}", [128, 8, DE], BF16, st) for i in range(2)]; wub = [Buf() for _ in range(2)]
                    wd = [sb(f"wd{i}", [128, 4, D], BF16, st) for i in range(2)]; wdb = [Buf() for _ in range(2)]
                    xs = [sb(f"xs{i}", [128, D], F32, st) for i in range(3)]; xsb = [Buf() for _ in range(3)]
                    xsT = [sb(f"xsT{i}", [128, 8, 128], BF16, st) for i in range(2)]; xsTb = [Buf() for _ in range(2)]
                    sg = [sb(f"sg{i}", [128, DE], F32, st) for i in range(2)]; sgb = [Buf() for _ in range(2)]
                    hd = [sb(f"hd{i}", [128, DE], F32, st) for i in range(2)]; hdb = [Buf() for _ in range(2)]
                    hdT = [sb(f"hdT{i}", [128, 4, 128], BF16, st) for i in range(2)]; hdTb = [Buf() for _ in range(2)]
                    yt = [sb(f"yt{i}", [128, D], F32, st) for i in range(2)]; ytb = [Buf() for _ in range(2)]

                    wg_rows = wg_d.rearrange("e p n -> (e p) n"); wu_rows = wu_d.rearrange("e p n -> (e p) n"); wd_rows = wd_d.rearrange("e p n -> (e p) n")

                    def load_gu(s_):
                        i = s_ % 2
                        off = bass.IndirectOffsetOnAxis(ap=widx[:, s_:s_ + 1], axis=0)
                        fw.dma_ind(wg[i][:].rearrange("p k n -> p (k n)"), None, wg_rows, off, reads=[b_slot], writes=[wgb[i]])
                        fw.dma_ind(wu[i][:].rearrange("p k n -> p (k n)"), None, wu_rows, off, reads=[b_slot], writes=[wub[i]])

                    def load_d(s_):
                        i = s_ % 2
                        off = bass.IndirectOffsetOnAxis(ap=widx[:, s_:s_ + 1], axis=0)
                        fw.dma_ind(wd[i][:].rearrange("p k n -> p (k n)"), None, wd_rows, off, reads=[b_slot], writes=[wdb[i]])

                    def load_x(u_):
                        fw.dma("sp", xs[u_ % 3][:], Xs_d[u_ * 128:(u_ + 1) * 128, :], writes=[xsb[u_ % 3]])

                    def st_T(u_):
                        i = u_ % 2
                        x3 = u_ % 3
                        for half in range(2):
                            def f(e, half=half):
                                for c4 in range(4):
                                    c = half * 4 + c4
                                    ins = e.transpose(PS[half][:, c4 * 128:(c4 + 1) * 128], xs[x3][:, c * 128:(c + 1) * 128], ident[:])
                                return ins
                            OP("pe", f, [xsb[x3], b_ident], [PSB[half]])
                            cp(xsT[i][:, half * 4:(half + 1) * 4, :], PS[half][:, :].rearrange("p (c t) -> p c t", c=4), [PSB[half]], [xsTb[i]],
                               eng=("act" if half == 0 else "dve"))

                    def st_GU(u_):
                        i = u_ % 2
                        wi = (u_ // SUB) % 2
                        for (pp, w_, wb__) in ((2, wg[wi], wgb[wi]), (3, wu[wi], wub[wi])):
                            def f(e, pp=pp, w_=w_):
                                for kc in range(8):
                                    ins = e.matmul(PS[pp][:, :], xsT[i][:, kc, :], w_[:, kc, :], start=(kc == 0), stop=(kc == 7))
                                return ins
                            OP("pe", f, [xsTb[i], wb__], [PSB[pp]])
                        act(sg[i][:], PS[2][:, :], AF.Silu, [PSB[2]], [sgb[i]])
                        tt(hd[i][:], PS[3][:, :], sg[i][:], ALU.mult, [PSB[3], sgb[i]], [hdb[i]])

                    def st_HT(u_):
                        i = u_ % 2

                        def f(e):
                            for c4 in range(4):
                                ins = e.transpose(PS[4][:, c4 * 128:(c4 + 1) * 128], hd[i][:, c4 * 128:(c4 + 1) * 128], ident[:])
                            return ins
                        OP("pe", f, [hdb[i], b_ident], [PSB[4]])
                        cp(hdT[i][:], PS[4][:, :].rearrange("p (c t) -> p c t", c=4), [PSB[4]], [hdTb[i]], eng="act")

                    def st_D(u_):
                        i = u_ % 2
                        wi = (u_ // SUB) % 2
                        for half in range(2):
                            ps = 5 + half

                            def f(e, half=half, ps=ps):
                                for kc in range(4):
                                    ins = e.matmul(PS[ps][:, :], hdT[i][:, kc, :], wd[wi][:, kc, half * 512:(half + 1) * 512], start=(kc == 0), stop=(kc == 3))
                                return ins
                            OP("pe", f, [hdTb[i], wdb[wi]], [PSB[ps]])
                            cp(yt[i][:, half * 512:(half + 1) * 512], PS[ps][:, :], [PSB[ps]], [ytb[i]], eng=("dve" if half == 0 else "act"))
                        fw.dma("sp", Ys_d[u_ * 128:(u_ + 1) * 128, :], yt[i][:], reads=[ytb[i]])

                    NU = SUB * NSLOT
                    for s_ in range(2):
                        load_gu(s_)
                        load_d(s_)
                    for u_ in range(3):
                        load_x(u_)
                    for step in range(NU + 3):
                        if step < NU:
                            st_T(step)
                            if step + 3 < NU:
                                load_x(step + 3)
                        if 0 <= step - 1 < NU:
                            u_ = step - 1
                            st_GU(u_)
                            if u_ % SUB == SUB - 1 and u_ // SUB + 2 < NSLOT:
                                load_gu(u_ // SUB + 2)
                        if 0 <= step - 2 < NU:
                            st_HT(step - 2)
                        if 0 <= step - 3 < NU:
                            u_ = step - 3
                            st_D(u_)
                            if u_ % SUB == SUB - 1 and u_ // SUB + 2 < NSLOT:
                                load_d(u_ // SUB + 2)
                    fw.barrier()
                with ExitStack() as st:
                    y1 = [sb(f"y1_{i}", [128, D], F32, st) for i in range(2)]; y2 = [sb(f"y2_{i}", [128, D], F32, st) for i in range(2)]
                    y1b = [Buf() for _ in range(2)]; y2b = [Buf() for _ in range(2)]
                    ac = [sb(f"ac{i}", [128, D], F32, st) for i in range(2)]; acb = [Buf() for _ in range(2)]
                    xm = [sb(f"xm{i}", [128, 8, 128], F32, st) for i in range(2)]; xmb = [Buf() for _ in range(2)]
                    otile = [sb(f"otile{i}", [128, D], F32, st) for i in range(2)]; otb = [Buf() for _ in range(2)]
                    def issue5(tk):
                        i = tk % 2
                        fw.dma_ind(y1[i][:], None, Ys_d[:, :], bass.IndirectOffsetOnAxis(ap=r1i[:, tk:tk + 1], axis=0), reads=[b_rw], writes=[y1b[i]])
                        fw.dma_ind(y2[i][:], None, Ys_d[:, :], bass.IndirectOffsetOnAxis(ap=r2i[:, tk:tk + 1], axis=0), reads=[b_rw], writes=[y2b[i]])
                        fw.dma("sp", xm[i][:], xT_d[:, :, tk * 128:(tk + 1) * 128].rearrange("c p t -> p c t"), writes=[xmb[i]])
                    issue5(0)
                    for tk in range(32):
                        i = tk % 2
                        if tk + 1 < 32:
                            issue5(tk + 1)
                        ts(ac[i][:], y1[i][:], w1s[:, tk:tk + 1], None, ALU.mult, None, [y1b[i], b_rw], [acb[i]])
                        stt(ac[i][:], y2[i][:], w2s[:, tk:tk + 1], ac[i][:], ALU.mult, ALU.add, [y2b[i], b_rw, acb[i]], [acb[i]])
                        for half in range(2):
                            ps = 2 * (tk % 2) + half

                            def f(e, half=half, ps=ps):
                                for c4 in range(4):
                                    c = half * 4 + c4
                                    ins = e.transpose(PS[ps][:, c4 * 128:(c4 + 1) * 128], ac[i][:, c * 128:(c + 1) * 128], ident[:])
                                return ins
                            OP("pe", f, [acb[i], b_ident], [PSB[ps]])
                            for c4 in range(4):
                                c = half * 4 + c4
                                stt(xm[i][:, c, :], PS[ps][:, c4 * 128:(c4 + 1) * 128], G2[:, c:c + 1], xm[i][:, c, :], ALU.mult, ALU.add,
                                    [PSB[ps], b_mod, xmb[i]], [xmb[i]])
                        if not last:
                            fw.dma("sp", xT_d[:, :, tk * 128:(tk + 1) * 128].rearrange("c p t -> p c t"), xm[i][:], reads=[xmb[i]])
                        else:
                            for half in range(2):
                                ps = 4 + 2 * (tk % 2) + half

                                def f(e, half=half, ps=ps):
                                    for c4 in range(4):
                                        c = half * 4 + c4
                                        ins = e.transpose(PS[ps][:, c4 * 128:(c4 + 1) * 128], xm[i][:, c, :], ident[:])
                                    return ins
                                OP("pe", f, [xmb[i], b_ident], [PSB[ps]])
                                cp(otile[i][:, half * 512:(half + 1) * 512], PS[ps][:, :], [PSB[ps]], [otb[i]], eng=("act" if half == 0 else "dve"))
                            fw.dma("sp", out_d[tk * 128:(tk + 1) * 128, :], otile[i][:], reads=[otb[i]])
                    fw.barrier()
    fw.barrier()
    return nc, fw, dbg


def host_inputs(inp, b):
    f = np.float32
    A = np.ascontiguousarray
    m = {}
    m["x"] = A(inp["x"][b])
    m["c_fm"] = A(inp["c"][b].reshape(8, 128).T)
    m["ada_w"] = inp["ada_w"]
    m["ada_b_fm"] = A(inp["ada_b"].reshape(2, 48, 128).transpose(0, 2, 1))
    m["n1g"] = A(inp["norm1_g"].reshape(2, 8, 128).transpose(0, 2, 1))
    m["n2g"] = A(inp["norm2_g"].reshape(2, 8, 128).transpose(0, 2, 1))
    m["w_in"] = inp["w_in"]
    m["fb"] = A(inp["forget_b"].reshape(2, 8, 1))
    def gp_lay(a):
        return A(a.reshape(2, 8, 2, 64).transpose(0, 2, 3, 1).reshape(2, 128, 8))
    m["lam_re"] = gp_lay(inp["lam_re"])
    m["lam_im"] = gp_lay(inp["lam_im"])
    m["log_dt"] = gp_lay(np.broadcast_to(inp["log_dt"][:, :, None], (2, 16, 64)))
    m["sb_re"] = A(inp["ssm_b_re"].reshape(2, 8, 2, 64, 16).transpose(0, 2, 3, 1, 4).reshape(2, 128, 8, 16))
    m["sb_im"] = A(inp["ssm_b_im"].reshape(2, 8, 2, 64, 16).transpose(0, 2, 3, 1, 4).reshape(2, 128, 8, 16))
    m["sc_re"] = A(inp["ssm_c_re"].reshape(2, 8, 2, 16, 64).transpose(0, 2, 4, 1, 3).reshape(2, 128, 8, 16))
    m["sc_im"] = A(inp["ssm_c_im"].reshape(2, 8, 2, 16, 64).transpose(0, 2, 4, 1, 3).reshape(2, 128, 8, 16))
    m["ssm_d"] = A(inp["ssm_d"].reshape(2, 2, 128).transpose(0, 2, 1))
    m["glu_w"] = inp["glu_w"]
    m["glu_b"] = A(inp["glu_b"].reshape(2, 2, 128).transpose(0, 2, 1))
    m["qg"] = A(np.tile(inp["q_norm_g"], (1, 2)).reshape(2, 128, 1))
    m["kg"] = A(np.tile(inp["k_norm_g"], (1, 2)).reshape(2, 128, 1))
    m["conv_w"] = A(inp["conv_w"].reshape(2, 3, 2, 128).transpose(0, 3, 2, 1))
    m["og_fm"] = A(inp["out_norm_g"].reshape(2, 8, 128).transpose(0, 2, 1))
    m["og_at"] = A(inp["out_norm_g"][:, 256:768].reshape(2, 8, 64).transpose(0, 2, 1))
    m["w_out"] = inp["w_out"]
    m["w_router"] = inp["w_router"]
    m["rbias"] = A(np.broadcast_to(inp["router_bias"][None, :], (128, 16)))
    m["w_gate"] = inp["w_gate"]
    m["w_up"] = inp["w_up"]
    m["w_down"] = inp["w_down"]
    m["ident"] = np.eye(128, dtype=f)
    e127 = np.zeros((128, 128), f); e127[127, :] = 1
    m["e127"] = e127
    blk = np.zeros((128, 128), f); blk[:64, :64] = 1; blk[64:, 64:] = 1
    m["blk64"] = blk
    m["tri"] = np.triu(np.ones((128, 128), f))
    m["iota"] = A(np.broadcast_to(np.arange(TT + 1, dtype=f)[None, :], (128, TT + 1)))
    sel = np.zeros((16, 16, 128), f)
    for e in range(16):
        sel[e, e, :] = 1
    m["sel16"] = sel
    return {k: np.asarray(v, dtype=f) for k, v in m.items()}


_CACHE = {}


def kernel(**inputs):
    inp = {k: np.asarray(v) for k, v in inputs.items()}
    if "nc" not in _CACHE:
        _CACHE["nc"] = build_program()[0]
    nc = _CACHE["nc"]
    in_maps = [host_inputs(inp, b) for b in range(8)]
    res = run_bass_kernel_spmd(nc, in_maps, core_ids=list(range(8)))
    out = np.stack([np.asarray(r["out"]) for r in res.results], axis=0)
    return out.astype(np.float32)
```

```python
import numpy as np
from contextlib import ExitStack
import concourse.bass as bass
import concourse.mybir as mybir
from concourse.bass_utils import run_bass_kernel_spmd

F32 = mybir.dt.float32
BF16 = mybir.dt.bfloat16
I32 = mybir.dt.int32
AF = mybir.ActivationFunctionType
ALU = mybir.AluOpType

S = 4096
D = 1024
TT = 512
NT = S // TT
DIN = 2568
NE = 16
DE = 512
EPS = 1e-6
TWO_PI = 6.283185307179586
import os as _os
NOCONV = bool(_os.environ.get('NOCONV'))
POOLENG = _os.environ.get('POOLENG', 'pool')
OFF_U, OFF_Q, OFF_K, OFF_V, OFF_F, OFF_HC, OFF_BG, OFF_CG = 0, 256, 768, 1280, 1792, 1800, 2056, 2312


class Buf:
    __slots__ = ("name", "w", "r")

    def __init__(self, name=""):
        self.name = name
        self.w = {}
        self.r = {}


class Fw:
    ENG = ("pe", "act", "dve", "pool", "sp")

    def __init__(self, nc, ndma=20):
        self.nc = nc
        self.eng = dict(pe=nc.tensor, act=nc.scalar, dve=nc.vector, pool=nc.gpsimd, sp=nc.sync)
        self.sem = {e: nc.alloc_semaphore("sem_" + e) for e in self.ENG}
        self.cnt = {e: 0 for e in self.ENG}
        self.known = {e: {} for e in self.ENG}
        self.dq = {}
        for q in ("sp", "pool"):
            self.dq[q] = dict(sems=[nc.alloc_semaphore(f"dq_{q}_{i}") for i in range(ndma)],
                              uses=[0] * ndma, nxt=0)
        self.allsems = {}
        self.nwaits = 0
        self.bg_on = False
        self.bg_sems = {s_.num for s_ in self.dq["pool"]["sems"]}

    def _wait(self, e, sem, val):
        k = self.known[e]
        if k.get(sem.num, 0) >= val:
            return
        self.eng[e].wait_ge(sem, val)
        self.nwaits += 1
        k[sem.num] = val

    def _deps(self, e, reads, writes):
        need = {}
        mysem = self.sem[e].num

        def add(tok, same_ok):
            sem, val = tok
            if same_ok and sem.num == mysem and e == "pe":
                return
            if need.get(sem.num, (None, 0))[1] < val:
                need[sem.num] = (sem, val)

        for b in reads:
            for tok in b.w.values():
                add(tok, False)
        for b in writes:
            for tok in b.w.values():
                add(tok, True)
            for tok in b.r.values():
                add(tok, True)
        for sem, val in need.values():
            self._wait(e, sem, val)

    def op(self, e, fn, reads=(), writes=()):
        self._deps(e, reads, writes)
        ins = fn(self.eng[e])
        self.cnt[e] += 1
        sem = self.sem[e]
        ins.then_inc(sem, 1)
        tok = (sem, self.cnt[e])
        for b in reads:
            b.r[sem.num] = tok
        for b in writes:
            b.w = {sem.num: tok}
            b.r = {}
        self.allsems[sem.num] = tok
        return tok

    def dma(self, q, out, in_, reads=(), writes=()):
        d = self.dq[q]
        i = d["nxt"]
        d["nxt"] = (i + 1) % len(d["sems"])
        sem = d["sems"][i]
        if d["uses"][i] > 0:
            self._wait(q, sem, 16 * d["uses"][i])
        self._deps(q, reads, writes)
        ins = self.eng[q].dma_start(out=out, in_=in_)
        d["uses"][i] += 1
        tok = (sem, 16 * d["uses"][i])
        ins.then_inc(sem, 16)
        for b in reads:
            b.r[sem.num] = tok
        for b in writes:
            b.w = {sem.num: tok}
            b.r = {}
        self.allsems[sem.num] = tok
        return tok

    def dma_ind(self, out, out_off, in_, in_off, reads=(), writes=()):
        q = "pool"
        d = self.dq[q]
        i = d["nxt"]
        d["nxt"] = (i + 1) % len(d["sems"])
        sem = d["sems"][i]
        if d["uses"][i] > 0:
            self._wait(q, sem, 16 * d["uses"][i])
        self._deps(q, reads, writes)
        ins = self.eng[q].indirect_dma_start(out=out, out_offset=out_off, in_=in_, in_offset=in_off)
        d["uses"][i] += 1
        tok = (sem, 16 * d["uses"][i])
        ins.then_inc(sem, 16)
        for b in reads:
            b.r[sem.num] = tok
        for b in writes:
            b.w = {sem.num: tok}
            b.r = {}
        self.allsems[sem.num] = tok
        return tok

    def barrier(self):
        for e in self.ENG:
            for sem, val in list(self.allsems.values()):
                if sem.num == self.sem[e].num:
                    continue
                if self.bg_on and sem.num in self.bg_sems:
                    continue
                self._wait(e, sem, val)


def build_program(nlayers=2, debug=False, stop_after=None):
    nc = bass.Bass("TRN2", target_bir_lowering=False)
    fw = Fw(nc)
    dbg = {}

    def din(name, shape, dt=F32):
        return nc.dram_tensor(name, list(shape), dt, kind="ExternalInput").ap()

    def dscr(name, shape, dt=F32):
        if debug:
            return nc.dram_tensor(name, list(shape), dt, kind="ExternalOutput").ap()
        return nc.dram_tensor(name, list(shape), dt).ap()

    x_in = din("x", [S, D])
    c_fm = din("c_fm", [128, 8])
    ada_w = din("ada_w", [2, D, 6 * D])
    ada_b_fm = din("ada_b_fm", [2, 128, 48])
    n1g = din("n1g", [2, 128, 8])
    n2g = din("n2g", [2, 128, 8])
    w_in = din("w_in", [2, D, DIN])
    fb = din("fb", [2, 8, 1])
    lam_re = din("lam_re", [2, 128, 8])
    lam_im = din("lam_im", [2, 128, 8])
    log_dt = din("log_dt", [2, 128, 8])
    sb_re = din("sb_re", [2, 128, 8, 16])
    sb_im = din("sb_im", [2, 128, 8, 16])
    sc_re = din("sc_re", [2, 128, 8, 16])
    sc_im = din("sc_im", [2, 128, 8, 16])
    ssm_d = din("ssm_d", [2, 128, 2])
    glu_w = din("glu_w", [2, 256, 256])
    glu_b = din("glu_b", [2, 128, 2])
    qg = din("qg", [2, 128, 1])
    kg = din("kg", [2, 128, 1])
    conv_w = din("conv_w", [2, 128, 2, 3])
    og_fm = din("og_fm", [2, 128, 8])
    og_at = din("og_at", [2, 64, 8])
    w_out = din("w_out", [2, D, D])
    w_router = din("w_router", [D, NE])
    rbias = din("rbias", [128, NE])
    w_gate = din("w_gate", [2, NE, D, DE])
    w_up = din("w_up", [2, NE, D, DE])
    w_down = din("w_down", [2, NE, DE, D])
    ident_in = din("ident", [128, 128])
    e127_in = din("e127", [128, 128])
    blk64_in = din("blk64", [128, 128])
    tri_in = din("tri", [128, 128])
    iota_in = din("iota", [128, TT + 1])
    sel_in = din("sel16", [16, NE, 128])
    out_d = nc.dram_tensor("out", [S, D], F32, kind="ExternalOutput").ap()

    xT_d = dscr("xT_d", [8, 128, S])
    wg_d = dscr("wg_d", [2 * NE, 128, 8 * DE], BF16)
    wu_d = dscr("wu_d", [2 * NE, 128, 8 * DE], BF16)
    wd_d = dscr("wd_d", [2 * NE, 128, 4 * D], BF16)
    uT_d = dscr("uT_d", [2, 128, S])
    qT_d = dscr("qT_d", [8, 65, S], BF16)
    kT_d = dscr("kT_d", [8, 65, S], BF16)
    yh_d = dscr("yh_d", [4, 128, S], BF16)
    ya_d = dscr("ya_d", [8, 64, S], BF16)
    h2_d = dscr("h2_d", [S, D])
    Xs_d = dscr("Xs_d", [80 * 128, D])
    Ys_d = dscr("Ys_d", [80 * 128, D])
    if debug:
        for nm, shp, dt in (("dbg_h", [8, 128, S], BF16), ("dbg_cum", [8, S], F32), ("dbg_mod", [128, 48], F32),
                            ("dbg_v", [128, 32, 8, 65], BF16), ("dbg_ssmpre", [2, 128, S], F32),
                            ("dbg_comb", [S, NE], F32), ("dbg_xmid", [8, 128, S], F32)):
            dbg[nm] = nc.dram_tensor(nm, shp, dt, kind="ExternalOutput").ap()

    es = ExitStack()

    uid = [0]

    def sb(name, shape, dt=F32, stack=None):
        uid[0] += 1
        return (stack or es).enter_context(nc.sbuf_tensor(f"s{uid[0]}_{name}", list(shape), dt))

    PS = [es.enter_context(nc.psum_tensor(f"ps{i}", [128, 512], F32)) for i in range(8)]
    PSB = [Buf(f"ps{i}") for i in range(8)]

    ident = sb("ident", [128, 128]); b_ident = Buf()
    e127 = sb("e127", [128, 128]); b_e127 = Buf()
    tri_f = sb("tri_f", [128, 128]); b_tri = Buf()
    blk_f = sb("blk_f", [128, 128]); blk = sb("blk", [128, 128], BF16); b_blk = Buf()
    ones_b = sb("ones_b", [128, 128], BF16); b_ones = Buf()
    ones_f = sb("ones_f", [128, 128]); b_onesf = Buf()
    iota = sb("iota", [128, TT + 1]); b_iota = Buf()
    sel16_f = sb("sel16_f", [16, NE, 128]); b_sel = Buf()
    cfm = sb("cfm", [128, 8]); b_cfm = Buf()
    cact = sb("cact", [128, 8]); b_cact = Buf()
    rb_sb = sb("rb_sb", [128, NE]); b_rb = Buf()
    wr_sb = sb("wr_sb", [128, 8, NE]); b_wr = Buf()

    fw.dma("sp", ident[:], ident_in, writes=[b_ident])
    fw.dma("sp", e127[:], e127_in, writes=[b_e127])
    fw.dma("sp", tri_f[:], tri_in, writes=[b_tri])
    fw.dma("sp", blk_f[:], blk64_in, writes=[b_blk])
    fw.dma("sp", iota[:], iota_in, writes=[b_iota])
    fw.dma("sp", sel16_f[:], sel_in, writes=[b_sel])
    fw.dma("sp", cfm[:], c_fm, writes=[b_cfm])
    fw.dma("sp", rb_sb[:], rbias, writes=[b_rb])
    fw.dma("sp", wr_sb[:], w_router.rearrange("(kc p) n -> p kc n", p=128), writes=[b_wr])
    fw.op("dve", lambda e: e.tensor_copy(out=blk[:], in_=blk_f[:]), reads=[b_blk], writes=[b_blk])
    fw.op("dve", lambda e: e.memset(ones_b[:], 1.0), writes=[b_ones])
    fw.op("dve", lambda e: e.memset(ones_f[:], 1.0), writes=[b_onesf])
    fw.op("act", lambda e: e.activation(out=cact[:], in_=cfm[:], func=AF.Silu), reads=[b_cfm], writes=[b_cact])

    def OP(e, fn, reads=(), writes=()):
        return fw.op(e, fn, reads, writes)

    def act(out, in_, func, reads, writes, scale=1.0, bias=0.0):
        return fw.op("act", lambda e: e.activation(out=out, in_=in_, func=func, bias=bias, scale=scale), reads, writes)

    def tt(out, in0, in1, op, reads, writes, eng="dve"):
        return fw.op(eng, lambda e: e.tensor_tensor(out=out, in0=in0, in1=in1, op=op), reads, writes)

    def ts(out, in0, s1, s2, op0, op1, reads, writes, eng="dve"):
        if op1 is None:
            return fw.op(eng, lambda e: e.tensor_scalar(out=out, in0=in0, scalar1=s1, scalar2=None, op0=op0), reads, writes)
        return fw.op(eng, lambda e: e.tensor_scalar(out=out, in0=in0, scalar1=s1, scalar2=s2, op0=op0, op1=op1), reads, writes)

    def stt(out, in0, scalar, in1, op0, op1, reads, writes):
        return fw.op("dve", lambda e: e.scalar_tensor_tensor(out=out, in0=in0, scalar=scalar, in1=in1, op0=op0, op1=op1),
                     reads, writes)

    def cp(out, in_, reads, writes, eng="dve"):
        if eng == "act":
            return fw.op("act", lambda e: e.copy(out=out, in_=in_), reads, writes)
        return fw.op(eng, lambda e: e.tensor_copy(out=out, in_=in_), reads, writes)

    ntri = sb("ntri", [128, 128], BF16); ident_b = sb("ident_b", [128, 128], BF16); b_ntri = Buf()
    tt(tri_f[:], tri_f[:], ident[:], ALU.subtract, [b_tri, b_ident], [b_tri])
    ts(ntri[:], tri_f[:], -30000.0, None, ALU.mult, None, [b_tri], [b_ntri])
    cp(ident_b[:], ident[:], [b_ident], [b_ntri])
    eps_c = sb("eps_c", [128, 1]); b_eps = Buf()
    OP("dve", lambda e: e.memset(eps_c[:], EPS), writes=[b_eps])

    hn_sq = [sb(f"hn_sq{i}", [128, TT], BF16) for i in range(2)]; hn_sqb = [Buf() for _ in range(2)]
    hn_rt = [sb(f"hn_rt{i}", [128, TT]) for i in range(2)]; hn_rtb = [Buf() for _ in range(2)]
    hn_ctr = [0]
    HN_PS = 7

    def hnorm(src, srcb, g_ap, gb, out, outb, P=128, n=TT):
        i = hn_ctr[0] % 2
        hn_ctr[0] += 1
        act(hn_sq[i][0:P, 0:n], src, AF.Square, [srcb], [hn_sqb[i]])
        OP("pe", lambda e: e.matmul(PS[HN_PS][0:P, 0:n], blk[0:P, 0:P], hn_sq[i][0:P, 0:n], start=True, stop=True),
           [hn_sqb[i], b_blk], [PSB[HN_PS]])
        act(hn_rt[i][0:P, 0:n], PS[HN_PS][0:P, 0:n], AF.Ln, [PSB[HN_PS], b_eps], [hn_rtb[i]], scale=1.0 / 64, bias=eps_c[0:P, :])
        act(hn_rt[i][0:P, 0:n], hn_rt[i][0:P, 0:n], AF.Exp, [hn_rtb[i]], [hn_rtb[i]], scale=-0.5)
        stt(out, src, g_ap, hn_rt[i][0:P, 0:n], ALU.mult, ALU.mult, [srcb, gb, hn_rtb[i]], [outb])

    with ExitStack() as st:
        xin = [sb(f"xin{i}", [128, D], F32, st) for i in range(4)]
        xinb = [Buf() for _ in range(4)]
        xo = [sb(f"xo{i}", [128, 8, 128], F32, st) for i in range(4)]
        xob = [Buf() for _ in range(4)]
        for tk in range(S // 128):
            i = tk % 4
            fw.dma("sp", xin[i][:], x_in[tk * 128:(tk + 1) * 128, :], writes=[xinb[i]])
            for half in range(2):
                pb = (2 * tk + half) % 8

                def f(e, i=i, half=half, pb=pb):
                    for c4 in range(4):
                        c = half * 4 + c4
                        ins = e.transpose(PS[pb][:, c4 * 128:(c4 + 1) * 128], xin[i][:, c * 128:(c + 1) * 128], ident[:])
                    return ins
                OP("pe", f, [xinb[i], b_ident], [PSB[pb]])
                cp(xo[i][:, half * 4:(half + 1) * 4, :], PS[pb][:].rearrange("p (c t) -> p c t", c=4),
                   [PSB[pb]], [xob[i]], eng=("act" if half == 0 else "dve"))
            fw.dma("sp", xT_d[:, :, tk * 128:(tk + 1) * 128].rearrange("c p t -> p c t"), xo[i][:], reads=[xob[i]])
    fw.barrier()

    def norm_mod(st_, xt, xtb, A, B, ABb, hb, hbb, xn, xnb, sqb, sqbb, rt, rtb, psn):
        act(sqb[:], xt[:], AF.Square, [xtb], [sqbb])

        def f(e):
            for c in range(8):
                ins = e.matmul(PS[psn][:, :], ones_b[:], sqb[:, c, :], start=(c == 0), stop=(c == 7))
            return ins
        OP("pe", f, [sqbb, b_ones], [PSB[psn]])
        act(rt[:], PS[psn][:, :], AF.Ln, [PSB[psn], b_eps], [rtb], scale=1.0 / D, bias=eps_c[:])
        act(rt[:], rt[:], AF.Exp, [rtb], [rtb], scale=-0.5)
        tt(xn[:], xt[:], rt[:].unsqueeze(1).to_broadcast([128, 8, TT]), ALU.mult, [xtb, rtb], [xnb])
        for c in range(8):
            if c % 2 == 0:
                ts(xn[:, c, :], xn[:, c, :], A[:, c:c + 1], B[:, c:c + 1], ALU.mult, ALU.add, [xnb, ABb], [xnb])
            else:
                act(xn[:, c, :], xn[:, c, :], AF.Identity, [xnb, ABb], [xnb], scale=A[:, c:c + 1], bias=B[:, c:c + 1])
        cp(hb[:, 0:4, :], xn[:, 0:4, :], [xnb], [hbb], eng="dve")
        cp(hb[:, 4:8, :], xn[:, 4:8, :], [xnb], [hbb], eng="act")

    for l in range(nlayers if stop_after != "0" else 0):
        last = (l == nlayers - 1)
        with ExitStack() as lay:
            mod = sb("mod", [128, 48], F32, lay); b_mod = Buf()
            A1 = sb("A1", [128, 8], F32, lay); A2 = sb("A2", [128, 8], F32, lay)
            n1g_sb = sb("n1g_sb", [128, 8], F32, lay); n2g_sb = sb("n2g_sb", [128, 8], F32, lay); b_ng = Buf()
            adab = sb("adab", [128, 48], F32, lay); b_adab = Buf()
            cact2 = sb("cact2", [128, 8, 2], F32, lay); b_cact2 = Buf()
            fw.dma("sp", n1g_sb[:], n1g[l], writes=[b_ng])
            fw.dma("sp", n2g_sb[:], n2g[l], writes=[b_ng])
            fw.dma("sp", adab[:], ada_b_fm[l], writes=[b_adab])
            cp(cact2[:], cact[:].unsqueeze(2).to_broadcast([128, 8, 2]), [b_cact], [b_cact2])
            with ExitStack() as st:
                adw = [sb(f"adw{i}", [128, 8, D], F32, st) for i in range(2)]
                adwb = [Buf() for _ in range(2)]
                for j in range(6):
                    i = j % 2
                    fw.dma("sp", adw[i][:], ada_w[l, :, j * D:(j + 1) * D].rearrange("(kc p) n -> p kc n", p=128),
                           writes=[adwb[i]])
                    for c in range(8):
                        def f(e, i=i, j=j, c=c):
                            for kc in range(8):
                                col = 2 * (j * 8 + c)
                                ins = e.matmul(PS[0][:, col:col + 2], adw[i][:, kc, c * 128:(c + 1) * 128], cact2[:, kc, :],
                                               start=(kc == 0), stop=(kc == 7))
                            return ins
                        OP("pe", f, [adwb[i], b_cact2], [PSB[0]])
                tt(mod[:], PS[0][:, 0:96].rearrange("p (n two) -> p n two", two=2)[:, :, 0], adab[:], ALU.add,
                   [PSB[0], b_adab], [b_mod])
                stt(A1[:], mod[:, 8:16], 1.0, n1g_sb[:], ALU.add, ALU.mult, [b_mod, b_ng], [b_mod])
                stt(A2[:], mod[:, 32:40], 1.0, n2g_sb[:], ALU.add, ALU.mult, [b_mod, b_ng], [b_mod])
                if debug and l == 0:
                    fw.dma("sp", dbg["dbg_mod"], mod[:], reads=[b_mod])
                fw.barrier()
            B1 = mod[:, 0:8]; G1 = mod[:, 16:24]; B2 = mod[:, 24:32]; G2 = mod[:, 40:48]

            with ExitStack() as mix:
                V_sb = sb("V_sb", [128, 32, 8, 65], BF16, mix); b_V = Buf()
                cum = sb("cum", [8, S], F32, mix); b_cum = Buf()
                og_fm_sb = sb("og_fm_sb", [128, 8], F32, mix); og_at_sb = sb("og_at_sb", [64, 8], F32, mix); b_og = Buf()
                fw.dma("sp", og_fm_sb[:], og_fm[l], writes=[b_og])
                fw.dma("sp", og_at_sb[:], og_at[l], writes=[b_og])
                OP("dve", lambda e: e.memset(V_sb[:, :, :, 64:65], 1.0), writes=[b_V])
                fw.barrier()
                with ExitStack() as st:
                    win = sb("win", [128, 8, DIN], BF16, st); b_win = Buf()
                    for hh in range(2):
                        fw.dma("pool", win[:, :, hh * 1284:(hh + 1) * 1284],
                               w_in[l, :, hh * 1284:(hh + 1) * 1284].rearrange("(kc p) n -> p kc n", p=128), writes=[Buf()])
                    fw.barrier()
                    fbs = sb("fbs", [8, 1], F32, st); b_fb = Buf()
                    qgs = sb("qgs", [128, 1], F32, st); kgs = sb("kgs", [128, 1], F32, st); b_qk = Buf()
                    cws = sb("cws", [128, 2, 3], F32, st); b_cw = Buf()
                    fw.dma("sp", fbs[:], fb[l], writes=[b_fb])
                    fw.dma("sp", qgs[:], qg[l], writes=[b_qk])
                    fw.dma("sp", kgs[:], kg[l], writes=[b_qk])
                    fw.dma("sp", cws[:], conv_w[l], writes=[b_cw])
                    ts(fbs[:], fbs[:], -1.0, None, ALU.mult, None, [b_fb], [b_fb])
                    ts(qgs[:], qgs[:], 0.125, None, ALU.mult, None, [b_qk], [b_qk])
                    xt = [sb(f"xt{i}", [128, 8, TT], F32, st) for i in range(2)]; xtb = [Buf() for _ in range(2)]
                    sqb = sb("sqb", [128, 8, TT], BF16, st); sqbb = Buf()
                    rt = sb("rt", [128, TT], F32, st); rtb = Buf()
                    xn = sb("xn", [128, 8, TT], F32, st); xnb = Buf()
                    hb = [sb(f"hb{i}", [128, 8, TT], BF16, st) for i in range(2)]; hbb = [Buf() for _ in range(2)]
                    ev = [sb(f"ev{i}", [128, TT], F32, st) for i in range(4)]; evb = [Buf() for _ in range(4)]
                    evo = [sb(f"evo{i}", [128, TT], BF16, st) for i in range(4)]; evob = [Buf() for _ in range(4)]
                    zt = [[sb(f"zt{cc}{i}", [128, TT + 2], F32, st) for i in range(2)] for cc in range(2)]
                    ztb = [[Buf() for _ in range(2)] for _ in range(2)]
                    cy = sb("cy", [128, TT], F32, st); cyb = Buf()
                    fe = sb("fe", [8, TT], F32, st); feb = Buf()
                    evc = [0]
                    gen = [0]
                    for cc in range(2):
                        OP("dve", lambda e, cc=cc: e.memset(zt[cc][1][:, TT:TT + 2], 0.0), writes=[ztb[cc][1]])

                    def proj(ps, off, M, hbt, hbtb):
                        def f(e):
                            for kc in range(8):
                                ins = e.matmul(PS[ps][0:M, :], win[:, kc, off:off + M], hbt[:, kc, :], start=(kc == 0), stop=(kc == 7))
                            return ins
                        OP("pe", f, [hbtb], [PSB[ps]])

                    for it in range(NT):
                        t0 = it * TT
                        i = it % 2
                        fw.dma("sp", xt[i][:], xT_d[:, :, t0:t0 + TT].rearrange("c p t -> p c t"), writes=[xtb[i]])
                        norm_mod(st, xt[i], xtb[i], A1, B1, b_mod, hb[i], hbb[i], xn, xnb, sqb, sqbb, rt, rtb, 0)
                        if debug and l == 0:
                            fw.dma("sp", dbg["dbg_h"][:, :, t0:t0 + TT].rearrange("c p t -> p c t"), hb[i][:], reads=[hbb[i]])
                        for c in range(2):
                            ps = 1 + gen[0] % 2; gen[0] += 1
                            proj(ps, OFF_U + c * 128, 128, hb[i], hbb[i])
                            k = evc[0] % 4; evc[0] += 1
                            cp(ev[k][:], PS[ps][:, :], [PSB[ps]], [evb[k]], eng="act")
                            fw.dma("sp", uT_d[c, :, t0:t0 + TT], ev[k][:], reads=[evb[k]])
                        for (off, gsb, dst) in ((OFF_Q, qgs, qT_d), (OFF_K, kgs, kT_d)):
                            for c in range(4):
                                ps = 1 + gen[0] % 2; gen[0] += 1
                                proj(ps, off + c * 128, 128, hb[i], hbb[i])
                                k = evc[0] % 4; evc[0] += 1
                                cp(ev[k][:], PS[ps][:, :], [PSB[ps]], [evb[k]], eng="act")
                                hnorm(ev[k][:], evb[k], gsb[:, 0:1], b_qk, evo[k][:], evob[k])
                                fw.dma("sp", dst[2 * c, 0:64, t0:t0 + TT], evo[k][0:64, :], reads=[evob[k]])
                                fw.dma("sp", dst[2 * c + 1, 0:64, t0:t0 + TT], evo[k][64:128, :], reads=[evob[k]])
                        ps = 1 + gen[0] % 2; gen[0] += 1
                        proj(ps, OFF_F, 8, hb[i], hbb[i])
                        act(fe[:], PS[ps][0:8, :], AF.Exp, [PSB[ps], b_fb], [feb], scale=-1.0, bias=fbs[:])
                        act(fe[:], fe[:], AF.Ln, [feb], [feb], bias=1.0)
                        init = 0.0 if it == 0 else cum[:, t0 - 1:t0]
                        OP("dve", lambda e, t0=t0, init=init: e.tensor_tensor_scan(
                            out=cum[:, t0:t0 + TT], data0=ones_f[0:8, 0:1].to_broadcast([8, TT]), data1=fe[:], initial=init,
                            op0=ALU.mult, op1=ALU.subtract), [feb, b_onesf, b_cum], [b_cum])
                        for sub in range(4):
                            def f(e, sub=sub, i=i):
                                for kc in range(8):
                                    ins = e.matmul(PS[3][:, :], hb[i][:, kc, sub * 128:(sub + 1) * 128], win[:, kc, OFF_V:OFF_V + 512],
                                                   start=(kc == 0), stop=(kc == 7))
                                return ins
                            OP("pe", f, [hbb[i]], [PSB[3]])
                            cp(V_sb[:, 4 * it + sub, :, 0:64], PS[3][:, :].rearrange("p (h d) -> p h d", h=8), [PSB[3]], [b_V],
                               eng=("act" if sub % 2 else "dve"))
                        for cc in range(2):
                            proj(4, OFF_HC + cc * 128, 128, hb[i], hbb[i])
                            proj(5, OFF_CG + cc * 128, 128, hb[i], hbb[i])
                            proj(6, OFF_BG + cc * 128, 128, hb[i], hbb[i])
                            k = evc[0] % 4; evc[0] += 1
                            z, zb = zt[cc][i], ztb[cc][i]
                            zp, zpb = zt[cc][1 - i], ztb[cc][1 - i]
                            cp(ev[k][:], PS[5][:, :], [PSB[5]], [evb[k]], eng="act")
                            cp(z[:, 0:2], zp[:, TT:TT + 2], [zpb], [zb])
                            tt(z[:, 2:TT + 2], PS[4][:, :], ev[k][:], ALU.mult, [PSB[4], evb[k]], [zb])
                            ts(cy[:], z[:, 2:TT + 2], cws[:, cc, 2:3], None, ALU.mult, None, [zb, b_cw], [cyb])
                            stt(cy[:], z[:, 1:TT + 1], cws[:, cc, 1:2], cy[:], ALU.mult, ALU.add, [zb, b_cw, cyb], [cyb])
                            stt(cy[:], z[:, 0:TT], cws[:, cc, 0:1], cy[:], ALU.mult, ALU.add, [zb, b_cw, cyb], [cyb])
                            tt(ev[k][:], PS[6][:, :], cy[:], ALU.mult, [PSB[6], cyb], [evb[k]])
                            hnorm(ev[k][:], evb[k], og_fm_sb[:, 6 + cc:7 + cc], b_og, evo[k][:], evob[k])
                            fw.dma("sp", yh_d[2 + cc, :, t0:t0 + TT], evo[k][:], reads=[evob[k]])
                    if debug and l == 0:
                        fw.dma("sp", dbg["dbg_cum"], cum[:], reads=[b_cum])
                        fw.dma("sp", dbg["dbg_v"], V_sb[:], reads=[b_V])
                    fw.barrier()
                if stop_after == "A":
                    break
                with ExitStack() as st:
                    def t8(name):
                        return sb(name, [128, 8], F32, st)
                    lre, lim, ldt = t8("lre"), t8("lim"), t8("ldt"); b_p = Buf()
                    fw.dma("sp", lre[:], lam_re[l], writes=[b_p])
                    fw.dma("sp", lim[:], lam_im[l], writes=[b_p])
                    fw.dma("sp", ldt[:], log_dt[l], writes=[b_p])
                    bre = sb("bre", [128, 8, 16], F32, st); bim = sb("bim", [128, 8, 16], F32, st)
                    cre = sb("cre", [128, 8, 16], F32, st); cim = sb("cim", [128, 8, 16], F32, st); b_bc = Buf()
                    fw.dma("sp", bre[:], sb_re[l], writes=[b_bc]); fw.dma("sp", bim[:], sb_im[l], writes=[b_bc])
                    fw.dma("sp", cre[:], sc_re[l], writes=[b_bc]); fw.dma("sp", cim[:], sc_im[l], writes=[b_bc])
                    dsk = sb("dsk", [128, 2], F32, st); glb = sb("glb", [128, 2], F32, st); b_dg = Buf()
                    fw.dma("sp", dsk[:], ssm_d[l], writes=[b_dg]); fw.dma("sp", glb[:], glu_b[l], writes=[b_dg])
                    gluw = sb("gluw", [128, 2, 256], BF16, st); b_gw = Buf()
                    fw.dma("pool", gluw[:], glu_w[l].rearrange("(kc p) n -> p kc n", p=128), writes=[b_gw])
                    r_sb, th = t8("r_sb"), t8("th")
                    dtv, a_, cs, sn, t1_, t2_, zre, zim = t8("dtv"), t8("a_"), t8("cs"), t8("sn"), t8("t1_"), t8("t2_"), t8("zre"), t8("zim")
                    ti = sb("ti", [128, 8], I32, st)
                    C1 = 6.28125
                    C2 = TWO_PI - C1

                    def sincos(out, ang, shape, tmpf, tmpi, bq, shift):
                        ts(tmpf, ang, 1.0 / TWO_PI, shift / TWO_PI, ALU.mult, ALU.add, [bq], [bq])
                        cp(tmpi, tmpf, [bq], [bq])
                        cp(tmpf, tmpi, [bq], [bq])
                        if shift != 0.0:
                            ts(out, ang, shift, None, ALU.add, None, [bq], [bq])
                            stt(out, tmpf, -C1, out, ALU.mult, ALU.add, [bq], [bq])
                        else:
                            stt(out, tmpf, -C1, ang, ALU.mult, ALU.add, [bq], [bq])
                        stt(out, tmpf, -C2, out, ALU.mult, ALU.add, [bq], [bq])
                        ts(out, out, 3.1415925, -3.1415925, ALU.min, ALU.max, [bq], [bq])
                        act(out, out, AF.Sin, [bq], [bq])

                    ts(lre[:], lre[:], -1e-4, None, ALU.min, None, [b_p], [b_p])
                    act(dtv[:], ldt[:], AF.Exp, [b_p], [b_p])
                    tt(a_[:], lre[:], dtv[:], ALU.mult, [b_p], [b_p])
                    act(r_sb[:], a_[:], AF.Exp, [b_p], [b_p])
                    tt(th[:], lim[:], dtv[:], ALU.mult, [b_p], [b_p])
                    sincos(sn[:], th[:], None, t1_[:], ti[:], b_p, 0.0)
                    sincos(cs[:], th[:], None, t1_[:], ti[:], b_p, 1.5707963267948966)
                    tt(cs[:], cs[:], r_sb[:], ALU.mult, [b_p], [b_p])
                    tt(sn[:], sn[:], r_sb[:], ALU.mult, [b_p], [b_p])
                    ts(cs[:], cs[:], -1.0, None, ALU.add, None, [b_p], [b_p])
                    tt(t1_[:], lre[:], lre[:], ALU.mult, [b_p], [b_p])
                    tt(t2_[:], lim[:], lim[:], ALU.mult, [b_p], [b_p])
                    tt(t1_[:], t1_[:], t2_[:], ALU.add, [b_p], [b_p])
                    OP("dve", lambda e: e.reciprocal(out=t1_[:], in_=t1_[:]), [b_p], [b_p])
                    tt(zre[:], cs[:], lre[:], ALU.mult, [b_p], [b_p])
                    tt(t2_[:], sn[:], lim[:], ALU.mult, [b_p], [b_p])
                    tt(zre[:], zre[:], t2_[:], ALU.add, [b_p], [b_p])
                    tt(zre[:], zre[:], t1_[:], ALU.mult, [b_p], [b_p])
                    tt(zim[:], sn[:], lre[:], ALU.mult, [b_p], [b_p])
                    tt(t2_[:], cs[:], lim[:], ALU.mult, [b_p], [b_p])
                    tt(zim[:], zim[:], t2_[:], ALU.subtract, [b_p], [b_p])
                    tt(zim[:], zim[:], t1_[:], ALU.mult, [b_p], [b_p])
                    bbr = sb("bbr", [128, 8, 16], F32, st); bbi = sb("bbi", [128, 8, 16], F32, st); tb = sb("tb", [128, 8, 16], F32, st)
                    zre_b = zre[:].unsqueeze(2).to_broadcast([128, 8, 16]); zim_b = zim[:].unsqueeze(2).to_broadcast([128, 8, 16])
                    tt(bbr[:], bre[:], zre_b, ALU.mult, [b_p, b_bc], [b_bc])
                    tt(tb[:], bim[:], zim_b, ALU.mult, [b_p, b_bc], [b_bc])
                    tt(bbr[:], bbr[:], tb[:], ALU.subtract, [b_bc], [b_bc])
                    tt(bbi[:], bim[:], zre_b, ALU.mult, [b_p, b_bc], [b_bc])
                    tt(tb[:], bre[:], zim_b, ALU.mult, [b_p, b_bc], [b_bc])
                    tt(bbi[:], bbi[:], tb[:], ALU.add, [b_bc], [b_bc])
                    WT = []
                    for nm, src in (("re", bbr), ("im", bbi)):
                        w1 = sb("w1" + nm, [128, 8, 2, 16], F32, st); bw1 = Buf()
                        OP("dve", lambda e, w1=w1: e.memset(w1[:], 0.0), writes=[bw1])
                        cp(w1[0:64, :, 0, :], src[0:64], [b_bc], [bw1])
                        cp(w1[64:128, :, 1, :], src[64:128], [b_bc], [bw1])
                        wt = sb("wt" + nm, [128, 2, 128], BF16, st); bwt = Buf()
                        w1v = w1[:].rearrange("p g a c -> p (g a c)")
                        for ch in range(2):
                            OP("pe", lambda e, ch=ch, w1v=w1v: e.transpose(PS[0][:, 0:128], w1v[:, ch * 128:(ch + 1) * 128], ident[:]),
                               [bw1, b_ident], [PSB[0]])
                            cp(wt[:, ch, :], PS[0][:, 0:128], [PSB[0]], [bwt])
                        WT.append((wt, bwt))
                    CT = []
                    for nm, src, sgn in (("re", cre, 1.0), ("im", cim, -1.0)):
                        ct = sb("ct" + nm, [128, 8, 2, 16], BF16, st); bct = Buf()
                        OP("dve", lambda e, ct=ct: e.memset(ct[:], 0.0), writes=[bct])
                        ts(ct[0:64, :, 0, :], src[0:64], sgn, None, ALU.mult, None, [b_bc], [bct])
                        ts(ct[64:128, :, 1, :], src[64:128], sgn, None, ALU.mult, None, [b_bc], [bct])
                        CT.append((ct, bct))
                    cosT = sb("cosT", [128, 8, TT + 1], F32, st); sinT = sb("sinT", [128, 8, TT + 1], F32, st); b_tab = Buf()
                    with ExitStack() as st2:
                        ang = sb("ang", [128, 8, TT + 1], F32, st2); tf = sb("tf", [128, 8, TT + 1], F32, st2)
                        tii = sb("tii", [128, 8, TT + 1], I32, st2); b_ang = Buf()
                        for gp in range(8):
                            ts(ang[:, gp, :], iota[:], th[:, gp:gp + 1], None, ALU.mult, None, [b_iota, b_p], [b_ang])
                        sincos(sinT[:], ang[:], None, tf[:], tii[:], b_ang, 0.0)
                        sincos(cosT[:], ang[:], None, tf[:], tii[:], b_ang, 1.5707963267948966)
                        fw.barrier()
                    uf = [sb(f"uf{i}", [128, 2, TT], F32, st) for i in range(2)]; ufb = [Buf() for _ in range(2)]
                    ub = [sb(f"ub{i}", [128, 2, TT], BF16, st) for i in range(2)]; ubb = [Buf() for _ in range(2)]
                    ta = [sb(f"ta{i}", [128, TT], F32, st) for i in range(4)]; tab_ = [Buf() for _ in range(4)]
                    wre = [sb(f"wre{i}", [128, TT], F32, st) for i in range(2)]; wim = [sb(f"wim{i}", [128, TT], F32, st) for i in range(2)]
                    wb_ = [Buf() for _ in range(2)]
                    zr = [sb(f"zr{i}", [128, TT], BF16, st) for i in range(2)]; zi = [sb(f"zi{i}", [128, TT], BF16, st) for i in range(2)]
                    zb_ = [Buf() for _ in range(2)]
                    ini = sb("ini", [128, 8, 2], F32, st); b_ini = [Buf() for _ in range(8)]
                    tiny = sb("tiny", [128, 2], F32, st)
                    yp = sb("yp", [128, 2, TT], F32, st); ypb = [Buf() for _ in range(2)]
                    yg = sb("yg", [128, 2, TT], F32, st); ygb_f = [Buf() for _ in range(2)]
                    ygb = sb("ygb", [128, 2, TT], BF16, st); ygbb = Buf()
                    g1t = sb("g1t", [128, TT], F32, st); g1b = Buf(); g2t = sb("g2t", [128, TT], F32, st); g2b = Buf()
                    yo = [sb(f"yo{i}", [128, TT], F32, st) for i in range(2)]; yob = [Buf() for _ in range(2)]
                    yob16 = [sb(f"yob16{i}", [128, TT], BF16, st) for i in range(2)]; yob16b = [Buf() for _ in range(2)]
                    OP("dve", lambda e: e.memset(ini[:], 0.0), writes=b_ini)
                    k = 0
                    def gen_B():
                        k = 0
                        pend = []

                        def run_due(force=False):
                            keep = []
                            for item in list(pend):
                                item[0] -= 1
                                if force or item[0] <= 0:
                                    nxt = item[1]()
                                    while force and nxt is not None:
                                        nxt = nxt()
                                    if nxt is not None:
                                        keep.append([1, nxt])
                                else:
                                    keep.append(item)
                            pend[:] = keep
                        for it in range(NT):
                            t0 = it * TT
                            i = it % 2
                            fw.dma("sp", uf[i][:], uT_d[:, :, t0:t0 + TT].rearrange("c p t -> p c t"), writes=[ufb[i]])
                            cp(ub[i][:], uf[i][:], [ufb[i]], [ubb[i]], eng="act")
                            for gp in range(8):
                                ch, j = gp // 4, gp % 4
                                pa, pb = 0, 1
                                for (pp, (wt, bwt)) in ((pa, WT[0]), (pb, WT[1])):
                                    OP("pe", lambda e, pp=pp, wt=wt, ch=ch, j=j, i=i: e.matmul(
                                        PS[pp][:, :], wt[32 * j:32 * j + 32, ch, :], ub[i][32 * j:32 * j + 32, ch, :],
                                        start=True, stop=True, tile_position=(32 * j, 0)), [bwt, ubb[i]], [PSB[pp]])
                                run_due()
                                cT = cosT[:, gp, 0:TT]; sT = sinT[:, gp, 0:TT]
                                kk = k % 2; k += 1
                                tt(ta[0][:], PS[pa][:, :], cT, ALU.mult, [PSB[pa], b_tab], [tab_[0]])
                                tt(ta[1][:], PS[pb][:, :], sT, ALU.mult, [PSB[pb], b_tab], [tab_[1]])
                                tt(ta[0][:], ta[0][:], ta[1][:], ALU.add, [tab_[0], tab_[1]], [tab_[0]])
                                tt(ta[2][:], PS[pb][:, :], cT, ALU.mult, [PSB[pb], b_tab], [tab_[2]])
                                tt(ta[3][:], PS[pa][:, :], sT, ALU.mult, [PSB[pa], b_tab], [tab_[3]])
                                tt(ta[2][:], ta[2][:], ta[3][:], ALU.subtract, [tab_[2], tab_[3]], [tab_[2]])
                                rb = r_sb[:, gp:gp + 1].to_broadcast([128, TT])
                                OP("dve", lambda e, kk=kk, rb=rb, gp=gp: e.tensor_tensor_scan(
                                    out=wre[kk][:], data0=rb, data1=ta[0][:], initial=ini[:, gp, 0:1], op0=ALU.mult, op1=ALU.add),
                                    [tab_[0], b_p, b_ini[gp]], [wb_[kk]])
                                OP("dve", lambda e, kk=kk, rb=rb, gp=gp: e.tensor_tensor_scan(
                                    out=wim[kk][:], data0=rb, data1=ta[2][:], initial=ini[:, gp, 1:2], op0=ALU.mult, op1=ALU.add),
                                    [tab_[2], b_p, b_ini[gp]], [wb_[kk]])
                                tt(ta[0][:], wre[kk][:], cT, ALU.mult, [wb_[kk], b_tab], [tab_[0]])
                                tt(ta[1][:], wim[kk][:], sT, ALU.mult, [wb_[kk], b_tab], [tab_[1]])
                                tt(zr[kk][:], ta[0][:], ta[1][:], ALU.subtract, [tab_[0], tab_[1]], [zb_[kk]])
                                tt(ta[2][:], wre[kk][:], sT, ALU.mult, [wb_[kk], b_tab], [tab_[2]])
                                tt(ta[3][:], wim[kk][:], cT, ALU.mult, [wb_[kk], b_tab], [tab_[3]])
                                tt(zi[kk][:], ta[2][:], ta[3][:], ALU.add, [tab_[2], tab_[3]], [zb_[kk]])
                                cL = cosT[:, gp, TT:TT + 1]; sL = sinT[:, gp, TT:TT + 1]
                                ts(tiny[:, 0:1], wim[kk][:, TT - 1:TT], sL, None, ALU.mult, None, [wb_[kk], b_tab], [b_ini[gp]])
                                ts(tiny[:, 1:2], wim[kk][:, TT - 1:TT], cL, None, ALU.mult, None, [wb_[kk], b_tab], [b_ini[gp]])
                                stt(ini[:, gp, 0:1], wre[kk][:, TT - 1:TT], cL, tiny[:, 0:1], ALU.mult, ALU.subtract, [wb_[kk], b_tab, b_ini[gp]], [b_ini[gp]])
                                stt(ini[:, gp, 1:2], wre[kk][:, TT - 1:TT], sL, tiny[:, 1:2], ALU.mult, ALU.add, [wb_[kk], b_tab, b_ini[gp]], [b_ini[gp]])
                                py = 2

                                def tail(gp=gp, j=j, kk=kk, py=py, ch=ch, i=i, t0=t0):
                                  def f(e):
                                    e.matmul(PS[py][32 * j:32 * j + 32, :], CT[0][0][:, gp, :, :].rearrange("p a c -> p (a c)"), zr[kk][:],
                                             start=True, stop=False, tile_position=(0, 32 * j))
                                    return e.matmul(PS[py][32 * j:32 * j + 32, :], CT[1][0][:, gp, :, :].rearrange("p a c -> p (a c)"), zi[kk][:],
                                                    start=False, stop=True, tile_position=(0, 32 * j))
                                  OP("pe", f, [zb_[kk], CT[0][1], CT[1][1]], [PSB[py]])
                                  if j == 3:
                                    stt(yp[:, ch, :], uf[i][:, ch, :], dsk[:, ch:ch + 1], PS[py][:, :], ALU.mult, ALU.add,
                                        [ufb[i], b_dg, PSB[py]], [ypb[ch]])
                                    if debug and l == 0:
                                        fw.dma("sp", dbg["dbg_ssmpre"][ch, :, t0:t0 + TT], yp[:, ch, :], reads=[ypb[ch]])
                                    tt(g1t[:], yp[:, ch, :], yp[:, ch, :], ALU.mult, [ypb[ch]], [g1b])
                                    ts(g1t[:], g1t[:], 0.044715, 1.0, ALU.mult, ALU.add, [g1b], [g1b])
                                    tt(g1t[:], g1t[:], yp[:, ch, :], ALU.mult, [g1b, ypb[ch]], [g1b])

                                    def tailB():
                                        act(g1t[:], g1t[:], AF.Sigmoid, [g1b], [g1b], scale=1.5957691216057308)

                                        def tailC():
                                            tt(yg[:, ch, :], yp[:, ch, :], g1t[:], ALU.mult, [g1b, ypb[ch]], [ygb_f[ch]])
                                            cp(ygb[:, ch, :], yg[:, ch, :], [ygb_f[ch]], [ygbb])
                                            return None
                                        return tailC
                                    return tailB
                                  return None
                                pend.append([1, tail])
                                yield
                            def glu_block(t0=t0):
                                for mc in range(2):
                                    def f(e, mc=mc):
                                        e.matmul(PS[7][:, :], gluw[:, 0, mc * 128:(mc + 1) * 128], ygb[:, 0, :], start=True, stop=False)
                                        return e.matmul(PS[7][:, :], gluw[:, 1, mc * 128:(mc + 1) * 128], ygb[:, 1, :], start=False, stop=True)
                                    OP("pe", f, [ygbb, b_gw], [PSB[7]])
                                    act(g2t[:], PS[7][:, :], AF.Sigmoid, [PSB[7], b_dg], [g2b], bias=glb[:, mc:mc + 1])
                                    tt(yo[mc][:], yg[:, mc, :], g2t[:], ALU.mult, [g2b, ygb_f[mc]], [yob[mc]])
                                    hnorm(yo[mc][:], yob[mc], og_fm_sb[:, mc:mc + 1], b_og, yob16[mc][:], yob16b[mc])
                                    fw.dma("sp", yh_d[mc, :, t0:t0 + TT], yob16[mc][:], reads=[yob16b[mc]])
                                return None
                            pend.append([4, glu_block])
                            yield
                        run_due(force=True)
                        yield
                    ckT = sb("ckT", [128, 32, 8], F32, st); cref = sb("cref", [128, 32, 8], F32, st); b_ck = Buf()
                    st3 = ExitStack()
                    ce = sb("ce", [8, 32], F32, st3); dq = sb("dq", [8, 8, 4], F32, st3); b_ce = Buf()
                    dqrow = sb("dqrow", [8, 32, 128], BF16, st3); onesrow = sb("onesrow", [8, S], BF16, st3); b_row = Buf()
                    cp(ce[:], cum[:].rearrange("h (s j) -> h s j", j=128)[:, :, 127], [b_cum], [b_ce])
                    cev = ce[:].rearrange("h (q s) -> h q s", s=4)
                    tt(dq[:], cev, cev[:, :, 3:4].to_broadcast([8, 8, 4]), ALU.subtract, [b_ce], [b_ce])
                    cp(dqrow[:], dq[:].rearrange("h q s -> h (q s)").unsqueeze(2).to_broadcast([8, 32, 128]), [b_ce], [b_row])
                    OP("dve", lambda e: e.memset(onesrow[:], 1.0), writes=[b_row])
                    fw.dma("sp", qT_d[:, 64, :], dqrow[:].rearrange("h s j -> h (s j)"), reads=[b_row])
                    fw.dma("sp", kT_d[:, 64, :], onesrow[:], reads=[b_row])

                    def f(e):
                        for kt in range(32):
                            ins = e.transpose(PS[0][:, kt * 8:(kt + 1) * 8], cum[0:8, kt * 128:(kt + 1) * 128], ident[0:8, 0:8])
                        return ins
                    OP("pe", f, [b_cum, b_ident], [PSB[0]])
                    cp(ckT[:].rearrange("p k h -> p (k h)"), PS[0][:, 0:256], [PSB[0]], [b_ck])
                    OP("pe", lambda e: e.matmul(PS[1][:, 0:256], e127[:], ckT[:].rearrange("p k h -> p (k h)"), start=True, stop=True),
                       [b_ck, b_e127], [PSB[1]])
                    cp(cref[:].rearrange("p k h -> p (k h)"), PS[1][:, 0:256], [PSB[1]], [b_ck])
                    fw.barrier()
                    st3.close()
                    cv = [sb(f"cv{i}", [128, 2048], BF16, st) for i in range(2)]; cvb = [Buf() for _ in range(2)]; cvk = [0]
                    qa = [sb("qa0", [65, S], BF16, st)]; ka = [sb("ka0", [65, S], BF16, st)]
                    qab = [Buf()]; kab = [Buf()]
                    NP = 6
                    pT = [sb(f"pT{i}", [128, TT], BF16, st) for i in range(NP)]; pTb = [Buf() for _ in range(NP)]
                    biasT = [sb(f"biasT{i}", [128, 32], F32, st) for i in range(2)]; biasb = [Buf() for _ in range(2)]
                    osb = [sb(f"osb{i}", [65, TT], F32, st) for i in range(2)]; osbb = [Buf() for _ in range(2)]
                    yat = [sb(f"yat{i}", [64, TT], F32, st) for i in range(2)]; yatb = [Buf() for _ in range(2)]
                    yab = [sb(f"yab{i}", [64, TT], BF16, st) for i in range(2)] ; yabb = [Buf() for _ in range(2)]
                    def emit_bias(u_):
                        h_, qt_ = u_ // 8, u_ % 8
                        n_ = 4 * qt_ + 4
                        ts(biasT[u_ % 2][:, 0:n_], ckT[:, 0:n_, h_], cref[:, 4 * qt_ + 3, h_:h_ + 1], -1.0, ALU.subtract, ALU.mult,
                           [b_ck], [biasb[u_ % 2]])

                    def gen_C():
                        blkctr = 0
                        pend2 = pend3 = None
                        for h in range(8):
                            hi = 0
                            fw.dma("sp", qa[hi][:], qT_d[h], writes=[qab[hi]])
                            fw.dma("sp", ka[hi][:], kT_d[h], writes=[kab[hi]])
                            for qt in range(8):
                                nkt = 4 * qt + 4
                                bi = (h * 8 + qt) % 2
                                oi = bi
                                po = 6
                                if h * 8 + qt == 0:
                                    emit_bias(0)
                                if h * 8 + qt + 1 < 64:
                                    emit_bias(h * 8 + qt + 1)

                                SL = (3, 4, 5)
                                LA = 2

                                def s_mm(kt):
                                    slot = SL[(blkctr + kt) % 3]
                                    m = kt - 4 * qt
                                    c0 = 128 * m if m > 0 else 0
                                    def f(e):
                                        ins = e.matmul(PS[slot][:, c0:TT], ka[hi][:, kt * 128:(kt + 1) * 128],
                                                       qa[hi][:, qt * TT + c0:(qt + 1) * TT], start=True, stop=(m < 0))
                                        if m >= 0:
                                            ins = e.matmul(PS[slot][:, c0:c0 + 128], ntri[:], ident_b[:], start=False, stop=True)
                                        return ins
                                    OP("pe", f, [kab[hi], qab[hi], b_ntri], [PSB[slot]])
                                for kt in range(min(LA, nkt)):
                                    s_mm(kt)
                                for kt in range(nkt):
                                    slot = SL[(blkctr + kt) % 3]
                                    if kt + LA < nkt:
                                        s_mm(kt + LA)
                                    m = kt - 4 * qt
                                    c0 = 128 * m if m > 0 else 0
                                    pi = (blkctr + kt) % NP
                                    act(pT[pi][:, c0:TT], PS[slot][:, c0:TT], AF.Exp, [PSB[slot], biasb[bi]], [pTb[pi]],
                                        bias=biasT[bi][:, kt:kt + 1])
                                    OP("pe", lambda e, kt=kt, c0=c0, pi=pi: e.matmul(
                                        PS[po][0:65, c0:TT], V_sb[:, kt, h, :], pT[pi][:, c0:TT], start=(kt == 0), stop=(kt == nkt - 1)),
                                        [pTb[pi], b_V], [PSB[po]])
                                    if kt % 8 == 7 and kt + 1 < nkt:
                                        yield 8
                                blkctr += nkt
                                cp(osb[oi][:], PS[po][0:65, :], [PSB[po]], [osbb[oi]], eng="act")
                                OP("dve", lambda e, oi=oi: e.reciprocal(out=osb[oi][64:65, :], in_=osb[oi][64:65, :]), [osbb[oi]], [osbb[oi]])

                                def phase2(oi=oi, h=h, qt=qt):
                                    OP("pe", lambda e: e.matmul(PS[7][0:64, :], ones_f[64:65, 0:64], osb[oi][64:65, :], start=True, stop=True),
                                       [osbb[oi], b_onesf], [PSB[7]])
                                    tt(yat[oi][:], osb[oi][0:64, :], PS[7][0:64, :], ALU.mult, [osbb[oi], PSB[7]], [yatb[oi]])

                                    def phase3():
                                        hnorm(yat[oi][:], yatb[oi], og_at_sb[:, h:h + 1], b_og, yab[oi][:], yabb[oi], P=64)
                                        fw.dma("sp", ya_d[h, :, qt * TT:(qt + 1) * TT], yab[oi][:], reads=[yabb[oi]])
                                    return phase3
                                if pend3 is not None:
                                    pend3()
                                pend3 = pend2() if pend2 is not None else None
                                pend2 = phase2
                                yield ((nkt - 1) % 8) + 1
                        if pend3 is not None:
                            pend3()
                        if pend2 is not None:
                            pend2()()
                    if l == 0:
                        for l2 in range(nlayers):
                            for e_ in range(NE):
                                for (src, dst, pat) in ((w_gate, wg_d, 8), (w_up, wu_d, 8), (w_down, wd_d, 4)):
                                    for hf in range(2):
                                        i = cvk[0] % 2
                                        cvk[0] += 1
                                        kcs = pat // 2
                                        srcap = src[l2, e_].rearrange("(kc p) n -> p kc n", p=128)[:, hf * kcs:(hf + 1) * kcs, :]
                                        dstv = cv[i][:].rearrange("p (kc n) -> p kc n", kc=kcs)
                                        fw.dma("pool", dstv, srcap, writes=[cvb[i]])
                                        fw.dma("pool", dst[l2 * NE + e_][:, hf * 2048:(hf + 1) * 2048], cv[i][:], reads=[cvb[i]])
                    gB, gC = gen_B(), gen_C()
                    aliveB = aliveC = True
                    cdone, bdone = 0, 0
                    CTOT, BTOT = 8 * sum(4 * q_ + 4 for q_ in range(8)), NT * 9
                    while aliveB or aliveC:
                        if aliveC:
                            try:
                                cdone += next(gC)
                            except StopIteration:
                                aliveC = False
                        while aliveB and (not aliveC or bdone * CTOT <= cdone * BTOT):
                            try:
                                next(gB)
                                bdone += 1
                            except StopIteration:
                                aliveB = False
                    fw.barrier()
            if stop_after == "C":
                break
            NSLOT = 80
            RS = 128
            SUB = RS // 128
            BIG = 1.0e4
            with ExitStack() as dl:
                msk_all = sb("msk_all", [128, 32, 16], F32, dl); eq1_all = sb("eq1_all", [128, 32, 16], F32, dl)
                comb_all = sb("comb_all", [128, 32, 16], F32, dl); b_all = Buf()
                r1i = sb("r1i", [128, 32], I32, dl); r2i = sb("r2i", [128, 32], I32, dl)
                w1s = sb("w1s", [128, 32], F32, dl); w2s = sb("w2s", [128, 32], F32, dl); b_rw = Buf()
                widx = sb("widx", [128, NSLOT], I32, dl); b_slot = Buf()
                with ExitStack() as st:
                    maskT = sb("maskT", [16, S], F32, dl); b_mT = Buf()
                    woa = sb("woa", [128, 4, D], BF16, st); wob = sb("wob", [64, 8, D], BF16, st)
                    fw.dma("pool", woa[:, 0:2, :], w_out[l, 0:256, :].rearrange("(kc p) n -> p kc n", p=128), writes=[Buf()])
                    fw.dma("pool", woa[:, 2:4, :], w_out[l, 768:1024, :].rearrange("(kc p) n -> p kc n", p=128), writes=[Buf()])
                    fw.dma("pool", wob[:], w_out[l, 256:768, :].rearrange("(h p) n -> p h n", p=64), writes=[Buf()])
                    fw.barrier()
                    zrow = sb("zrow", [128, 2048], F32, st); b_z = Buf()
                    OP("dve", lambda e: e.memset(zrow[:], 0.0), writes=[b_z])
                    for c_ in range(NSLOT * RS // 256):
                        fw.dma("pool", Xs_d[c_ * 256:(c_ + 1) * 256, :].rearrange("(p two) n -> p (two n)", two=2), zrow[:], reads=[b_z])
                    xt = sb("xtD", [128, 8, TT], F32, st); xtb = Buf()
                    ys = sb("ys", [128, 4, TT], BF16, st); ysb = Buf()
                    yatt = sb("yatt", [64, 8, TT], BF16, st); yattb = Buf()
                    sqb = sb("sqbD", [128, 8, TT], BF16, st); sqbb = Buf()
                    rt = sb("rtD", [128, TT], F32, st); rtb = Buf()
                    h2f = sb("h2f", [128, 8, TT], F32, st); h2fb = Buf()
                    h2 = sb("h2", [128, 8, TT], BF16, st); h2b = Buf()
                    htok = [sb(f"htok{i}", [128, D], F32, st) for i in range(2)]; htokb = [Buf() for _ in range(2)]
                    aff = sb("aff", [128, 4, 16], F32, st); selv = sb("selv", [128, 4, 16], F32, st); rtmp = sb("rtmp", [128, 4, 16], F32, st)
                    m1 = sb("m1", [128, 16], F32, st); m2 = sb("m2", [128, 16], F32, st); gm = sb("gm", [128, 4], F32, st)
                    b_r = Buf()
                    for it in range(NT):
                        t0 = it * TT
                        msk = msk_all[:, 4 * it:4 * it + 4, :]; comb = comb_all[:, 4 * it:4 * it + 4, :]; eq1 = eq1_all[:, 4 * it:4 * it + 4, :]
                        fw.dma("sp", xt[:], xT_d[:, :, t0:t0 + TT].rearrange("c p t -> p c t"), writes=[xtb])
                        fw.dma("sp", ys[:], yh_d[:, :, t0:t0 + TT].rearrange("c p t -> p c t"), writes=[ysb])
                        fw.dma("sp", yatt[:], ya_d[:, :, t0:t0 + TT].rearrange("h p t -> p h t"), writes=[yattb])
                        for mc in range(8):
                            ps = 4 + mc % 2

                            def f(e, mc=mc, ps=ps):
                                for kc in range(4):
                                    e.matmul(PS[ps][:, :], woa[:, kc, mc * 128:(mc + 1) * 128], ys[:, kc, :], start=(kc == 0), stop=False)
                                for hh in range(8):
                                    ins = e.matmul(PS[ps][:, :], wob[:, hh, mc * 128:(mc + 1) * 128], yatt[:, hh, :], start=False, stop=(hh == 7))
                                return ins
                            OP("pe", f, [ysb, yattb], [PSB[ps]])
                            stt(xt[:, mc, :], PS[ps][:, :], G1[:, mc:mc + 1], xt[:, mc, :], ALU.mult, ALU.add, [PSB[ps], b_mod, xtb], [xtb])
                        if debug and l == 0:
                            fw.dma("sp", dbg["dbg_xmid"][:, :, t0:t0 + TT].rearrange("c p t -> p c t"), xt[:], reads=[xtb])
                        fw.dma("sp", xT_d[:, :, t0:t0 + TT].rearrange("c p t -> p c t"), xt[:], reads=[xtb])
                        norm_mod(st, xt, xtb, A2, B2, b_mod, h2, h2b, h2f, h2fb, sqb, sqbb, rt, rtb, 7)
                        for sub in range(4):
                            hi_ = sub % 2
                            for half in range(2):
                                ps = half

                                def f(e, sub=sub, half=half, ps=ps):
                                    for c4 in range(4):
                                        c = half * 4 + c4
                                        ins = e.transpose(PS[ps][:, c4 * 128:(c4 + 1) * 128], h2f[:, c, sub * 128:(sub + 1) * 128], ident[:])
                                    return ins
                                OP("pe", f, [h2fb, b_ident], [PSB[ps]])
                                cp(htok[hi_][:, half * 512:(half + 1) * 512], PS[ps][:, :], [PSB[ps]], [htokb[hi_]],
                                   eng=("act" if half == 0 else "dve"))
                            fw.dma("sp", h2_d[t0 + sub * 128:t0 + (sub + 1) * 128, :], htok[hi_][:], reads=[htokb[hi_]])
                        for sub in range(4):
                            def f(e, sub=sub):
                                for kc in range(8):
                                    ins = e.matmul(PS[6][:, sub * 16:(sub + 1) * 16], h2f[:, kc, sub * 128:(sub + 1) * 128], wr_sb[:, kc, :],
                                                   start=(kc == 0), stop=(kc == 7))
                                return ins
                            OP("pe", f, [h2fb, b_wr], [PSB[6]])
                        act(aff[:].rearrange("p s e -> p (s e)"), PS[6][:, 0:64], AF.Sigmoid, [PSB[6]], [b_r])
                        tt(selv[:], aff[:], rb_sb[:].unsqueeze(1).to_broadcast([128, 4, 16]), ALU.add, [b_r, b_rb], [b_r])
                        s44 = selv[:].rearrange("p s (g e) -> p (s g) e", e=4)
                        r44 = rtmp[:].rearrange("p s (g e) -> p (s g) e", e=4)
                        RD = lambda o, i_, op: OP("dve", lambda e: e.tensor_reduce(out=o, in_=i_, axis=mybir.AxisListType.X, op=op), [b_r, b_all], [b_r, b_all])
                        RD(m1[:], s44, ALU.max)
                        tt(r44, s44, m1[:].unsqueeze(2).to_broadcast([128, 16, 4]), ALU.is_equal, [b_r], [b_r])
                        stt(r44, r44, -BIG, s44, ALU.mult, ALU.add, [b_r], [b_r])
                        RD(m2[:], r44, ALU.max)
                        tt(m1[:], m1[:], m2[:], ALU.add, [b_r], [b_r])
                        gs = m1[:].rearrange("p (s g) -> p s g", g=4)
                        RD(gm[:], gs, ALU.max)
                        m2v = m2[:].rearrange("p (s g) -> p s g", g=4)
                        tt(m2v, gs, gm[:].unsqueeze(2).to_broadcast([128, 4, 4]), ALU.is_equal, [b_r], [b_r])
                        ts(m2[:], m2[:], BIG, -BIG, ALU.mult, ALU.add, [b_r], [b_r])
                        tt(r44, s44, m2[:].unsqueeze(2).to_broadcast([128, 16, 4]), ALU.add, [b_r], [b_r])
                        RD(gm[:], rtmp[:], ALU.max)
                        tt(eq1, rtmp[:], gm[:].unsqueeze(2).to_broadcast([128, 4, 16]), ALU.is_equal, [b_r, b_all], [b_r, b_all])
                        stt(msk, eq1, -BIG, rtmp[:], ALU.mult, ALU.add, [b_r, b_all], [b_r, b_all])
                        RD(gm[:], msk, ALU.max)
                        tt(msk, rtmp[:], gm[:].unsqueeze(2).to_broadcast([128, 4, 16]), ALU.is_ge, [b_r, b_all], [b_r, b_all])
                        tt(comb, aff[:], msk, ALU.mult, [b_r, b_all], [b_r, b_all])
                        RD(gm[:], comb, ALU.add)
                        OP("dve", lambda e: e.reciprocal(out=gm[:], in_=gm[:]), [b_r], [b_r])
                        tt(comb, comb, gm[:].unsqueeze(2).to_broadcast([128, 4, 16]), ALU.mult, [b_r, b_all], [b_r, b_all])
                        if debug and l == 0:
                            fw.dma("sp", dbg["dbg_comb"][t0:t0 + TT, :].rearrange("(s p) e -> p s e", p=128), comb, reads=[b_all])

                        def f(e, it=it):
                            for sub in range(4):
                                ins = e.transpose(PS[6][0:16, sub * 128:(sub + 1) * 128], msk_all[:, 4 * it + sub, :], ident[:])
                            return ins
                        OP("pe", f, [b_all, b_ident], [PSB[6]])
                        cp(maskT[:, t0:t0 + TT], PS[6][0:16, :], [PSB[6]], [b_mT])
                    fw.barrier()
                with ExitStack() as st:
                    inc = sb("inc", [16, S], F32, st); b_s = Buf()
                    cntf = sb("cntf", [16, 2], F32, st); slf = sb("slf", [16, 2], F32, st); offf = sb("offf", [16, 1], F32, st)
                    endf = sb("endf", [16, 1], F32, st); cnti = sb("cnti", [16, 2], I32, st)
                    cmpt = sb("cmpt", [16, NSLOT], F32, st); sef = sb("sef", [128, NSLOT], F32, st); pidx = sb("pidx", [128, 1], F32, st); pit = sb("pit", [128, 128], F32, st)
                    pos_all = sb("pos_all", [128, 32, 16], F32, st); tmp3 = sb("tmp3", [128, 32, 16], F32, st)
                    rf = sb("rf", [128, 32], F32, st)
                    OP("dve", lambda e: e.tensor_tensor_scan(out=inc[:], data0=ones_f[0:16, 0:1].to_broadcast([16, S]), data1=maskT[:],
                                                             initial=0.0, op0=ALU.mult, op1=ALU.add), [b_mT, b_onesf], [b_s])
                    ts(cntf[:], inc[:, S - 1:S].to_broadcast([16, 2]), 1.0 / RS, (RS - 1.0) / RS - (RS - 1.0) / (2 * RS), ALU.mult, ALU.add, [b_s], [b_s])
                    cp(cnti[:], cntf[:], [b_s], [b_s])
                    cp(slf[:], cnti[:], [b_s], [b_s])
                    OP("pe", lambda e: e.matmul(PS[0][0:16, 0:2], tri_f[0:16, 0:16], slf[:], start=True, stop=True), [b_s, b_tri], [PSB[0]])
                    cp(offf[:], PS[0][0:16, 0:1], [PSB[0]], [b_s])
                    tt(endf[:], offf[:], slf[:, 0:1], ALU.add, [b_s], [b_s])
                    ts(offf[:], offf[:], float(RS), None, ALU.mult, None, [b_s], [b_s])
                    tt(inc[:], inc[:], maskT[:], ALU.subtract, [b_s, b_mT], [b_s])
                    ts(inc[:], inc[:], offf[:, 0:1], None, ALU.add, None, [b_s], [b_s])

                    def f(e):
                        for tk in range(32):
                            ins = e.transpose(PS[1][:, tk * 16:(tk + 1) * 16], inc[:, tk * 128:(tk + 1) * 128], ident[0:16, 0:16])
                        return ins
                    OP("pe", f, [b_s, b_ident], [PSB[1]])
                    cp(pos_all[:].rearrange("p k e -> p (k e)"), PS[1][:, :], [PSB[1]], [b_s])
                    RD2 = lambda o, i_: OP("dve", lambda e: e.tensor_reduce(out=o, in_=i_, axis=mybir.AxisListType.X, op=ALU.add), [b_s, b_all], [b_s, b_rw])
                    tt(tmp3[:], eq1_all[:], pos_all[:], ALU.mult, [b_s, b_all], [b_s])
                    RD2(rf[:], tmp3[:])
                    cp(r1i[:], rf[:], [b_s], [b_rw])
                    tt(tmp3[:], eq1_all[:], comb_all[:], ALU.mult, [b_s, b_all], [b_s])
                    RD2(w1s[:], tmp3[:])
                    tt(eq1_all[:], msk_all[:], eq1_all[:], ALU.subtract, [b_all], [b_all])
                    tt(tmp3[:], eq1_all[:], pos_all[:], ALU.mult, [b_s, b_all], [b_s])
                    RD2(rf[:], tmp3[:])
                    cp(r2i[:], rf[:], [b_s], [b_rw])
                    tt(tmp3[:], eq1_all[:], comb_all[:], ALU.mult, [b_s, b_all], [b_s])
                    RD2(w2s[:], tmp3[:])
                    ts(cmpt[:], iota[0:16, 0:NSLOT], endf[:, 0:1], None, ALU.is_ge, None, [b_iota, b_s], [b_s])
                    OP("pe", lambda e: e.matmul(PS[2][:, 0:NSLOT], ones_f[0:16, :], cmpt[:], start=True, stop=True), [b_s, b_onesf], [PSB[2]])
                    ts(sef[:], PS[2][:, 0:NSLOT], 15.0, float(l * NE), ALU.min, ALU.add, [PSB[2]], [b_s])
                    tt(pit[:], ident[:], iota[:, 0:128], ALU.mult, [b_ident, b_iota], [b_s])
                    OP("dve", lambda e: e.tensor_reduce(out=pidx[:], in_=pit[:], axis=mybir.AxisListType.X, op=ALU.add), [b_s], [b_s])
                    stt(sef[:], sef[:], 128.0, pidx[:, 0:1].to_broadcast([128, NSLOT]), ALU.mult, ALU.add, [b_s], [b_s])
                    cp(widx[:], sef[:], [b_s], [b_slot])
                    fw.barrier()
                with ExitStack() as st:
                    hrow = [sb(f"hrow{i}", [128, D], F32, st) for i in range(3)]; hrowb = [Buf() for _ in range(3)]
                    for tk in range(32):
                        i = tk % 3
                        fw.dma("sp", hrow[i][:], h2_d[tk * 128:(tk + 1) * 128, :], writes=[hrowb[i]])
                        for ri in (r1i, r2i):
                            fw.dma_ind(Xs_d[:, :], bass.IndirectOffsetOnAxis(ap=ri[:, tk:tk + 1], axis=0), hrow[i][:], None,
                                       reads=[hrowb[i], b_rw])
                    fw.barrier()
                with ExitStack() as st:
                    wg = [sb(f"wg{i}", [128, 8, DE], BF16, st) for i in range(2)]; wgb = [Buf() for _ in range(2)]
                    wu = [sb(f"wu{i}", [128, 8, DE], BF16, st) for i in range(2)]; wub = [Buf() for _ in range(2)]
                    wd = [sb(f"wd{i}", [128, 4, D], BF16, st) for i in range(2)]; wdb = [Buf() for _ in range(2)]
                    xs = [sb(f"xs{i}", [128, D], F32, st) for i in range(3)]; xsb = [Buf() for _ in range(3)]
                    xsT = [sb(f"xsT{i}", [128, 8, 128], BF16, st) for i in range(2)]; xsTb = [Buf() for _ in range(2)]
                    sg = [sb(f"sg{i}", [128, DE], F32, st) for i in range(2)]; sgb = [Buf() for _ in range(2)]
                    hd = [sb(f"hd{i}", [128, DE], F32, st) for i in range(2)]; hdb = [Buf() for _ in range(2)]
                    hdT = [sb(f"hdT{i}", [128, 4, 128], BF16, st) for i in range(2)]; hdTb = [Buf() for _ in range(2)]
                    yt = [sb(f"yt{i}", [128, D], F32, st) for i in range(2)]; ytb = [Buf() for _ in range(2)]

                    wg_rows = wg_d.rearrange("e p n -> (e p) n"); wu_rows = wu_d.rearrange("e p n -> (e p) n"); wd_rows = wd_d.rearrange("e p n -> (e p) n")

                    def load_gu(s_):
                        i = s_ % 2
                        off = bass.IndirectOffsetOnAxis(ap=widx[:, s_:s_ + 1], axis=0)
                        fw.dma_ind(wg[i][:].rearrange("p k n -> p (k n)"), None, wg_rows, off, reads=[b_slot], writes=[wgb[i]])
                        fw.dma_ind(wu[i][:].rearrange("p k n -> p (k n)"), None, wu_rows, off, reads=[b_slot], writes=[wub[i]])

                    def load_d(s_):
                        i = s_ % 2
                        off = bass.IndirectOffsetOnAxis(ap=widx[:, s_:s_ + 1], axis=0)
                        fw.dma_ind(wd[i][:].rearrange("p k n -> p (k n)"), None, wd_rows, off, reads=[b_slot], writes=[wdb[i]])

                    def load_x(u_):
                        fw.dma("sp", xs[u_ % 3][:], Xs_d[u_ * 128:(u_ + 1) * 128, :], writes=[xsb[u_ % 3]])

                    def st_T(u_):
                        i = u_ % 2
                        x3 = u_ % 3
                        for half in range(2):
                            def f(e, half=half):
                                for c4 in range(4):
                                    c = half * 4 + c4
                                    ins = e.transpose(PS[half][:, c4 * 128:(c4 + 1) * 128], xs[x3][:, c * 128:(c + 1) * 128], ident[:])
                                return ins
                            OP("pe", f, [xsb[x3], b_ident], [PSB[half]])
                            cp(xsT[i][:, half * 4:(half + 1) * 4, :], PS[half][:, :].rearrange("p (c t) -> p c t", c=4), [PSB[half]], [xsTb[i]],
                               eng=("act" if half == 0 else "dve"))

                    def st_GU(u_):
                        i = u_ % 2
                        wi = (u_ // SUB) % 2
                        for (pp, w_, wb__) in ((2, wg[wi], wgb[wi]), (3, wu[wi], wub[wi])):
                            def f(e, pp=pp, w_=w_):
                                for kc in range(8):
                                    ins = e.matmul(PS[pp][:, :], xsT[i][:, kc, :], w_[:, kc, :], start=(kc == 0), stop=(kc == 7))
                                return ins
                            OP("pe", f, [xsTb[i], wb__], [PSB[pp]])
                        act(sg[i][:], PS[2][:, :], AF.Silu, [PSB[2]], [sgb[i]])
                        tt(hd[i][:], PS[3][:, :], sg[i][:], ALU.mult, [PSB[3], sgb[i]], [hdb[i]])

                    def st_HT(u_):
                        i = u_ % 2

                        def f(e):
                            for c4 in range(4):
                                ins = e.transpose(PS[4][:, c4 * 128:(c4 + 1) * 128], hd[i][:, c4 * 128:(c4 + 1) * 128], ident[:])
                            return ins
                        OP("pe", f, [hdb[i], b_ident], [PSB[4]])
                        cp(hdT[i][:], PS[4][:, :].rearrange("p (c t) -> p c t", c=4), [PSB[4]], [hdTb[i]], eng="act")

                    def st_D(u_):
                        i = u_ % 2
                        wi = (u_ // SUB) % 2
                        for half in range(2):
                            ps = 5 + half

                            def f(e, half=half, ps=ps):
                                for kc in range(4):
                                    ins = e.matmul(PS[ps][:, :], hdT[i][:, kc, :], wd[wi][:, kc, half * 512:(half + 1) * 512], start=(kc == 0), stop=(kc == 3))
                                return ins
                            OP("pe", f, [hdTb[i], wdb[wi]], [PSB[ps]])
                            cp(yt[i][:, half * 512:(half + 1) * 512], PS[ps][:, :], [PSB[ps]], [ytb[i]], eng=("dve" if half == 0 else "act"))
                        fw.dma("sp", Ys_d[u_ * 128:(u_ + 1) * 128, :], yt[i][:], reads=[ytb[i]])

                    NU = SUB * NSLOT
                    for s_ in range(2):
                        load_gu(s_)
                        load_d(s_)
                    for u_ in range(3):
                        load_x(u_)
                    for step in range(NU + 3):
                        if step < NU:
                            st_T(step)
                            if step + 3 < NU:
                                load_x(step + 3)
                        if 0 <= step - 1 < NU:
                            u_ = step - 1
                            st_GU(u_)
                            if u_ % SUB == SUB - 1 and u_ // SUB + 2 < NSLOT:
                                load_gu(u_ // SUB + 2)
                        if 0 <= step - 2 < NU:
                            st_HT(step - 2)
                        if 0 <= step - 3 < NU:
                            u_ = step - 3
                            st_D(u_)
                            if u_ % SUB == SUB - 1 and u_ // SUB + 2 < NSLOT:
                                load_d(u_ // SUB + 2)
                    fw.barrier()
                with ExitStack() as st:
                    y1 = [sb(f"y1_{i}", [128, D], F32, st) for i in range(2)]; y2 = [sb(f"y2_{i}", [128, D], F32, st) for i in range(2)]
                    y1b = [Buf() for _ in range(2)]; y2b = [Buf() for _ in range(2)]
                    ac = [sb(f"ac{i}", [128, D], F32, st) for i in range(2)]; acb = [Buf() for _ in range(2)]
                    xm = [sb(f"xm{i}", [128, 8, 128], F32, st) for i in range(2)]; xmb = [Buf() for _ in range(2)]
                    otile = [sb(f"otile{i}", [128, D], F32, st) for i in range(2)]; otb = [Buf() for _ in range(2)]
                    def issue5(tk):
                        i = tk % 2
                        fw.dma_ind(y1[i][:], None, Ys_d[:, :], bass.IndirectOffsetOnAxis(ap=r1i[:, tk:tk + 1], axis=0), reads=[b_rw], writes=[y1b[i]])
                        fw.dma_ind(y2[i][:], None, Ys_d[:, :], bass.IndirectOffsetOnAxis(ap=r2i[:, tk:tk + 1], axis=0), reads=[b_rw], writes=[y2b[i]])
                        fw.dma("sp", xm[i][:], xT_d[:, :, tk * 128:(tk + 1) * 128].rearrange("c p t -> p c t"), writes=[xmb[i]])
                    issue5(0)
                    for tk in range(32):
                        i = tk % 2
                        if tk + 1 < 32:
                            issue5(tk + 1)
                        ts(ac[i][:], y1[i][:], w1s[:, tk:tk + 1], None, ALU.mult, None, [y1b[i], b_rw], [acb[i]])
                        stt(ac[i][:], y2[i][:], w2s[:, tk:tk + 1], ac[i][:], ALU.mult, ALU.add, [y2b[i], b_rw, acb[i]], [acb[i]])
                        for half in range(2):
                            ps = 2 * (tk % 2) + half

                            def f(e, half=half, ps=ps):
                                for c4 in range(4):
                                    c = half * 4 + c4
                                    ins = e.transpose(PS[ps][:, c4 * 128:(c4 + 1) * 128], ac[i][:, c * 128:(c + 1) * 128], ident[:])
                                return ins
                            OP("pe", f, [acb[i], b_ident], [PSB[ps]])
                            for c4 in range(4):
                                c = half * 4 + c4
                                stt(xm[i][:, c, :], PS[ps][:, c4 * 128:(c4 + 1) * 128], G2[:, c:c + 1], xm[i][:, c, :], ALU.mult, ALU.add,
                                    [PSB[ps], b_mod, xmb[i]], [xmb[i]])
                        if not last:
                            fw.dma("sp", xT_d[:, :, tk * 128:(tk + 1) * 128].rearrange("c p t -> p c t"), xm[i][:], reads=[xmb[i]])
                        else:
                            for half in range(2):
                                ps = 4 + 2 * (tk % 2) + half

                                def f(e, half=half, ps=ps):
                                    for c4 in range(4):
                                        c = half * 4 + c4
                                        ins = e.transpose(PS[ps][:, c4 * 128:(c4 + 1) * 128], xm[i][:, c, :], ident[:])
                                    return ins
                                OP("pe", f, [xmb[i], b_ident], [PSB[ps]])
                                cp(otile[i][:, half * 512:(half + 1) * 512], PS[ps][:, :], [PSB[ps]], [otb[i]], eng=("act" if half == 0 else "dve"))
                            fw.dma("sp", out_d[tk * 128:(tk + 1) * 128, :], otile[i][:], reads=[otb[i]])
                    fw.barrier()
    fw.barrier()
    return nc, fw, dbg


def host_inputs(inp, b):
    f = np.float32
    A = np.ascontiguousarray
    m = {}
    m["x"] = A(inp["x"][b])
    m["c_fm"] = A(inp["c"][b].reshape(8, 128).T)
    m["ada_w"] = inp["ada_w"]
    m["ada_b_fm"] = A(inp["ada_b"].reshape(2, 48, 128).transpose(0, 2, 1))
    m["n1g"] = A(inp["norm1_g"].reshape(2, 8, 128).transpose(0, 2, 1))
    m["n2g"] = A(inp["norm2_g"].reshape(2, 8, 128).transpose(0, 2, 1))
    m["w_in"] = inp["w_in"]
    m["fb"] = A(inp["forget_b"].reshape(2, 8, 1))
    def gp_lay(a):
        return A(a.reshape(2, 8, 2, 64).transpose(0, 2, 3, 1).reshape(2, 128, 8))
    m["lam_re"] = gp_lay(inp["lam_re"])
    m["lam_im"] = gp_lay(inp["lam_im"])
    m["log_dt"] = gp_lay(np.broadcast_to(inp["log_dt"][:, :, None], (2, 16, 64)))
    m["sb_re"] = A(inp["ssm_b_re"].reshape(2, 8, 2, 64, 16).transpose(0, 2, 3, 1, 4).reshape(2, 128, 8, 16))
    m["sb_im"] = A(inp["ssm_b_im"].reshape(2, 8, 2, 64, 16).transpose(0, 2, 3, 1, 4).reshape(2, 128, 8, 16))
    m["sc_re"] = A(inp["ssm_c_re"].reshape(2, 8, 2, 16, 64).transpose(0, 2, 4, 1, 3).reshape(2, 128, 8, 16))
    m["sc_im"] = A(inp["ssm_c_im"].reshape(2, 8, 2, 16, 64).transpose(0, 2, 4, 1, 3).reshape(2, 128, 8, 16))
    m["ssm_d"] = A(inp["ssm_d"].reshape(2, 2, 128).transpose(0, 2, 1))
    m["glu_w"] = inp["glu_w"]
    m["glu_b"] = A(inp["glu_b"].reshape(2, 2, 128).transpose(0, 2, 1))
    m["qg"] = A(np.tile(inp["q_norm_g"], (1, 2)).reshape(2, 128, 1))
    m["kg"] = A(np.tile(inp["k_norm_g"], (1, 2)).reshape(2, 128, 1))
    m["conv_w"] = A(inp["conv_w"].reshape(2, 3, 2, 128).transpose(0, 3, 2, 1))
    m["og_fm"] = A(inp["out_norm_g"].reshape(2, 8, 128).transpose(0, 2, 1))
    m["og_at"] = A(inp["out_norm_g"][:, 256:768].reshape(2, 8, 64).transpose(0, 2, 1))
    m["w_out"] = inp["w_out"]
    m["w_router"] = inp["w_router"]
    m["rbias"] = A(np.broadcast_to(inp["router_bias"][None, :], (128, 16)))
    m["w_gate"] = inp["w_gate"]
    m["w_up"] = inp["w_up"]
    m["w_down"] = inp["w_down"]
    m["ident"] = np.eye(128, dtype=f)
    e127 = np.zeros((128, 128), f); e127[127, :] = 1
    m["e127"] = e127
    blk = np.zeros((128, 128), f); blk[:64, :64] = 1; blk[64:, 64:] = 1
    m["blk64"] = blk
    m["tri"] = np.triu(np.ones((128, 128), f))
    m["iota"] = A(np.broadcast_to(np.arange(TT + 1, dtype=f)[None, :], (128, TT + 1)))
    sel = np.zeros((16, 16, 128), f)
    for e in range(16):
        sel[e, e, :] = 1
    m["sel16"] = sel
    return {k: np.asarray(v, dtype=f) for k, v in m.items()}


_CACHE = {}


def kernel(**inputs):
    inp = {k: np.asarray(v) for k, v in inputs.items()}
    if "nc" not in _CACHE:
        _CACHE["nc"] = build_program()[0]
    nc = _CACHE["nc"]
    in_maps = [host_inputs(inp, b) for b in range(8)]
    res = run_bass_kernel_spmd(nc, in_maps, core_ids=list(range(8)))
    out = np.stack([np.asarray(r["out"]) for r in res.results], axis=0)
    return out.astype(np.float32)
```

```python
import numpy as np
from contextlib import ExitStack
import concourse.bass as bass
import concourse.mybir as mybir
from concourse.bass_utils import run_bass_kernel_spmd

F32 = mybir.dt.float32
BF16 = mybir.dt.bfloat16
I32 = mybir.dt.int32
AF = mybir.ActivationFunctionType
ALU = mybir.AluOpType

S = 4096
D = 1024
TT = 512
NT = S // TT
DIN = 2568
NE = 16
DE = 512
EPS = 1e-6
TWO_PI = 6.283185307179586
import os as _os
NOCONV = bool(_os.environ.get('NOCONV'))
POOLENG = _os.environ.get('POOLENG', 'pool')
OFF_U, OFF_Q, OFF_K, OFF_V, OFF_F, OFF_HC, OFF_BG, OFF_CG = 0, 256, 768, 1280, 1792, 1800, 2056, 2312


class Buf:
    __slots__ = ("name", "w", "r")

    def __init__(self, name=""):
        self.name = name
        self.w = {}
        self.r = {}


class Fw:
    ENG = ("pe", "act", "dve", "pool", "sp")

    def __init__(self, nc, ndma=20):
        self.nc = nc
        self.eng = dict(pe=nc.tensor, act=nc.scalar, dve=nc.vector, pool=nc.gpsimd, sp=nc.sync)
        self.sem = {e: nc.alloc_semaphore("sem_" + e) for e in self.ENG}
        self.cnt = {e: 0 for e in self.ENG}
        self.known = {e: {} for e in self.ENG}
        self.dq = {}
        for q in ("sp", "pool"):
            self.dq[q] = dict(sems=[nc.alloc_semaphore(f"dq_{q}_{i}") for i in range(ndma)],
                              uses=[0] * ndma, nxt=0)
        self.allsems = {}
        self.nwaits = 0
        self.bg_on = False
        self.bg_sems = {s_.num for s_ in self.dq["pool"]["sems"]}

    def _wait(self, e, sem, val):
        k = self.known[e]
        if k.get(sem.num, 0) >= val:
            return
        self.eng[e].wait_ge(sem, val)
        self.nwaits += 1
        k[sem.num] = val

    def _deps(self, e, reads, writes):
        need = {}
        mysem = self.sem[e].num

        def add(tok, same_ok):
            sem, val = tok
            if same_ok and sem.num == mysem and e == "pe":
                return
            if need.get(sem.num, (None, 0))[1] < val:
                need[sem.num] = (sem, val)

        for b in reads:
            for tok in b.w.values():
                add(tok, False)
        for b in writes:
            for tok in b.w.values():
                add(tok, True)
            for tok in b.r.values():
                add(tok, True)
        for sem, val in need.values():
            self._wait(e, sem, val)

    def op(self, e, fn, reads=(), writes=()):
        self._deps(e, reads, writes)
        ins = fn(self.eng[e])
        self.cnt[e] += 1
        sem = self.sem[e]
        ins.then_inc(sem, 1)
        tok = (sem, self.cnt[e])
        for b in reads:
            b.r[sem.num] = tok
        for b in writes:
            b.w = {sem.num: tok}
            b.r = {}
        self.allsems[sem.num] = tok
        return tok

    def dma(self, q, out, in_, reads=(), writes=()):
        d = self.dq[q]
        i = d["nxt"]
        d["nxt"] = (i + 1) % len(d["sems"])
        sem = d["sems"][i]
        if d["uses"][i] > 0:
            self._wait(q, sem, 16 * d["uses"][i])
        self._deps(q, reads, writes)
        ins = self.eng[q].dma_start(out=out, in_=in_)
        d["uses"][i] += 1
        tok = (sem, 16 * d["uses"][i])
        ins.then_inc(sem, 16)
        for b in reads:
            b.r[sem.num] = tok
        for b in writes:
            b.w = {sem.num: tok}
            b.r = {}
        self.allsems[sem.num] = tok
        return tok

    def dma_ind(self, out, out_off, in_, in_off, reads=(), writes=()):
        q = "pool"
        d = self.dq[q]
        i = d["nxt"]
        d["nxt"] = (i + 1) % len(d["sems"])
        sem = d["sems"][i]
        if d["uses"][i] > 0:
            self._wait(q, sem, 16 * d["uses"][i])
        self._deps(q, reads, writes)
        ins = self.eng[q].indirect_dma_start(out=out, out_offset=out_off, in_=in_, in_offset=in_off)
        d["uses"][i] += 1
        tok = (sem, 16 * d["uses"][i])
        ins.then_inc(sem, 16)
        for b in reads:
            b.r[sem.num] = tok
        for b in writes:
            b.w = {sem.num: tok}
            b.r = {}
        self.allsems[sem.num] = tok
        return tok

    def barrier(self):
        for e in self.ENG:
            for sem, val in list(self.allsems.values()):
                if sem.num == self.sem[e].num:
                    continue
                if self.bg_on and sem.num in self.bg_sems:
                    continue
                self._wait(e, sem, val)


def build_program(nlayers=2, debug=False, stop_after=None):
    nc = bass.Bass("TRN2", target_bir_lowering=False)
    fw = Fw(nc)
    dbg = {}

    def din(name, shape, dt=F32):
        return nc.dram_tensor(name, list(shape), dt, kind="ExternalInput").ap()

    def dscr(name, shape, dt=F32):
        if debug:
            return nc.dram_tensor(name, list(shape), dt, kind="ExternalOutput").ap()
        return nc.dram_tensor(name, list(shape), dt).ap()

    x_in = din("x", [S, D])
    c_fm = din("c_fm", [128, 8])
    ada_w = din("ada_w", [2, D, 6 * D])
    ada_b_fm = din("ada_b_fm", [2, 128, 48])
    n1g = din("n1g", [2, 128, 8])
    n2g = din("n2g", [2, 128, 8])
    w_in = din("w_in", [2, D, DIN])
    fb = din("fb", [2, 8, 1])
    lam_re = din("lam_re", [2, 128, 8])
    lam_im = din("lam_im", [2, 128, 8])
    log_dt = din("log_dt", [2, 128, 8])
    sb_re = din("sb_re", [2, 128, 8, 16])
    sb_im = din("sb_im", [2, 128, 8, 16])
    sc_re = din("sc_re", [2, 128, 8, 16])
    sc_im = din("sc_im", [2, 128, 8, 16])
    ssm_d = din("ssm_d", [2, 128, 2])
    glu_w = din("glu_w", [2, 256, 256])
    glu_b = din("glu_b", [2, 128, 2])
    qg = din("qg", [2, 128, 1])
    kg = din("kg", [2, 128, 1])
    conv_w = din("conv_w", [2, 128, 2, 3])
    og_fm = din("og_fm", [2, 128, 8])
    og_at = din("og_at", [2, 64, 8])
    w_out = din("w_out", [2, D, D])
    w_router = din("w_router", [D, NE])
    rbias = din("rbias", [128, NE])
    w_gate = din("w_gate", [2, NE, D, DE])
    w_up = din("w_up", [2, NE, D, DE])
    w_down = din("w_down", [2, NE, DE, D])
    ident_in = din("ident", [128, 128])
    e127_in = din("e127", [128, 128])
    blk64_in = din("blk64", [128, 128])
    tri_in = din("tri", [128, 128])
    iota_in = din("iota", [128, TT + 1])
    sel_in = din("sel16", [16, NE, 128])
    out_d = nc.dram_tensor("out", [S, D], F32, kind="ExternalOutput").ap()

    xT_d = dscr("xT_d", [8, 128, S])
    wg_d = dscr("wg_d", [2 * NE, 128, 8 * DE], BF16)
    wu_d = dscr("wu_d", [2 * NE, 128, 8 * DE], BF16)
    wd_d = dscr("wd_d", [2 * NE, 128, 4 * D], BF16)
    uT_d = dscr("uT_d", [2, 128, S])
    qT_d = dscr("qT_d", [8, 65, S], BF16)
    kT_d = dscr("kT_d", [8, 65, S], BF16)
    yh_d = dscr("yh_d", [4, 128, S], BF16)
    ya_d = dscr("ya_d", [8, 64, S], BF16)
    h2_d = dscr("h2_d", [S, D])
    Xs_d = dscr("Xs_d", [80 * 128, D])
    Ys_d = dscr("Ys_d", [80 * 128, D])
    if debug:
        for nm, shp, dt in (("dbg_h", [8, 128, S], BF16), ("dbg_cum", [8, S], F32), ("dbg_mod", [128, 48], F32),
                            ("dbg_v", [128, 32, 8, 65], BF16), ("dbg_ssmpre", [2, 128, S], F32),
                            ("dbg_comb", [S, NE], F32), ("dbg_xmid", [8, 128, S], F32)):
            dbg[nm] = nc.dram_tensor(nm, shp, dt, kind="ExternalOutput").ap()

    es = ExitStack()

    uid = [0]

    def sb(name, shape, dt=F32, stack=None):
        uid[0] += 1
        return (stack or es).enter_context(nc.sbuf_tensor(f"s{uid[0]}_{name}", list(shape), dt))

    PS = [es.enter_context(nc.psum_tensor(f"ps{i}", [128, 512], F32)) for i in range(8)]
    PSB = [Buf(f"ps{i}") for i in range(8)]

    ident = sb("ident", [128, 128]); b_ident = Buf()
    e127 = sb("e127", [128, 128]); b_e127 = Buf()
    tri_f = sb("tri_f", [128, 128]); b_tri = Buf()
    blk_f = sb("blk_f", [128, 128]); blk = sb("blk", [128, 128], BF16); b_blk = Buf()
    ones_b = sb("ones_b", [128, 128], BF16); b_ones = Buf()
    ones_f = sb("ones_f", [128, 128]); b_onesf = Buf()
    iota = sb("iota", [128, TT + 1]); b_iota = Buf()
    sel16_f = sb("sel16_f", [16, NE, 128]); b_sel = Buf()
    cfm = sb("cfm", [128, 8]); b_cfm = Buf()
    cact = sb("cact", [128, 8]); b_cact = Buf()
    rb_sb = sb("rb_sb", [128, NE]); b_rb = Buf()
    wr_sb = sb("wr_sb", [128, 8, NE]); b_wr = Buf()

    fw.dma("sp", ident[:], ident_in, writes=[b_ident])
    fw.dma("sp", e127[:], e127_in, writes=[b_e127])
    fw.dma("sp", tri_f[:], tri_in, writes=[b_tri])
    fw.dma("sp", blk_f[:], blk64_in, writes=[b_blk])
    fw.dma("sp", iota[:], iota_in, writes=[b_iota])
    fw.dma("sp", sel16_f[:], sel_in, writes=[b_sel])
    fw.dma("sp", cfm[:], c_fm, writes=[b_cfm])
    fw.dma("sp", rb_sb[:], rbias, writes=[b_rb])
    fw.dma("sp", wr_sb[:], w_router.rearrange("(kc p) n -> p kc n", p=128), writes=[b_wr])
    fw.op("dve", lambda e: e.tensor_copy(out=blk[:], in_=blk_f[:]), reads=[b_blk], writes=[b_blk])
    fw.op("dve", lambda e: e.memset(ones_b[:], 1.0), writes=[b_ones])
    fw.op("dve", lambda e: e.memset(ones_f[:], 1.0), writes=[b_onesf])
    fw.op("act", lambda e: e.activation(out=cact[:], in_=cfm[:], func=AF.Silu), reads=[b_cfm], writes=[b_cact])

    def OP(e, fn, reads=(), writes=()):
        return fw.op(e, fn, reads, writes)

    def act(out, in_, func, reads, writes, scale=1.0, bias=0.0):
        return fw.op("act", lambda e: e.activation(out=out, in_=in_, func=func, bias=bias, scale=scale), reads, writes)

    def tt(out, in0, in1, op, reads, writes, eng="dve"):
        return fw.op(eng, lambda e: e.tensor_tensor(out=out, in0=in0, in1=in1, op=op), reads, writes)

    def ts(out, in0, s1, s2, op0, op1, reads, writes, eng="dve"):
        if op1 is None:
            return fw.op(eng, lambda e: e.tensor_scalar(out=out, in0=in0, scalar1=s1, scalar2=None, op0=op0), reads, writes)
        return fw.op(eng, lambda e: e.tensor_scalar(out=out, in0=in0, scalar1=s1, scalar2=s2, op0=op0, op1=op1), reads, writes)

    def stt(out, in0, scalar, in1, op0, op1, reads, writes):
        return fw.op("dve", lambda e: e.scalar_tensor_tensor(out=out, in0=in0, scalar=scalar, in1=in1, op0=op0, op1=op1),
                     reads, writes)

    def cp(out, in_, reads, writes, eng="dve"):
        if eng == "act":
            return fw.op("act", lambda e: e.copy(out=out, in_=in_), reads, writes)
        return fw.op(eng, lambda e: e.tensor_copy(out=out, in_=in_), reads, writes)

    ntri = sb("ntri", [128, 128], BF16); ident_b = sb("ident_b", [128, 128], BF16); b_ntri = Buf()
    tt(tri_f[:], tri_f[:], ident[:], ALU.subtract, [b_tri, b_ident], [b_tri])
    ts(ntri[:], tri_f[:], -30000.0, None, ALU.mult, None, [b_tri], [b_ntri])
    cp(ident_b[:], ident[:], [b_ident], [b_ntri])
    eps_c = sb("eps_c", [128, 1]); b_eps = Buf()
    OP("dve", lambda e: e.memset(eps_c[:], EPS), writes=[b_eps])

    hn_sq = [sb(f"hn_sq{i}", [128, TT], BF16) for i in range(2)]; hn_sqb = [Buf() for _ in range(2)]
    hn_rt = [sb(f"hn_rt{i}", [128, TT]) for i in range(2)]; hn_rtb = [Buf() for _ in range(2)]
    hn_ctr = [0]
    HN_PS = 7

    def hnorm(src, srcb, g_ap, gb, out, outb, P=128, n=TT):
        i = hn_ctr[0] % 2
        hn_ctr[0] += 1
        act(hn_sq[i][0:P, 0:n], src, AF.Square, [srcb], [hn_sqb[i]])
        OP("pe", lambda e: e.matmul(PS[HN_PS][0:P, 0:n], blk[0:P, 0:P], hn_sq[i][0:P, 0:n], start=True, stop=True),
           [hn_sqb[i], b_blk], [PSB[HN_PS]])
        act(hn_rt[i][0:P, 0:n], PS[HN_PS][0:P, 0:n], AF.Ln, [PSB[HN_PS], b_eps], [hn_rtb[i]], scale=1.0 / 64, bias=eps_c[0:P, :])
        act(hn_rt[i][0:P, 0:n], hn_rt[i][0:P, 0:n], AF.Exp, [hn_rtb[i]], [hn_rtb[i]], scale=-0.5)
        stt(out, src, g_ap, hn_rt[i][0:P, 0:n], ALU.mult, ALU.mult, [srcb, gb, hn_rtb[i]], [outb])

    with ExitStack() as st:
        xin = [sb(f"xin{i}", [128, D], F32, st) for i in range(4)]
        xinb = [Buf() for _ in range(4)]
        xo = [sb(f"xo{i}", [128, 8, 128], F32, st) for i in range(4)]
        xob = [Buf() for _ in range(4)]
        for tk in range(S // 128):
            i = tk % 4
            fw.dma("sp", xin[i][:], x_in[tk * 128:(tk + 1) * 128, :], writes=[xinb[i]])
            for half in range(2):
                pb = (2 * tk + half) % 8

                def f(e, i=i, half=half, pb=pb):
                    for c4 in range(4):
                        c = half * 4 + c4
                        ins = e.transpose(PS[pb][:, c4 * 128:(c4 + 1) * 128], xin[i][:, c * 128:(c + 1) * 128], ident[:])
                    return ins
                OP("pe", f, [xinb[i], b_ident], [PSB[pb]])
                cp(xo[i][:, half * 4:(half + 1) * 4, :], PS[pb][:].rearrange("p (c t) -> p c t", c=4),
                   [PSB[pb]], [xob[i]], eng=("act" if half == 0 else "dve"))
            fw.dma("sp", xT_d[:, :, tk * 128:(tk + 1) * 128].rearrange("c p t -> p c t"), xo[i][:], reads=[xob[i]])
    fw.barrier()

    def norm_mod(st_, xt, xtb, A, B, ABb, hb, hbb, xn, xnb, sqb, sqbb, rt, rtb, psn):
        act(sqb[:], xt[:], AF.Square, [xtb], [sqbb])

        def f(e):
            for c in range(8):
                ins = e.matmul(PS[psn][:, :], ones_b[:], sqb[:, c, :], start=(c == 0), stop=(c == 7))
            return ins
        OP("pe", f, [sqbb, b_ones], [PSB[psn]])
        act(rt[:], PS[psn][:, :], AF.Ln, [PSB[psn], b_eps], [rtb], scale=1.0 / D, bias=eps_c[:])
        act(rt[:], rt[:], AF.Exp, [rtb], [rtb], scale=-0.5)
        tt(xn[:], xt[:], rt[:].unsqueeze(1).to_broadcast([128, 8, TT]), ALU.mult, [xtb, rtb], [xnb])
        for c in range(8):
            if c % 2 == 0:
                ts(xn[:, c, :], xn[:, c, :], A[:, c:c + 1], B[:, c:c + 1], ALU.mult, ALU.add, [xnb, ABb], [xnb])
            else:
                act(xn[:, c, :], xn[:, c, :], AF.Identity, [xnb, ABb], [xnb], scale=A[:, c:c + 1], bias=B[:, c:c + 1])
        cp(hb[:, 0:4, :], xn[:, 0:4, :], [xnb], [hbb], eng="dve")
        cp(hb[:, 4:8, :], xn[:, 4:8, :], [xnb], [hbb], eng="act")

    for l in range(nlayers if stop_after != "0" else 0):
        last = (l == nlayers - 1)
        with ExitStack() as lay:
            mod = sb("mod", [128, 48], F32, lay); b_mod = Buf()
            A1 = sb("A1", [128, 8], F32, lay); A2 = sb("A2", [128, 8], F32, lay)
            n1g_sb = sb("n1g_sb", [128, 8], F32, lay); n2g_sb = sb("n2g_sb", [128, 8], F32, lay); b_ng = Buf()
            adab = sb("adab", [128, 48], F32, lay); b_adab = Buf()
            cact2 = sb("cact2", [128, 8, 2], F32, lay); b_cact2 = Buf()
            fw.dma("sp", n1g_sb[:], n1g[l], writes=[b_ng])
            fw.dma("sp", n2g_sb[:], n2g[l], writes=[b_ng])
            fw.dma("sp", adab[:], ada_b_fm[l], writes=[b_adab])
            cp(cact2[:], cact[:].unsqueeze(2).to_broadcast([128, 8, 2]), [b_cact], [b_cact2])
            with ExitStack() as st:
                adw = [sb(f"adw{i}", [128, 8, D], F32, st) for i in range(2)]
                adwb = [Buf() for _ in range(2)]
                for j in range(6):
                    i = j % 2
                    fw.dma("sp", adw[i][:], ada_w[l, :, j * D:(j + 1) * D].rearrange("(kc p) n -> p kc n", p=128),
                           writes=[adwb[i]])
                    for c in range(8):
                        def f(e, i=i, j=j, c=c):
                            for kc in range(8):
                                col = 2 * (j * 8 + c)
                                ins = e.matmul(PS[0][:, col:col + 2], adw[i][:, kc, c * 128:(c + 1) * 128], cact2[:, kc, :],
                                               start=(kc == 0), stop=(kc == 7))
                            return ins
                        OP("pe", f, [adwb[i], b_cact2], [PSB[0]])
                tt(mod[:], PS[0][:, 0:96].rearrange("p (n two) -> p n two", two=2)[:, :, 0], adab[:], ALU.add,
                   [PSB[0], b_adab], [b_mod])
                stt(A1[:], mod[:, 8:16], 1.0, n1g_sb[:], ALU.add, ALU.mult, [b_mod, b_ng], [b_mod])
                stt(A2[:], mod[:, 32:40], 1.0, n2g_sb[:], ALU.add, ALU.mult, [b_mod, b_ng], [b_mod])
                if debug and l == 0:
                    fw.dma("sp", dbg["dbg_mod"], mod[:], reads=[b_mod])
                fw.barrier()
            B1 = mod[:, 0:8]; G1 = mod[:, 16:24]; B2 = mod[:, 24:32]; G2 = mod[:, 40:48]

            with ExitStack() as mix:
                V_sb = sb("V_sb", [128, 32, 8, 65], BF16, mix); b_V = Buf()
                cum = sb("cum", [8, S], F32, mix); b_cum = Buf()
                og_fm_sb = sb("og_fm_sb", [128, 8], F32, mix); og_at_sb = sb("og_at_sb", [64, 8], F32, mix); b_og = Buf()
                fw.dma("sp", og_fm_sb[:], og_fm[l], writes=[b_og])
                fw.dma("sp", og_at_sb[:], og_at[l], writes=[b_og])
                OP("dve", lambda e: e.memset(V_sb[:, :, :, 64:65], 1.0), writes=[b_V])
                if l == 0:
                    cv = [sb(f"cv{i}", [128, 2048], BF16, mix) for i in range(2)]; cvb = [Buf() for _ in range(2)]; cvk = [0]
                fw.barrier()
                with ExitStack() as st:
                    win = sb("win", [128, 8, DIN], BF16, st); b_win = Buf()
                    for hh in range(2):
                        fw.dma("pool", win[:, :, hh * 1284:(hh + 1) * 1284],
                               w_in[l, :, hh * 1284:(hh + 1) * 1284].rearrange("(kc p) n -> p kc n", p=128), writes=[Buf()])
                    fw.barrier()
                    if l == 0 and not NOCONV:
                        for l2 in range(nlayers):
                            for e_ in range(NE):
                                for (src, dst, pat) in ((w_gate, wg_d, 8), (w_up, wu_d, 8), (w_down, wd_d, 4)):
                                    for hf in range(2):
                                        i = cvk[0] % 2
                                        cvk[0] += 1
                                        kcs = pat // 2
                                        srcap = src[l2, e_].rearrange("(kc p) n -> p kc n", p=128)[:, hf * kcs:(hf + 1) * kcs, :]
                                        dstv = cv[i][:].rearrange("p (kc n) -> p kc n", kc=kcs)
                                        fw.dma("pool", dstv, srcap, writes=[cvb[i]])
                                        fw.dma("pool", dst[l2 * NE + e_][:, hf * 2048:(hf + 1) * 2048], cv[i][:], reads=[cvb[i]])
                        fw.bg_on = True
                    fbs = sb("fbs", [8, 1], F32, st); b_fb = Buf()
                    qgs = sb("qgs", [128, 1], F32, st); kgs = sb("kgs", [128, 1], F32, st); b_qk = Buf()
                    cws = sb("cws", [128, 2, 3], F32, st); b_cw = Buf()
                    fw.dma("sp", fbs[:], fb[l], writes=[b_fb])
                    fw.dma("sp", qgs[:], qg[l], writes=[b_qk])
                    fw.dma("sp", kgs[:], kg[l], writes=[b_qk])
                    fw.dma("sp", cws[:], conv_w[l], writes=[b_cw])
                    ts(fbs[:], fbs[:], -1.0, None, ALU.mult, None, [b_fb], [b_fb])
                    ts(qgs[:], qgs[:], 0.125, None, ALU.mult, None, [b_qk], [b_qk])
                    if l == 0:
                        xt = [sb("xt0", [128, 8, TT], F32, st)] * 2; xtb = [Buf()] * 2
                    else:
                        xt = [sb(f"xt{i}", [128, 8, TT], F32, st) for i in range(2)]; xtb = [Buf() for _ in range(2)]
                    sqb = sb("sqb", [128, 8, TT], BF16, st); sqbb = Buf()
                    rt = sb("rt", [128, TT], F32, st); rtb = Buf()
                    xn = sb("xn", [128, 8, TT], F32, st); xnb = Buf()
                    hb = [sb(f"hb{i}", [128, 8, TT], BF16, st) for i in range(2)]; hbb = [Buf() for _ in range(2)]
                    ev = [sb(f"ev{i}", [128, TT], F32, st) for i in range(4)]; evb = [Buf() for _ in range(4)]
                    evo = [sb(f"evo{i}", [128, TT], BF16, st) for i in range(4)]; evob = [Buf() for _ in range(4)]
                    zt = [[sb(f"zt{cc}{i}", [128, TT + 2], F32, st) for i in range(2)] for cc in range(2)]
                    ztb = [[Buf() for _ in range(2)] for _ in range(2)]
                    cy = sb("cy", [128, TT], F32, st); cyb = Buf()
                    fe = sb("fe", [8, TT], F32, st); feb = Buf()
                    evc = [0]
                    gen = [0]
                    for cc in range(2):
                        OP("dve", lambda e, cc=cc: e.memset(zt[cc][1][:, TT:TT + 2], 0.0), writes=[ztb[cc][1]])

                    def proj(ps, off, M, hbt, hbtb):
                        def f(e):
                            for kc in range(8):
                                ins = e.matmul(PS[ps][0:M, :], win[:, kc, off:off + M], hbt[:, kc, :], start=(kc == 0), stop=(kc == 7))
                            return ins
                        OP("pe", f, [hbtb], [PSB[ps]])

                    for it in range(NT):
                        t0 = it * TT
                        i = it % 2
                        fw.dma("sp", xt[i][:], xT_d[:, :, t0:t0 + TT].rearrange("c p t -> p c t"), writes=[xtb[i]])
                        norm_mod(st, xt[i], xtb[i], A1, B1, b_mod, hb[i], hbb[i], xn, xnb, sqb, sqbb, rt, rtb, 0)
                        if debug and l == 0:
                            fw.dma("sp", dbg["dbg_h"][:, :, t0:t0 + TT].rearrange("c p t -> p c t"), hb[i][:], reads=[hbb[i]])
                        for c in range(2):
                            ps = 1 + gen[0] % 2; gen[0] += 1
                            proj(ps, OFF_U + c * 128, 128, hb[i], hbb[i])
                            k = evc[0] % 4; evc[0] += 1
                            cp(ev[k][:], PS[ps][:, :], [PSB[ps]], [evb[k]], eng="act")
                            fw.dma("sp", uT_d[c, :, t0:t0 + TT], ev[k][:], reads=[evb[k]])
                        for (off, gsb, dst) in ((OFF_Q, qgs, qT_d), (OFF_K, kgs, kT_d)):
                            for c in range(4):
                                ps = 1 + gen[0] % 2; gen[0] += 1
                                proj(ps, off + c * 128, 128, hb[i], hbb[i])
                                k = evc[0] % 4; evc[0] += 1
                                cp(ev[k][:], PS[ps][:, :], [PSB[ps]], [evb[k]], eng="act")
                                hnorm(ev[k][:], evb[k], gsb[:, 0:1], b_qk, evo[k][:], evob[k])
                                fw.dma("sp", dst[2 * c, 0:64, t0:t0 + TT], evo[k][0:64, :], reads=[evob[k]])
                                fw.dma("sp", dst[2 * c + 1, 0:64, t0:t0 + TT], evo[k][64:128, :], reads=[evob[k]])
                        ps = 1 + gen[0] % 2; gen[0] += 1
                        proj(ps, OFF_F, 8, hb[i], hbb[i])
                        act(fe[:], PS[ps][0:8, :], AF.Exp, [PSB[ps], b_fb], [feb], scale=-1.0, bias=fbs[:])
                        act(fe[:], fe[:], AF.Ln, [feb], [feb], bias=1.0)
                        init = 0.0 if it == 0 else cum[:, t0 - 1:t0]
                        OP("dve", lambda e, t0=t0, init=init: e.tensor_tensor_scan(
                            out=cum[:, t0:t0 + TT], data0=ones_f[0:8, 0:1].to_broadcast([8, TT]), data1=fe[:], initial=init,
                            op0=ALU.mult, op1=ALU.subtract), [feb, b_onesf, b_cum], [b_cum])
                        for sub in range(4):
                            def f(e, sub=sub, i=i):
                                for kc in range(8):
                                    ins = e.matmul(PS[3][:, :], hb[i][:, kc, sub * 128:(sub + 1) * 128], win[:, kc, OFF_V:OFF_V + 512],
                                                   start=(kc == 0), stop=(kc == 7))
                                return ins
                            OP("pe", f, [hbb[i]], [PSB[3]])
                            cp(V_sb[:, 4 * it + sub, :, 0:64], PS[3][:, :].rearrange("p (h d) -> p h d", h=8), [PSB[3]], [b_V],
                               eng=("act" if sub % 2 else "dve"))
                        for cc in range(2):
                            proj(4, OFF_HC + cc * 128, 128, hb[i], hbb[i])
                            proj(5, OFF_CG + cc * 128, 128, hb[i], hbb[i])
                            proj(6, OFF_BG + cc * 128, 128, hb[i], hbb[i])
                            k = evc[0] % 4; evc[0] += 1
                            z, zb = zt[cc][i], ztb[cc][i]
                            zp, zpb = zt[cc][1 - i], ztb[cc][1 - i]
                            cp(ev[k][:], PS[5][:, :], [PSB[5]], [evb[k]], eng="act")
                            cp(z[:, 0:2], zp[:, TT:TT + 2], [zpb], [zb])
                            tt(z[:, 2:TT + 2], PS[4][:, :], ev[k][:], ALU.mult, [PSB[4], evb[k]], [zb])
                            ts(cy[:], z[:, 2:TT + 2], cws[:, cc, 2:3], None, ALU.mult, None, [zb, b_cw], [cyb])
                            stt(cy[:], z[:, 1:TT + 1], cws[:, cc, 1:2], cy[:], ALU.mult, ALU.add, [zb, b_cw, cyb], [cyb])
                            stt(cy[:], z[:, 0:TT], cws[:, cc, 0:1], cy[:], ALU.mult, ALU.add, [zb, b_cw, cyb], [cyb])
                            tt(ev[k][:], PS[6][:, :], cy[:], ALU.mult, [PSB[6], cyb], [evb[k]])
                            hnorm(ev[k][:], evb[k], og_fm_sb[:, 6 + cc:7 + cc], b_og, evo[k][:], evob[k])
                            fw.dma("sp", yh_d[2 + cc, :, t0:t0 + TT], evo[k][:], reads=[evob[k]])
                    if debug and l == 0:
                        fw.dma("sp", dbg["dbg_cum"], cum[:], reads=[b_cum])
                        fw.dma("sp", dbg["dbg_v"], V_sb[:], reads=[b_V])
                    fw.barrier()
                if stop_after == "A":
                    break
                with ExitStack() as st:
                    def t8(name):
                        return sb(name, [128, 8], F32, st)
                    lre, lim, ldt = t8("lre"), t8("lim"), t8("ldt"); b_p = Buf()
                    fw.dma("sp", lre[:], lam_re[l], writes=[b_p])
                    fw.dma("sp", lim[:], lam_im[l], writes=[b_p])
                    fw.dma("sp", ldt[:], log_dt[l], writes=[b_p])
                    bre = sb("bre", [128, 8, 16], F32, st); bim = sb("bim", [128, 8, 16], F32, st)
                    cre = sb("cre", [128, 8, 16], F32, st); cim = sb("cim", [128, 8, 16], F32, st); b_bc = Buf()
                    fw.dma("sp", bre[:], sb_re[l], writes=[b_bc]); fw.dma("sp", bim[:], sb_im[l], writes=[b_bc])
                    fw.dma("sp", cre[:], sc_re[l], writes=[b_bc]); fw.dma("sp", cim[:], sc_im[l], writes=[b_bc])
                    dsk = sb("dsk", [128, 2], F32, st); glb = sb("glb", [128, 2], F32, st); b_dg = Buf()
                    fw.dma("sp", dsk[:], ssm_d[l], writes=[b_dg]); fw.dma("sp", glb[:], glu_b[l], writes=[b_dg])
                    gluw = sb("gluw", [128, 2, 256], BF16, st); gluwf = sb("gluwf", [128, 2, 256], F32, st); b_gw = Buf()
                    fw.dma("sp", gluwf[:], glu_w[l].rearrange("(kc p) n -> p kc n", p=128), writes=[b_gw])
                    cp(gluw[:], gluwf[:], [b_gw], [b_gw], eng="act")
                    r_sb, th = t8("r_sb"), t8("th")
                    dtv, a_, cs, sn, t1_, t2_, zre, zim = t8("dtv"), t8("a_"), t8("cs"), t8("sn"), t8("t1_"), t8("t2_"), t8("zre"), t8("zim")
                    ti = sb("ti", [128, 8], I32, st)
                    C1 = 6.28125
                    C2 = TWO_PI - C1

                    def sincos(out, ang, shape, tmpf, tmpi, bq, shift):
                        ts(tmpf, ang, 1.0 / TWO_PI, shift / TWO_PI, ALU.mult, ALU.add, [bq], [bq])
                        cp(tmpi, tmpf, [bq], [bq])
                        cp(tmpf, tmpi, [bq], [bq])
                        if shift != 0.0:
                            ts(out, ang, shift, None, ALU.add, None, [bq], [bq])
                            stt(out, tmpf, -C1, out, ALU.mult, ALU.add, [bq], [bq])
                        else:
                            stt(out, tmpf, -C1, ang, ALU.mult, ALU.add, [bq], [bq])
                        stt(out, tmpf, -C2, out, ALU.mult, ALU.add, [bq], [bq])
                        ts(out, out, 3.1415925, -3.1415925, ALU.min, ALU.max, [bq], [bq])
                        act(out, out, AF.Sin, [bq], [bq])

                    ts(lre[:], lre[:], -1e-4, None, ALU.min, None, [b_p], [b_p])
                    act(dtv[:], ldt[:], AF.Exp, [b_p], [b_p])
                    tt(a_[:], lre[:], dtv[:], ALU.mult, [b_p], [b_p])
                    act(r_sb[:], a_[:], AF.Exp, [b_p], [b_p])
                    tt(th[:], lim[:], dtv[:], ALU.mult, [b_p], [b_p])
                    sincos(sn[:], th[:], None, t1_[:], ti[:], b_p, 0.0)
                    sincos(cs[:], th[:], None, t1_[:], ti[:], b_p, 1.5707963267948966)
                    tt(cs[:], cs[:], r_sb[:], ALU.mult, [b_p], [b_p])
                    tt(sn[:], sn[:], r_sb[:], ALU.mult, [b_p], [b_p])
                    ts(cs[:], cs[:], -1.0, None, ALU.add, None, [b_p], [b_p])
                    tt(t1_[:], lre[:], lre[:], ALU.mult, [b_p], [b_p])
                    tt(t2_[:], lim[:], lim[:], ALU.mult, [b_p], [b_p])
                    tt(t1_[:], t1_[:], t2_[:], ALU.add, [b_p], [b_p])
                    OP("dve", lambda e: e.reciprocal(out=t1_[:], in_=t1_[:]), [b_p], [b_p])
                    tt(zre[:], cs[:], lre[:], ALU.mult, [b_p], [b_p])
                    tt(t2_[:], sn[:], lim[:], ALU.mult, [b_p], [b_p])
                    tt(zre[:], zre[:], t2_[:], ALU.add, [b_p], [b_p])
                    tt(zre[:], zre[:], t1_[:], ALU.mult, [b_p], [b_p])
                    tt(zim[:], sn[:], lre[:], ALU.mult, [b_p], [b_p])
                    tt(t2_[:], cs[:], lim[:], ALU.mult, [b_p], [b_p])
                    tt(zim[:], zim[:], t2_[:], ALU.subtract, [b_p], [b_p])
                    tt(zim[:], zim[:], t1_[:], ALU.mult, [b_p], [b_p])
                    bbr = sb("bbr", [128, 8, 16], F32, st); bbi = sb("bbi", [128, 8, 16], F32, st); tb = sb("tb", [128, 8, 16], F32, st)
                    zre_b = zre[:].unsqueeze(2).to_broadcast([128, 8, 16]); zim_b = zim[:].unsqueeze(2).to_broadcast([128, 8, 16])
                    tt(bbr[:], bre[:], zre_b, ALU.mult, [b_p, b_bc], [b_bc])
                    tt(tb[:], bim[:], zim_b, ALU.mult, [b_p, b_bc], [b_bc])
                    tt(bbr[:], bbr[:], tb[:], ALU.subtract, [b_bc], [b_bc])
                    tt(bbi[:], bim[:], zre_b, ALU.mult, [b_p, b_bc], [b_bc])
                    tt(tb[:], bre[:], zim_b, ALU.mult, [b_p, b_bc], [b_bc])
                    tt(bbi[:], bbi[:], tb[:], ALU.add, [b_bc], [b_bc])
                    WT = []
                    for nm, src in (("re", bbr), ("im", bbi)):
                        w1 = sb("w1" + nm, [128, 8, 2, 16], F32, st); bw1 = Buf()
                        OP("dve", lambda e, w1=w1: e.memset(w1[:], 0.0), writes=[bw1])
                        cp(w1[0:64, :, 0, :], src[0:64], [b_bc], [bw1])
                        cp(w1[64:128, :, 1, :], src[64:128], [b_bc], [bw1])
                        wt = sb("wt" + nm, [128, 2, 128], BF16, st); bwt = Buf()
                        w1v = w1[:].rearrange("p g a c -> p (g a c)")
                        for ch in range(2):
                            OP("pe", lambda e, ch=ch, w1v=w1v: e.transpose(PS[0][:, 0:128], w1v[:, ch * 128:(ch + 1) * 128], ident[:]),
                               [bw1, b_ident], [PSB[0]])
                            cp(wt[:, ch, :], PS[0][:, 0:128], [PSB[0]], [bwt])
                        WT.append((wt, bwt))
                    CT = []
                    for nm, src, sgn in (("re", cre, 1.0), ("im", cim, -1.0)):
                        ct = sb("ct" + nm, [128, 8, 2, 16], BF16, st); bct = Buf()
                        OP("dve", lambda e, ct=ct: e.memset(ct[:], 0.0), writes=[bct])
                        ts(ct[0:64, :, 0, :], src[0:64], sgn, None, ALU.mult, None, [b_bc], [bct])
                        ts(ct[64:128, :, 1, :], src[64:128], sgn, None, ALU.mult, None, [b_bc], [bct])
                        CT.append((ct, bct))
                    cosT = sb("cosT", [128, 8, TT + 1], F32, st); sinT = sb("sinT", [128, 8, TT + 1], F32, st); b_tab = Buf()
                    with ExitStack() as st2:
                        ang = sb("ang", [128, 8, TT + 1], F32, st2); tf = sb("tf", [128, 8, TT + 1], F32, st2)
                        tii = sb("tii", [128, 8, TT + 1], I32, st2); b_ang = Buf()
                        for gp in range(8):
                            ts(ang[:, gp, :], iota[:], th[:, gp:gp + 1], None, ALU.mult, None, [b_iota, b_p], [b_ang])
                        sincos(sinT[:], ang[:], None, tf[:], tii[:], b_ang, 0.0)
                        sincos(cosT[:], ang[:], None, tf[:], tii[:], b_ang, 1.5707963267948966)
                        fw.barrier()
                    uf = [sb(f"uf{i}", [128, 2, TT], F32, st) for i in range(2)]; ufb = [Buf() for _ in range(2)]
                    ub = [sb(f"ub{i}", [128, 2, TT], BF16, st) for i in range(2)]; ubb = [Buf() for _ in range(2)]
                    ta = [sb(f"ta{i}", [128, TT], F32, st) for i in range(4)]; tab_ = [Buf() for _ in range(4)]
                    wre = [sb(f"wre{i}", [128, TT], F32, st) for i in range(2)]; wim = [sb(f"wim{i}", [128, TT], F32, st) for i in range(2)]
                    wb_ = [Buf() for _ in range(2)]
                    zr = [sb(f"zr{i}", [128, TT], BF16, st) for i in range(2)]; zi = [sb(f"zi{i}", [128, TT], BF16, st) for i in range(2)]
                    zb_ = [Buf() for _ in range(2)]
                    ini = sb("ini", [128, 8, 2], F32, st); b_ini = [Buf() for _ in range(8)]
                    tiny = sb("tiny", [128, 2], F32, st)
                    yp = sb("yp", [128, 2, TT], F32, st); ypb = [Buf() for _ in range(2)]
                    yg = sb("yg", [128, 2, TT], F32, st); ygb_f = [Buf() for _ in range(2)]
                    ygb = sb("ygb", [128, 2, TT], BF16, st); ygbb = Buf()
                    g1t = sb("g1t", [128, TT], F32, st); g1b = Buf(); g2t = sb("g2t", [128, TT], F32, st); g2b = Buf()
                    yo = [sb(f"yo{i}", [128, TT], F32, st) for i in range(2)]; yob = [Buf() for _ in range(2)]
                    yob16 = [sb(f"yob16{i}", [128, TT], BF16, st) for i in range(2)]; yob16b = [Buf() for _ in range(2)]
                    OP("dve", lambda e: e.memset(ini[:], 0.0), writes=b_ini)
                    k = 0
                    def gen_B():
                        k = 0
                        pend = []

                        def run_due(force=False):
                            keep = []
                            for item in list(pend):
                                item[0] -= 1
                                if force or item[0] <= 0:
                                    nxt = item[1]()
                                    while force and nxt is not None:
                                        nxt = nxt()
                                    if nxt is not None:
                                        keep.append([1, nxt])
                                else:
                                    keep.append(item)
                            pend[:] = keep
                        for it in range(NT):
                            t0 = it * TT
                            i = it % 2
                            fw.dma("sp", uf[i][:], uT_d[:, :, t0:t0 + TT].rearrange("c p t -> p c t"), writes=[ufb[i]])
                            cp(ub[i][:], uf[i][:], [ufb[i]], [ubb[i]], eng="act")
                            for gp in range(8):
                                ch, j = gp // 4, gp % 4
                                pa, pb = 0, 1
                                for (pp, (wt, bwt)) in ((pa, WT[0]), (pb, WT[1])):
                                    OP("pe", lambda e, pp=pp, wt=wt, ch=ch, j=j, i=i: e.matmul(
                                        PS[pp][:, :], wt[32 * j:32 * j + 32, ch, :], ub[i][32 * j:32 * j + 32, ch, :],
                                        start=True, stop=True, tile_position=(32 * j, 0)), [bwt, ubb[i]], [PSB[pp]])
                                run_due()
                                cT = cosT[:, gp, 0:TT]; sT = sinT[:, gp, 0:TT]
                                kk = k % 2; k += 1
                                tt(ta[0][:], PS[pa][:, :], cT, ALU.mult, [PSB[pa], b_tab], [tab_[0]])
                                tt(ta[1][:], PS[pb][:, :], sT, ALU.mult, [PSB[pb], b_tab], [tab_[1]])
                                tt(ta[0][:], ta[0][:], ta[1][:], ALU.add, [tab_[0], tab_[1]], [tab_[0]])
                                tt(ta[2][:], PS[pb][:, :], cT, ALU.mult, [PSB[pb], b_tab], [tab_[2]])
                                tt(ta[3][:], PS[pa][:, :], sT, ALU.mult, [PSB[pa], b_tab], [tab_[3]])
                                tt(ta[2][:], ta[2][:], ta[3][:], ALU.subtract, [tab_[2], tab_[3]], [tab_[2]])
                                rb = r_sb[:, gp:gp + 1].to_broadcast([128, TT])
                                OP("dve", lambda e, kk=kk, rb=rb, gp=gp: e.tensor_tensor_scan(
                                    out=wre[kk][:], data0=rb, data1=ta[0][:], initial=ini[:, gp, 0:1], op0=ALU.mult, op1=ALU.add),
                                    [tab_[0], b_p, b_ini[gp]], [wb_[kk]])
                                OP("dve", lambda e, kk=kk, rb=rb, gp=gp: e.tensor_tensor_scan(
                                    out=wim[kk][:], data0=rb, data1=ta[2][:], initial=ini[:, gp, 1:2], op0=ALU.mult, op1=ALU.add),
                                    [tab_[2], b_p, b_ini[gp]], [wb_[kk]])
                                tt(ta[0][:], wre[kk][:], cT, ALU.mult, [wb_[kk], b_tab], [tab_[0]])
                                tt(ta[1][:], wim[kk][:], sT, ALU.mult, [wb_[kk], b_tab], [tab_[1]])
                                tt(zr[kk][:], ta[0][:], ta[1][:], ALU.subtract, [tab_[0], tab_[1]], [zb_[kk]])
                                tt(ta[2][:], wre[kk][:], sT, ALU.mult, [wb_[kk], b_tab], [tab_[2]])
                                tt(ta[3][:], wim[kk][:], cT, ALU.mult, [wb_[kk], b_tab], [tab_[3]])
                                tt(zi[kk][:], ta[2][:], ta[3][:], ALU.add, [tab_[2], tab_[3]], [zb_[kk]])
                                cL = cosT[:, gp, TT:TT + 1]; sL = sinT[:, gp, TT:TT + 1]
                                ts(tiny[:, 0:1], wim[kk][:, TT - 1:TT], sL, None, ALU.mult, None, [wb_[kk], b_tab], [b_ini[gp]])
                                ts(tiny[:, 1:2], wim[kk][:, TT - 1:TT], cL, None, ALU.mult, None, [wb_[kk], b_tab], [b_ini[gp]])
                                stt(ini[:, gp, 0:1], wre[kk][:, TT - 1:TT], cL, tiny[:, 0:1], ALU.mult, ALU.subtract, [wb_[kk], b_tab, b_ini[gp]], [b_ini[gp]])
                                stt(ini[:, gp, 1:2], wre[kk][:, TT - 1:TT], sL, tiny[:, 1:2], ALU.mult, ALU.add, [wb_[kk], b_tab, b_ini[gp]], [b_ini[gp]])
                                py = 2

                                def tail(gp=gp, j=j, kk=kk, py=py, ch=ch, i=i, t0=t0):
                                  def f(e):
                                    e.matmul(PS[py][32 * j:32 * j + 32, :], CT[0][0][:, gp, :, :].rearrange("p a c -> p (a c)"), zr[kk][:],
                                             start=True, stop=False, tile_position=(0, 32 * j))
                                    return e.matmul(PS[py][32 * j:32 * j + 32, :], CT[1][0][:, gp, :, :].rearrange("p a c -> p (a c)"), zi[kk][:],
                                                    start=False, stop=True, tile_position=(0, 32 * j))
                                  OP("pe", f, [zb_[kk], CT[0][1], CT[1][1]], [PSB[py]])
                                  if j == 3:
                                    stt(yp[:, ch, :], uf[i][:, ch, :], dsk[:, ch:ch + 1], PS[py][:, :], ALU.mult, ALU.add,
                                        [ufb[i], b_dg, PSB[py]], [ypb[ch]])
                                    if debug and l == 0:
                                        fw.dma("sp", dbg["dbg_ssmpre"][ch, :, t0:t0 + TT], yp[:, ch, :], reads=[ypb[ch]])
                                    tt(g1t[:], yp[:, ch, :], yp[:, ch, :], ALU.mult, [ypb[ch]], [g1b])
                                    ts(g1t[:], g1t[:], 0.044715, 1.0, ALU.mult, ALU.add, [g1b], [g1b])
                                    tt(g1t[:], g1t[:], yp[:, ch, :], ALU.mult, [g1b, ypb[ch]], [g1b])

                                    def tailB():
                                        act(g1t[:], g1t[:], AF.Sigmoid, [g1b], [g1b], scale=1.5957691216057308)

                                        def tailC():
                                            tt(yg[:, ch, :], yp[:, ch, :], g1t[:], ALU.mult, [g1b, ypb[ch]], [ygb_f[ch]])
                                            cp(ygb[:, ch, :], yg[:, ch, :], [ygb_f[ch]], [ygbb])
                                            return None
                                        return tailC
                                    return tailB
                                  return None
                                pend.append([1, tail])
                                yield
                            def glu_block(t0=t0):
                                for mc in range(2):
                                    def f(e, mc=mc):
                                        e.matmul(PS[7][:, :], gluw[:, 0, mc * 128:(mc + 1) * 128], ygb[:, 0, :], start=True, stop=False)
                                        return e.matmul(PS[7][:, :], gluw[:, 1, mc * 128:(mc + 1) * 128], ygb[:, 1, :], start=False, stop=True)
                                    OP("pe", f, [ygbb, b_gw], [PSB[7]])
                                    act(g2t[:], PS[7][:, :], AF.Sigmoid, [PSB[7], b_dg], [g2b], bias=glb[:, mc:mc + 1])
                                    tt(yo[mc][:], yg[:, mc, :], g2t[:], ALU.mult, [g2b, ygb_f[mc]], [yob[mc]])
                                    hnorm(yo[mc][:], yob[mc], og_fm_sb[:, mc:mc + 1], b_og, yob16[mc][:], yob16b[mc])
                                    fw.dma("sp", yh_d[mc, :, t0:t0 + TT], yob16[mc][:], reads=[yob16b[mc]])
                                return None
                            pend.append([4, glu_block])
                            yield
                        run_due(force=True)
                        yield
                    ckT = sb("ckT", [128, 32, 8], F32, st); cref = sb("cref", [128, 32, 8], F32, st); b_ck = Buf()
                    st3 = ExitStack()
                    ce = sb("ce", [8, 32], F32, st3); dq = sb("dq", [8, 8, 4], F32, st3); b_ce = Buf()
                    dqrow = sb("dqrow", [8, 32, 128], BF16, st3); onesrow = sb("onesrow", [8, S], BF16, st3); b_row = Buf()
                    cp(ce[:], cum[:].rearrange("h (s j) -> h s j", j=128)[:, :, 127], [b_cum], [b_ce])
                    cev = ce[:].rearrange("h (q s) -> h q s", s=4)
                    tt(dq[:], cev, cev[:, :, 3:4].to_broadcast([8, 8, 4]), ALU.subtract, [b_ce], [b_ce])
                    cp(dqrow[:], dq[:].rearrange("h q s -> h (q s)").unsqueeze(2).to_broadcast([8, 32, 128]), [b_ce], [b_row])
                    OP("dve", lambda e: e.memset(onesrow[:], 1.0), writes=[b_row])
                    fw.dma("sp", qT_d[:, 64, :], dqrow[:].rearrange("h s j -> h (s j)"), reads=[b_row])
                    fw.dma("sp", kT_d[:, 64, :], onesrow[:], reads=[b_row])

                    def f(e):
                        for kt in range(32):
                            ins = e.transpose(PS[0][:, kt * 8:(kt + 1) * 8], cum[0:8, kt * 128:(kt + 1) * 128], ident[0:8, 0:8])
                        return ins
                    OP("pe", f, [b_cum, b_ident], [PSB[0]])
                    cp(ckT[:].rearrange("p k h -> p (k h)"), PS[0][:, 0:256], [PSB[0]], [b_ck])
                    OP("pe", lambda e: e.matmul(PS[1][:, 0:256], e127[:], ckT[:].rearrange("p k h -> p (k h)"), start=True, stop=True),
                       [b_ck, b_e127], [PSB[1]])
                    cp(cref[:].rearrange("p k h -> p (k h)"), PS[1][:, 0:256], [PSB[1]], [b_ck])
                    fw.barrier()
                    st3.close()
                    qa = [sb("qa0", [65, S], BF16, st)]; ka = [sb("ka0", [65, S], BF16, st)]
                    qab = [Buf()]; kab = [Buf()]
                    NP = 6
                    pT = [sb(f"pT{i}", [128, TT], BF16, st) for i in range(NP)]; pTb = [Buf() for _ in range(NP)]
                    biasT = [sb(f"biasT{i}", [128, 32], F32, st) for i in range(2)]; biasb = [Buf() for _ in range(2)]
                    osb = [sb(f"osb{i}", [65, TT], F32, st) for i in range(2)]; osbb = [Buf() for _ in range(2)]
                    yat = [sb(f"yat{i}", [64, TT], F32, st) for i in range(2)]; yatb = [Buf() for _ in range(2)]
                    yab = [sb(f"yab{i}", [64, TT], BF16, st) for i in range(2)] ; yabb = [Buf() for _ in range(2)]
                    def emit_bias(u_):
                        h_, qt_ = u_ // 8, u_ % 8
                        n_ = 4 * qt_ + 4
                        ts(biasT[u_ % 2][:, 0:n_], ckT[:, 0:n_, h_], cref[:, 4 * qt_ + 3, h_:h_ + 1], -1.0, ALU.subtract, ALU.mult,
                           [b_ck], [biasb[u_ % 2]])

                    def gen_C():
                        blkctr = 0
                        pend2 = pend3 = None
                        for h in range(8):
                            hi = 0
                            fw.dma("sp", qa[hi][:], qT_d[h], writes=[qab[hi]])
                            fw.dma("sp", ka[hi][:], kT_d[h], writes=[kab[hi]])
                            for qt in range(8):
                                nkt = 4 * qt + 4
                                bi = (h * 8 + qt) % 2
                                oi = bi
                                po = 6
                                if h * 8 + qt == 0:
                                    emit_bias(0)
                                if h * 8 + qt + 1 < 64:
                                    emit_bias(h * 8 + qt + 1)

                                SL = (3, 4, 5)
                                LA = 2

                                def s_mm(kt):
                                    slot = SL[(blkctr + kt) % 3]
                                    m = kt - 4 * qt
                                    c0 = 128 * m if m > 0 else 0
                                    def f(e):
                                        ins = e.matmul(PS[slot][:, c0:TT], ka[hi][:, kt * 128:(kt + 1) * 128],
                                                       qa[hi][:, qt * TT + c0:(qt + 1) * TT], start=True, stop=(m < 0))
                                        if m >= 0:
                                            ins = e.matmul(PS[slot][:, c0:c0 + 128], ntri[:], ident_b[:], start=False, stop=True)
                                        return ins
                                    OP("pe", f, [kab[hi], qab[hi], b_ntri], [PSB[slot]])
                                for kt in range(min(LA, nkt)):
                                    s_mm(kt)
                                for kt in range(nkt):
                                    slot = SL[(blkctr + kt) % 3]
                                    if kt + LA < nkt:
                                        s_mm(kt + LA)
                                    m = kt - 4 * qt
                                    c0 = 128 * m if m > 0 else 0
                                    pi = (blkctr + kt) % NP
                                    act(pT[pi][:, c0:TT], PS[slot][:, c0:TT], AF.Exp, [PSB[slot], biasb[bi]], [pTb[pi]],
                                        bias=biasT[bi][:, kt:kt + 1])
                                    OP("pe", lambda e, kt=kt, c0=c0, pi=pi: e.matmul(
                                        PS[po][0:65, c0:TT], V_sb[:, kt, h, :], pT[pi][:, c0:TT], start=(kt == 0), stop=(kt == nkt - 1)),
                                        [pTb[pi], b_V], [PSB[po]])
                                    if kt % 8 == 7 and kt + 1 < nkt:
                                        yield 8
                                blkctr += nkt
                                cp(osb[oi][:], PS[po][0:65, :], [PSB[po]], [osbb[oi]], eng="act")
                                OP("dve", lambda e, oi=oi: e.reciprocal(out=osb[oi][64:65, :], in_=osb[oi][64:65, :]), [osbb[oi]], [osbb[oi]])

                                def phase2(oi=oi, h=h, qt=qt):
                                    OP("pe", lambda e: e.matmul(PS[7][0:64, :], ones_f[64:65, 0:64], osb[oi][64:65, :], start=True, stop=True),
                                       [osbb[oi], b_onesf], [PSB[7]])
                                    tt(yat[oi][:], osb[oi][0:64, :], PS[7][0:64, :], ALU.mult, [osbb[oi], PSB[7]], [yatb[oi]])

                                    def phase3():
                                        hnorm(yat[oi][:], yatb[oi], og_at_sb[:, h:h + 1], b_og, yab[oi][:], yabb[oi], P=64)
                                        fw.dma("sp", ya_d[h, :, qt * TT:(qt + 1) * TT], yab[oi][:], reads=[yabb[oi]])
                                    return phase3
                                if pend3 is not None:
                                    pend3()
                                pend3 = pend2() if pend2 is not None else None
                                pend2 = phase2
                                yield ((nkt - 1) % 8) + 1
                        if pend3 is not None:
                            pend3()
                        if pend2 is not None:
                            pend2()()
                    gB, gC = gen_B(), gen_C()
                    aliveB = aliveC = True
                    cdone, bdone = 0, 0
                    CTOT, BTOT = 8 * sum(4 * q_ + 4 for q_ in range(8)), NT * 9
                    while aliveB or aliveC:
                        if aliveC:
                            try:
                                cdone += next(gC)
                            except StopIteration:
                                aliveC = False
                        while aliveB and (not aliveC or bdone * CTOT <= cdone * BTOT):
                            try:
                                next(gB)
                                bdone += 1
                            except StopIteration:
                                aliveB = False
                    fw.bg_on = False
                    fw.barrier()
            if stop_after == "C":
                break
            NSLOT = 80
            RS = 128
            SUB = RS // 128
            BIG = 1.0e4
            with ExitStack() as dl:
                msk_all = sb("msk_all", [128, 32, 16], F32, dl); eq1_all = sb("eq1_all", [128, 32, 16], F32, dl)
                comb_all = sb("comb_all", [128, 32, 16], F32, dl); b_all = Buf()
                r1i = sb("r1i", [128, 32], I32, dl); r2i = sb("r2i", [128, 32], I32, dl)
                w1s = sb("w1s", [128, 32], F32, dl); w2s = sb("w2s", [128, 32], F32, dl); b_rw = Buf()
                widx = sb("widx", [128, NSLOT], I32, dl); b_slot = Buf()
                with ExitStack() as st:
                    maskT = sb("maskT", [16, S], F32, dl); b_mT = Buf()
                    woa = sb("woa", [128, 4, D], BF16, st); wob = sb("wob", [64, 8, D], BF16, st)
                    fw.dma("pool", woa[:, 0:2, :], w_out[l, 0:256, :].rearrange("(kc p) n -> p kc n", p=128), writes=[Buf()])
                    fw.dma("pool", woa[:, 2:4, :], w_out[l, 768:1024, :].rearrange("(kc p) n -> p kc n", p=128), writes=[Buf()])
                    fw.dma("pool", wob[:], w_out[l, 256:768, :].rearrange("(h p) n -> p h n", p=64), writes=[Buf()])
                    fw.barrier()
                    zrow = sb("zrow", [128, 2048], F32, st); b_z = Buf()
                    OP("dve", lambda e: e.memset(zrow[:], 0.0), writes=[b_z])
                    for c_ in range(NSLOT * RS // 256):
                        fw.dma("pool", Xs_d[c_ * 256:(c_ + 1) * 256, :].rearrange("(p two) n -> p (two n)", two=2), zrow[:], reads=[b_z])
                    xt = sb("xtD", [128, 8, TT], F32, st); xtb = Buf()
                    ys = sb("ys", [128, 4, TT], BF16, st); ysb = Buf()
                    yatt = sb("yatt", [64, 8, TT], BF16, st); yattb = Buf()
                    sqb = sb("sqbD", [128, 8, TT], BF16, st); sqbb = Buf()
                    rt = sb("rtD", [128, TT], F32, st); rtb = Buf()
                    h2f = sb("h2f", [128, 8, TT], F32, st); h2fb = Buf()
                    h2 = sb("h2", [128, 8, TT], BF16, st); h2b = Buf()
                    htok = [sb(f"htok{i}", [128, D], F32, st) for i in range(2)]; htokb = [Buf() for _ in range(2)]
                    aff = sb("aff", [128, 4, 16], F32, st); selv = sb("selv", [128, 4, 16], F32, st); rtmp = sb("rtmp", [128, 4, 16], F32, st)
                    m1 = sb("m1", [128, 16], F32, st); m2 = sb("m2", [128, 16], F32, st); gm = sb("gm", [128, 4], F32, st)
                    b_r = Buf()
                    for it in range(NT):
                        t0 = it * TT
                        msk = msk_all[:, 4 * it:4 * it + 4, :]; comb = comb_all[:, 4 * it:4 * it + 4, :]; eq1 = eq1_all[:, 4 * it:4 * it + 4, :]
                        fw.dma("sp", xt[:], xT_d[:, :, t0:t0 + TT].rearrange("c p t -> p c t"), writes=[xtb])
                        fw.dma("sp", ys[:], yh_d[:, :, t0:t0 + TT].rearrange("c p t -> p c t"), writes=[ysb])
                        fw.dma("sp", yatt[:], ya_d[:, :, t0:t0 + TT].rearrange("h p t -> p h t"), writes=[yattb])
                        for mc in range(8):
                            ps = 4 + mc % 2

                            def f(e, mc=mc, ps=ps):
                                for kc in range(4):
                                    e.matmul(PS[ps][:, :], woa[:, kc, mc * 128:(mc + 1) * 128], ys[:, kc, :], start=(kc == 0), stop=False)
                                for hh in range(8):
                                    ins = e.matmul(PS[ps][:, :], wob[:, hh, mc * 128:(mc + 1) * 128], yatt[:, hh, :], start=False, stop=(hh == 7))
                                return ins
                            OP("pe", f, [ysb, yattb], [PSB[ps]])
                            stt(xt[:, mc, :], PS[ps][:, :], G1[:, mc:mc + 1], xt[:, mc, :], ALU.mult, ALU.add, [PSB[ps], b_mod, xtb], [xtb])
                        if debug and l == 0:
                            fw.dma("sp", dbg["dbg_xmid"][:, :, t0:t0 + TT].rearrange("c p t -> p c t"), xt[:], reads=[xtb])
                        fw.dma("sp", xT_d[:, :, t0:t0 + TT].rearrange("c p t -> p c t"), xt[:], reads=[xtb])
                        norm_mod(st, xt, xtb, A2, B2, b_mod, h2, h2b, h2f, h2fb, sqb, sqbb, rt, rtb, 7)
                        for sub in range(4):
                            hi_ = sub % 2
                            for half in range(2):
                                ps = half

                                def f(e, sub=sub, half=half, ps=ps):
                                    for c4 in range(4):
                                        c = half * 4 + c4
                                        ins = e.transpose(PS[ps][:, c4 * 128:(c4 + 1) * 128], h2f[:, c, sub * 128:(sub + 1) * 128], ident[:])
                                    return ins
                                OP("pe", f, [h2fb, b_ident], [PSB[ps]])
                                cp(htok[hi_][:, half * 512:(half + 1) * 512], PS[ps][:, :], [PSB[ps]], [htokb[hi_]],
                                   eng=("act" if half == 0 else "dve"))
                            fw.dma("sp", h2_d[t0 + sub * 128:t0 + (sub + 1) * 128, :], htok[hi_][:], reads=[htokb[hi_]])
                        for sub in range(4):
                            def f(e, sub=sub):
                                for kc in range(8):
                                    ins = e.matmul(PS[6][:, sub * 16:(sub + 1) * 16], h2f[:, kc, sub * 128:(sub + 1) * 128], wr_sb[:, kc, :],
                                                   start=(kc == 0), stop=(kc == 7))
                                return ins
                            OP("pe", f, [h2fb, b_wr], [PSB[6]])
                        act(aff[:].rearrange("p s e -> p (s e)"), PS[6][:, 0:64], AF.Sigmoid, [PSB[6]], [b_r])
                        tt(selv[:], aff[:], rb_sb[:].unsqueeze(1).to_broadcast([128, 4, 16]), ALU.add, [b_r, b_rb], [b_r])
                        s44 = selv[:].rearrange("p s (g e) -> p (s g) e", e=4)
                        r44 = rtmp[:].rearrange("p s (g e) -> p (s g) e", e=4)
                        RD = lambda o, i_, op: OP("dve", lambda e: e.tensor_reduce(out=o, in_=i_, axis=mybir.AxisListType.X, op=op), [b_r, b_all], [b_r, b_all])
                        RD(m1[:], s44, ALU.max)
                        tt(r44, s44, m1[:].unsqueeze(2).to_broadcast([128, 16, 4]), ALU.is_equal, [b_r], [b_r])
                        stt(r44, r44, -BIG, s44, ALU.mult, ALU.add, [b_r], [b_r])
                        RD(m2[:], r44, ALU.max)
                        tt(m1[:], m1[:], m2[:], ALU.add, [b_r], [b_r])
                        gs = m1[:].rearrange("p (s g) -> p s g", g=4)
                        RD(gm[:], gs, ALU.max)
                        m2v = m2[:].rearrange("p (s g) -> p s g", g=4)
                        tt(m2v, gs, gm[:].unsqueeze(2).to_broadcast([128, 4, 4]), ALU.is_equal, [b_r], [b_r])
                        ts(m2[:], m2[:], BIG, -BIG, ALU.mult, ALU.add, [b_r], [b_r])
                        tt(r44, s44, m2[:].unsqueeze(2).to_broadcast([128, 16, 4]), ALU.add, [b_r], [b_r])
                        RD(gm[:], rtmp[:], ALU.max)
                        tt(eq1, rtmp[:], gm[:].unsqueeze(2).to_broadcast([128, 4, 16]), ALU.is_equal, [b_r, b_all], [b_r, b_all])
                        stt(msk, eq1, -BIG, rtmp[:], ALU.mult, ALU.add, [b_r, b_all], [b_r, b_all])
                        RD(gm[:], msk, ALU.max)
                        tt(msk, rtmp[:], gm[:].unsqueeze(2).to_broadcast([128, 4, 16]), ALU.is_ge, [b_r, b_all], [b_r, b_all])
                        tt(comb, aff[:], msk, ALU.mult, [b_r, b_all], [b_r, b_all])
                        RD(gm[:], comb, ALU.add)
                        OP("dve", lambda e: e.reciprocal(out=gm[:], in_=gm[:]), [b_r], [b_r])
                        tt(comb, comb, gm[:].unsqueeze(2).to_broadcast([128, 4, 16]), ALU.mult, [b_r, b_all], [b_r, b_all])
                        if debug and l == 0:
                            fw.dma("sp", dbg["dbg_comb"][t0:t0 + TT, :].rearrange("(s p) e -> p s e", p=128), comb, reads=[b_all])

                        def f(e, it=it):
                            for sub in range(4):
                                ins = e.transpose(PS[6][0:16, sub * 128:(sub + 1) * 128], msk_all[:, 4 * it + sub, :], ident[:])
                            return ins
                        OP("pe", f, [b_all, b_ident], [PSB[6]])
                        cp(maskT[:, t0:t0 + TT], PS[6][0:16, :], [PSB[6]], [b_mT])
                    fw.barrier()
                with ExitStack() as st:
                    inc = sb("inc", [16, S], F32, st); b_s = Buf()
                    cntf = sb("cntf", [16, 2], F32, st); slf = sb("slf", [16, 2], F32, st); offf = sb("offf", [16, 1], F32, st)
                    endf = sb("endf", [16, 1], F32, st); cnti = sb("cnti", [16, 2], I32, st)
                    cmpt = sb("cmpt", [16, NSLOT], F32, st); sef = sb("sef", [128, NSLOT], F32, st); pidx = sb("pidx", [128, 1], F32, st); pit = sb("pit", [128, 128], F32, st)
                    pos_all = sb("pos_all", [128, 32, 16], F32, st); tmp3 = sb("tmp3", [128, 32, 16], F32, st)
                    rf = sb("rf", [128, 32], F32, st)
                    OP("dve", lambda e: e.tensor_tensor_scan(out=inc[:], data0=ones_f[0:16, 0:1].to_broadcast([16, S]), data1=maskT[:],
                                                             initial=0.0, op0=ALU.mult, op1=ALU.add), [b_mT, b_onesf], [b_s])
                    ts(cntf[:], inc[:, S - 1:S].to_broadcast([16, 2]), 1.0 / RS, (RS - 1.0) / RS - (RS - 1.0) / (2 * RS), ALU.mult, ALU.add, [b_s], [b_s])
                    cp(cnti[:], cntf[:], [b_s], [b_s])
                    cp(slf[:], cnti[:], [b_s], [b_s])
                    OP("pe", lambda e: e.matmul(PS[0][0:16, 0:2], tri_f[0:16, 0:16], slf[:], start=True, stop=True), [b_s, b_tri], [PSB[0]])
                    cp(offf[:], PS[0][0:16, 0:1], [PSB[0]], [b_s])
                    tt(endf[:], offf[:], slf[:, 0:1], ALU.add, [b_s], [b_s])
                    ts(offf[:], offf[:], float(RS), None, ALU.mult, None, [b_s], [b_s])
                    tt(inc[:], inc[:], maskT[:], ALU.subtract, [b_s, b_mT], [b_s])
                    ts(inc[:], inc[:], offf[:, 0:1], None, ALU.add, None, [b_s], [b_s])

                    def f(e):
                        for tk in range(32):
                            ins = e.transpose(PS[1][:, tk * 16:(tk + 1) * 16], inc[:, tk * 128:(tk + 1) * 128], ident[0:16, 0:16])
                        return ins
                    OP("pe", f, [b_s, b_ident], [PSB[1]])
                    cp(pos_all[:].rearrange("p k e -> p (k e)"), PS[1][:, :], [PSB[1]], [b_s])
                    RD2 = lambda o, i_: OP("dve", lambda e: e.tensor_reduce(out=o, in_=i_, axis=mybir.AxisListType.X, op=ALU.add), [b_s, b_all], [b_s, b_rw])
                    tt(tmp3[:], eq1_all[:], pos_all[:], ALU.mult, [b_s, b_all], [b_s])
                    RD2(rf[:], tmp3[:])
                    cp(r1i[:], rf[:], [b_s], [b_rw])
                    tt(tmp3[:], eq1_all[:], comb_all[:], ALU.mult, [b_s, b_all], [b_s])
                    RD2(w1s[:], tmp3[:])
                    tt(eq1_all[:], msk_all[:], eq1_all[:], ALU.subtract, [b_all], [b_all])
                    tt(tmp3[:], eq1_all[:], pos_all[:], ALU.mult, [b_s, b_all], [b_s])
                    RD2(rf[:], tmp3[:])
                    cp(r2i[:], rf[:], [b_s], [b_rw])
                    tt(tmp3[:], eq1_all[:], comb_all[:], ALU.mult, [b_s, b_all], [b_s])
                    RD2(w2s[:], tmp3[:])
                    ts(cmpt[:], iota[0:16, 0:NSLOT], endf[:, 0:1], None, ALU.is_ge, None, [b_iota, b_s], [b_s])
                    OP("pe", lambda e: e.matmul(PS[2][:, 0:NSLOT], ones_f[0:16, :], cmpt[:], start=True, stop=True), [b_s, b_onesf], [PSB[2]])
                    ts(sef[:], PS[2][:, 0:NSLOT], 15.0, float(l * NE), ALU.min, ALU.add, [PSB[2]], [b_s])
                    tt(pit[:], ident[:], iota[:, 0:128], ALU.mult, [b_ident, b_iota], [b_s])
                    OP("dve", lambda e: e.tensor_reduce(out=pidx[:], in_=pit[:], axis=mybir.AxisListType.X, op=ALU.add), [b_s], [b_s])
                    stt(sef[:], sef[:], 128.0, pidx[:, 0:1].to_broadcast([128, NSLOT]), ALU.mult, ALU.add, [b_s], [b_s])
                    cp(widx[:], sef[:], [b_s], [b_slot])
                    fw.barrier()
                with ExitStack() as st:
                    hrow = [sb(f"hrow{i}", [128, D], F32, st) for i in range(3)]; hrowb = [Buf() for _ in range(3)]
                    for tk in range(32):
                        i = tk % 3
                        fw.dma("sp", hrow[i][:], h2_d[tk * 128:(tk + 1) * 128, :], writes=[hrowb[i]])
                        for ri in (r1i, r2i):
                            fw.dma_ind(Xs_d[:, :], bass.IndirectOffsetOnAxis(ap=ri[:, tk:tk + 1], axis=0), hrow[i][:], None,
                                       reads=[hrowb[i], b_rw])
                    fw.barrier()
                with ExitStack() as st:
                    wg = [sb(f"wg{i}", [128, 8, DE], BF16, st) for i in range(2)]; wgb = [Buf() for _ in range(2)]
                    wu = [sb(f"wu{i}", [128, 8, DE], BF16, st) for i in range(2)]; wub = [Buf() for _ in range(2)]
                    wd = [sb(f"wd{i}", [128, 4, D], BF16, st) for i in range(2)]; wdb = [Buf() for _ in range(2)]
                    xs = [sb(f"xs{i}", [128, D], F32, st) for i in range(3)]; xsb = [Buf() for _ in range(3)]
                    xsT = [sb(f"xsT{i}", [128, 8, 128], BF16, st) for i in range(2)]; xsTb = [Buf() for _ in range(2)]
                    sg = [sb(f"sg{i}", [128, DE], F32, st) for i in range(2)]; sgb = [Buf() for _ in range(2)]
                    hd = [sb(f"hd{i}", [128, DE], F32, st) for i in range(2)]; hdb = [Buf() for _ in range(2)]
                    hdT = [sb(f"hdT{i}", [128, 4, 128], BF16, st) for i in range(2)]; hdTb = [Buf() for _ in range(2)]
                    yt = [sb(f"yt{i}", [128, D], F32, st) for i in range(2)]; ytb = [Buf() for _ in range(2)]

                    wg_rows = wg_d.rearrange("e p n -> (e p) n"); wu_rows = wu_d.rearrange("e p n -> (e p) n"); wd_rows = wd_d.rearrange("e p n -> (e p) n")

                    def load_gu(s_):
                        i = s_ % 2
                        off = bass.IndirectOffsetOnAxis(ap=widx[:, s_:s_ + 1], axis=0)
                        fw.dma_ind(wg[i][:].rearrange("p k n -> p (k n)"), None, wg_rows, off, reads=[b_slot], writes=[wgb[i]])
                        fw.dma_ind(wu[i][:].rearrange("p k n -> p (k n)"), None, wu_rows, off, reads=[b_slot], writes=[wub[i]])

                    def load_d(s_):
                        i = s_ % 2
                        off = bass.IndirectOffsetOnAxis(ap=widx[:, s_:s_ + 1], axis=0)
                        fw.dma_ind(wd[i][:].rearrange("p k n -> p (k n)"), None, wd_rows, off, reads=[b_slot], writes=[wdb[i]])

                    def load_x(u_):
                        fw.dma("sp", xs[u_ % 3][:], Xs_d[u_ * 128:(u_ + 1) * 128, :], writes=[xsb[u_ % 3]])

                    def st_T(u_):
                        i = u_ % 2
                        x3 = u_ % 3
                        for half in range(2):
                            def f(e, half=half):
                                for c4 in range(4):
                                    c = half * 4 + c4
                                    ins = e.transpose(PS[half][:, c4 * 128:(c4 + 1) * 128], xs[x3][:, c * 128:(c + 1) * 128], ident[:])
                                return ins
                            OP("pe", f, [xsb[x3], b_ident], [PSB[half]])
                            cp(xsT[i][:, half * 4:(half + 1) * 4, :], PS[half][:, :].rearrange("p (c t) -> p c t", c=4), [PSB[half]], [xsTb[i]],
                               eng=("act" if half == 0 else "dve"))

                    def st_GU(u_):
                        i = u_ % 2
                        wi = (u_ // SUB) % 2
                        for (pp, w_, wb__) in ((2, wg[wi], wgb[wi]), (3, wu[wi], wub[wi])):
                            def f(e, pp=pp, w_=w_):
                                for kc in range(8):
                                    ins = e.matmul(PS[pp][:, :], xsT[i][:, kc, :], w_[:, kc, :], start=(kc == 0), stop=(kc == 7))
                                return ins
                            OP("pe", f, [xsTb[i], wb__], [PSB[pp]])
                        act(sg[i][:], PS[2][:, :], AF.Silu, [PSB[2]], [sgb[i]])
                        tt(hd[i][:], PS[3][:, :], sg[i][:], ALU.mult, [PSB[3], sgb[i]], [hdb[i]])

                    def st_HT(u_):
                        i = u_ % 2

                        def f(e):
                            for c4 in range(4):
                                ins = e.transpose(PS[4][:, c4 * 128:(c4 + 1) * 128], hd[i][:, c4 * 128:(c4 + 1) * 128], ident[:])
                            return ins
                        OP("pe", f, [hdb[i], b_ident], [PSB[4]])
                        cp(hdT[i][:], PS[4][:, :].rearrange("p (c t) -> p c t", c=4), [PSB[4]], [hdTb[i]], eng="act")

                    def st_D(u_):
                        i = u_ % 2
                        wi = (u_ // SUB) % 2
                        for half in range(2):
                            ps = 5 + half

                            def f(e, half=half, ps=ps):
                                for kc in range(4):
                                    ins = e.matmul(PS[ps][:, :], hdT[i][:, kc, :], wd[wi][:, kc, half * 512:(half + 1) * 512], start=(kc == 0), stop=(kc == 3))
                                return ins
                            OP("pe", f, [hdTb[i], wdb[wi]], [PSB[ps]])
                            cp(yt[i][:, half * 512:(half + 1) * 512], PS[ps][:, :], [PSB[ps]], [ytb[i]], eng=("dve" if half == 0 else "act"))
                        fw.dma("sp", Ys_d[u_ * 128:(u_ + 1) * 128, :], yt[i][:], reads=[ytb[i]])

                    NU = SUB * NSLOT
                    for s_ in range(2):
                        load_gu(s_)
                        load_d(s_)
                    for u_ in range(3):
                        load_x(u_)
                    for step in range(NU + 3):
                        if step < NU:
                            st_T(step)
                            if step + 3 < NU:
                                load_x(step + 3)
                        if 0 <= step - 1 < NU:
                            u_ = step - 1
                            st_GU(u_)
                            if u_ % SUB == SUB - 1 and u_ // SUB + 2 < NSLOT:
                                load_gu(u_ // SUB + 2)
                        if 0 <= step - 2 < NU:
                            st_HT(step - 2)
                        if 0 <= step - 3 < NU:
                            u_ = step - 3
                            st_D(u_)
                            if u_ % SUB == SUB - 1 and u_ // SUB + 2 < NSLOT:
                                load_d(u_ // SUB + 2)
                    fw.barrier()
                with ExitStack() as st:
                    y1 = [sb(f"y1_{i}", [128, D], F32, st) for i in range(2)]; y2 = [sb(f"y2_{i}", [128, D], F32, st) for i in range(2)]
                    y1b = [Buf() for _ in range(2)]; y2b = [Buf() for _ in range(2)]
                    ac = [sb(f"ac{i}", [128, D], F32, st) for i in range(2)]; acb = [Buf() for _ in range(2)]
                    xm = [sb(f"xm{i}", [128, 8, 128], F32, st) for i in range(2)]; xmb = [Buf() for _ in range(2)]
                    otile = [sb(f"otile{i}", [128, D], F32, st) for i in range(2)]; otb = [Buf() for _ in range(2)]
                    def issue5(tk):
                        i = tk % 2
                        fw.dma_ind(y1[i][:], None, Ys_d[:, :], bass.IndirectOffsetOnAxis(ap=r1i[:, tk:tk + 1], axis=0), reads=[b_rw], writes=[y1b[i]])
                        fw.dma_ind(y2[i][:], None, Ys_d[:, :], bass.IndirectOffsetOnAxis(ap=r2i[:, tk:tk + 1], axis=0), reads=[b_rw], writes=[y2b[i]])
                        fw.dma("sp", xm[i][:], xT_d[:, :, tk * 128:(tk + 1) * 128].rearrange("c p t -> p c t"), writes=[xmb[i]])
                    issue5(0)
                    for tk in range(32):
                        i = tk % 2
                        if tk + 1 < 32:
                            issue5(tk + 1)
                        ts(ac[i][:], y1[i][:], w1s[:, tk:tk + 1], None, ALU.mult, None, [y1b[i], b_rw], [acb[i]])
                        stt(ac[i][:], y2[i][:], w2s[:, tk:tk + 1], ac[i][:], ALU.mult, ALU.add, [y2b[i], b_rw, acb[i]], [acb[i]])
                        for half in range(2):
                            ps = 2 * (tk % 2) + half

                            def f(e, half=half, ps=ps):
                                for c4 in range(4):
                                    c = half * 4 + c4
                                    ins = e.transpose(PS[ps][:, c4 * 128:(c4 + 1) * 128], ac[i][:, c * 128:(c + 1) * 128], ident[:])
                                return ins
                            OP("pe", f, [acb[i], b_ident], [PSB[ps]])
                            for c4 in range(4):
                                c = half * 4 + c4
                                stt(xm[i][:, c, :], PS[ps][:, c4 * 128:(c4 + 1) * 128], G2[:, c:c + 1], xm[i][:, c, :], ALU.mult, ALU.add,
                                    [PSB[ps], b_mod, xmb[i]], [xmb[i]])
                        if not last:
                            fw.dma("sp", xT_d[:, :, tk * 128:(tk + 1) * 128].rearrange("c p t -> p c t"), xm[i][:], reads=[xmb[i]])
                        else:
                            for half in range(2):
                                ps = 4 + 2 * (tk % 2) + half

                                def f(e, half=half, ps=ps):
                                    for c4 in range(4):
                                        c = half * 4 + c4
                                        ins = e.transpose(PS[ps][:, c4 * 128:(c4 + 1) * 128], xm[i][:, c, :], ident[:])
                                    return ins
                                OP("pe", f, [xmb[i], b_ident], [PSB[ps]])
                                cp(otile[i][:, half * 512:(half + 1) * 512], PS[ps][:, :], [PSB[ps]], [otb[i]], eng=("act" if half == 0 else "dve"))
                            fw.dma("sp", out_d[tk * 128:(tk + 1) * 128, :], otile[i][:], reads=[otb[i]])
                    fw.barrier()
    fw.barrier()
    return nc, fw, dbg


def host_inputs(inp, b):
    f = np.float32
    A = np.ascontiguousarray
    m = {}
    m["x"] = A(inp["x"][b])
    m["c_fm"] = A(inp["c"][b].reshape(8, 128).T)
    m["ada_w"] = inp["ada_w"]
    m["ada_b_fm"] = A(inp["ada_b"].reshape(2, 48, 128).transpose(0, 2, 1))
    m["n1g"] = A(inp["norm1_g"].reshape(2, 8, 128).transpose(0, 2, 1))
    m["n2g"] = A(inp["norm2_g"].reshape(2, 8, 128).transpose(0, 2, 1))
    m["w_in"] = inp["w_in"]
    m["fb"] = A(inp["forget_b"].reshape(2, 8, 1))
    def gp_lay(a):
        return A(a.reshape(2, 8, 2, 64).transpose(0, 2, 3, 1).reshape(2, 128, 8))
    m["lam_re"] = gp_lay(inp["lam_re"])
    m["lam_im"] = gp_lay(inp["lam_im"])
    m["log_dt"] = gp_lay(np.broadcast_to(inp["log_dt"][:, :, None], (2, 16, 64)))
    m["sb_re"] = A(inp["ssm_b_re"].reshape(2, 8, 2, 64, 16).transpose(0, 2, 3, 1, 4).reshape(2, 128, 8, 16))
    m["sb_im"] = A(inp["ssm_b_im"].reshape(2, 8, 2, 64, 16).transpose(0, 2, 3, 1, 4).reshape(2, 128, 8, 16))
    m["sc_re"] = A(inp["ssm_c_re"].reshape(2, 8, 2, 16, 64).transpose(0, 2, 4, 1, 3).reshape(2, 128, 8, 16))
    m["sc_im"] = A(inp["ssm_c_im"].reshape(2, 8, 2, 16, 64).transpose(0, 2, 4, 1, 3).reshape(2, 128, 8, 16))
    m["ssm_d"] = A(inp["ssm_d"].reshape(2, 2, 128).transpose(0, 2, 1))
    m["glu_w"] = inp["glu_w"]
    m["glu_b"] = A(inp["glu_b"].reshape(2, 2, 128).transpose(0, 2, 1))
    m["qg"] = A(np.tile(inp["q_norm_g"], (1, 2)).reshape(2, 128, 1))
    m["kg"] = A(np.tile(inp["k_norm_g"], (1, 2)).reshape(2, 128, 1))
    m["conv_w"] = A(inp["conv_w"].reshape(2, 3, 2, 128).transpose(0, 3, 2, 1))
    m["og_fm"] = A(inp["out_norm_g"].reshape(2, 8, 128).transpose(0, 2, 1))
    m["og_at"] = A(inp["out_norm_g"][:, 256:768].reshape(2, 8, 64).transpose(0, 2, 1))
    m["w_out"] = inp["w_out"]
    m["w_router"] = inp["w_router"]
    m["rbias"] = A(np.broadcast_to(inp["router_bias"][None, :], (128, 16)))
    m["w_gate"] = inp["w_gate"]
    m["w_up"] = inp["w_up"]
    m["w_down"] = inp["w_down"]
    m["ident"] = np.eye(128, dtype=f)
    e127 = np.zeros((128, 128), f); e127[127, :] = 1
    m["e127"] = e127
    blk = np.zeros((128, 128), f); blk[:64, :64] = 1; blk[64:, 64:] = 1
    m["blk64"] = blk
    m["tri"] = np.triu(np.ones((128, 128), f))
    m["iota"] = A(np.broadcast_to(np.arange(TT + 1, dtype=f)[None, :], (128, TT + 1)))
    sel = np.zeros((16, 16, 128), f)
    for e in range(16):
        sel[e, e, :] = 1
    m["sel16"] = sel
    return {k: np.asarray(v, dtype=f) for k, v in m.items()}


_CACHE = {}


def kernel(**inputs):
    inp = {k: np.asarray(v) for k, v in inputs.items()}
    if "nc" not in _CACHE:
        _CACHE["nc"] = build_program()[0]
    nc = _CACHE["nc"]
    in_maps = [host_inputs(inp, b) for b in range(8)]
    res = run_bass_kernel_spmd(nc, in_maps, core_ids=list(range(8)))
    out = np.stack([np.asarray(r["out"]) for r in res.results], axis=0)
    return out.astype(np.float32)
```

```python
import numpy as np
from contextlib import ExitStack
import concourse.bass as bass
import concourse.mybir as mybir
from concourse.bass_utils import run_bass_kernel_spmd

F32 = mybir.dt.float32
BF16 = mybir.dt.bfloat16
I32 = mybir.dt.int32
AF = mybir.ActivationFunctionType
ALU = mybir.AluOpType

S = 4096
D = 1024
TT = 512
NT = S // TT
DIN = 2568
NE = 16
DE = 512
EPS = 1e-6
TWO_PI = 6.283185307179586
import os as _os
NOCONV = bool(_os.environ.get('NOCONV'))
POOLENG = _os.environ.get('POOLENG', 'pool')
OFF_U, OFF_Q, OFF_K, OFF_V, OFF_F, OFF_HC, OFF_BG, OFF_CG = 0, 256, 768, 1280, 1792, 1800, 2056, 2312


class Buf:
    __slots__ = ("name", "w", "r")

    def __init__(self, name=""):
        self.name = name
        self.w = {}
        self.r = {}


class Fw:
    ENG = ("pe", "act", "dve", "pool", "sp")

    def __init__(self, nc, ndma=20):
        self.nc = nc
        self.eng = dict(pe=nc.tensor, act=nc.scalar, dve=nc.vector, pool=nc.gpsimd, sp=nc.sync)
        self.sem = {e: nc.alloc_semaphore("sem_" + e) for e in self.ENG}
        self.cnt = {e: 0 for e in self.ENG}
        self.known = {e: {} for e in self.ENG}
        self.dq = {}
        for q in ("sp", "pool"):
            self.dq[q] = dict(sems=[nc.alloc_semaphore(f"dq_{q}_{i}") for i in range(ndma)],
                              uses=[0] * ndma, nxt=0)
        self.allsems = {}
        self.nwaits = 0
        self.bg_on = False
        self.bg_sems = {s_.num for s_ in self.dq["pool"]["sems"]}

    def _wait(self, e, sem, val):
        k = self.known[e]
        if k.get(sem.num, 0) >= val:
            return
        self.eng[e].wait_ge(sem, val)
        self.nwaits += 1
        k[sem.num] = val

    def _deps(self, e, reads, writes):
        need = {}
        mysem = self.sem[e].num

        def add(tok, same_ok):
            sem, val = tok
            if same_ok and sem.num == mysem and e == "pe":
                return
            if need.get(sem.num, (None, 0))[1] < val:
                need[sem.num] = (sem, val)

        for b in reads:
            for tok in b.w.values():
                add(tok, False)
        for b in writes:
            for tok in b.w.values():
                add(tok, True)
            for tok in b.r.values():
                add(tok, True)
        for sem, val in need.values():
            self._wait(e, sem, val)

    def op(self, e, fn, reads=(), writes=()):
        self._deps(e, reads, writes)
        ins = fn(self.eng[e])
        self.cnt[e] += 1
        sem = self.sem[e]
        ins.then_inc(sem, 1)
        tok = (sem, self.cnt[e])
        for b in reads:
            b.r[sem.num] = tok
        for b in writes:
            b.w = {sem.num: tok}
            b.r = {}
        self.allsems[sem.num] = tok
        return tok

    def dma(self, q, out, in_, reads=(), writes=()):
        d = self.dq[q]
        i = d["nxt"]
        d["nxt"] = (i + 1) % len(d["sems"])
        sem = d["sems"][i]
        if d["uses"][i] > 0:
            self._wait(q, sem, 16 * d["uses"][i])
        self._deps(q, reads, writes)
        ins = self.eng[q].dma_start(out=out, in_=in_)
        d["uses"][i] += 1
        tok = (sem, 16 * d["uses"][i])
        ins.then_inc(sem, 16)
        for b in reads:
            b.r[sem.num] = tok
        for b in writes:
            b.w = {sem.num: tok}
            b.r = {}
        self.allsems[sem.num] = tok
        return tok

    def dma_ind(self, out, out_off, in_, in_off, reads=(), writes=()):
        q = "pool"
        d = self.dq[q]
        i = d["nxt"]
        d["nxt"] = (i + 1) % len(d["sems"])
        sem = d["sems"][i]
        if d["uses"][i] > 0:
            self._wait(q, sem, 16 * d["uses"][i])
        self._deps(q, reads, writes)
        ins = self.eng[q].indirect_dma_start(out=out, out_offset=out_off, in_=in_, in_offset=in_off)
        d["uses"][i] += 1
        tok = (sem, 16 * d["uses"][i])
        ins.then_inc(sem, 16)
        for b in reads:
            b.r[sem.num] = tok
        for b in writes:
            b.w = {sem.num: tok}
            b.r = {}
        self.allsems[sem.num] = tok
        return tok

    def barrier(self):
        for e in self.ENG:
            for sem, val in list(self.allsems.values()):
                if sem.num == self.sem[e].num:
                    continue
                if self.bg_on and sem.num in self.bg_sems:
                    continue
                self._wait(e, sem, val)


def build_program(nlayers=2, debug=False, stop_after=None):
    nc = bass.Bass("TRN2", target_bir_lowering=False)
    fw = Fw(nc)
    dbg = {}

    def din(name, shape, dt=F32):
        return nc.dram_tensor(name, list(shape), dt, kind="ExternalInput").ap()

    def dscr(name, shape, dt=F32):
        if debug:
            return nc.dram_tensor(name, list(shape), dt, kind="ExternalOutput").ap()
        return nc.dram_tensor(name, list(shape), dt).ap()

    x_in = din("x", [S, D])
    c_fm = din("c_fm", [128, 8])
    ada_w = din("ada_w", [2, D, 6 * D])
    ada_b_fm = din("ada_b_fm", [2, 128, 48])
    n1g = din("n1g", [2, 128, 8])
    n2g = din("n2g", [2, 128, 8])
    w_in = din("w_in", [2, D, DIN])
    fb = din("fb", [2, 8, 1])
    lam_re = din("lam_re", [2, 128, 8])
    lam_im = din("lam_im", [2, 128, 8])
    log_dt = din("log_dt", [2, 128, 8])
    sb_re = din("sb_re", [2, 128, 8, 16])
    sb_im = din("sb_im", [2, 128, 8, 16])
    sc_re = din("sc_re", [2, 128, 8, 16])
    sc_im = din("sc_im", [2, 128, 8, 16])
    ssm_d = din("ssm_d", [2, 128, 2])
    glu_w = din("glu_w", [2, 256, 256])
    glu_b = din("glu_b", [2, 128, 2])
    qg = din("qg", [2, 128, 1])
    kg = din("kg", [2, 128, 1])
    conv_w = din("conv_w", [2, 128, 2, 3])
    og_fm = din("og_fm", [2, 128, 8])
    og_at = din("og_at", [2, 64, 8])
    w_out = din("w_out", [2, D, D])
    w_router = din("w_router", [D, NE])
    rbias = din("rbias", [128, NE])
    w_gate = din("w_gate", [2, NE, D, DE])
    w_up = din("w_up", [2, NE, D, DE])
    w_down = din("w_down", [2, NE, DE, D])
    ident_in = din("ident", [128, 128])
    e127_in = din("e127", [128, 128])
    blk64_in = din("blk64", [128, 128])
    tri_in = din("tri", [128, 128])
    iota_in = din("iota", [128, TT + 1])
    sel_in = din("sel16", [16, NE, 128])
    out_d = nc.dram_tensor("out", [S, D], F32, kind="ExternalOutput").ap()

    xT_d = dscr("xT_d", [8, 128, S])
    wg_d = dscr("wg_d", [2 * NE, 128, 8 * DE], BF16)
    wu_d = dscr("wu_d", [2 * NE, 128, 8 * DE], BF16)
    wd_d = dscr("wd_d", [2 * NE, 128, 4 * D], BF16)
    uT_d = dscr("uT_d", [2, 128, S])
    qT_d = dscr("qT_d", [8, 65, S], BF16)
    kT_d = dscr("kT_d", [8, 65, S], BF16)
    yh_d = dscr("yh_d", [4, 128, S], BF16)
    ya_d = dscr("ya_d", [8, 64, S], BF16)
    h2_d = dscr("h2_d", [S, D])
    Xs_d = dscr("Xs_d", [80 * 128, D])
    Ys_d = dscr("Ys_d", [80 * 128, D])
    if debug:
        for nm, shp, dt in (("dbg_h", [8, 128, S], BF16), ("dbg_cum", [8, S], F32), ("dbg_mod", [128, 48], F32),
                            ("dbg_v", [128, 32, 8, 65], BF16), ("dbg_ssmpre", [2, 128, S], F32),
                            ("dbg_comb", [S, NE], F32), ("dbg_xmid", [8, 128, S], F32)):
            dbg[nm] = nc.dram_tensor(nm, shp, dt, kind="ExternalOutput").ap()

    es = ExitStack()

    uid = [0]

    def sb(name, shape, dt=F32, stack=None):
        uid[0] += 1
        return (stack or es).enter_context(nc.sbuf_tensor(f"s{uid[0]}_{name}", list(shape), dt))

    PS = [es.enter_context(nc.psum_tensor(f"ps{i}", [128, 512], F32)) for i in range(8)]
    PSB = [Buf(f"ps{i}") for i in range(8)]

    ident = sb("ident", [128, 128]); b_ident = Buf()
    e127 = sb("e127", [128, 128]); b_e127 = Buf()
    tri_f = sb("tri_f", [128, 128]); b_tri = Buf()
    blk_f = sb("blk_f", [128, 128]); blk = sb("blk", [128, 128], BF16); b_blk = Buf()
    ones_b = sb("ones_b", [128, 128], BF16); b_ones = Buf()
    ones_f = sb("ones_f", [128, 128]); b_onesf = Buf()
    iota = sb("iota", [128, TT + 1]); b_iota = Buf()
    sel16_f = sb("sel16_f", [16, NE, 128]); b_sel = Buf()
    cfm = sb("cfm", [128, 8]); b_cfm = Buf()
    cact = sb("cact", [128, 8]); b_cact = Buf()
    rb_sb = sb("rb_sb", [128, NE]); b_rb = Buf()
    wr_sb = sb("wr_sb", [128, 8, NE]); b_wr = Buf()

    fw.dma("sp", ident[:], ident_in, writes=[b_ident])
    fw.dma("sp", e127[:], e127_in, writes=[b_e127])
    fw.dma("sp", tri_f[:], tri_in, writes=[b_tri])
    fw.dma("sp", blk_f[:], blk64_in, writes=[b_blk])
    fw.dma("sp", iota[:], iota_in, writes=[b_iota])
    fw.dma("sp", sel16_f[:], sel_in, writes=[b_sel])
    fw.dma("sp", cfm[:], c_fm, writes=[b_cfm])
    fw.dma("sp", rb_sb[:], rbias, writes=[b_rb])
    fw.dma("sp", wr_sb[:], w_router.rearrange("(kc p) n -> p kc n", p=128), writes=[b_wr])
    fw.op("dve", lambda e: e.tensor_copy(out=blk[:], in_=blk_f[:]), reads=[b_blk], writes=[b_blk])
    fw.op("dve", lambda e: e.memset(ones_b[:], 1.0), writes=[b_ones])
    fw.op("dve", lambda e: e.memset(ones_f[:], 1.0), writes=[b_onesf])
    fw.op("act", lambda e: e.activation(out=cact[:], in_=cfm[:], func=AF.Silu), reads=[b_cfm], writes=[b_cact])

    def OP(e, fn, reads=(), writes=()):
        return fw.op(e, fn, reads, writes)

    def act(out, in_, func, reads, writes, scale=1.0, bias=0.0):
        return fw.op("act", lambda e: e.activation(out=out, in_=in_, func=func, bias=bias, scale=scale), reads, writes)

    def tt(out, in0, in1, op, reads, writes, eng="dve"):
        return fw.op(eng, lambda e: e.tensor_tensor(out=out, in0=in0, in1=in1, op=op), reads, writes)

    def ts(out, in0, s1, s2, op0, op1, reads, writes, eng="dve"):
        if op1 is None:
            return fw.op(eng, lambda e: e.tensor_scalar(out=out, in0=in0, scalar1=s1, scalar2=None, op0=op0), reads, writes)
        return fw.op(eng, lambda e: e.tensor_scalar(out=out, in0=in0, scalar1=s1, scalar2=s2, op0=op0, op1=op1), reads, writes)

    def stt(out, in0, scalar, in1, op0, op1, reads, writes):
        return fw.op("dve", lambda e: e.scalar_tensor_tensor(out=out, in0=in0, scalar=scalar, in1=in1, op0=op0, op1=op1),
                     reads, writes)

    def cp(out, in_, reads, writes, eng="dve"):
        if eng == "act":
            return fw.op("act", lambda e: e.copy(out=out, in_=in_), reads, writes)
        return fw.op(eng, lambda e: e.tensor_copy(out=out, in_=in_), reads, writes)

    ntri = sb("ntri", [128, 128], BF16); ident_b = sb("ident_b", [128, 128], BF16); b_ntri = Buf()
    tt(tri_f[:], tri_f[:], ident[:], ALU.subtract, [b_tri, b_ident], [b_tri])
    ts(ntri[:], tri_f[:], -30000.0, None, ALU.mult, None, [b_tri], [b_ntri])
    cp(ident_b[:], ident[:], [b_ident], [b_ntri])
    eps_c = sb("eps_c", [128, 1]); b_eps = Buf()
    OP("dve", lambda e: e.memset(eps_c[:], EPS), writes=[b_eps])

    hn_sq = [sb(f"hn_sq{i}", [128, TT], BF16) for i in range(2)]; hn_sqb = [Buf() for _ in range(2)]
    hn_rt = [sb(f"hn_rt{i}", [128, TT]) for i in range(2)]; hn_rtb = [Buf() for _ in range(2)]
    hn_ctr = [0]
    HN_PS = 7

    def hnorm(src, srcb, g_ap, gb, out, outb, P=128, n=TT):
        i = hn_ctr[0] % 2
        hn_ctr[0] += 1
        act(hn_sq[i][0:P, 0:n], src, AF.Square, [srcb], [hn_sqb[i]])
        OP("pe", lambda e: e.matmul(PS[HN_PS][0:P, 0:n], blk[0:P, 0:P], hn_sq[i][0:P, 0:n], start=True, stop=True),
           [hn_sqb[i], b_blk], [PSB[HN_PS]])
        act(hn_rt[i][0:P, 0:n], PS[HN_PS][0:P, 0:n], AF.Ln, [PSB[HN_PS], b_eps], [hn_rtb[i]], scale=1.0 / 64, bias=eps_c[0:P, :])
        act(hn_rt[i][0:P, 0:n], hn_rt[i][0:P, 0:n], AF.Exp, [hn_rtb[i]], [hn_rtb[i]], scale=-0.5)
        stt(out, src, g_ap, hn_rt[i][0:P, 0:n], ALU.mult, ALU.mult, [srcb, gb, hn_rtb[i]], [outb])

    mods, A1s, A2s, b_mods = [], [], [], []
    for l_ in range(nlayers):
        mods.append(sb(f"mod{l_}", [128, 48])); A1s.append(sb(f"A1_{l_}", [128, 8])); A2s.append(sb(f"A2_{l_}", [128, 8])); b_mods.append(Buf())
    with ExitStack() as st:
        xin = [sb(f"xin{i}", [128, D], F32, st) for i in range(4)]
        xinb = [Buf() for _ in range(4)]
        xo = [sb(f"xo{i}", [128, 8, 128], F32, st) for i in range(4)]
        xob = [Buf() for _ in range(4)]
        n1g_sb = sb("n1g_sb", [128, nlayers, 8], F32, st); n2g_sb = sb("n2g_sb", [128, nlayers, 8], F32, st); b_ng = Buf()
        adab = sb("adab", [128, nlayers, 48], F32, st); b_adab = Buf()
        cact2 = sb("cact2", [128, 8, 2], F32, st); b_cact2 = Buf()
        adw = [sb(f"adw{i}", [128, 8, D], F32, st) for i in range(2)]
        adwb = [Buf() for _ in range(2)]
        for l_ in range(nlayers):
            fw.dma("sp", n1g_sb[:, l_, :], n1g[l_], writes=[b_ng])
            fw.dma("sp", n2g_sb[:, l_, :], n2g[l_], writes=[b_ng])
            fw.dma("sp", adab[:, l_, :], ada_b_fm[l_], writes=[b_adab])
        cp(cact2[:], cact[:].unsqueeze(2).to_broadcast([128, 8, 2]), [b_cact], [b_cact2])

        def ada_units():
            for l_ in range(nlayers):
                for j in range(6):
                    i = (l_ * 6 + j) % 2
                    fw.dma("sp", adw[i][:], ada_w[l_, :, j * D:(j + 1) * D].rearrange("(kc p) n -> p kc n", p=128), writes=[adwb[i]])
                    for c in range(8):
                        def f(e, i=i, j=j, c=c, l_=l_):
                            for kc in range(8):
                                col = 2 * (j * 8 + c)
                                ins = e.matmul(PS[l_][:, col:col + 2], adw[i][:, kc, c * 128:(c + 1) * 128], cact2[:, kc, :],
                                               start=(kc == 0), stop=(kc == 7))
                            return ins
                        OP("pe", f, [adwb[i], b_cact2], [PSB[l_]])
                        yield
        au = ada_units()
        per = (nlayers * 48 + 31) // 32
        for tk in range(S // 128):
            i = tk % 4
            fw.dma("sp", xin[i][:], x_in[tk * 128:(tk + 1) * 128, :], writes=[xinb[i]])
            for _ in range(per):
                next(au, None)
            for half in range(2):
                pb = 2 + (2 * tk + half) % 6

                def f(e, i=i, half=half, pb=pb):
                    for c4 in range(4):
                        c = half * 4 + c4
                        ins = e.transpose(PS[pb][:, c4 * 128:(c4 + 1) * 128], xin[i][:, c * 128:(c + 1) * 128], ident[:])
                    return ins
                OP("pe", f, [xinb[i], b_ident], [PSB[pb]])
                cp(xo[i][:, half * 4:(half + 1) * 4, :], PS[pb][:].rearrange("p (c t) -> p c t", c=4),
                   [PSB[pb]], [xob[i]], eng=("act" if half == 0 else "dve"))
            fw.dma("sp", xT_d[:, :, tk * 128:(tk + 1) * 128].rearrange("c p t -> p c t"), xo[i][:], reads=[xob[i]])
        for _ in au:
            pass
        for l_ in range(nlayers):
            tt(mods[l_][:], PS[l_][:, 0:96].rearrange("p (n two) -> p n two", two=2)[:, :, 0], adab[:, l_, :], ALU.add,
               [PSB[l_], b_adab], [b_mods[l_]])
            stt(A1s[l_][:], mods[l_][:, 8:16], 1.0, n1g_sb[:, l_, :], ALU.add, ALU.mult, [b_mods[l_], b_ng], [b_mods[l_]])
            stt(A2s[l_][:], mods[l_][:, 32:40], 1.0, n2g_sb[:, l_, :], ALU.add, ALU.mult, [b_mods[l_], b_ng], [b_mods[l_]])
        if debug:
            fw.dma("sp", dbg["dbg_mod"], mods[0][:], reads=[b_mods[0]])
        fw.barrier()

    def norm_mod(st_, xt, xtb, A, B, ABb, hb, hbb, xn, xnb, sqb, sqbb, rt, rtb, psn):
        act(sqb[:], xt[:], AF.Square, [xtb], [sqbb])

        def f(e):
            for c in range(8):
                ins = e.matmul(PS[psn][:, :], ones_b[:], sqb[:, c, :], start=(c == 0), stop=(c == 7))
            return ins
        OP("pe", f, [sqbb, b_ones], [PSB[psn]])
        act(rt[:], PS[psn][:, :], AF.Ln, [PSB[psn], b_eps], [rtb], scale=1.0 / D, bias=eps_c[:])
        act(rt[:], rt[:], AF.Exp, [rtb], [rtb], scale=-0.5)
        tt(xn[:], xt[:], rt[:].unsqueeze(1).to_broadcast([128, 8, TT]), ALU.mult, [xtb, rtb], [xnb])
        for c in range(8):
            if c % 2 == 0:
                ts(xn[:, c, :], xn[:, c, :], A[:, c:c + 1], B[:, c:c + 1], ALU.mult, ALU.add, [xnb, ABb], [xnb])
            else:
                act(xn[:, c, :], xn[:, c, :], AF.Identity, [xnb, ABb], [xnb], scale=A[:, c:c + 1], bias=B[:, c:c + 1])
        cp(hb[:, 0:4, :], xn[:, 0:4, :], [xnb], [hbb], eng="dve")
        cp(hb[:, 4:8, :], xn[:, 4:8, :], [xnb], [hbb], eng="act")

    for l in range(nlayers if stop_after != "0" else 0):
        last = (l == nlayers - 1)
        with ExitStack() as lay:
            mod = mods[l]; A1 = A1s[l]; A2 = A2s[l]; b_mod = b_mods[l]
            B1 = mod[:, 0:8]; G1 = mod[:, 16:24]; B2 = mod[:, 24:32]; G2 = mod[:, 40:48]

            with ExitStack() as mix:
                V_sb = sb("V_sb", [128, 32, 8, 65], BF16, mix); b_V = Buf()
                cum = sb("cum", [8, S], F32, mix); b_cum = Buf()
                og_fm_sb = sb("og_fm_sb", [128, 8], F32, mix); og_at_sb = sb("og_at_sb", [64, 8], F32, mix); b_og = Buf()
                fw.dma("sp", og_fm_sb[:], og_fm[l], writes=[b_og])
                fw.dma("sp", og_at_sb[:], og_at[l], writes=[b_og])
                OP("dve", lambda e: e.memset(V_sb[:, :, :, 64:65], 1.0), writes=[b_V])
                if l == 0:
                    cv = [sb(f"cv{i}", [128, 2048], BF16, mix) for i in range(2)]; cvb = [Buf() for _ in range(2)]; cvk = [0]
                fw.barrier()
                with ExitStack() as st:
                    win = sb("win", [128, 8, DIN], BF16, st); b_win = Buf()
                    for hh in range(2):
                        fw.dma("pool", win[:, :, hh * 1284:(hh + 1) * 1284],
                               w_in[l, :, hh * 1284:(hh + 1) * 1284].rearrange("(kc p) n -> p kc n", p=128), writes=[Buf()])
                    fw.barrier()
                    if l == 0 and not NOCONV:
                        for l2 in range(nlayers):
                            for e_ in range(NE):
                                for (src, dst, pat) in ((w_gate, wg_d, 8), (w_up, wu_d, 8), (w_down, wd_d, 4)):
                                    for hf in range(2):
                                        i = cvk[0] % 2
                                        cvk[0] += 1
                                        kcs = pat // 2
                                        srcap = src[l2, e_].rearrange("(kc p) n -> p kc n", p=128)[:, hf * kcs:(hf + 1) * kcs, :]
                                        dstv = cv[i][:].rearrange("p (kc n) -> p kc n", kc=kcs)
                                        fw.dma("pool", dstv, srcap, writes=[cvb[i]])
                                        fw.dma("pool", dst[l2 * NE + e_][:, hf * 2048:(hf + 1) * 2048], cv[i][:], reads=[cvb[i]])
                        fw.bg_on = True
                    fbs = sb("fbs", [8, 1], F32, st); b_fb = Buf()
                    qgs = sb("qgs", [128, 1], F32, st); kgs = sb("kgs", [128, 1], F32, st); b_qk = Buf()
                    cws = sb("cws", [128, 2, 3], F32, st); b_cw = Buf()
                    fw.dma("sp", fbs[:], fb[l], writes=[b_fb])
                    fw.dma("sp", qgs[:], qg[l], writes=[b_qk])
                    fw.dma("sp", kgs[:], kg[l], writes=[b_qk])
                    fw.dma("sp", cws[:], conv_w[l], writes=[b_cw])
                    ts(fbs[:], fbs[:], -1.0, None, ALU.mult, None, [b_fb], [b_fb])
                    ts(qgs[:], qgs[:], 0.125, None, ALU.mult, None, [b_qk], [b_qk])
                    if l == 0:
                        xt = [sb("xt0", [128, 8, TT], F32, st)] * 2; xtb = [Buf()] * 2
                    else:
                        xt = [sb(f"xt{i}", [128, 8, TT], F32, st) for i in range(2)]; xtb = [Buf() for _ in range(2)]
                    sqb = sb("sqb", [128, 8, TT], BF16, st); sqbb = Buf()
                    rt = sb("rt", [128, TT], F32, st); rtb = Buf()
                    xn = sb("xn", [128, 8, TT], F32, st); xnb = Buf()
                    hb = [sb(f"hb{i}", [128, 8, TT], BF16, st) for i in range(2)]; hbb = [Buf() for _ in range(2)]
                    ev = [sb(f"ev{i}", [128, TT], F32, st) for i in range(4)]; evb = [Buf() for _ in range(4)]
                    evo = [sb(f"evo{i}", [128, TT], BF16, st) for i in range(4)]; evob = [Buf() for _ in range(4)]
                    zt = [[sb(f"zt{cc}{i}", [128, TT + 2], F32, st) for i in range(2)] for cc in range(2)]
                    ztb = [[Buf() for _ in range(2)] for _ in range(2)]
                    cy = sb("cy", [128, TT], F32, st); cyb = Buf()
                    fe = sb("fe", [8, TT], F32, st); feb = Buf()
                    evc = [0]
                    gen = [0]
                    for cc in range(2):
                        OP("dve", lambda e, cc=cc: e.memset(zt[cc][1][:, TT:TT + 2], 0.0), writes=[ztb[cc][1]])

                    def proj(ps, off, M, hbt, hbtb):
                        def f(e):
                            for kc in range(8):
                                ins = e.matmul(PS[ps][0:M, :], win[:, kc, off:off + M], hbt[:, kc, :], start=(kc == 0), stop=(kc == 7))
                            return ins
                        OP("pe", f, [hbtb], [PSB[ps]])

                    for it in range(NT):
                        t0 = it * TT
                        i = it % 2
                        fw.dma("sp", xt[i][:], xT_d[:, :, t0:t0 + TT].rearrange("c p t -> p c t"), writes=[xtb[i]])
                        norm_mod(st, xt[i], xtb[i], A1, B1, b_mod, hb[i], hbb[i], xn, xnb, sqb, sqbb, rt, rtb, 0)
                        if debug and l == 0:
                            fw.dma("sp", dbg["dbg_h"][:, :, t0:t0 + TT].rearrange("c p t -> p c t"), hb[i][:], reads=[hbb[i]])
                        for c in range(2):
                            ps = 1 + gen[0] % 2; gen[0] += 1
                            proj(ps, OFF_U + c * 128, 128, hb[i], hbb[i])
                            k = evc[0] % 4; evc[0] += 1
                            cp(ev[k][:], PS[ps][:, :], [PSB[ps]], [evb[k]], eng="act")
                            fw.dma("sp", uT_d[c, :, t0:t0 + TT], ev[k][:], reads=[evb[k]])
                        for (off, gsb, dst) in ((OFF_Q, qgs, qT_d), (OFF_K, kgs, kT_d)):
                            for c in range(4):
                                ps = 1 + gen[0] % 2; gen[0] += 1
                                proj(ps, off + c * 128, 128, hb[i], hbb[i])
                                k = evc[0] % 4; evc[0] += 1
                                cp(ev[k][:], PS[ps][:, :], [PSB[ps]], [evb[k]], eng="act")
                                hnorm(ev[k][:], evb[k], gsb[:, 0:1], b_qk, evo[k][:], evob[k])
                                fw.dma("sp", dst[2 * c, 0:64, t0:t0 + TT], evo[k][0:64, :], reads=[evob[k]])
                                fw.dma("sp", dst[2 * c + 1, 0:64, t0:t0 + TT], evo[k][64:128, :], reads=[evob[k]])
                        ps = 1 + gen[0] % 2; gen[0] += 1
                        proj(ps, OFF_F, 8, hb[i], hbb[i])
                        act(fe[:], PS[ps][0:8, :], AF.Exp, [PSB[ps], b_fb], [feb], scale=-1.0, bias=fbs[:])
                        act(fe[:], fe[:], AF.Ln, [feb], [feb], bias=1.0)
                        init = 0.0 if it == 0 else cum[:, t0 - 1:t0]
                        OP("dve", lambda e, t0=t0, init=init: e.tensor_tensor_scan(
                            out=cum[:, t0:t0 + TT], data0=ones_f[0:8, 0:1].to_broadcast([8, TT]), data1=fe[:], initial=init,
                            op0=ALU.mult, op1=ALU.subtract), [feb, b_onesf, b_cum], [b_cum])
                        for sub in range(4):
                            def f(e, sub=sub, i=i):
                                for kc in range(8):
                                    ins = e.matmul(PS[3][:, :], hb[i][:, kc, sub * 128:(sub + 1) * 128], win[:, kc, OFF_V:OFF_V + 512],
                                                   start=(kc == 0), stop=(kc == 7))
                                return ins
                            OP("pe", f, [hbb[i]], [PSB[3]])
                            cp(V_sb[:, 4 * it + sub, :, 0:64], PS[3][:, :].rearrange("p (h d) -> p h d", h=8), [PSB[3]], [b_V],
                               eng=("act" if sub % 2 else "dve"))
                        for cc in range(2):
                            proj(4, OFF_HC + cc * 128, 128, hb[i], hbb[i])
                            proj(5, OFF_CG + cc * 128, 128, hb[i], hbb[i])
                            proj(6, OFF_BG + cc * 128, 128, hb[i], hbb[i])
                            k = evc[0] % 4; evc[0] += 1
                            z, zb = zt[cc][i], ztb[cc][i]
                            zp, zpb = zt[cc][1 - i], ztb[cc][1 - i]
                            cp(ev[k][:], PS[5][:, :], [PSB[5]], [evb[k]], eng="act")
                            cp(z[:, 0:2], zp[:, TT:TT + 2], [zpb], [zb])
                            tt(z[:, 2:TT + 2], PS[4][:, :], ev[k][:], ALU.mult, [PSB[4], evb[k]], [zb])
                            ts(cy[:], z[:, 2:TT + 2], cws[:, cc, 2:3], None, ALU.mult, None, [zb, b_cw], [cyb])
                            stt(cy[:], z[:, 1:TT + 1], cws[:, cc, 1:2], cy[:], ALU.mult, ALU.add, [zb, b_cw, cyb], [cyb])
                            stt(cy[:], z[:, 0:TT], cws[:, cc, 0:1], cy[:], ALU.mult, ALU.add, [zb, b_cw, cyb], [cyb])
                            tt(ev[k][:], PS[6][:, :], cy[:], ALU.mult, [PSB[6], cyb], [evb[k]])
                            hnorm(ev[k][:], evb[k], og_fm_sb[:, 6 + cc:7 + cc], b_og, evo[k][:], evob[k])
                            fw.dma("sp", yh_d[2 + cc, :, t0:t0 + TT], evo[k][:], reads=[evob[k]])
                    if debug and l == 0:
                        fw.dma("sp", dbg["dbg_cum"], cum[:], reads=[b_cum])
                        fw.dma("sp", dbg["dbg_v"], V_sb[:], reads=[b_V])
                    fw.barrier()
                if stop_after == "A":
                    break
                with ExitStack() as st:
                    def t8(name):
                        return sb(name, [128, 8], F32, st)
                    lre, lim, ldt = t8("lre"), t8("lim"), t8("ldt"); b_p = Buf()
                    fw.dma("sp", lre[:], lam_re[l], writes=[b_p])
                    fw.dma("sp", lim[:], lam_im[l], writes=[b_p])
                    fw.dma("sp", ldt[:], log_dt[l], writes=[b_p])
                    bre = sb("bre", [128, 8, 16], F32, st); bim = sb("bim", [128, 8, 16], F32, st)
                    cre = sb("cre", [128, 8, 16], F32, st); cim = sb("cim", [128, 8, 16], F32, st); b_bc = Buf()
                    fw.dma("sp", bre[:], sb_re[l], writes=[b_bc]); fw.dma("sp", bim[:], sb_im[l], writes=[b_bc])
                    fw.dma("sp", cre[:], sc_re[l], writes=[b_bc]); fw.dma("sp", cim[:], sc_im[l], writes=[b_bc])
                    dsk = sb("dsk", [128, 2], F32, st); glb = sb("glb", [128, 2], F32, st); b_dg = Buf()
                    fw.dma("sp", dsk[:], ssm_d[l], writes=[b_dg]); fw.dma("sp", glb[:], glu_b[l], writes=[b_dg])
                    gluw = sb("gluw", [128, 2, 256], BF16, st); gluwf = sb("gluwf", [128, 2, 256], F32, st); b_gw = Buf()
                    fw.dma("sp", gluwf[:], glu_w[l].rearrange("(kc p) n -> p kc n", p=128), writes=[b_gw])
                    cp(gluw[:], gluwf[:], [b_gw], [b_gw], eng="act")
                    r_sb, th = t8("r_sb"), t8("th")
                    dtv, a_, cs, sn, t1_, t2_, zre, zim = t8("dtv"), t8("a_"), t8("cs"), t8("sn"), t8("t1_"), t8("t2_"), t8("zre"), t8("zim")
                    ti = sb("ti", [128, 8], I32, st)
                    C1 = 6.28125
                    C2 = TWO_PI - C1

                    def sincos(out, ang, shape, tmpf, tmpi, bq, shift):
                        ts(tmpf, ang, 1.0 / TWO_PI, shift / TWO_PI, ALU.mult, ALU.add, [bq], [bq])
                        cp(tmpi, tmpf, [bq], [bq])
                        cp(tmpf, tmpi, [bq], [bq])
                        if shift != 0.0:
                            ts(out, ang, shift, None, ALU.add, None, [bq], [bq])
                            stt(out, tmpf, -C1, out, ALU.mult, ALU.add, [bq], [bq])
                        else:
                            stt(out, tmpf, -C1, ang, ALU.mult, ALU.add, [bq], [bq])
                        stt(out, tmpf, -C2, out, ALU.mult, ALU.add, [bq], [bq])
                        ts(out, out, 3.1415925, -3.1415925, ALU.min, ALU.max, [bq], [bq])
                        act(out, out, AF.Sin, [bq], [bq])

                    ts(lre[:], lre[:], -1e-4, None, ALU.min, None, [b_p], [b_p])
                    act(dtv[:], ldt[:], AF.Exp, [b_p], [b_p])
                    tt(a_[:], lre[:], dtv[:], ALU.mult, [b_p], [b_p])
                    act(r_sb[:], a_[:], AF.Exp, [b_p], [b_p])
                    tt(th[:], lim[:], dtv[:], ALU.mult, [b_p], [b_p])
                    sincos(sn[:], th[:], None, t1_[:], ti[:], b_p, 0.0)
                    sincos(cs[:], th[:], None, t1_[:], ti[:], b_p, 1.5707963267948966)
                    tt(cs[:], cs[:], r_sb[:], ALU.mult, [b_p], [b_p])
                    tt(sn[:], sn[:], r_sb[:], ALU.mult, [b_p], [b_p])
                    ts(cs[:], cs[:], -1.0, None, ALU.add, None, [b_p], [b_p])
                    tt(t1_[:], lre[:], lre[:], ALU.mult, [b_p], [b_p])
                    tt(t2_[:], lim[:], lim[:], ALU.mult, [b_p], [b_p])
                    tt(t1_[:], t1_[:], t2_[:], ALU.add, [b_p], [b_p])
                    OP("dve", lambda e: e.reciprocal(out=t1_[:], in_=t1_[:]), [b_p], [b_p])
                    tt(zre[:], cs[:], lre[:], ALU.mult, [b_p], [b_p])
                    tt(t2_[:], sn[:], lim[:], ALU.mult, [b_p], [b_p])
                    tt(zre[:], zre[:], t2_[:], ALU.add, [b_p], [b_p])
                    tt(zre[:], zre[:], t1_[:], ALU.mult, [b_p], [b_p])
                    tt(zim[:], sn[:], lre[:], ALU.mult, [b_p], [b_p])
                    tt(t2_[:], cs[:], lim[:], ALU.mult, [b_p], [b_p])
                    tt(zim[:], zim[:], t2_[:], ALU.subtract, [b_p], [b_p])
                    tt(zim[:], zim[:], t1_[:], ALU.mult, [b_p], [b_p])
                    bbr = sb("bbr", [128, 8, 16], F32, st); bbi = sb("bbi", [128, 8, 16], F32, st); tb = sb("tb", [128, 8, 16], F32, st)
                    zre_b = zre[:].unsqueeze(2).to_broadcast([128, 8, 16]); zim_b = zim[:].unsqueeze(2).to_broadcast([128, 8, 16])
                    tt(bbr[:], bre[:], zre_b, ALU.mult, [b_p, b_bc], [b_bc])
                    tt(tb[:], bim[:], zim_b, ALU.mult, [b_p, b_bc], [b_bc])
                    tt(bbr[:], bbr[:], tb[:], ALU.subtract, [b_bc], [b_bc])
                    tt(bbi[:], bim[:], zre_b, ALU.mult, [b_p, b_bc], [b_bc])
                    tt(tb[:], bre[:], zim_b, ALU.mult, [b_p, b_bc], [b_bc])
                    tt(bbi[:], bbi[:], tb[:], ALU.add, [b_bc], [b_bc])
                    WT = []
                    for nm, src in (("re", bbr), ("im", bbi)):
                        w1 = sb("w1" + nm, [128, 8, 2, 16], F32, st); bw1 = Buf()
                        OP("dve", lambda e, w1=w1: e.memset(w1[:], 0.0), writes=[bw1])
                        cp(w1[0:64, :, 0, :], src[0:64], [b_bc], [bw1])
                        cp(w1[64:128, :, 1, :], src[64:128], [b_bc], [bw1])
                        wt = sb("wt" + nm, [128, 2, 128], BF16, st); bwt = Buf()
                        w1v = w1[:].rearrange("p g a c -> p (g a c)")
                        for ch in range(2):
                            OP("pe", lambda e, ch=ch, w1v=w1v: e.transpose(PS[0][:, 0:128], w1v[:, ch * 128:(ch + 1) * 128], ident[:]),
                               [bw1, b_ident], [PSB[0]])
                            cp(wt[:, ch, :], PS[0][:, 0:128], [PSB[0]], [bwt])
                        WT.append((wt, bwt))
                    CT = []
                    for nm, src, sgn in (("re", cre, 1.0), ("im", cim, -1.0)):
                        ct = sb("ct" + nm, [128, 8, 2, 16], BF16, st); bct = Buf()
                        OP("dve", lambda e, ct=ct: e.memset(ct[:], 0.0), writes=[bct])
                        ts(ct[0:64, :, 0, :], src[0:64], sgn, None, ALU.mult, None, [b_bc], [bct])
                        ts(ct[64:128, :, 1, :], src[64:128], sgn, None, ALU.mult, None, [b_bc], [bct])
                        CT.append((ct, bct))
                    cosT = sb("cosT", [128, 8, TT + 1], F32, st); sinT = sb("sinT", [128, 8, TT + 1], F32, st); b_tab = Buf()
                    with ExitStack() as st2:
                        ang = sb("ang", [128, 8, TT + 1], F32, st2); tf = sb("tf", [128, 8, TT + 1], F32, st2)
                        tii = sb("tii", [128, 8, TT + 1], I32, st2); b_ang = Buf()
                        for gp in range(8):
                            ts(ang[:, gp, :], iota[:], th[:, gp:gp + 1], None, ALU.mult, None, [b_iota, b_p], [b_ang])
                        sincos(sinT[:], ang[:], None, tf[:], tii[:], b_ang, 0.0)
                        sincos(cosT[:], ang[:], None, tf[:], tii[:], b_ang, 1.5707963267948966)
                        fw.barrier()
                    uf = [sb(f"uf{i}", [128, 2, TT], F32, st) for i in range(2)]; ufb = [Buf() for _ in range(2)]
                    ub = [sb(f"ub{i}", [128, 2, TT], BF16, st) for i in range(2)]; ubb = [Buf() for _ in range(2)]
                    ta = [sb(f"ta{i}", [128, TT], F32, st) for i in range(4)]; tab_ = [Buf() for _ in range(4)]
                    wre = [sb(f"wre{i}", [128, TT], F32, st) for i in range(2)]; wim = [sb(f"wim{i}", [128, TT], F32, st) for i in range(2)]
                    wb_ = [Buf() for _ in range(2)]
                    zr = [sb(f"zr{i}", [128, TT], BF16, st) for i in range(2)]; zi = [sb(f"zi{i}", [128, TT], BF16, st) for i in range(2)]
                    zb_ = [Buf() for _ in range(2)]
                    ini = sb("ini", [128, 8, 2], F32, st); b_ini = [Buf() for _ in range(8)]
                    tiny = sb("tiny", [128, 2], F32, st)
                    yp = sb("yp", [128, 2, TT], F32, st); ypb = [Buf() for _ in range(2)]
                    yg = sb("yg", [128, 2, TT], F32, st); ygb_f = [Buf() for _ in range(2)]
                    ygb = sb("ygb", [128, 2, TT], BF16, st); ygbb = Buf()
                    g1t = sb("g1t", [128, TT], F32, st); g1b = Buf(); g2t = sb("g2t", [128, TT], F32, st); g2b = Buf()
                    yo = [sb(f"yo{i}", [128, TT], F32, st) for i in range(2)]; yob = [Buf() for _ in range(2)]
                    yob16 = [sb(f"yob16{i}", [128, TT], BF16, st) for i in range(2)]; yob16b = [Buf() for _ in range(2)]
                    OP("dve", lambda e: e.memset(ini[:], 0.0), writes=b_ini)
                    k = 0
                    def gen_B():
                        k = 0
                        pend = []

                        def run_due(force=False):
                            keep = []
                            for item in list(pend):
                                item[0] -= 1
                                if force or item[0] <= 0:
                                    nxt = item[1]()
                                    while force and nxt is not None:
                                        nxt = nxt()
                                    if nxt is not None:
                                        keep.append([1, nxt])
                                else:
                                    keep.append(item)
                            pend[:] = keep
                        for it in range(NT):
                            t0 = it * TT
                            i = it % 2
                            fw.dma("sp", uf[i][:], uT_d[:, :, t0:t0 + TT].rearrange("c p t -> p c t"), writes=[ufb[i]])
                            cp(ub[i][:], uf[i][:], [ufb[i]], [ubb[i]], eng="act")
                            for gp in range(8):
                                ch, j = gp // 4, gp % 4
                                pa, pb = 0, 1
                                for (pp, (wt, bwt)) in ((pa, WT[0]), (pb, WT[1])):
                                    OP("pe", lambda e, pp=pp, wt=wt, ch=ch, j=j, i=i: e.matmul(
                                        PS[pp][:, :], wt[32 * j:32 * j + 32, ch, :], ub[i][32 * j:32 * j + 32, ch, :],
                                        start=True, stop=True, tile_position=(32 * j, 0)), [bwt, ubb[i]], [PSB[pp]])
                                run_due()
                                cT = cosT[:, gp, 0:TT]; sT = sinT[:, gp, 0:TT]
                                kk = k % 2; k += 1
                                tt(ta[0][:], PS[pa][:, :], cT, ALU.mult, [PSB[pa], b_tab], [tab_[0]])
                                tt(ta[1][:], PS[pb][:, :], sT, ALU.mult, [PSB[pb], b_tab], [tab_[1]])
                                tt(ta[0][:], ta[0][:], ta[1][:], ALU.add, [tab_[0], tab_[1]], [tab_[0]])
                                tt(ta[2][:], PS[pb][:, :], cT, ALU.mult, [PSB[pb], b_tab], [tab_[2]])
                                tt(ta[3][:], PS[pa][:, :], sT, ALU.mult, [PSB[pa], b_tab], [tab_[3]])
                                tt(ta[2][:], ta[2][:], ta[3][:], ALU.subtract, [tab_[2], tab_[3]], [tab_[2]])
                                rb = r_sb[:, gp:gp + 1].to_broadcast([128, TT])
                                OP("dve", lambda e, kk=kk, rb=rb, gp=gp: e.tensor_tensor_scan(
                                    out=wre[kk][:], data0=rb, data1=ta[0][:], initial=ini[:, gp, 0:1], op0=ALU.mult, op1=ALU.add),
                                    [tab_[0], b_p, b_ini[gp]], [wb_[kk]])
                                OP("dve", lambda e, kk=kk, rb=rb, gp=gp: e.tensor_tensor_scan(
                                    out=wim[kk][:], data0=rb, data1=ta[2][:], initial=ini[:, gp, 1:2], op0=ALU.mult, op1=ALU.add),
                                    [tab_[2], b_p, b_ini[gp]], [wb_[kk]])
                                tt(ta[0][:], wre[kk][:], cT, ALU.mult, [wb_[kk], b_tab], [tab_[0]])
                                tt(ta[1][:], wim[kk][:], sT, ALU.mult, [wb_[kk], b_tab], [tab_[1]])
                                tt(zr[kk][:], ta[0][:], ta[1][:], ALU.subtract, [tab_[0], tab_[1]], [zb_[kk]])
                                tt(ta[2][:], wre[kk][:], sT, ALU.mult, [wb_[kk], b_tab], [tab_[2]])
                                tt(ta[3][:], wim[kk][:], cT, ALU.mult, [wb_[kk], b_tab], [tab_[3]])
                                tt(zi[kk][:], ta[2][:], ta[3][:], ALU.add, [tab_[2], tab_[3]], [zb_[kk]])
                                cL = cosT[:, gp, TT:TT + 1]; sL = sinT[:, gp, TT:TT + 1]
                                ts(tiny[:, 0:1], wim[kk][:, TT - 1:TT], sL, None, ALU.mult, None, [wb_[kk], b_tab], [b_ini[gp]])
                                ts(tiny[:, 1:2], wim[kk][:, TT - 1:TT], cL, None, ALU.mult, None, [wb_[kk], b_tab], [b_ini[gp]])
                                stt(ini[:, gp, 0:1], wre[kk][:, TT - 1:TT], cL, tiny[:, 0:1], ALU.mult, ALU.subtract, [wb_[kk], b_tab, b_ini[gp]], [b_ini[gp]])
                                stt(ini[:, gp, 1:2], wre[kk][:, TT - 1:TT], sL, tiny[:, 1:2], ALU.mult, ALU.add, [wb_[kk], b_tab, b_ini[gp]], [b_ini[gp]])
                                py = 2

                                def tail(gp=gp, j=j, kk=kk, py=py, ch=ch, i=i, t0=t0):
                                  def f(e):
                                    e.matmul(PS[py][32 * j:32 * j + 32, :], CT[0][0][:, gp, :, :].rearrange("p a c -> p (a c)"), zr[kk][:],
                                             start=True, stop=False, tile_position=(0, 32 * j))
                                    return e.matmul(PS[py][32 * j:32 * j + 32, :], CT[1][0][:, gp, :, :].rearrange("p a c -> p (a c)"), zi[kk][:],
                                                    start=False, stop=True, tile_position=(0, 32 * j))
                                  OP("pe", f, [zb_[kk], CT[0][1], CT[1][1]], [PSB[py]])
                                  if j == 3:
                                    stt(yp[:, ch, :], uf[i][:, ch, :], dsk[:, ch:ch + 1], PS[py][:, :], ALU.mult, ALU.add,
                                        [ufb[i], b_dg, PSB[py]], [ypb[ch]])
                                    if debug and l == 0:
                                        fw.dma("sp", dbg["dbg_ssmpre"][ch, :, t0:t0 + TT], yp[:, ch, :], reads=[ypb[ch]])
                                    tt(g1t[:], yp[:, ch, :], yp[:, ch, :], ALU.mult, [ypb[ch]], [g1b])
                                    ts(g1t[:], g1t[:], 0.044715, 1.0, ALU.mult, ALU.add, [g1b], [g1b])
                                    tt(g1t[:], g1t[:], yp[:, ch, :], ALU.mult, [g1b, ypb[ch]], [g1b])

                                    def tailB():
                                        act(g1t[:], g1t[:], AF.Sigmoid, [g1b], [g1b], scale=1.5957691216057308)

                                        def tailC():
                                            tt(yg[:, ch, :], yp[:, ch, :], g1t[:], ALU.mult, [g1b, ypb[ch]], [ygb_f[ch]])
                                            cp(ygb[:, ch, :], yg[:, ch, :], [ygb_f[ch]], [ygbb])
                                            return None
                                        return tailC
                                    return tailB
                                  return None
                                pend.append([1, tail])
                                yield
                            def glu_block(t0=t0):
                                for mc in range(2):
                                    def f(e, mc=mc):
                                        e.matmul(PS[7][:, :], gluw[:, 0, mc * 128:(mc + 1) * 128], ygb[:, 0, :], start=True, stop=False)
                                        return e.matmul(PS[7][:, :], gluw[:, 1, mc * 128:(mc + 1) * 128], ygb[:, 1, :], start=False, stop=True)
                                    OP("pe", f, [ygbb, b_gw], [PSB[7]])
                                    act(g2t[:], PS[7][:, :], AF.Sigmoid, [PSB[7], b_dg], [g2b], bias=glb[:, mc:mc + 1])
                                    tt(yo[mc][:], yg[:, mc, :], g2t[:], ALU.mult, [g2b, ygb_f[mc]], [yob[mc]])
                                    hnorm(yo[mc][:], yob[mc], og_fm_sb[:, mc:mc + 1], b_og, yob16[mc][:], yob16b[mc])
                                    fw.dma("sp", yh_d[mc, :, t0:t0 + TT], yob16[mc][:], reads=[yob16b[mc]])
                                return None
                            pend.append([4, glu_block])
                            yield
                        run_due(force=True)
                        yield
                    ckT = sb("ckT", [128, 32, 8], F32, st); cref = sb("cref", [128, 32, 8], F32, st); b_ck = Buf()
                    st3 = ExitStack()
                    ce = sb("ce", [8, 32], F32, st3); dq = sb("dq", [8, 8, 4], F32, st3); b_ce = Buf()
                    dqrow = sb("dqrow", [8, 32, 128], BF16, st3); onesrow = sb("onesrow", [8, S], BF16, st3); b_row = Buf()
                    cp(ce[:], cum[:].rearrange("h (s j) -> h s j", j=128)[:, :, 127], [b_cum], [b_ce])
                    cev = ce[:].rearrange("h (q s) -> h q s", s=4)
                    tt(dq[:], cev, cev[:, :, 3:4].to_broadcast([8, 8, 4]), ALU.subtract, [b_ce], [b_ce])
                    cp(dqrow[:], dq[:].rearrange("h q s -> h (q s)").unsqueeze(2).to_broadcast([8, 32, 128]), [b_ce], [b_row])
                    OP("dve", lambda e: e.memset(onesrow[:], 1.0), writes=[b_row])
                    fw.dma("sp", qT_d[:, 64, :], dqrow[:].rearrange("h s j -> h (s j)"), reads=[b_row])
                    fw.dma("sp", kT_d[:, 64, :], onesrow[:], reads=[b_row])

                    def f(e):
                        for kt in range(32):
                            ins = e.transpose(PS[0][:, kt * 8:(kt + 1) * 8], cum[0:8, kt * 128:(kt + 1) * 128], ident[0:8, 0:8])
                        return ins
                    OP("pe", f, [b_cum, b_ident], [PSB[0]])
                    cp(ckT[:].rearrange("p k h -> p (k h)"), PS[0][:, 0:256], [PSB[0]], [b_ck])
                    OP("pe", lambda e: e.matmul(PS[1][:, 0:256], e127[:], ckT[:].rearrange("p k h -> p (k h)"), start=True, stop=True),
                       [b_ck, b_e127], [PSB[1]])
                    cp(cref[:].rearrange("p k h -> p (k h)"), PS[1][:, 0:256], [PSB[1]], [b_ck])
                    fw.barrier()
                    st3.close()
                    qa = [sb("qa0", [65, S], BF16, st)]; ka = [sb("ka0", [65, S], BF16, st)]
                    qab = [Buf()]; kab = [Buf()]
                    NP = 6
                    pT = [sb(f"pT{i}", [128, TT], BF16, st) for i in range(NP)]; pTb = [Buf() for _ in range(NP)]
                    biasT = [sb(f"biasT{i}", [128, 32], F32, st) for i in range(2)]; biasb = [Buf() for _ in range(2)]
                    osb = [sb(f"osb{i}", [65, TT], F32, st) for i in range(2)]; osbb = [Buf() for _ in range(2)]
                    yat = [sb(f"yat{i}", [64, TT], F32, st) for i in range(2)]; yatb = [Buf() for _ in range(2)]
                    yab = [sb(f"yab{i}", [64, TT], BF16, st) for i in range(2)] ; yabb = [Buf() for _ in range(2)]
                    def emit_bias(u_):
                        h_, qt_ = u_ // 8, u_ % 8
                        n_ = 4 * qt_ + 4
                        ts(biasT[u_ % 2][:, 0:n_], ckT[:, 0:n_, h_], cref[:, 4 * qt_ + 3, h_:h_ + 1], -1.0, ALU.subtract, ALU.mult,
                           [b_ck], [biasb[u_ % 2]])

                    def gen_C():
                        blkctr = 0
                        pend2 = pend3 = None
                        for h in range(8):
                            hi = 0
                            fw.dma("sp", qa[hi][:], qT_d[h], writes=[qab[hi]])
                            fw.dma("sp", ka[hi][:], kT_d[h], writes=[kab[hi]])
                            for qt in range(8):
                                nkt = 4 * qt + 4
                                bi = (h * 8 + qt) % 2
                                oi = bi
                                po = 6
                                if h * 8 + qt == 0:
                                    emit_bias(0)
                                if h * 8 + qt + 1 < 64:
                                    emit_bias(h * 8 + qt + 1)

                                SL = (3, 4, 5)
                                LA = 2

                                def s_mm(kt):
                                    slot = SL[(blkctr + kt) % 3]
                                    m = kt - 4 * qt
                                    c0 = 128 * m if m > 0 else 0
                                    def f(e):
                                        ins = e.matmul(PS[slot][:, c0:TT], ka[hi][:, kt * 128:(kt + 1) * 128],
                                                       qa[hi][:, qt * TT + c0:(qt + 1) * TT], start=True, stop=(m < 0))
                                        if m >= 0:
                                            ins = e.matmul(PS[slot][:, c0:c0 + 128], ntri[:], ident_b[:], start=False, stop=True)
                                        return ins
                                    OP("pe", f, [kab[hi], qab[hi], b_ntri], [PSB[slot]])
                                for kt in range(min(LA, nkt)):
                                    s_mm(kt)
                                for kt in range(nkt):
                                    slot = SL[(blkctr + kt) % 3]
                                    if kt + LA < nkt:
                                        s_mm(kt + LA)
                                    m = kt - 4 * qt
                                    c0 = 128 * m if m > 0 else 0
                                    pi = (blkctr + kt) % NP
                                    act(pT[pi][:, c0:TT], PS[slot][:, c0:TT], AF.Exp, [PSB[slot], biasb[bi]], [pTb[pi]],
                                        bias=biasT[bi][:, kt:kt + 1])
                                    OP("pe", lambda e, kt=kt, c0=c0, pi=pi: e.matmul(
                                        PS[po][0:65, c0:TT], V_sb[:, kt, h, :], pT[pi][:, c0:TT], start=(kt == 0), stop=(kt == nkt - 1)),
                                        [pTb[pi], b_V], [PSB[po]])
                                    if kt % 8 == 7 and kt + 1 < nkt:
                                        yield 8
                                blkctr += nkt
                                cp(osb[oi][:], PS[po][0:65, :], [PSB[po]], [osbb[oi]], eng="act")
                                OP("dve", lambda e, oi=oi: e.reciprocal(out=osb[oi][64:65, :], in_=osb[oi][64:65, :]), [osbb[oi]], [osbb[oi]])

                                def phase2(oi=oi, h=h, qt=qt):
                                    OP("pe", lambda e: e.matmul(PS[7][0:64, :], ones_f[64:65, 0:64], osb[oi][64:65, :], start=True, stop=True),
                                       [osbb[oi], b_onesf], [PSB[7]])
                                    tt(yat[oi][:], osb[oi][0:64, :], PS[7][0:64, :], ALU.mult, [osbb[oi], PSB[7]], [yatb[oi]])

                                    def phase3():
                                        hnorm(yat[oi][:], yatb[oi], og_at_sb[:, h:h + 1], b_og, yab[oi][:], yabb[oi], P=64)
                                        fw.dma("sp", ya_d[h, :, qt * TT:(qt + 1) * TT], yab[oi][:], reads=[yabb[oi]])
                                    return phase3
                                if pend3 is not None:
                                    pend3()
                                pend3 = pend2() if pend2 is not None else None
                                pend2 = phase2
                                yield ((nkt - 1) % 8) + 1
                        if pend3 is not None:
                            pend3()
                        if pend2 is not None:
                            pend2()()
                    gB, gC = gen_B(), gen_C()
                    aliveB = aliveC = True
                    cdone, bdone = 0, 0
                    CTOT, BTOT = 8 * sum(4 * q_ + 4 for q_ in range(8)), NT * 9
                    while aliveB or aliveC:
                        if aliveC:
                            try:
                                cdone += next(gC)
                            except StopIteration:
                                aliveC = False
                        while aliveB and (not aliveC or bdone * CTOT <= cdone * BTOT):
                            try:
                                next(gB)
                                bdone += 1
                            except StopIteration:
                                aliveB = False
                    fw.bg_on = False
                    fw.barrier()
            if stop_after == "C":
                break
            NSLOT = 80
            RS = 128
            SUB = RS // 128
            BIG = 1.0e4
            with ExitStack() as dl:
                msk_all = sb("msk_all", [128, 32, 16], F32, dl); eq1_all = sb("eq1_all", [128, 32, 16], F32, dl)
                comb_all = sb("comb_all", [128, 32, 16], F32, dl); b_all = Buf()
                r1i = sb("r1i", [128, 32], I32, dl); r2i = sb("r2i", [128, 32], I32, dl)
                w1s = sb("w1s", [128, 32], F32, dl); w2s = sb("w2s", [128, 32], F32, dl); b_rw = Buf()
                widx = sb("widx", [128, NSLOT], I32, dl); b_slot = Buf()
                with ExitStack() as st:
                    maskT = sb("maskT", [16, S], F32, dl); b_mT = Buf()
                    woa = sb("woa", [128, 4, D], BF16, st); wob = sb("wob", [64, 8, D], BF16, st)
                    fw.dma("pool", woa[:, 0:2, :], w_out[l, 0:256, :].rearrange("(kc p) n -> p kc n", p=128), writes=[Buf()])
                    fw.dma("pool", woa[:, 2:4, :], w_out[l, 768:1024, :].rearrange("(kc p) n -> p kc n", p=128), writes=[Buf()])
                    fw.dma("pool", wob[:], w_out[l, 256:768, :].rearrange("(h p) n -> p h n", p=64), writes=[Buf()])
                    fw.barrier()
                    zrow = sb("zrow", [128, 2048], F32, st); b_z = Buf()
                    OP("dve", lambda e: e.memset(zrow[:], 0.0), writes=[b_z])
                    for c_ in range(NSLOT * RS // 256):
                        fw.dma("pool", Xs_d[c_ * 256:(c_ + 1) * 256, :].rearrange("(p two) n -> p (two n)", two=2), zrow[:], reads=[b_z])
                    xt = sb("xtD", [128, 8, TT], F32, st); xtb = Buf()
                    ys = sb("ys", [128, 4, TT], BF16, st); ysb = Buf()
                    yatt = sb("yatt", [64, 8, TT], BF16, st); yattb = Buf()
                    sqb = sb("sqbD", [128, 8, TT], BF16, st); sqbb = Buf()
                    rt = sb("rtD", [128, TT], F32, st); rtb = Buf()
                    h2f = sb("h2f", [128, 8, TT], F32, st); h2fb = Buf()
                    h2 = sb("h2", [128, 8, TT], BF16, st); h2b = Buf()
                    htok = [sb(f"htok{i}", [128, D], F32, st) for i in range(2)]; htokb = [Buf() for _ in range(2)]
                    aff = sb("aff", [128, 4, 16], F32, st); selv = sb("selv", [128, 4, 16], F32, st); rtmp = sb("rtmp", [128, 4, 16], F32, st)
                    m1 = sb("m1", [128, 16], F32, st); m2 = sb("m2", [128, 16], F32, st); gm = sb("gm", [128, 4], F32, st)
                    b_r = Buf()
                    for it in range(NT):
                        t0 = it * TT
                        msk = msk_all[:, 4 * it:4 * it + 4, :]; comb = comb_all[:, 4 * it:4 * it + 4, :]; eq1 = eq1_all[:, 4 * it:4 * it + 4, :]
                        fw.dma("sp", xt[:], xT_d[:, :, t0:t0 + TT].rearrange("c p t -> p c t"), writes=[xtb])
                        fw.dma("sp", ys[:], yh_d[:, :, t0:t0 + TT].rearrange("c p t -> p c t"), writes=[ysb])
                        fw.dma("sp", yatt[:], ya_d[:, :, t0:t0 + TT].rearrange("h p t -> p h t"), writes=[yattb])
                        for mc in range(8):
                            ps = 4 + mc % 2

                            def f(e, mc=mc, ps=ps):
                                for kc in range(4):
                                    e.matmul(PS[ps][:, :], woa[:, kc, mc * 128:(mc + 1) * 128], ys[:, kc, :], start=(kc == 0), stop=False)
                                for hh in range(8):
                                    ins = e.matmul(PS[ps][:, :], wob[:, hh, mc * 128:(mc + 1) * 128], yatt[:, hh, :], start=False, stop=(hh == 7))
                                return ins
                            OP("pe", f, [ysb, yattb], [PSB[ps]])
                            stt(xt[:, mc, :], PS[ps][:, :], G1[:, mc:mc + 1], xt[:, mc, :], ALU.mult, ALU.add, [PSB[ps], b_mod, xtb], [xtb])
                        if debug and l == 0:
                            fw.dma("sp", dbg["dbg_xmid"][:, :, t0:t0 + TT].rearrange("c p t -> p c t"), xt[:], reads=[xtb])
                        fw.dma("sp", xT_d[:, :, t0:t0 + TT].rearrange("c p t -> p c t"), xt[:], reads=[xtb])
                        norm_mod(st, xt, xtb, A2, B2, b_mod, h2, h2b, h2f, h2fb, sqb, sqbb, rt, rtb, 7)
                        for sub in range(4):
                            hi_ = sub % 2
                            for half in range(2):
                                ps = half

                                def f(e, sub=sub, half=half, ps=ps):
                                    for c4 in range(4):
                                        c = half * 4 + c4
                                        ins = e.transpose(PS[ps][:, c4 * 128:(c4 + 1) * 128], h2f[:, c, sub * 128:(sub + 1) * 128], ident[:])
                                    return ins
                                OP("pe", f, [h2fb, b_ident], [PSB[ps]])
                                cp(htok[hi_][:, half * 512:(half + 1) * 512], PS[ps][:, :], [PSB[ps]], [htokb[hi_]],
                                   eng=("act" if half == 0 else "dve"))
                            fw.dma("sp", h2_d[t0 + sub * 128:t0 + (sub + 1) * 128, :], htok[hi_][:], reads=[htokb[hi_]])
                        for sub in range(4):
                            def f(e, sub=sub):
                                for kc in range(8):
                                    ins = e.matmul(PS[6][:, sub * 16:(sub + 1) * 16], h2f[:, kc, sub * 128:(sub + 1) * 128], wr_sb[:, kc, :],
                                                   start=(kc == 0), stop=(kc == 7))
                                return ins
                            OP("pe", f, [h2fb, b_wr], [PSB[6]])
                        act(aff[:].rearrange("p s e -> p (s e)"), PS[6][:, 0:64], AF.Sigmoid, [PSB[6]], [b_r])
                        tt(selv[:], aff[:], rb_sb[:].unsqueeze(1).to_broadcast([128, 4, 16]), ALU.add, [b_r, b_rb], [b_r])
                        s44 = selv[:].rearrange("p s (g e) -> p (s g) e", e=4)
                        r44 = rtmp[:].rearrange("p s (g e) -> p (s g) e", e=4)
                        RD = lambda o, i_, op: OP("dve", lambda e: e.tensor_reduce(out=o, in_=i_, axis=mybir.AxisListType.X, op=op), [b_r, b_all], [b_r, b_all])
                        RD(m1[:], s44, ALU.max)
                        tt(r44, s44, m1[:].unsqueeze(2).to_broadcast([128, 16, 4]), ALU.is_equal, [b_r], [b_r])
                        stt(r44, r44, -BIG, s44, ALU.mult, ALU.add, [b_r], [b_r])
                        RD(m2[:], r44, ALU.max)
                        tt(m1[:], m1[:], m2[:], ALU.add, [b_r], [b_r])
                        gs = m1[:].rearrange("p (s g) -> p s g", g=4)
                        RD(gm[:], gs, ALU.max)
                        m2v = m2[:].rearrange("p (s g) -> p s g", g=4)
                        tt(m2v, gs, gm[:].unsqueeze(2).to_broadcast([128, 4, 4]), ALU.is_equal, [b_r], [b_r])
                        ts(m2[:], m2[:], BIG, -BIG, ALU.mult, ALU.add, [b_r], [b_r])
                        tt(r44, s44, m2[:].unsqueeze(2).to_broadcast([128, 16, 4]), ALU.add, [b_r], [b_r])
                        RD(gm[:], rtmp[:], ALU.max)
                        tt(eq1, rtmp[:], gm[:].unsqueeze(2).to_broadcast([128, 4, 16]), ALU.is_equal, [b_r, b_all], [b_r, b_all])
                        stt(msk, eq1, -BIG, rtmp[:], ALU.mult, ALU.add, [b_r, b_all], [b_r, b_all])
                        RD(gm[:], msk, ALU.max)
                        tt(msk, rtmp[:], gm[:].unsqueeze(2).to_broadcast([128, 4, 16]), ALU.is_ge, [b_r, b_all], [b_r, b_all])
                        tt(comb, aff[:], msk, ALU.mult, [b_r, b_all], [b_r, b_all])
                        RD(gm[:], comb, ALU.add)
                        OP("dve", lambda e: e.reciprocal(out=gm[:], in_=gm[:]), [b_r], [b_r])
                        tt(comb, comb, gm[:].unsqueeze(2).to_broadcast([128, 4, 16]), ALU.mult, [b_r, b_all], [b_r, b_all])
                        if debug and l == 0:
                            fw.dma("sp", dbg["dbg_comb"][t0:t0 + TT, :].rearrange("(s p) e -> p s e", p=128), comb, reads=[b_all])

                        def f(e, it=it):
                            for sub in range(4):
                                ins = e.transpose(PS[6][0:16, sub * 128:(sub + 1) * 128], msk_all[:, 4 * it + sub, :], ident[:])
                            return ins
                        OP("pe", f, [b_all, b_ident], [PSB[6]])
                        cp(maskT[:, t0:t0 + TT], PS[6][0:16, :], [PSB[6]], [b_mT])
                    fw.barrier()
                with ExitStack() as st:
                    inc = sb("inc", [16, S], F32, st); b_s = Buf()
                    cntf = sb("cntf", [16, 2], F32, st); slf = sb("slf", [16, 2], F32, st); offf = sb("offf", [16, 1], F32, st)
                    endf = sb("endf", [16, 1], F32, st); cnti = sb("cnti", [16, 2], I32, st)
                    cmpt = sb("cmpt", [16, NSLOT], F32, st); sef = sb("sef", [128, NSLOT], F32, st); pidx = sb("pidx", [128, 1], F32, st); pit = sb("pit", [128, 128], F32, st)
                    pos_all = sb("pos_all", [128, 32, 16], F32, st); tmp3 = sb("tmp3", [128, 32, 16], F32, st)
                    rf = sb("rf", [128, 32], F32, st)
                    OP("dve", lambda e: e.tensor_tensor_scan(out=inc[:], data0=ones_f[0:16, 0:1].to_broadcast([16, S]), data1=maskT[:],
                                                             initial=0.0, op0=ALU.mult, op1=ALU.add), [b_mT, b_onesf], [b_s])
                    ts(cntf[:], inc[:, S - 1:S].to_broadcast([16, 2]), 1.0 / RS, (RS - 1.0) / RS - (RS - 1.0) / (2 * RS), ALU.mult, ALU.add, [b_s], [b_s])
                    cp(cnti[:], cntf[:], [b_s], [b_s])
                    cp(slf[:], cnti[:], [b_s], [b_s])
                    OP("pe", lambda e: e.matmul(PS[0][0:16, 0:2], tri_f[0:16, 0:16], slf[:], start=True, stop=True), [b_s, b_tri], [PSB[0]])
                    cp(offf[:], PS[0][0:16, 0:1], [PSB[0]], [b_s])
                    tt(endf[:], offf[:], slf[:, 0:1], ALU.add, [b_s], [b_s])
                    ts(offf[:], offf[:], float(RS), None, ALU.mult, None, [b_s], [b_s])
                    tt(inc[:], inc[:], maskT[:], ALU.subtract, [b_s, b_mT], [b_s])
                    ts(inc[:], inc[:], offf[:, 0:1], None, ALU.add, None, [b_s], [b_s])

                    def f(e):
                        for tk in range(32):
                            ins = e.transpose(PS[1][:, tk * 16:(tk + 1) * 16], inc[:, tk * 128:(tk + 1) * 128], ident[0:16, 0:16])
                        return ins
                    OP("pe", f, [b_s, b_ident], [PSB[1]])
                    cp(pos_all[:].rearrange("p k e -> p (k e)"), PS[1][:, :], [PSB[1]], [b_s])
                    RD2 = lambda o, i_: OP("dve", lambda e: e.tensor_reduce(out=o, in_=i_, axis=mybir.AxisListType.X, op=ALU.add), [b_s, b_all], [b_s, b_rw])
                    tt(tmp3[:], eq1_all[:], pos_all[:], ALU.mult, [b_s, b_all], [b_s])
                    RD2(rf[:], tmp3[:])
                    cp(r1i[:], rf[:], [b_s], [b_rw])
                    tt(tmp3[:], eq1_all[:], comb_all[:], ALU.mult, [b_s, b_all], [b_s])
                    RD2(w1s[:], tmp3[:])
                    tt(eq1_all[:], msk_all[:], eq1_all[:], ALU.subtract, [b_all], [b_all])
                    tt(tmp3[:], eq1_all[:], pos_all[:], ALU.mult, [b_s, b_all], [b_s])
                    RD2(rf[:], tmp3[:])
                    cp(r2i[:], rf[:], [b_s], [b_rw])
                    tt(tmp3[:], eq1_all[:], comb_all[:], ALU.mult, [b_s, b_all], [b_s])
                    RD2(w2s[:], tmp3[:])
                    ts(cmpt[:], iota[0:16, 0:NSLOT], endf[:, 0:1], None, ALU.is_ge, None, [b_iota, b_s], [b_s])
                    OP("pe", lambda e: e.matmul(PS[2][:, 0:NSLOT], ones_f[0:16, :], cmpt[:], start=True, stop=True), [b_s, b_onesf], [PSB[2]])
                    ts(sef[:], PS[2][:, 0:NSLOT], 15.0, float(l * NE), ALU.min, ALU.add, [PSB[2]], [b_s])
                    tt(pit[:], ident[:], iota[:, 0:128], ALU.mult, [b_ident, b_iota], [b_s])
                    OP("dve", lambda e: e.tensor_reduce(out=pidx[:], in_=pit[:], axis=mybir.AxisListType.X, op=ALU.add), [b_s], [b_s])
                    stt(sef[:], sef[:], 128.0, pidx[:, 0:1].to_broadcast([128, NSLOT]), ALU.mult, ALU.add, [b_s], [b_s])
                    cp(widx[:], sef[:], [b_s], [b_slot])
                    fw.barrier()
                with ExitStack() as st:
                    hrow = [sb(f"hrow{i}", [128, D], F32, st) for i in range(3)]; hrowb = [Buf() for _ in range(3)]
                    for tk in range(32):
                        i = tk % 3
                        fw.dma("sp", hrow[i][:], h2_d[tk * 128:(tk + 1) * 128, :], writes=[hrowb[i]])
                        for ri in (r1i, r2i):
                            fw.dma_ind(Xs_d[:, :], bass.IndirectOffsetOnAxis(ap=ri[:, tk:tk + 1], axis=0), hrow[i][:], None,
                                       reads=[hrowb[i], b_rw])
                    fw.barrier()
                with ExitStack() as st:
                    wg = [sb(f"wg{i}", [128, 8, DE], BF16, st) for i in range(2)]; wgb = [Buf() for _ in range(2)]
                    wu = [sb(f"wu{i}", [128, 8, DE], BF16, st) for i in range(2)]; wub = [Buf() for _ in range(2)]
                    wd = [sb(f"wd{i}", [128, 4, D], BF16, st) for i in range(2)]; wdb = [Buf() for _ in range(2)]
                    xs = [sb(f"xs{i}", [128, D], F32, st) for i in range(3)]; xsb = [Buf() for _ in range(3)]
                    xsT = [sb(f"xsT{i}", [128, 8, 128], BF16, st) for i in range(2)]; xsTb = [Buf() for _ in range(2)]
                    sg = [sb(f"sg{i}", [128, DE], F32, st) for i in range(2)]; sgb = [Buf() for _ in range(2)]
                    hd = [sb(f"hd{i}", [128, DE], F32, st) for i in range(2)]; hdb = [Buf() for _ in range(2)]
                    hdT = [sb(f"hdT{i}", [128, 4, 128], BF16, st) for i in range(2)]; hdTb = [Buf() for _ in range(2)]
                    yt = [sb(f"yt{i}", [128, D], F32, st) for i in range(2)]; ytb = [Buf() for _ in range(2)]

                    wg_rows = wg_d.rearrange("e p n -> (e p) n"); wu_rows = wu_d.rearrange("e p n -> (e p) n"); wd_rows = wd_d.rearrange("e p n -> (e p) n")

                    def load_gu(s_):
                        i = s_ % 2
                        off = bass.IndirectOffsetOnAxis(ap=widx[:, s_:s_ + 1], axis=0)
                        fw.dma_ind(wg[i][:].rearrange("p k n -> p (k n)"), None, wg_rows, off, reads=[b_slot], writes=[wgb[i]])
                        fw.dma_ind(wu[i][:].rearrange("p k n -> p (k n)"), None, wu_rows, off, reads=[b_slot], writes=[wub[i]])

                    def load_d(s_):
                        i = s_ % 2
                        off = bass.IndirectOffsetOnAxis(ap=widx[:, s_:s_ + 1], axis=0)
                        fw.dma_ind(wd[i][:].rearrange("p k n -> p (k n)"), None, wd_rows, off, reads=[b_slot], writes=[wdb[i]])

                    def load_x(u_):
                        fw.dma("sp", xs[u_ % 3][:], Xs_d[u_ * 128:(u_ + 1) * 128, :], writes=[xsb[u_ % 3]])

                    def st_T(u_):
                        i = u_ % 2
                        x3 = u_ % 3
                        for half in range(2):
                            def f(e, half=half):
                                for c4 in range(4):
                                    c = half * 4 + c4
                                    ins = e.transpose(PS[half][:, c4 * 128:(c4 + 1) * 128], xs[x3][:, c * 128:(c + 1) * 128], ident[:])
                                return ins
                            OP("pe", f, [xsb[x3], b_ident], [PSB[half]])
                            cp(xsT[i][:, half * 4:(half + 1) * 4, :], PS[half][:, :].rearrange("p (c t) -> p c t", c=4), [PSB[half]], [xsTb[i]],
                               eng=("act" if half == 0 else "dve"))

                    def st_GU(u_):
                        i = u_ % 2
                        wi = (u_ // SUB) % 2
                        for (pp, w_, wb__) in ((2, wg[wi], wgb[wi]), (3, wu[wi], wub[wi])):
                            def f(e, pp=pp, w_=w_):
                                for kc in range(8):
                                    ins = e.matmul(PS[pp][:, :], xsT[i][:, kc, :], w_[:, kc, :], start=(kc == 0), stop=(kc == 7))
                                return ins
                            OP("pe", f, [xsTb[i], wb__], [PSB[pp]])
                        act(sg[i][:], PS[2][:, :], AF.Silu, [PSB[2]], [sgb[i]])
                        tt(hd[i][:], PS[3][:, :], sg[i][:], ALU.mult, [PSB[3], sgb[i]], [hdb[i]])

                    def st_HT(u_):
                        i = u_ % 2

                        def f(e):
                            for c4 in range(4):
                                ins = e.transpose(PS[4][:, c4 * 128:(c4 + 1) * 128], hd[i][:, c4 * 128:(c4 + 1) * 128], ident[:])
                            return ins
                        OP("pe", f, [hdb[i], b_ident], [PSB[4]])
                        cp(hdT[i][:], PS[4][:, :].rearrange("p (c t) -> p c t", c=4), [PSB[4]], [hdTb[i]], eng="act")

                    def st_D(u_):
                        i = u_ % 2
                        wi = (u_ // SUB) % 2
                        for half in range(2):
                            ps = 5 + half

                            def f(e, half=half, ps=ps):
                                for kc in range(4):
                                    ins = e.matmul(PS[ps][:, :], hdT[i][:, kc, :], wd[wi][:, kc, half * 512:(half + 1) * 512], start=(kc == 0), stop=(kc == 3))
                                return ins
                            OP("pe", f, [hdTb[i], wdb[wi]], [PSB[ps]])
                            cp(yt[i][:, half * 512:(half + 1) * 512], PS[ps][:, :], [PSB[ps]], [ytb[i]], eng=("dve" if half == 0 else "act"))
                        fw.dma("sp", Ys_d[u_ * 128:(u_ + 1) * 128, :], yt[i][:], reads=[ytb[i]])

                    NU = SUB * NSLOT
                    for s_ in range(2):
                        load_gu(s_)
                        load_d(s_)
                    for u_ in range(3):
                        load_x(u_)
                    for step in range(NU + 3):
                        if step < NU:
                            st_T(step)
                            if step + 3 < NU:
                                load_x(step + 3)
                        if 0 <= step - 1 < NU:
                            u_ = step - 1
                            st_GU(u_)
                            if u_ % SUB == SUB - 1 and u_ // SUB + 2 < NSLOT:
                                load_gu(u_ // SUB + 2)
                        if 0 <= step - 2 < NU:
                            st_HT(step - 2)
                        if 0 <= step - 3 < NU:
                            u_ = step - 3
                            st_D(u_)
                            if u_ % SUB == SUB - 1 and u_ // SUB + 2 < NSLOT:
                                load_d(u_ // SUB + 2)
                    fw.barrier()
                with ExitStack() as st:
                    y1 = [sb(f"y1_{i}", [128, D], F32, st) for i in range(2)]; y2 = [sb(f"y2_{i}", [128, D], F32, st) for i in range(2)]
                    y1b = [Buf() for _ in range(2)]; y2b = [Buf() for _ in range(2)]
                    ac = [sb(f"ac{i}", [128, D], F32, st) for i in range(2)]; acb = [Buf() for _ in range(2)]
                    xm = [sb(f"xm{i}", [128, 8, 128], F32, st) for i in range(2)]; xmb = [Buf() for _ in range(2)]
                    otile = [sb(f"otile{i}", [128, D], F32, st) for i in range(2)]; otb = [Buf() for _ in range(2)]
                    def issue5(tk):
                        i = tk % 2
                        fw.dma_ind(y1[i][:], None, Ys_d[:, :], bass.IndirectOffsetOnAxis(ap=r1i[:, tk:tk + 1], axis=0), reads=[b_rw], writes=[y1b[i]])
                        fw.dma_ind(y2[i][:], None, Ys_d[:, :], bass.IndirectOffsetOnAxis(ap=r2i[:, tk:tk + 1], axis=0), reads=[b_rw], writes=[y2b[i]])
                        fw.dma("sp", xm[i][:], xT_d[:, :, tk * 128:(tk + 1) * 128].rearrange("c p t -> p c t"), writes=[xmb[i]])
                    issue5(0)
                    for tk in range(32):
                        i = tk % 2
                        if tk + 1 < 32:
                            issue5(tk + 1)
                        ts(ac[i][:], y1[i][:], w1s[:, tk:tk + 1], None, ALU.mult, None, [y1b[i], b_rw], [acb[i]])
                        stt(ac[i][:], y2[i][:], w2s[:, tk:tk + 1], ac[i][:], ALU.mult, ALU.add, [y2b[i], b_rw, acb[i]], [acb[i]])
                        for half in range(2):
                            ps = 2 * (tk % 2) + half

                            def f(e, half=half, ps=ps):
                                for c4 in range(4):
                                    c = half * 4 + c4
                                    ins = e.transpose(PS[ps][:, c4 * 128:(c4 + 1) * 128], ac[i][:, c * 128:(c + 1) * 128], ident[:])
                                return ins
                            OP("pe", f, [acb[i], b_ident], [PSB[ps]])
                            for c4 in range(4):
                                c = half * 4 + c4
                                stt(xm[i][:, c, :], PS[ps][:, c4 * 128:(c4 + 1) * 128], G2[:, c:c + 1], xm[i][:, c, :], ALU.mult, ALU.add,
                                    [PSB[ps], b_mod, xmb[i]], [xmb[i]])
                        if not last:
                            fw.dma("sp", xT_d[:, :, tk * 128:(tk + 1) * 128].rearrange("c p t -> p c t"), xm[i][:], reads=[xmb[i]])
                        else:
                            for half in range(2):
                                ps = 4 + 2 * (tk % 2) + half

                                def f(e, half=half, ps=ps):
                                    for c4 in range(4):
                                        c = half * 4 + c4
                                        ins = e.transpose(PS[ps][:, c4 * 128:(c4 + 1) * 128], xm[i][:, c, :], ident[:])
                                    return ins
                                OP("pe", f, [xmb[i], b_ident], [PSB[ps]])
                                cp(otile[i][:, half * 512:(half + 1) * 512], PS[ps][:, :], [PSB[ps]], [otb[i]], eng=("act" if half == 0 else "dve"))
                            fw.dma("sp", out_d[tk * 128:(tk + 1) * 128, :], otile[i][:], reads=[otb[i]])
                    fw.barrier()
    fw.barrier()
    return nc, fw, dbg


def host_inputs(inp, b):
    f = np.float32
    A = np.ascontiguousarray
    m = {}
    m["x"] = A(inp["x"][b])
    m["c_fm"] = A(inp["c"][b].reshape(8, 128).T)
    m["ada_w"] = inp["ada_w"]
    m["ada_b_fm"] = A(inp["ada_b"].reshape(2, 48, 128).transpose(0, 2, 1))
    m["n1g"] = A(inp["norm1_g"].reshape(2, 8, 128).transpose(0, 2, 1))
    m["n2g"] = A(inp["norm2_g"].reshape(2, 8, 128).transpose(0, 2, 1))
    m["w_in"] = inp["w_in"]
    m["fb"] = A(inp["forget_b"].reshape(2, 8, 1))
    def gp_lay(a):
        return A(a.reshape(2, 8, 2, 64).transpose(0, 2, 3, 1).reshape(2, 128, 8))
    m["lam_re"] = gp_lay(inp["lam_re"])
    m["lam_im"] = gp_lay(inp["lam_im"])
    m["log_dt"] = gp_lay(np.broadcast_to(inp["log_dt"][:, :, None], (2, 16, 64)))
    m["sb_re"] = A(inp["ssm_b_re"].reshape(2, 8, 2, 64, 16).transpose(0, 2, 3, 1, 4).reshape(2, 128, 8, 16))
    m["sb_im"] = A(inp["ssm_b_im"].reshape(2, 8, 2, 64, 16).transpose(0, 2, 3, 1, 4).reshape(2, 128, 8, 16))
    m["sc_re"] = A(inp["ssm_c_re"].reshape(2, 8, 2, 16, 64).transpose(0, 2, 4, 1, 3).reshape(2, 128, 8, 16))
    m["sc_im"] = A(inp["ssm_c_im"].reshape(2, 8, 2, 16, 64).transpose(0, 2, 4, 1, 3).reshape(2, 128, 8, 16))
    m["ssm_d"] = A(inp["ssm_d"].reshape(2, 2, 128).transpose(0, 2, 1))
    m["glu_w"] = inp["glu_w"]
    m["glu_b"] = A(inp["glu_b"].reshape(2, 2, 128).transpose(0, 2, 1))
    m["qg"] = A(np.tile(inp["q_norm_g"], (1, 2)).reshape(2, 128, 1))
    m["kg"] = A(np.tile(inp["k_norm_g"], (1, 2)).reshape(2, 128, 1))
    m["conv_w"] = A(inp["conv_w"].reshape(2, 3, 2, 128).transpose(0, 3, 2, 1))
    m["og_fm"] = A(inp["out_norm_g"].reshape(2, 8, 128).transpose(0, 2, 1))
    m["og_at"] = A(inp["out_norm_g"][:, 256:768].reshape(2, 8, 64).transpose(0, 2, 1))
    m["w_out"] = inp["w_out"]
    m["w_router"] = inp["w_router"]
    m["rbias"] = A(np.broadcast_to(inp["router_bias"][None, :], (128, 16)))
    m["w_gate"] = inp["w_gate"]
    m["w_up"] = inp["w_up"]
    m["w_down"] = inp["w_down"]
    m["ident"] = np.eye(128, dtype=f)
    e127 = np.zeros((128, 128), f); e127[127, :] = 1
    m["e127"] = e127
    blk = np.zeros((128, 128), f); blk[:64, :64] = 1; blk[64:, 64:] = 1
    m["blk64"] = blk
    m["tri"] = np.triu(np.ones((128, 128), f))
    m["iota"] = A(np.broadcast_to(np.arange(TT + 1, dtype=f)[None, :], (128, TT + 1)))
    sel = np.zeros((16, 16, 128), f)
    for e in range(16):
        sel[e, e, :] = 1
    m["sel16"] = sel
    return {k: np.asarray(v, dtype=f) for k, v in m.items()}


_CACHE = {}


def kernel(**inputs):
    inp = {k: np.asarray(v) for k, v in inputs.items()}
    if "nc" not in _CACHE:
        _CACHE["nc"] = build_program()[0]
    nc = _CACHE["nc"]
    in_maps = [host_inputs(inp, b) for b in range(8)]
    res = run_bass_kernel_spmd(nc, in_maps, core_ids=list(range(8)))
    out = np.stack([np.asarray(r["out"]) for r in res.results], axis=0)
    return out.astype(np.float32)
```

```python
import numpy as np
from contextlib import ExitStack
import concourse.bass as bass
import concourse.mybir as mybir
from concourse.bass_utils import run_bass_kernel_spmd

F32 = mybir.dt.float32
BF16 = mybir.dt.bfloat16
I32 = mybir.dt.int32
AF = mybir.ActivationFunctionType
ALU = mybir.AluOpType

S = 4096
D = 1024
TT = 512
NT = S // TT
DIN = 2568
NE = 16
DE = 512
EPS = 1e-6
TWO_PI = 6.283185307179586
import os as _os
NOCONV = bool(_os.environ.get('NOCONV'))
POOLENG = _os.environ.get('POOLENG', 'pool')
OFF_U, OFF_Q, OFF_K, OFF_V, OFF_F, OFF_HC, OFF_BG, OFF_CG = 0, 256, 768, 1280, 1792, 1800, 2056, 2312


class Buf:
    __slots__ = ("name", "w", "r")

    def __init__(self, name=""):
        self.name = name
        self.w = {}
        self.r = {}


class Fw:
    ENG = ("pe", "act", "dve", "pool", "sp")

    def __init__(self, nc, ndma=20):
        self.nc = nc
        self.eng = dict(pe=nc.tensor, act=nc.scalar, dve=nc.vector, pool=nc.gpsimd, sp=nc.sync)
        self.sem = {e: nc.alloc_semaphore("sem_" + e) for e in self.ENG}
        self.cnt = {e: 0 for e in self.ENG}
        self.known = {e: {} for e in self.ENG}
        self.dq = {}
        for q in ("sp", "pool"):
            self.dq[q] = dict(sems=[nc.alloc_semaphore(f"dq_{q}_{i}") for i in range(ndma)],
                              uses=[0] * ndma, nxt=0)
        self.allsems = {}
        self.nwaits = 0
        self.bg_on = False
        self.bg_sems = {s_.num for s_ in self.dq["pool"]["sems"]}

    def _wait(self, e, sem, val):
        k = self.known[e]
        if k.get(sem.num, 0) >= val:
            return
        self.eng[e].wait_ge(sem, val)
        self.nwaits += 1
        k[sem.num] = val

    def _deps(self, e, reads, writes):
        need = {}
        mysem = self.sem[e].num

        def add(tok, same_ok):
            sem, val = tok
            if same_ok and sem.num == mysem and e == "pe":
                return
            if need.get(sem.num, (None, 0))[1] < val:
                need[sem.num] = (sem, val)

        for b in reads:
            for tok in b.w.values():
                add(tok, False)
        for b in writes:
            for tok in b.w.values():
                add(tok, True)
            for tok in b.r.values():
                add(tok, True)
        for sem, val in need.values():
            self._wait(e, sem, val)

    def op(self, e, fn, reads=(), writes=()):
        self._deps(e, reads, writes)
        ins = fn(self.eng[e])
        self.cnt[e] += 1
        sem = self.sem[e]
        ins.then_inc(sem, 1)
        tok = (sem, self.cnt[e])
        for b in reads:
            b.r[sem.num] = tok
        for b in writes:
            b.w = {sem.num: tok}
            b.r = {}
        self.allsems[sem.num] = tok
        return tok

    def dma(self, q, out, in_, reads=(), writes=()):
        d = self.dq[q]
        i = d["nxt"]
        d["nxt"] = (i + 1) % len(d["sems"])
        sem = d["sems"][i]
        if d["uses"][i] > 0:
            self._wait(q, sem, 16 * d["uses"][i])
        self._deps(q, reads, writes)
        ins = self.eng[q].dma_start(out=out, in_=in_)
        d["uses"][i] += 1
        tok = (sem, 16 * d["uses"][i])
        ins.then_inc(sem, 16)
        for b in reads:
            b.r[sem.num] = tok
        for b in writes:
            b.w = {sem.num: tok}
            b.r = {}
        self.allsems[sem.num] = tok
        return tok

    def dma_ind(self, out, out_off, in_, in_off, reads=(), writes=(), bounds_check=None):
        q = "pool"
        d = self.dq[q]
        i = d["nxt"]
        d["nxt"] = (i + 1) % len(d["sems"])
        sem = d["sems"][i]
        if d["uses"][i] > 0:
            self._wait(q, sem, 16 * d["uses"][i])
        self._deps(q, reads, writes)
        if bounds_check is None:
            ins = self.eng[q].indirect_dma_start(out=out, out_offset=out_off, in_=in_, in_offset=in_off)
        else:
            ins = self.eng[q].indirect_dma_start(out=out, out_offset=out_off, in_=in_, in_offset=in_off,
                                                 bounds_check=bounds_check, oob_is_err=False)
        d["uses"][i] += 1
        tok = (sem, 16 * d["uses"][i])
        ins.then_inc(sem, 16)
        for b in reads:
            b.r[sem.num] = tok
        for b in writes:
            b.w = {sem.num: tok}
            b.r = {}
        self.allsems[sem.num] = tok
        return tok

    def barrier(self):
        for e in self.ENG:
            for sem, val in list(self.allsems.values()):
                if sem.num == self.sem[e].num:
                    continue
                if self.bg_on and sem.num in self.bg_sems:
                    continue
                self._wait(e, sem, val)


def build_program(nlayers=2, debug=False, stop_after=None):
    nc = bass.Bass("TRN2", target_bir_lowering=False)
    fw = Fw(nc)
    dbg = {}

    def din(name, shape, dt=F32):
        return nc.dram_tensor(name, list(shape), dt, kind="ExternalInput").ap()

    def dscr(name, shape, dt=F32):
        if debug:
            return nc.dram_tensor(name, list(shape), dt, kind="ExternalOutput").ap()
        return nc.dram_tensor(name, list(shape), dt).ap()

    x_in = din("x", [S, D])
    c_fm = din("c_fm", [128, 8])
    ada_w = din("ada_w", [2, D, 6 * D])
    ada_b_fm = din("ada_b_fm", [2, 128, 48])
    n1g = din("n1g", [2, 128, 8])
    n2g = din("n2g", [2, 128, 8])
    w_in = din("w_in", [2, D, DIN])
    fb = din("fb", [2, 8, 1])
    lam_re = din("lam_re", [2, 128, 8])
    lam_im = din("lam_im", [2, 128, 8])
    log_dt = din("log_dt", [2, 128, 8])
    sb_re = din("sb_re", [2, 128, 8, 16])
    sb_im = din("sb_im", [2, 128, 8, 16])
    sc_re = din("sc_re", [2, 128, 8, 16])
    sc_im = din("sc_im", [2, 128, 8, 16])
    ssm_d = din("ssm_d", [2, 128, 2])
    glu_w = din("glu_w", [2, 256, 256])
    glu_b = din("glu_b", [2, 128, 2])
    qg = din("qg", [2, 128, 1])
    kg = din("kg", [2, 128, 1])
    conv_w = din("conv_w", [2, 128, 2, 3])
    og_fm = din("og_fm", [2, 128, 8])
    og_at = din("og_at", [2, 64, 8])
    w_out = din("w_out", [2, D, D])
    w_router = din("w_router", [D, NE])
    rbias = din("rbias", [128, NE])
    w_gate = din("w_gate", [2, NE, D, DE])
    w_up = din("w_up", [2, NE, D, DE])
    w_down = din("w_down", [2, NE, DE, D])
    ident_in = din("ident", [128, 128])
    e127_in = din("e127", [128, 128])
    blk64_in = din("blk64", [128, 128])
    tri_in = din("tri", [128, 128])
    iota_in = din("iota", [128, TT + 1])
    sel_in = din("sel16", [16, NE, 128])
    out_d = nc.dram_tensor("out", [S, D], F32, kind="ExternalOutput").ap()

    xT_d = dscr("xT_d", [8, 128, S])
    wg_d = dscr("wg_d", [2 * NE, 128, 8 * DE], BF16)
    wu_d = dscr("wu_d", [2 * NE, 128, 8 * DE], BF16)
    wd_d = dscr("wd_d", [2 * NE, 128, 4 * D], BF16)
    uT_d = dscr("uT_d", [2, 128, S])
    qT_d = dscr("qT_d", [8, 65, S], BF16)
    kT_d = dscr("kT_d", [8, 65, S], BF16)
    yh_d = dscr("yh_d", [4, 128, S], BF16)
    ya_d = dscr("ya_d", [8, 64, S], BF16)
    h2_d = dscr("h2_d", [S, D])
    Xs_d = dscr("Xs_d", [80 * 128, D])
    Ys_d = dscr("Ys_d", [80 * 128, D])
    if debug:
        for nm, shp, dt in (("dbg_h", [8, 128, S], BF16), ("dbg_cum", [8, S], F32), ("dbg_mod", [128, 48], F32),
                            ("dbg_v", [128, 32, 8, 65], BF16), ("dbg_ssmpre", [2, 128, S], F32),
                            ("dbg_comb", [S, NE], F32), ("dbg_xmid", [8, 128, S], F32)):
            dbg[nm] = nc.dram_tensor(nm, shp, dt, kind="ExternalOutput").ap()

    es = ExitStack()

    uid = [0]

    def sb(name, shape, dt=F32, stack=None):
        uid[0] += 1
        return (stack or es).enter_context(nc.sbuf_tensor(f"s{uid[0]}_{name}", list(shape), dt))

    PS = [es.enter_context(nc.psum_tensor(f"ps{i}", [128, 512], F32)) for i in range(8)]
    PSB = [Buf(f"ps{i}") for i in range(8)]

    ident = sb("ident", [128, 128]); b_ident = Buf()
    e127 = sb("e127", [128, 128]); b_e127 = Buf()
    tri_f = sb("tri_f", [128, 128]); b_tri = Buf()
    blk_f = sb("blk_f", [128, 128]); blk = sb("blk", [128, 128], BF16); b_blk = Buf()
    ones_b = sb("ones_b", [128, 128], BF16); b_ones = Buf()
    ones_f = sb("ones_f", [128, 128]); b_onesf = Buf()
    iota = sb("iota", [128, TT + 1]); b_iota = Buf()
    sel16_f = sb("sel16_f", [16, NE, 128]); b_sel = Buf()
    cfm = sb("cfm", [128, 8]); b_cfm = Buf()
    cact = sb("cact", [128, 8]); b_cact = Buf()
    rb_sb = sb("rb_sb", [128, NE]); b_rb = Buf()
    wr_sb = sb("wr_sb", [128, 8, NE]); b_wr = Buf()

    fw.dma("sp", ident[:], ident_in, writes=[b_ident])
    fw.dma("sp", e127[:], e127_in, writes=[b_e127])
    fw.dma("sp", tri_f[:], tri_in, writes=[b_tri])
    fw.dma("sp", blk_f[:], blk64_in, writes=[b_blk])
    fw.dma("sp", iota[:], iota_in, writes=[b_iota])
    fw.dma("sp", sel16_f[:], sel_in, writes=[b_sel])
    fw.dma("sp", cfm[:], c_fm, writes=[b_cfm])
    fw.dma("sp", rb_sb[:], rbias, writes=[b_rb])
    fw.dma("sp", wr_sb[:], w_router.rearrange("(kc p) n -> p kc n", p=128), writes=[b_wr])
    fw.op("dve", lambda e: e.tensor_copy(out=blk[:], in_=blk_f[:]), reads=[b_blk], writes=[b_blk])
    fw.op("dve", lambda e: e.memset(ones_b[:], 1.0), writes=[b_ones])
    fw.op("dve", lambda e: e.memset(ones_f[:], 1.0), writes=[b_onesf])
    fw.op("act", lambda e: e.activation(out=cact[:], in_=cfm[:], func=AF.Silu), reads=[b_cfm], writes=[b_cact])

    def OP(e, fn, reads=(), writes=()):
        return fw.op(e, fn, reads, writes)

    def act(out, in_, func, reads, writes, scale=1.0, bias=0.0):
        return fw.op("act", lambda e: e.activation(out=out, in_=in_, func=func, bias=bias, scale=scale), reads, writes)

    def tt(out, in0, in1, op, reads, writes, eng="dve"):
        return fw.op(eng, lambda e: e.tensor_tensor(out=out, in0=in0, in1=in1, op=op), reads, writes)

    def ts(out, in0, s1, s2, op0, op1, reads, writes, eng="dve"):
        if op1 is None:
            return fw.op(eng, lambda e: e.tensor_scalar(out=out, in0=in0, scalar1=s1, scalar2=None, op0=op0), reads, writes)
        return fw.op(eng, lambda e: e.tensor_scalar(out=out, in0=in0, scalar1=s1, scalar2=s2, op0=op0, op1=op1), reads, writes)

    def stt(out, in0, scalar, in1, op0, op1, reads, writes):
        return fw.op("dve", lambda e: e.scalar_tensor_tensor(out=out, in0=in0, scalar=scalar, in1=in1, op0=op0, op1=op1),
                     reads, writes)

    def cp(out, in_, reads, writes, eng="dve"):
        if eng == "act":
            return fw.op("act", lambda e: e.copy(out=out, in_=in_), reads, writes)
        return fw.op(eng, lambda e: e.tensor_copy(out=out, in_=in_), reads, writes)

    ntri = sb("ntri", [128, 128], BF16); ident_b = sb("ident_b", [128, 128], BF16); b_ntri = Buf()
    tt(tri_f[:], tri_f[:], ident[:], ALU.subtract, [b_tri, b_ident], [b_tri])
    ts(ntri[:], tri_f[:], -30000.0, None, ALU.mult, None, [b_tri], [b_ntri])
    cp(ident_b[:], ident[:], [b_ident], [b_ntri])
    eps_c = sb("eps_c", [128, 1]); b_eps = Buf()
    OP("dve", lambda e: e.memset(eps_c[:], EPS), writes=[b_eps])

    hn_sq = [sb(f"hn_sq{i}", [128, TT], BF16) for i in range(2)]; hn_sqb = [Buf() for _ in range(2)]
    hn_rt = [sb(f"hn_rt{i}", [128, TT]) for i in range(2)]; hn_rtb = [Buf() for _ in range(2)]
    hn_ctr = [0]
    HN_PS = 7

    def hnorm(src, srcb, g_ap, gb, out, outb, P=128, n=TT):
        i = hn_ctr[0] % 2
        hn_ctr[0] += 1
        act(hn_sq[i][0:P, 0:n], src, AF.Square, [srcb], [hn_sqb[i]])
        OP("pe", lambda e: e.matmul(PS[HN_PS][0:P, 0:n], blk[0:P, 0:P], hn_sq[i][0:P, 0:n], start=True, stop=True),
           [hn_sqb[i], b_blk], [PSB[HN_PS]])
        act(hn_rt[i][0:P, 0:n], PS[HN_PS][0:P, 0:n], AF.Ln, [PSB[HN_PS], b_eps], [hn_rtb[i]], scale=1.0 / 64, bias=eps_c[0:P, :])
        act(hn_rt[i][0:P, 0:n], hn_rt[i][0:P, 0:n], AF.Exp, [hn_rtb[i]], [hn_rtb[i]], scale=-0.5)
        stt(out, src, g_ap, hn_rt[i][0:P, 0:n], ALU.mult, ALU.mult, [srcb, gb, hn_rtb[i]], [outb])

    mods, A1s, A2s, b_mods = [], [], [], []
    for l_ in range(nlayers):
        mods.append(sb(f"mod{l_}", [128, 48])); A1s.append(sb(f"A1_{l_}", [128, 8])); A2s.append(sb(f"A2_{l_}", [128, 8])); b_mods.append(Buf())
    with ExitStack() as st:
        xin = [sb(f"xin{i}", [128, D], F32, st) for i in range(4)]
        xinb = [Buf() for _ in range(4)]
        xo = [sb(f"xo{i}", [128, 8, 128], F32, st) for i in range(4)]
        xob = [Buf() for _ in range(4)]
        n1g_sb = sb("n1g_sb", [128, nlayers, 8], F32, st); n2g_sb = sb("n2g_sb", [128, nlayers, 8], F32, st); b_ng = Buf()
        adab = sb("adab", [128, nlayers, 48], F32, st); b_adab = Buf()
        cact2 = sb("cact2", [128, 8, 2], F32, st); b_cact2 = Buf()
        adw = [sb(f"adw{i}", [128, 8, D], F32, st) for i in range(2)]
        adwb = [Buf() for _ in range(2)]
        for l_ in range(nlayers):
            fw.dma("sp", n1g_sb[:, l_, :], n1g[l_], writes=[b_ng])
            fw.dma("sp", n2g_sb[:, l_, :], n2g[l_], writes=[b_ng])
            fw.dma("sp", adab[:, l_, :], ada_b_fm[l_], writes=[b_adab])
        cp(cact2[:], cact[:].unsqueeze(2).to_broadcast([128, 8, 2]), [b_cact], [b_cact2])

        def ada_units():
            for l_ in range(nlayers):
                for j in range(6):
                    i = (l_ * 6 + j) % 2
                    fw.dma("sp", adw[i][:], ada_w[l_, :, j * D:(j + 1) * D].rearrange("(kc p) n -> p kc n", p=128), writes=[adwb[i]])
                    for c in range(8):
                        def f(e, i=i, j=j, c=c, l_=l_):
                            for kc in range(8):
                                col = 2 * (j * 8 + c)
                                ins = e.matmul(PS[l_][:, col:col + 2], adw[i][:, kc, c * 128:(c + 1) * 128], cact2[:, kc, :],
                                               start=(kc == 0), stop=(kc == 7))
                            return ins
                        OP("pe", f, [adwb[i], b_cact2], [PSB[l_]])
                        yield
        au = ada_units()
        per = (nlayers * 48 + 31) // 32
        for tk in range(S // 128):
            i = tk % 4
            fw.dma("sp", xin[i][:], x_in[tk * 128:(tk + 1) * 128, :], writes=[xinb[i]])
            for _ in range(per):
                next(au, None)
            for half in range(2):
                pb = 2 + (2 * tk + half) % 6

                def f(e, i=i, half=half, pb=pb):
                    for c4 in range(4):
                        c = half * 4 + c4
                        ins = e.transpose(PS[pb][:, c4 * 128:(c4 + 1) * 128], xin[i][:, c * 128:(c + 1) * 128], ident[:])
                    return ins
                OP("pe", f, [xinb[i], b_ident], [PSB[pb]])
                cp(xo[i][:, half * 4:(half + 1) * 4, :], PS[pb][:].rearrange("p (c t) -> p c t", c=4),
                   [PSB[pb]], [xob[i]], eng=("act" if half == 0 else "dve"))
            fw.dma("sp", xT_d[:, :, tk * 128:(tk + 1) * 128].rearrange("c p t -> p c t"), xo[i][:], reads=[xob[i]])
        for _ in au:
            pass
        for l_ in range(nlayers):
            tt(mods[l_][:], PS[l_][:, 0:96].rearrange("p (n two) -> p n two", two=2)[:, :, 0], adab[:, l_, :], ALU.add,
               [PSB[l_], b_adab], [b_mods[l_]])
            stt(A1s[l_][:], mods[l_][:, 8:16], 1.0, n1g_sb[:, l_, :], ALU.add, ALU.mult, [b_mods[l_], b_ng], [b_mods[l_]])
            stt(A2s[l_][:], mods[l_][:, 32:40], 1.0, n2g_sb[:, l_, :], ALU.add, ALU.mult, [b_mods[l_], b_ng], [b_mods[l_]])
        if debug:
            fw.dma("sp", dbg["dbg_mod"], mods[0][:], reads=[b_mods[0]])
        fw.barrier()

    def norm_mod(st_, xt, xtb, A, B, ABb, hb, hbb, xn, xnb, sqb, sqbb, rt, rtb, psn):
        act(sqb[:], xt[:], AF.Square, [xtb], [sqbb])

        def f(e):
            for c in range(8):
                ins = e.matmul(PS[psn][:, :], ones_b[:], sqb[:, c, :], start=(c == 0), stop=(c == 7))
            return ins
        OP("pe", f, [sqbb, b_ones], [PSB[psn]])
        act(rt[:], PS[psn][:, :], AF.Ln, [PSB[psn], b_eps], [rtb], scale=1.0 / D, bias=eps_c[:])
        act(rt[:], rt[:], AF.Exp, [rtb], [rtb], scale=-0.5)
        tt(xn[:], xt[:], rt[:].unsqueeze(1).to_broadcast([128, 8, TT]), ALU.mult, [xtb, rtb], [xnb])
        for c in range(8):
            if c % 2 == 0:
                ts(xn[:, c, :], xn[:, c, :], A[:, c:c + 1], B[:, c:c + 1], ALU.mult, ALU.add, [xnb, ABb], [xnb])
            else:
                act(xn[:, c, :], xn[:, c, :], AF.Identity, [xnb, ABb], [xnb], scale=A[:, c:c + 1], bias=B[:, c:c + 1])
        cp(hb[:, 0:4, :], xn[:, 0:4, :], [xnb], [hbb], eng="dve")
        cp(hb[:, 4:8, :], xn[:, 4:8, :], [xnb], [hbb], eng="act")

    bc_val = {}
    for l in range(nlayers if stop_after != "0" else 0):
        last = (l == nlayers - 1)
        with ExitStack() as lay:
            mod = mods[l]; A1 = A1s[l]; A2 = A2s[l]; b_mod = b_mods[l]
            B1 = mod[:, 0:8]; G1 = mod[:, 16:24]; B2 = mod[:, 24:32]; G2 = mod[:, 40:48]

            with ExitStack() as mix:
                V_sb = sb("V_sb", [128, 32, 8, 65], BF16, mix); b_V = Buf()
                cum = sb("cum", [8, S], F32, mix); b_cum = Buf()
                og_fm_sb = sb("og_fm_sb", [128, 8], F32, mix); og_at_sb = sb("og_at_sb", [64, 8], F32, mix); b_og = Buf()
                fw.dma("sp", og_fm_sb[:], og_fm[l], writes=[b_og])
                fw.dma("sp", og_at_sb[:], og_at[l], writes=[b_og])
                OP("dve", lambda e: e.memset(V_sb[:, :, :, 64:65], 1.0), writes=[b_V])
                if l == 0:
                    cv = [sb(f"cv{i}", [128, 2048], BF16, mix) for i in range(2)]; cvb = [Buf() for _ in range(2)]; cvk = [0]
                fw.barrier()
                with ExitStack() as st:
                    win = sb("win", [128, 8, DIN], BF16, st); b_win = Buf()
                    for hh in range(2):
                        fw.dma("pool", win[:, :, hh * 1284:(hh + 1) * 1284],
                               w_in[l, :, hh * 1284:(hh + 1) * 1284].rearrange("(kc p) n -> p kc n", p=128), writes=[Buf()])
                    fw.barrier()
                    if l == 0 and not NOCONV:
                        for l2 in range(nlayers):
                            for e_ in range(NE):
                                for (src, dst, pat) in ((w_gate, wg_d, 8), (w_up, wu_d, 8), (w_down, wd_d, 4)):
                                    for hf in range(2):
                                        i = cvk[0] % 2
                                        cvk[0] += 1
                                        kcs = pat // 2
                                        srcap = src[l2, e_].rearrange("(kc p) n -> p kc n", p=128)[:, hf * kcs:(hf + 1) * kcs, :]
                                        dstv = cv[i][:].rearrange("p (kc n) -> p kc n", kc=kcs)
                                        fw.dma("pool", dstv, srcap, writes=[cvb[i]])
                                        fw.dma("pool", dst[l2 * NE + e_][:, hf * 2048:(hf + 1) * 2048], cv[i][:], reads=[cvb[i]])
                        fw.bg_on = True
                    fbs = sb("fbs", [8, 1], F32, st); b_fb = Buf()
                    qgs = sb("qgs", [128, 1], F32, st); kgs = sb("kgs", [128, 1], F32, st); b_qk = Buf()
                    cws = sb("cws", [128, 2, 3], F32, st); b_cw = Buf()
                    fw.dma("sp", fbs[:], fb[l], writes=[b_fb])
                    fw.dma("sp", qgs[:], qg[l], writes=[b_qk])
                    fw.dma("sp", kgs[:], kg[l], writes=[b_qk])
                    fw.dma("sp", cws[:], conv_w[l], writes=[b_cw])
                    ts(fbs[:], fbs[:], -1.0, None, ALU.mult, None, [b_fb], [b_fb])
                    ts(qgs[:], qgs[:], 0.125, None, ALU.mult, None, [b_qk], [b_qk])
                    if l == 0:
                        xt = [sb("xt0", [128, 8, TT], F32, st)] * 2; xtb = [Buf()] * 2
                    else:
                        xt = [sb(f"xt{i}", [128, 8, TT], F32, st) for i in range(2)]; xtb = [Buf() for _ in range(2)]
                    sqb = sb("sqb", [128, 8, TT], BF16, st); sqbb = Buf()
                    rt = sb("rt", [128, TT], F32, st); rtb = Buf()
                    xn = sb("xn", [128, 8, TT], F32, st); xnb = Buf()
                    hb = [sb(f"hb{i}", [128, 8, TT], BF16, st) for i in range(2)]; hbb = [Buf() for _ in range(2)]
                    ev = [sb(f"ev{i}", [128, TT], F32, st) for i in range(4)]; evb = [Buf() for _ in range(4)]
                    evo = [sb(f"evo{i}", [128, TT], BF16, st) for i in range(4)]; evob = [Buf() for _ in range(4)]
                    zt = [[sb(f"zt{cc}{i}", [128, TT + 2], F32, st) for i in range(2)] for cc in range(2)]
                    ztb = [[Buf() for _ in range(2)] for _ in range(2)]
                    cy = sb("cy", [128, TT], F32, st); cyb = Buf()
                    fe = sb("fe", [8, TT], F32, st); feb = Buf()
                    evc = [0]
                    gen = [0]
                    for cc in range(2):
                        OP("dve", lambda e, cc=cc: e.memset(zt[cc][1][:, TT:TT + 2], 0.0), writes=[ztb[cc][1]])

                    def proj(ps, off, M, hbt, hbtb):
                        def f(e):
                            for kc in range(8):
                                ins = e.matmul(PS[ps][0:M, :], win[:, kc, off:off + M], hbt[:, kc, :], start=(kc == 0), stop=(kc == 7))
                            return ins
                        OP("pe", f, [hbtb], [PSB[ps]])

                    for it in range(NT):
                        t0 = it * TT
                        i = it % 2
                        fw.dma("sp", xt[i][:], xT_d[:, :, t0:t0 + TT].rearrange("c p t -> p c t"), writes=[xtb[i]])
                        norm_mod(st, xt[i], xtb[i], A1, B1, b_mod, hb[i], hbb[i], xn, xnb, sqb, sqbb, rt, rtb, 0)
                        if debug and l == 0:
                            fw.dma("sp", dbg["dbg_h"][:, :, t0:t0 + TT].rearrange("c p t -> p c t"), hb[i][:], reads=[hbb[i]])
                        for c in range(2):
                            ps = 1 + gen[0] % 2; gen[0] += 1
                            proj(ps, OFF_U + c * 128, 128, hb[i], hbb[i])
                            k = evc[0] % 4; evc[0] += 1
                            cp(ev[k][:], PS[ps][:, :], [PSB[ps]], [evb[k]], eng="act")
                            fw.dma("sp", uT_d[c, :, t0:t0 + TT], ev[k][:], reads=[evb[k]])
                        for (off, gsb, dst) in ((OFF_Q, qgs, qT_d), (OFF_K, kgs, kT_d)):
                            for c in range(4):
                                ps = 1 + gen[0] % 2; gen[0] += 1
                                proj(ps, off + c * 128, 128, hb[i], hbb[i])
                                k = evc[0] % 4; evc[0] += 1
                                cp(ev[k][:], PS[ps][:, :], [PSB[ps]], [evb[k]], eng="act")
                                hnorm(ev[k][:], evb[k], gsb[:, 0:1], b_qk, evo[k][:], evob[k])
                                fw.dma("sp", dst[2 * c, 0:64, t0:t0 + TT], evo[k][0:64, :], reads=[evob[k]])
                                fw.dma("sp", dst[2 * c + 1, 0:64, t0:t0 + TT], evo[k][64:128, :], reads=[evob[k]])
                        ps = 1 + gen[0] % 2; gen[0] += 1
                        proj(ps, OFF_F, 8, hb[i], hbb[i])
                        act(fe[:], PS[ps][0:8, :], AF.Exp, [PSB[ps], b_fb], [feb], scale=-1.0, bias=fbs[:])
                        act(fe[:], fe[:], AF.Ln, [feb], [feb], bias=1.0)
                        init = 0.0 if it == 0 else cum[:, t0 - 1:t0]
                        OP("dve", lambda e, t0=t0, init=init: e.tensor_tensor_scan(
                            out=cum[:, t0:t0 + TT], data0=ones_f[0:8, 0:1].to_broadcast([8, TT]), data1=fe[:], initial=init,
                            op0=ALU.mult, op1=ALU.subtract), [feb, b_onesf, b_cum], [b_cum])
                        for sub in range(4):
                            def f(e, sub=sub, i=i):
                                for kc in range(8):
                                    ins = e.matmul(PS[3][:, :], hb[i][:, kc, sub * 128:(sub + 1) * 128], win[:, kc, OFF_V:OFF_V + 512],
                                                   start=(kc == 0), stop=(kc == 7))
                                return ins
                            OP("pe", f, [hbb[i]], [PSB[3]])
                            cp(V_sb[:, 4 * it + sub, :, 0:64], PS[3][:, :].rearrange("p (h d) -> p h d", h=8), [PSB[3]], [b_V],
                               eng=("act" if sub % 2 else "dve"))
                        for cc in range(2):
                            proj(4, OFF_HC + cc * 128, 128, hb[i], hbb[i])
                            proj(5, OFF_CG + cc * 128, 128, hb[i], hbb[i])
                            proj(6, OFF_BG + cc * 128, 128, hb[i], hbb[i])
                            k = evc[0] % 4; evc[0] += 1
                            z, zb = zt[cc][i], ztb[cc][i]
                            zp, zpb = zt[cc][1 - i], ztb[cc][1 - i]
                            cp(ev[k][:], PS[5][:, :], [PSB[5]], [evb[k]], eng="act")
                            cp(z[:, 0:2], zp[:, TT:TT + 2], [zpb], [zb])
                            tt(z[:, 2:TT + 2], PS[4][:, :], ev[k][:], ALU.mult, [PSB[4], evb[k]], [zb])
                            ts(cy[:], z[:, 2:TT + 2], cws[:, cc, 2:3], None, ALU.mult, None, [zb, b_cw], [cyb])
                            stt(cy[:], z[:, 1:TT + 1], cws[:, cc, 1:2], cy[:], ALU.mult, ALU.add, [zb, b_cw, cyb], [cyb])
                            stt(cy[:], z[:, 0:TT], cws[:, cc, 0:1], cy[:], ALU.mult, ALU.add, [zb, b_cw, cyb], [cyb])
                            tt(ev[k][:], PS[6][:, :], cy[:], ALU.mult, [PSB[6], cyb], [evb[k]])
                            hnorm(ev[k][:], evb[k], og_fm_sb[:, 6 + cc:7 + cc], b_og, evo[k][:], evob[k])
                            fw.dma("sp", yh_d[2 + cc, :, t0:t0 + TT], evo[k][:], reads=[evob[k]])
                    if debug and l == 0:
                        fw.dma("sp", dbg["dbg_cum"], cum[:], reads=[b_cum])
                        fw.dma("sp", dbg["dbg_v"], V_sb[:], reads=[b_V])
                    fw.barrier()
                if stop_after == "A":
                    break
                with ExitStack() as st:
                    def t8(name):
                        return sb(name, [128, 8], F32, st)
                    lre, lim, ldt = t8("lre"), t8("lim"), t8("ldt"); b_p = Buf()
                    fw.dma("sp", lre[:], lam_re[l], writes=[b_p])
                    fw.dma("sp", lim[:], lam_im[l], writes=[b_p])
                    fw.dma("sp", ldt[:], log_dt[l], writes=[b_p])
                    bre = sb("bre", [128, 8, 16], F32, st); bim = sb("bim", [128, 8, 16], F32, st)
                    cre = sb("cre", [128, 8, 16], F32, st); cim = sb("cim", [128, 8, 16], F32, st); b_bc = Buf()
                    fw.dma("sp", bre[:], sb_re[l], writes=[b_bc]); fw.dma("sp", bim[:], sb_im[l], writes=[b_bc])
                    fw.dma("sp", cre[:], sc_re[l], writes=[b_bc]); fw.dma("sp", cim[:], sc_im[l], writes=[b_bc])
                    dsk = sb("dsk", [128, 2], F32, st); glb = sb("glb", [128, 2], F32, st); b_dg = Buf()
                    fw.dma("sp", dsk[:], ssm_d[l], writes=[b_dg]); fw.dma("sp", glb[:], glu_b[l], writes=[b_dg])
                    gluw = sb("gluw", [128, 2, 256], BF16, st); gluwf = sb("gluwf", [128, 2, 256], F32, st); b_gw = Buf()
                    fw.dma("sp", gluwf[:], glu_w[l].rearrange("(kc p) n -> p kc n", p=128), writes=[b_gw])
                    cp(gluw[:], gluwf[:], [b_gw], [b_gw], eng="act")
                    r_sb, th = t8("r_sb"), t8("th")
                    dtv, a_, cs, sn, t1_, t2_, zre, zim = t8("dtv"), t8("a_"), t8("cs"), t8("sn"), t8("t1_"), t8("t2_"), t8("zre"), t8("zim")
                    ti = sb("ti", [128, 8], I32, st)
                    C1 = 6.28125
                    C2 = TWO_PI - C1

                    def sincos(out, ang, shape, tmpf, tmpi, bq, shift):
                        ts(tmpf, ang, 1.0 / TWO_PI, shift / TWO_PI, ALU.mult, ALU.add, [bq], [bq])
                        cp(tmpi, tmpf, [bq], [bq])
                        cp(tmpf, tmpi, [bq], [bq])
                        if shift != 0.0:
                            ts(out, ang, shift, None, ALU.add, None, [bq], [bq])
                            stt(out, tmpf, -C1, out, ALU.mult, ALU.add, [bq], [bq])
                        else:
                            stt(out, tmpf, -C1, ang, ALU.mult, ALU.add, [bq], [bq])
                        stt(out, tmpf, -C2, out, ALU.mult, ALU.add, [bq], [bq])
                        ts(out, out, 3.1415925, -3.1415925, ALU.min, ALU.max, [bq], [bq])
                        act(out, out, AF.Sin, [bq], [bq])

                    ts(lre[:], lre[:], -1e-4, None, ALU.min, None, [b_p], [b_p])
                    act(dtv[:], ldt[:], AF.Exp, [b_p], [b_p])
                    tt(a_[:], lre[:], dtv[:], ALU.mult, [b_p], [b_p])
                    act(r_sb[:], a_[:], AF.Exp, [b_p], [b_p])
                    tt(th[:], lim[:], dtv[:], ALU.mult, [b_p], [b_p])
                    sincos(sn[:], th[:], None, t1_[:], ti[:], b_p, 0.0)
                    sincos(cs[:], th[:], None, t1_[:], ti[:], b_p, 1.5707963267948966)
                    tt(cs[:], cs[:], r_sb[:], ALU.mult, [b_p], [b_p])
                    tt(sn[:], sn[:], r_sb[:], ALU.mult, [b_p], [b_p])
                    ts(cs[:], cs[:], -1.0, None, ALU.add, None, [b_p], [b_p])
                    tt(t1_[:], lre[:], lre[:], ALU.mult, [b_p], [b_p])
                    tt(t2_[:], lim[:], lim[:], ALU.mult, [b_p], [b_p])
                    tt(t1_[:], t1_[:], t2_[:], ALU.add, [b_p], [b_p])
                    OP("dve", lambda e: e.reciprocal(out=t1_[:], in_=t1_[:]), [b_p], [b_p])
                    tt(zre[:], cs[:], lre[:], ALU.mult, [b_p], [b_p])
                    tt(t2_[:], sn[:], lim[:], ALU.mult, [b_p], [b_p])
                    tt(zre[:], zre[:], t2_[:], ALU.add, [b_p], [b_p])
                    tt(zre[:], zre[:], t1_[:], ALU.mult, [b_p], [b_p])
                    tt(zim[:], sn[:], lre[:], ALU.mult, [b_p], [b_p])
                    tt(t2_[:], cs[:], lim[:], ALU.mult, [b_p], [b_p])
                    tt(zim[:], zim[:], t2_[:], ALU.subtract, [b_p], [b_p])
                    tt(zim[:], zim[:], t1_[:], ALU.mult, [b_p], [b_p])
                    bbr = sb("bbr", [128, 8, 16], F32, st); bbi = sb("bbi", [128, 8, 16], F32, st); tb = sb("tb", [128, 8, 16], F32, st)
                    zre_b = zre[:].unsqueeze(2).to_broadcast([128, 8, 16]); zim_b = zim[:].unsqueeze(2).to_broadcast([128, 8, 16])
                    tt(bbr[:], bre[:], zre_b, ALU.mult, [b_p, b_bc], [b_bc])
                    tt(tb[:], bim[:], zim_b, ALU.mult, [b_p, b_bc], [b_bc])
                    tt(bbr[:], bbr[:], tb[:], ALU.subtract, [b_bc], [b_bc])
                    tt(bbi[:], bim[:], zre_b, ALU.mult, [b_p, b_bc], [b_bc])
                    tt(tb[:], bre[:], zim_b, ALU.mult, [b_p, b_bc], [b_bc])
                    tt(bbi[:], bbi[:], tb[:], ALU.add, [b_bc], [b_bc])
                    WT = []
                    for nm, src in (("re", bbr), ("im", bbi)):
                        w1 = sb("w1" + nm, [128, 8, 2, 16], F32, st); bw1 = Buf()
                        OP("dve", lambda e, w1=w1: e.memset(w1[:], 0.0), writes=[bw1])
                        cp(w1[0:64, :, 0, :], src[0:64], [b_bc], [bw1])
                        cp(w1[64:128, :, 1, :], src[64:128], [b_bc], [bw1])
                        wt = sb("wt" + nm, [128, 2, 128], BF16, st); bwt = Buf()
                        w1v = w1[:].rearrange("p g a c -> p (g a c)")
                        for ch in range(2):
                            OP("pe", lambda e, ch=ch, w1v=w1v: e.transpose(PS[0][:, 0:128], w1v[:, ch * 128:(ch + 1) * 128], ident[:]),
                               [bw1, b_ident], [PSB[0]])
                            cp(wt[:, ch, :], PS[0][:, 0:128], [PSB[0]], [bwt])
                        WT.append((wt, bwt))
                    CT = []
                    for nm, src, sgn in (("re", cre, 1.0), ("im", cim, -1.0)):
                        ct = sb("ct" + nm, [128, 8, 2, 16], BF16, st); bct = Buf()
                        OP("dve", lambda e, ct=ct: e.memset(ct[:], 0.0), writes=[bct])
                        ts(ct[0:64, :, 0, :], src[0:64], sgn, None, ALU.mult, None, [b_bc], [bct])
                        ts(ct[64:128, :, 1, :], src[64:128], sgn, None, ALU.mult, None, [b_bc], [bct])
                        CT.append((ct, bct))
                    cosT = sb("cosT", [128, 8, TT + 1], F32, st); sinT = sb("sinT", [128, 8, TT + 1], F32, st); b_tab = Buf()
                    with ExitStack() as st2:
                        ang = sb("ang", [128, 8, TT + 1], F32, st2); tf = sb("tf", [128, 8, TT + 1], F32, st2)
                        tii = sb("tii", [128, 8, TT + 1], I32, st2); b_ang = Buf()
                        for gp in range(8):
                            ts(ang[:, gp, :], iota[:], th[:, gp:gp + 1], None, ALU.mult, None, [b_iota, b_p], [b_ang])
                        sincos(sinT[:], ang[:], None, tf[:], tii[:], b_ang, 0.0)
                        sincos(cosT[:], ang[:], None, tf[:], tii[:], b_ang, 1.5707963267948966)
                        fw.barrier()
                    uf = [sb(f"uf{i}", [128, 2, TT], F32, st) for i in range(2)]; ufb = [Buf() for _ in range(2)]
                    ub = [sb(f"ub{i}", [128, 2, TT], BF16, st) for i in range(2)]; ubb = [Buf() for _ in range(2)]
                    ta = [sb(f"ta{i}", [128, TT], F32, st) for i in range(4)]; tab_ = [Buf() for _ in range(4)]
                    wre = [sb(f"wre{i}", [128, TT], F32, st) for i in range(2)]; wim = [sb(f"wim{i}", [128, TT], F32, st) for i in range(2)]
                    wb_ = [Buf() for _ in range(2)]
                    zr = [sb(f"zr{i}", [128, TT], BF16, st) for i in range(2)]; zi = [sb(f"zi{i}", [128, TT], BF16, st) for i in range(2)]
                    zb_ = [Buf() for _ in range(2)]
                    ini = sb("ini", [128, 8, 2], F32, st); b_ini = [Buf() for _ in range(8)]
                    tiny = sb("tiny", [128, 2], F32, st)
                    yp = sb("yp", [128, 2, TT], F32, st); ypb = [Buf() for _ in range(2)]
                    yg = sb("yg", [128, 2, TT], F32, st); ygb_f = [Buf() for _ in range(2)]
                    ygb = sb("ygb", [128, 2, TT], BF16, st); ygbb = Buf()
                    g1t = sb("g1t", [128, TT], F32, st); g1b = Buf(); g2t = sb("g2t", [128, TT], F32, st); g2b = Buf()
                    yo = [sb(f"yo{i}", [128, TT], F32, st) for i in range(2)]; yob = [Buf() for _ in range(2)]
                    yob16 = [sb(f"yob16{i}", [128, TT], BF16, st) for i in range(2)]; yob16b = [Buf() for _ in range(2)]
                    OP("dve", lambda e: e.memset(ini[:], 0.0), writes=b_ini)
                    k = 0
                    def gen_B():
                        k = 0
                        pend = []

                        def run_due(force=False):
                            keep = []
                            for item in list(pend):
                                item[0] -= 1
                                if force or item[0] <= 0:
                                    nxt = item[1]()
                                    while force and nxt is not None:
                                        nxt = nxt()
                                    if nxt is not None:
                                        keep.append([1, nxt])
                                else:
                                    keep.append(item)
                            pend[:] = keep
                        for it in range(NT):
                            t0 = it * TT
                            i = it % 2
                            fw.dma("sp", uf[i][:], uT_d[:, :, t0:t0 + TT].rearrange("c p t -> p c t"), writes=[ufb[i]])
                            cp(ub[i][:], uf[i][:], [ufb[i]], [ubb[i]], eng="act")
                            for gp in range(8):
                                ch, j = gp // 4, gp % 4
                                pa, pb = 0, 1
                                for (pp, (wt, bwt)) in ((pa, WT[0]), (pb, WT[1])):
                                    OP("pe", lambda e, pp=pp, wt=wt, ch=ch, j=j, i=i: e.matmul(
                                        PS[pp][:, :], wt[32 * j:32 * j + 32, ch, :], ub[i][32 * j:32 * j + 32, ch, :],
                                        start=True, stop=True, tile_position=(32 * j, 0)), [bwt, ubb[i]], [PSB[pp]])
                                run_due()
                                cT = cosT[:, gp, 0:TT]; sT = sinT[:, gp, 0:TT]
                                kk = k % 2; k += 1
                                tt(ta[0][:], PS[pa][:, :], cT, ALU.mult, [PSB[pa], b_tab], [tab_[0]])
                                tt(ta[1][:], PS[pb][:, :], sT, ALU.mult, [PSB[pb], b_tab], [tab_[1]])
                                tt(ta[0][:], ta[0][:], ta[1][:], ALU.add, [tab_[0], tab_[1]], [tab_[0]])
                                tt(ta[2][:], PS[pb][:, :], cT, ALU.mult, [PSB[pb], b_tab], [tab_[2]])
                                tt(ta[3][:], PS[pa][:, :], sT, ALU.mult, [PSB[pa], b_tab], [tab_[3]])
                                tt(ta[2][:], ta[2][:], ta[3][:], ALU.subtract, [tab_[2], tab_[3]], [tab_[2]])
                                rb = r_sb[:, gp:gp + 1].to_broadcast([128, TT])
                                OP("dve", lambda e, kk=kk, rb=rb, gp=gp: e.tensor_tensor_scan(
                                    out=wre[kk][:], data0=rb, data1=ta[0][:], initial=ini[:, gp, 0:1], op0=ALU.mult, op1=ALU.add),
                                    [tab_[0], b_p, b_ini[gp]], [wb_[kk]])
                                OP("dve", lambda e, kk=kk, rb=rb, gp=gp: e.tensor_tensor_scan(
                                    out=wim[kk][:], data0=rb, data1=ta[2][:], initial=ini[:, gp, 1:2], op0=ALU.mult, op1=ALU.add),
                                    [tab_[2], b_p, b_ini[gp]], [wb_[kk]])
                                tt(ta[0][:], wre[kk][:], cT, ALU.mult, [wb_[kk], b_tab], [tab_[0]])
                                tt(ta[1][:], wim[kk][:], sT, ALU.mult, [wb_[kk], b_tab], [tab_[1]])
                                tt(zr[kk][:], ta[0][:], ta[1][:], ALU.subtract, [tab_[0], tab_[1]], [zb_[kk]])
                                tt(ta[2][:], wre[kk][:], sT, ALU.mult, [wb_[kk], b_tab], [tab_[2]])
                                tt(ta[3][:], wim[kk][:], cT, ALU.mult, [wb_[kk], b_tab], [tab_[3]])
                                tt(zi[kk][:], ta[2][:], ta[3][:], ALU.add, [tab_[2], tab_[3]], [zb_[kk]])
                                cL = cosT[:, gp, TT:TT + 1]; sL = sinT[:, gp, TT:TT + 1]
                                ts(tiny[:, 0:1], wim[kk][:, TT - 1:TT], sL, None, ALU.mult, None, [wb_[kk], b_tab], [b_ini[gp]])
                                ts(tiny[:, 1:2], wim[kk][:, TT - 1:TT], cL, None, ALU.mult, None, [wb_[kk], b_tab], [b_ini[gp]])
                                stt(ini[:, gp, 0:1], wre[kk][:, TT - 1:TT], cL, tiny[:, 0:1], ALU.mult, ALU.subtract, [wb_[kk], b_tab, b_ini[gp]], [b_ini[gp]])
                                stt(ini[:, gp, 1:2], wre[kk][:, TT - 1:TT], sL, tiny[:, 1:2], ALU.mult, ALU.add, [wb_[kk], b_tab, b_ini[gp]], [b_ini[gp]])
                                py = 2

                                def tail(gp=gp, j=j, kk=kk, py=py, ch=ch, i=i, t0=t0):
                                  def f(e):
                                    e.matmul(PS[py][32 * j:32 * j + 32, :], CT[0][0][:, gp, :, :].rearrange("p a c -> p (a c)"), zr[kk][:],
                                             start=True, stop=False, tile_position=(0, 32 * j))
                                    return e.matmul(PS[py][32 * j:32 * j + 32, :], CT[1][0][:, gp, :, :].rearrange("p a c -> p (a c)"), zi[kk][:],
                                                    start=False, stop=True, tile_position=(0, 32 * j))
                                  OP("pe", f, [zb_[kk], CT[0][1], CT[1][1]], [PSB[py]])
                                  if j == 3:
                                    stt(yp[:, ch, :], uf[i][:, ch, :], dsk[:, ch:ch + 1], PS[py][:, :], ALU.mult, ALU.add,
                                        [ufb[i], b_dg, PSB[py]], [ypb[ch]])
                                    if debug and l == 0:
                                        fw.dma("sp", dbg["dbg_ssmpre"][ch, :, t0:t0 + TT], yp[:, ch, :], reads=[ypb[ch]])
                                    tt(g1t[:], yp[:, ch, :], yp[:, ch, :], ALU.mult, [ypb[ch]], [g1b])
                                    ts(g1t[:], g1t[:], 0.044715, 1.0, ALU.mult, ALU.add, [g1b], [g1b])
                                    tt(g1t[:], g1t[:], yp[:, ch, :], ALU.mult, [g1b, ypb[ch]], [g1b])

                                    def tailB():
                                        act(g1t[:], g1t[:], AF.Sigmoid, [g1b], [g1b], scale=1.5957691216057308)

                                        def tailC():
                                            tt(yg[:, ch, :], yp[:, ch, :], g1t[:], ALU.mult, [g1b, ypb[ch]], [ygb_f[ch]])
                                            cp(ygb[:, ch, :], yg[:, ch, :], [ygb_f[ch]], [ygbb])
                                            return None
                                        return tailC
                                    return tailB
                                  return None
                                pend.append([1, tail])
                                yield
                            def glu_block(t0=t0):
                                for mc in range(2):
                                    def f(e, mc=mc):
                                        e.matmul(PS[7][:, :], gluw[:, 0, mc * 128:(mc + 1) * 128], ygb[:, 0, :], start=True, stop=False)
                                        return e.matmul(PS[7][:, :], gluw[:, 1, mc * 128:(mc + 1) * 128], ygb[:, 1, :], start=False, stop=True)
                                    OP("pe", f, [ygbb, b_gw], [PSB[7]])
                                    act(g2t[:], PS[7][:, :], AF.Sigmoid, [PSB[7], b_dg], [g2b], bias=glb[:, mc:mc + 1])
                                    tt(yo[mc][:], yg[:, mc, :], g2t[:], ALU.mult, [g2b, ygb_f[mc]], [yob[mc]])
                                    hnorm(yo[mc][:], yob[mc], og_fm_sb[:, mc:mc + 1], b_og, yob16[mc][:], yob16b[mc])
                                    fw.dma("sp", yh_d[mc, :, t0:t0 + TT], yob16[mc][:], reads=[yob16b[mc]])
                                return None
                            pend.append([4, glu_block])
                            yield
                        run_due(force=True)
                        yield
                    ckT = sb("ckT", [128, 32, 8], F32, st); cref = sb("cref", [128, 32, 8], F32, st); b_ck = Buf()
                    st3 = ExitStack()
                    ce = sb("ce", [8, 32], F32, st3); dq = sb("dq", [8, 8, 4], F32, st3); b_ce = Buf()
                    dqrow = sb("dqrow", [8, 32, 128], BF16, st3); onesrow = sb("onesrow", [8, S], BF16, st3); b_row = Buf()
                    cp(ce[:], cum[:].rearrange("h (s j) -> h s j", j=128)[:, :, 127], [b_cum], [b_ce])
                    cev = ce[:].rearrange("h (q s) -> h q s", s=4)
                    tt(dq[:], cev, cev[:, :, 3:4].to_broadcast([8, 8, 4]), ALU.subtract, [b_ce], [b_ce])
                    cp(dqrow[:], dq[:].rearrange("h q s -> h (q s)").unsqueeze(2).to_broadcast([8, 32, 128]), [b_ce], [b_row])
                    OP("dve", lambda e: e.memset(onesrow[:], 1.0), writes=[b_row])
                    fw.dma("sp", qT_d[:, 64, :], dqrow[:].rearrange("h s j -> h (s j)"), reads=[b_row])
                    fw.dma("sp", kT_d[:, 64, :], onesrow[:], reads=[b_row])

                    def f(e):
                        for kt in range(32):
                            ins = e.transpose(PS[0][:, kt * 8:(kt + 1) * 8], cum[0:8, kt * 128:(kt + 1) * 128], ident[0:8, 0:8])
                        return ins
                    OP("pe", f, [b_cum, b_ident], [PSB[0]])
                    cp(ckT[:].rearrange("p k h -> p (k h)"), PS[0][:, 0:256], [PSB[0]], [b_ck])
                    OP("pe", lambda e: e.matmul(PS[1][:, 0:256], e127[:], ckT[:].rearrange("p k h -> p (k h)"), start=True, stop=True),
                       [b_ck, b_e127], [PSB[1]])
                    cp(cref[:].rearrange("p k h -> p (k h)"), PS[1][:, 0:256], [PSB[1]], [b_ck])
                    fw.barrier()
                    st3.close()
                    qa = [sb("qa0", [65, S], BF16, st)]; ka = [sb("ka0", [65, S], BF16, st)]
                    qab = [Buf()]; kab = [Buf()]
                    NP = 6
                    pT = [sb(f"pT{i}", [128, TT], BF16, st) for i in range(NP)]; pTb = [Buf() for _ in range(NP)]
                    biasT = [sb(f"biasT{i}", [128, 32], F32, st) for i in range(2)]; biasb = [Buf() for _ in range(2)]
                    osb = [sb(f"osb{i}", [65, TT], F32, st) for i in range(2)]; osbb = [Buf() for _ in range(2)]
                    yat = [sb(f"yat{i}", [64, TT], F32, st) for i in range(2)]; yatb = [Buf() for _ in range(2)]
                    yab = [sb(f"yab{i}", [64, TT], BF16, st) for i in range(2)] ; yabb = [Buf() for _ in range(2)]
                    def emit_bias(u_):
                        h_, qt_ = u_ // 8, u_ % 8
                        n_ = 4 * qt_ + 4
                        ts(biasT[u_ % 2][:, 0:n_], ckT[:, 0:n_, h_], cref[:, 4 * qt_ + 3, h_:h_ + 1], -1.0, ALU.subtract, ALU.mult,
                           [b_ck], [biasb[u_ % 2]])

                    def gen_C():
                        blkctr = 0
                        pend2 = pend3 = None
                        for h in range(8):
                            hi = 0
                            fw.dma("sp", qa[hi][:], qT_d[h], writes=[qab[hi]])
                            fw.dma("sp", ka[hi][:], kT_d[h], writes=[kab[hi]])
                            for qt in range(8):
                                nkt = 4 * qt + 4
                                bi = (h * 8 + qt) % 2
                                oi = bi
                                po = 6
                                if h * 8 + qt == 0:
                                    emit_bias(0)
                                if h * 8 + qt + 1 < 64:
                                    emit_bias(h * 8 + qt + 1)

                                SL = (3, 4, 5)
                                LA = 2

                                def s_mm(kt):
                                    slot = SL[(blkctr + kt) % 3]
                                    m = kt - 4 * qt
                                    c0 = 128 * m if m > 0 else 0
                                    def f(e):
                                        ins = e.matmul(PS[slot][:, c0:TT], ka[hi][:, kt * 128:(kt + 1) * 128],
                                                       qa[hi][:, qt * TT + c0:(qt + 1) * TT], start=True, stop=(m < 0))
                                        if m >= 0:
                                            ins = e.matmul(PS[slot][:, c0:c0 + 128], ntri[:], ident_b[:], start=False, stop=True)
                                        return ins
                                    OP("pe", f, [kab[hi], qab[hi], b_ntri], [PSB[slot]])
                                for kt in range(min(LA, nkt)):
                                    s_mm(kt)
                                for kt in range(nkt):
                                    slot = SL[(blkctr + kt) % 3]
                                    if kt + LA < nkt:
                                        s_mm(kt + LA)
                                    m = kt - 4 * qt
                                    c0 = 128 * m if m > 0 else 0
                                    pi = (blkctr + kt) % NP
                                    act(pT[pi][:, c0:TT], PS[slot][:, c0:TT], AF.Exp, [PSB[slot], biasb[bi]], [pTb[pi]],
                                        bias=biasT[bi][:, kt:kt + 1])
                                    OP("pe", lambda e, kt=kt, c0=c0, pi=pi: e.matmul(
                                        PS[po][0:65, c0:TT], V_sb[:, kt, h, :], pT[pi][:, c0:TT], start=(kt == 0), stop=(kt == nkt - 1)),
                                        [pTb[pi], b_V], [PSB[po]])
                                    if kt % 8 == 7 and kt + 1 < nkt:
                                        yield 8
                                blkctr += nkt
                                cp(osb[oi][:], PS[po][0:65, :], [PSB[po]], [osbb[oi]], eng="act")
                                OP("dve", lambda e, oi=oi: e.reciprocal(out=osb[oi][64:65, :], in_=osb[oi][64:65, :]), [osbb[oi]], [osbb[oi]])

                                def phase2(oi=oi, h=h, qt=qt):
                                    OP("pe", lambda e: e.matmul(PS[7][0:64, :], ones_f[64:65, 0:64], osb[oi][64:65, :], start=True, stop=True),
                                       [osbb[oi], b_onesf], [PSB[7]])
                                    tt(yat[oi][:], osb[oi][0:64, :], PS[7][0:64, :], ALU.mult, [osbb[oi], PSB[7]], [yatb[oi]])

                                    def phase3():
                                        hnorm(yat[oi][:], yatb[oi], og_at_sb[:, h:h + 1], b_og, yab[oi][:], yabb[oi], P=64)
                                        fw.dma("sp", ya_d[h, :, qt * TT:(qt + 1) * TT], yab[oi][:], reads=[yabb[oi]])
                                    return phase3
                                if pend3 is not None:
                                    pend3()
                                pend3 = pend2() if pend2 is not None else None
                                pend2 = phase2
                                yield ((nkt - 1) % 8) + 1
                        if pend3 is not None:
                            pend3()
                        if pend2 is not None:
                            pend2()()
                    gB, gC = gen_B(), gen_C()
                    aliveB = aliveC = True
                    cdone, bdone = 0, 0
                    CTOT, BTOT = 8 * sum(4 * q_ + 4 for q_ in range(8)), NT * 9
                    while aliveB or aliveC:
                        if aliveC:
                            try:
                                cdone += next(gC)
                            except StopIteration:
                                aliveC = False
                        while aliveB and (not aliveC or bdone * CTOT <= cdone * BTOT):
                            try:
                                next(gB)
                                bdone += 1
                            except StopIteration:
                                aliveB = False
                    fw.bg_on = False
                    fw.barrier()
            if stop_after == "C":
                break
            NSLOT = 80
            RS = 128
            SUB = RS // 128
            BIG = 1.0e4
            with ExitStack() as dl:
                msk_all = sb("msk_all", [128, 32, 16], F32, dl); eq1_all = sb("eq1_all", [128, 32, 16], F32, dl)
                comb_all = sb("comb_all", [128, 32, 16], F32, dl); b_all = Buf()
                r1i = sb("r1i", [128, 32], I32, dl); r2i = sb("r2i", [128, 32], I32, dl)
                w1s = sb("w1s", [128, 32], F32, dl); w2s = sb("w2s", [128, 32], F32, dl); b_rw = Buf()
                widx = sb("widx", [128, NSLOT], I32, dl); b_slot = Buf()
                with ExitStack() as st:
                    maskT = sb("maskT", [16, S], F32, dl); b_mT = Buf()
                    woa = sb("woa", [128, 4, D], BF16, st); wob = sb("wob", [64, 8, D], BF16, st)
                    fw.dma("pool", woa[:, 0:2, :], w_out[l, 0:256, :].rearrange("(kc p) n -> p kc n", p=128), writes=[Buf()])
                    fw.dma("pool", woa[:, 2:4, :], w_out[l, 768:1024, :].rearrange("(kc p) n -> p kc n", p=128), writes=[Buf()])
                    fw.dma("pool", wob[:], w_out[l, 256:768, :].rearrange("(h p) n -> p h n", p=64), writes=[Buf()])
                    fw.barrier()
                    zrow = sb("zrow", [128, 2048], F32, st); b_z = Buf()
                    OP("dve", lambda e: e.memset(zrow[:], 0.0), writes=[b_z])
                    for c_ in range(NSLOT * RS // 256):
                        fw.dma("pool", Xs_d[c_ * 256:(c_ + 1) * 256, :].rearrange("(p two) n -> p (two n)", two=2), zrow[:], reads=[b_z])
                    xt = sb("xtD", [128, 8, TT], F32, st); xtb = Buf()
                    ys = sb("ys", [128, 4, TT], BF16, st); ysb = Buf()
                    yatt = sb("yatt", [64, 8, TT], BF16, st); yattb = Buf()
                    sqb = sb("sqbD", [128, 8, TT], BF16, st); sqbb = Buf()
                    rt = sb("rtD", [128, TT], F32, st); rtb = Buf()
                    h2f = sb("h2f", [128, 8, TT], F32, st); h2fb = Buf()
                    h2 = sb("h2", [128, 8, TT], BF16, st); h2b = Buf()
                    htok = [sb(f"htok{i}", [128, D], F32, st) for i in range(2)]; htokb = [Buf() for _ in range(2)]
                    aff = sb("aff", [128, 4, 16], F32, st); selv = sb("selv", [128, 4, 16], F32, st); rtmp = sb("rtmp", [128, 4, 16], F32, st)
                    m1 = sb("m1", [128, 16], F32, st); m2 = sb("m2", [128, 16], F32, st); gm = sb("gm", [128, 4], F32, st)
                    b_r = Buf()
                    for it in range(NT):
                        t0 = it * TT
                        msk = msk_all[:, 4 * it:4 * it + 4, :]; comb = comb_all[:, 4 * it:4 * it + 4, :]; eq1 = eq1_all[:, 4 * it:4 * it + 4, :]
                        fw.dma("sp", xt[:], xT_d[:, :, t0:t0 + TT].rearrange("c p t -> p c t"), writes=[xtb])
                        fw.dma("sp", ys[:], yh_d[:, :, t0:t0 + TT].rearrange("c p t -> p c t"), writes=[ysb])
                        fw.dma("sp", yatt[:], ya_d[:, :, t0:t0 + TT].rearrange("h p t -> p h t"), writes=[yattb])
                        for mc in range(8):
                            ps = 4 + mc % 2

                            def f(e, mc=mc, ps=ps):
                                for kc in range(4):
                                    e.matmul(PS[ps][:, :], woa[:, kc, mc * 128:(mc + 1) * 128], ys[:, kc, :], start=(kc == 0), stop=False)
                                for hh in range(8):
                                    ins = e.matmul(PS[ps][:, :], wob[:, hh, mc * 128:(mc + 1) * 128], yatt[:, hh, :], start=False, stop=(hh == 7))
                                return ins
                            OP("pe", f, [ysb, yattb], [PSB[ps]])
                            stt(xt[:, mc, :], PS[ps][:, :], G1[:, mc:mc + 1], xt[:, mc, :], ALU.mult, ALU.add, [PSB[ps], b_mod, xtb], [xtb])
                        if debug and l == 0:
                            fw.dma("sp", dbg["dbg_xmid"][:, :, t0:t0 + TT].rearrange("c p t -> p c t"), xt[:], reads=[xtb])
                        fw.dma("sp", xT_d[:, :, t0:t0 + TT].rearrange("c p t -> p c t"), xt[:], reads=[xtb])
                        norm_mod(st, xt, xtb, A2, B2, b_mod, h2, h2b, h2f, h2fb, sqb, sqbb, rt, rtb, 7)
                        for sub in range(4):
                            hi_ = sub % 2
                            for half in range(2):
                                ps = half

                                def f(e, sub=sub, half=half, ps=ps):
                                    for c4 in range(4):
                                        c = half * 4 + c4
                                        ins = e.transpose(PS[ps][:, c4 * 128:(c4 + 1) * 128], h2f[:, c, sub * 128:(sub + 1) * 128], ident[:])
                                    return ins
                                OP("pe", f, [h2fb, b_ident], [PSB[ps]])
                                cp(htok[hi_][:, half * 512:(half + 1) * 512], PS[ps][:, :], [PSB[ps]], [htokb[hi_]],
                                   eng=("act" if half == 0 else "dve"))
                            fw.dma("sp", h2_d[t0 + sub * 128:t0 + (sub + 1) * 128, :], htok[hi_][:], reads=[htokb[hi_]])
                        for sub in range(4):
                            def f(e, sub=sub):
                                for kc in range(8):
                                    ins = e.matmul(PS[6][:, sub * 16:(sub + 1) * 16], h2f[:, kc, sub * 128:(sub + 1) * 128], wr_sb[:, kc, :],
                                                   start=(kc == 0), stop=(kc == 7))
                                return ins
                            OP("pe", f, [h2fb, b_wr], [PSB[6]])
                        act(aff[:].rearrange("p s e -> p (s e)"), PS[6][:, 0:64], AF.Sigmoid, [PSB[6]], [b_r])
                        tt(selv[:], aff[:], rb_sb[:].unsqueeze(1).to_broadcast([128, 4, 16]), ALU.add, [b_r, b_rb], [b_r])
                        s44 = selv[:].rearrange("p s (g e) -> p (s g) e", e=4)
                        r44 = rtmp[:].rearrange("p s (g e) -> p (s g) e", e=4)
                        RD = lambda o, i_, op: OP("dve", lambda e: e.tensor_reduce(out=o, in_=i_, axis=mybir.AxisListType.X, op=op), [b_r, b_all], [b_r, b_all])
                        RD(m1[:], s44, ALU.max)
                        tt(r44, s44, m1[:].unsqueeze(2).to_broadcast([128, 16, 4]), ALU.is_equal, [b_r], [b_r])
                        stt(r44, r44, -BIG, s44, ALU.mult, ALU.add, [b_r], [b_r])
                        RD(m2[:], r44, ALU.max)
                        tt(m1[:], m1[:], m2[:], ALU.add, [b_r], [b_r])
                        gs = m1[:].rearrange("p (s g) -> p s g", g=4)
                        RD(gm[:], gs, ALU.max)
                        m2v = m2[:].rearrange("p (s g) -> p s g", g=4)
                        tt(m2v, gs, gm[:].unsqueeze(2).to_broadcast([128, 4, 4]), ALU.is_equal, [b_r], [b_r])
                        ts(m2[:], m2[:], BIG, -BIG, ALU.mult, ALU.add, [b_r], [b_r])
                        tt(r44, s44, m2[:].unsqueeze(2).to_broadcast([128, 16, 4]), ALU.add, [b_r], [b_r])
                        RD(gm[:], rtmp[:], ALU.max)
                        tt(eq1, rtmp[:], gm[:].unsqueeze(2).to_broadcast([128, 4, 16]), ALU.is_equal, [b_r, b_all], [b_r, b_all])
                        stt(msk, eq1, -BIG, rtmp[:], ALU.mult, ALU.add, [b_r, b_all], [b_r, b_all])
                        RD(gm[:], msk, ALU.max)
                        tt(msk, rtmp[:], gm[:].unsqueeze(2).to_broadcast([128, 4, 16]), ALU.is_ge, [b_r, b_all], [b_r, b_all])
                        tt(comb, aff[:], msk, ALU.mult, [b_r, b_all], [b_r, b_all])
                        RD(gm[:], comb, ALU.add)
                        OP("dve", lambda e: e.reciprocal(out=gm[:], in_=gm[:]), [b_r], [b_r])
                        tt(comb, comb, gm[:].unsqueeze(2).to_broadcast([128, 4, 16]), ALU.mult, [b_r, b_all], [b_r, b_all])
                        if debug and l == 0:
                            fw.dma("sp", dbg["dbg_comb"][t0:t0 + TT, :].rearrange("(s p) e -> p s e", p=128), comb, reads=[b_all])

                        def f(e, it=it):
                            for sub in range(4):
                                ins = e.transpose(PS[6][0:16, sub * 128:(sub + 1) * 128], msk_all[:, 4 * it + sub, :], ident[:])
                            return ins
                        OP("pe", f, [b_all, b_ident], [PSB[6]])
                        cp(maskT[:, t0:t0 + TT], PS[6][0:16, :], [PSB[6]], [b_mT])
                    fw.barrier()
                with ExitStack() as st:
                    inc = sb("inc", [16, S], F32, st); b_s = Buf()
                    cntf = sb("cntf", [16, 2], F32, st); slf = sb("slf", [16, 2], F32, st); offf = sb("offf", [16, 1], F32, st)
                    endf = sb("endf", [16, 1], F32, st); cnti = sb("cnti", [16, 2], I32, st)
                    cmpt = sb("cmpt", [16, NSLOT], F32, st); sef = sb("sef", [128, NSLOT], F32, st); pidx = sb("pidx", [128, 1], F32, st); pit = sb("pit", [128, 128], F32, st)
                    pos_all = sb("pos_all", [128, 32, 16], F32, st); tmp3 = sb("tmp3", [128, 32, 16], F32, st)
                    rf = sb("rf", [128, 32], F32, st)
                    OP("dve", lambda e: e.tensor_tensor_scan(out=inc[:], data0=ones_f[0:16, 0:1].to_broadcast([16, S]), data1=maskT[:],
                                                             initial=0.0, op0=ALU.mult, op1=ALU.add), [b_mT, b_onesf], [b_s])
                    ts(cntf[:], inc[:, S - 1:S].to_broadcast([16, 2]), 1.0 / RS, (RS - 1.0) / RS - (RS - 1.0) / (2 * RS), ALU.mult, ALU.add, [b_s], [b_s])
                    cp(cnti[:], cntf[:], [b_s], [b_s])
                    cp(slf[:], cnti[:], [b_s], [b_s])
                    OP("pe", lambda e: e.matmul(PS[0][0:16, 0:2], tri_f[0:16, 0:16], slf[:], start=True, stop=True), [b_s, b_tri], [PSB[0]])
                    cp(offf[:], PS[0][0:16, 0:1], [PSB[0]], [b_s])
                    tt(endf[:], offf[:], slf[:, 0:1], ALU.add, [b_s], [b_s])
                    ts(offf[:], offf[:], float(RS), None, ALU.mult, None, [b_s], [b_s])
                    tt(inc[:], inc[:], maskT[:], ALU.subtract, [b_s, b_mT], [b_s])
                    ts(inc[:], inc[:], offf[:, 0:1], None, ALU.add, None, [b_s], [b_s])

                    def f(e):
                        for tk in range(32):
                            ins = e.transpose(PS[1][:, tk * 16:(tk + 1) * 16], inc[:, tk * 128:(tk + 1) * 128], ident[0:16, 0:16])
                        return ins
                    OP("pe", f, [b_s, b_ident], [PSB[1]])
                    cp(pos_all[:].rearrange("p k e -> p (k e)"), PS[1][:, :], [PSB[1]], [b_s])
                    RD2 = lambda o, i_: OP("dve", lambda e: e.tensor_reduce(out=o, in_=i_, axis=mybir.AxisListType.X, op=ALU.add), [b_s, b_all], [b_s, b_rw])
                    tt(tmp3[:], eq1_all[:], pos_all[:], ALU.mult, [b_s, b_all], [b_s])
                    RD2(rf[:], tmp3[:])
                    cp(r1i[:], rf[:], [b_s], [b_rw])
                    tt(tmp3[:], eq1_all[:], comb_all[:], ALU.mult, [b_s, b_all], [b_s])
                    RD2(w1s[:], tmp3[:])
                    tt(eq1_all[:], msk_all[:], eq1_all[:], ALU.subtract, [b_all], [b_all])
                    tt(tmp3[:], eq1_all[:], pos_all[:], ALU.mult, [b_s, b_all], [b_s])
                    RD2(rf[:], tmp3[:])
                    cp(r2i[:], rf[:], [b_s], [b_rw])
                    tt(tmp3[:], eq1_all[:], comb_all[:], ALU.mult, [b_s, b_all], [b_s])
                    RD2(w2s[:], tmp3[:])
                    ts(cmpt[:], iota[0:16, 0:NSLOT], endf[:, 0:1], None, ALU.is_ge, None, [b_iota, b_s], [b_s])
                    OP("pe", lambda e: e.matmul(PS[2][:, 0:NSLOT], ones_f[0:16, :], cmpt[:], start=True, stop=True), [b_s, b_onesf], [PSB[2]])
                    ts(sef[:], PS[2][:, 0:NSLOT], 15.0, float(l * NE), ALU.min, ALU.add, [PSB[2]], [b_s])
                    tt(pit[:], ident[:], iota[:, 0:128], ALU.mult, [b_ident, b_iota], [b_s])
                    OP("dve", lambda e: e.tensor_reduce(out=pidx[:], in_=pit[:], axis=mybir.AxisListType.X, op=ALU.add), [b_s], [b_s])
                    sk = sb("sk", [128, NSLOT], F32, st)
                    OP("dve", lambda e: e.memset(sk[:, 0:2], 0.0), writes=[b_s])
                    tt(sk[:, 2:NSLOT], sef[:, 2:NSLOT], sef[:, 0:NSLOT - 2], ALU.is_equal, [b_s], [b_s])
                    stt(sef[:], sef[:], 128.0, pidx[:, 0:1].to_broadcast([128, NSLOT]), ALU.mult, ALU.add, [b_s], [b_s])
                    stt(sef[:], sk[:], 1.0e6, sef[:], ALU.mult, ALU.add, [b_s], [b_s])
                    cp(widx[:], sef[:], [b_s], [b_slot])
                    fw.barrier()
                with ExitStack() as st:
                    hrow = [sb(f"hrow{i}", [128, D], F32, st) for i in range(3)]; hrowb = [Buf() for _ in range(3)]
                    for tk in range(32):
                        i = tk % 3
                        fw.dma("sp", hrow[i][:], h2_d[tk * 128:(tk + 1) * 128, :], writes=[hrowb[i]])
                        for ri in (r1i, r2i):
                            fw.dma_ind(Xs_d[:, :], bass.IndirectOffsetOnAxis(ap=ri[:, tk:tk + 1], axis=0), hrow[i][:], None,
                                       reads=[hrowb[i], b_rw])
                    fw.barrier()
                with ExitStack() as st:
                    wg = [sb(f"wg{i}", [128, 8, DE], BF16, st) for i in range(2)]; wgb = [Buf() for _ in range(2)]
                    wu = [sb(f"wu{i}", [128, 8, DE], BF16, st) for i in range(2)]; wub = [Buf() for _ in range(2)]
                    wd = [sb(f"wd{i}", [128, 4, D], BF16, st) for i in range(2)]; wdb = [Buf() for _ in range(2)]
                    xs = [sb(f"xs{i}", [128, D], F32, st) for i in range(3)]; xsb = [Buf() for _ in range(3)]
                    xsT = [sb(f"xsT{i}", [128, 8, 128], BF16, st) for i in range(2)]; xsTb = [Buf() for _ in range(2)]
                    sg = [sb(f"sg{i}", [128, DE], F32, st) for i in range(2)]; sgb = [Buf() for _ in range(2)]
                    hd = [sb(f"hd{i}", [128, DE], F32, st) for i in range(2)]; hdb = [Buf() for _ in range(2)]
                    hdT = [sb(f"hdT{i}", [128, 4, 128], BF16, st) for i in range(2)]; hdTb = [Buf() for _ in range(2)]
                    yt = [sb(f"yt{i}", [128, D], F32, st) for i in range(2)]; ytb = [Buf() for _ in range(2)]

                    if "bc" not in bc_val:
                        bc_reg = nc.gpsimd.alloc_register("bc_reg")
                        nc.gpsimd.reg_mov(bc_reg, 2 * NE * 128 - 1)
                        bc_val["bc"] = nc.gpsimd.snap(bc_reg, donate=True)
                    wg_rows = wg_d.rearrange("e p n -> (e p) n"); wu_rows = wu_d.rearrange("e p n -> (e p) n"); wd_rows = wd_d.rearrange("e p n -> (e p) n")

                    def load_gu(s_):
                        i = s_ % 2
                        off = bass.IndirectOffsetOnAxis(ap=widx[:, s_:s_ + 1], axis=0)
                        fw.dma_ind(wg[i][:].rearrange("p k n -> p (k n)"), None, wg_rows, off, reads=[b_slot], writes=[wgb[i]], bounds_check=bc_val["bc"])
                        fw.dma_ind(wu[i][:].rearrange("p k n -> p (k n)"), None, wu_rows, off, reads=[b_slot], writes=[wub[i]], bounds_check=bc_val["bc"])

                    def load_d(s_):
                        i = s_ % 2
                        off = bass.IndirectOffsetOnAxis(ap=widx[:, s_:s_ + 1], axis=0)
                        fw.dma_ind(wd[i][:].rearrange("p k n -> p (k n)"), None, wd_rows, off, reads=[b_slot], writes=[wdb[i]], bounds_check=bc_val["bc"])

                    def load_x(u_):
                        fw.dma("sp", xs[u_ % 3][:], Xs_d[u_ * 128:(u_ + 1) * 128, :], writes=[xsb[u_ % 3]])

                    def st_T(u_):
                        i = u_ % 2
                        x3 = u_ % 3
                        for half in range(2):
                            def f(e, half=half):
                                for c4 in range(4):
                                    c = half * 4 + c4
                                    ins = e.transpose(PS[half][:, c4 * 128:(c4 + 1) * 128], xs[x3][:, c * 128:(c + 1) * 128], ident[:])
                                return ins
                            OP("pe", f, [xsb[x3], b_ident], [PSB[half]])
                            cp(xsT[i][:, half * 4:(half + 1) * 4, :], PS[half][:, :].rearrange("p (c t) -> p c t", c=4), [PSB[half]], [xsTb[i]],
                               eng=("act" if half == 0 else "dve"))

                    def st_GU(u_):
                        i = u_ % 2
                        wi = (u_ // SUB) % 2
                        for (pp, w_, wb__) in ((2, wg[wi], wgb[wi]), (3, wu[wi], wub[wi])):
                            def f(e, pp=pp, w_=w_):
                                for kc in range(8):
                                    ins = e.matmul(PS[pp][:, :], xsT[i][:, kc, :], w_[:, kc, :], start=(kc == 0), stop=(kc == 7))
                                return ins
                            OP("pe", f, [xsTb[i], wb__], [PSB[pp]])
                        act(sg[i][:], PS[2][:, :], AF.Silu, [PSB[2]], [sgb[i]])
                        tt(hd[i][:], PS[3][:, :], sg[i][:], ALU.mult, [PSB[3], sgb[i]], [hdb[i]])

                    def st_HT(u_):
                        i = u_ % 2

                        def f(e):
                            for c4 in range(4):
                                ins = e.transpose(PS[4][:, c4 * 128:(c4 + 1) * 128], hd[i][:, c4 * 128:(c4 + 1) * 128], ident[:])
                            return ins
                        OP("pe", f, [hdb[i], b_ident], [PSB[4]])
                        cp(hdT[i][:], PS[4][:, :].rearrange("p (c t) -> p c t", c=4), [PSB[4]], [hdTb[i]], eng="act")

                    def st_D(u_):
                        i = u_ % 2
                        wi = (u_ // SUB) % 2
                        for half in range(2):
                            ps = 5 + half

                            def f(e, half=half, ps=ps):
                                for kc in range(4):
                                    ins = e.matmul(PS[ps][:, :], hdT[i][:, kc, :], wd[wi][:, kc, half * 512:(half + 1) * 512], start=(kc == 0), stop=(kc == 3))
                                return ins
                            OP("pe", f, [hdTb[i], wdb[wi]], [PSB[ps]])
                            cp(yt[i][:, half * 512:(half + 1) * 512], PS[ps][:, :], [PSB[ps]], [ytb[i]], eng=("dve" if half == 0 else "act"))
                        fw.dma("sp", Ys_d[u_ * 128:(u_ + 1) * 128, :], yt[i][:], reads=[ytb[i]])

                    NU = SUB * NSLOT
                    for s_ in range(2):
                        load_gu(s_)
                        load_d(s_)
                    for u_ in range(3):
                        load_x(u_)
                    for step in range(NU + 3):
                        if step < NU:
                            st_T(step)
                            if step + 3 < NU:
                                load_x(step + 3)
                        if 0 <= step - 1 < NU:
                            u_ = step - 1
                            st_GU(u_)
                            if u_ % SUB == SUB - 1 and u_ // SUB + 2 < NSLOT:
                                load_gu(u_ // SUB + 2)
                        if 0 <= step - 2 < NU:
                            st_HT(step - 2)
                        if 0 <= step - 3 < NU:
                            u_ = step - 3
                            st_D(u_)
                            if u_ % SUB == SUB - 1 and u_ // SUB + 2 < NSLOT:
                                load_d(u_ // SUB + 2)
                    fw.barrier()
                with ExitStack() as st:
                    y1 = [sb(f"y1_{i}", [128, D], F32, st) for i in range(3)]; y2 = [sb(f"y2_{i}", [128, D], F32, st) for i in range(3)]
                    y1b = [Buf() for _ in range(3)]; y2b = [Buf() for _ in range(3)]
                    ac = [sb(f"ac{i}", [128, D], F32, st) for i in range(2)]; acb = [Buf() for _ in range(2)]
                    xm = [sb(f"xm{i}", [128, 8, 128], F32, st) for i in range(3)]; xmb = [Buf() for _ in range(3)]
                    otile = [sb(f"otile{i}", [128, D], F32, st) for i in range(2)]; otb = [Buf() for _ in range(2)]
                    def issue5(tk):
                        i = tk % 3
                        fw.dma_ind(y1[i][:], None, Ys_d[:, :], bass.IndirectOffsetOnAxis(ap=r1i[:, tk:tk + 1], axis=0), reads=[b_rw], writes=[y1b[i]])
                        fw.dma_ind(y2[i][:], None, Ys_d[:, :], bass.IndirectOffsetOnAxis(ap=r2i[:, tk:tk + 1], axis=0), reads=[b_rw], writes=[y2b[i]])
                        fw.dma("sp", xm[i][:], xT_d[:, :, tk * 128:(tk + 1) * 128].rearrange("c p t -> p c t"), writes=[xmb[i]])
                    issue5(0)
                    issue5(1)
                    for tk in range(32):
                        i = tk % 2
                        j3 = tk % 3
                        if tk + 2 < 32:
                            issue5(tk + 2)
                        ts(ac[i][:], y1[j3][:], w1s[:, tk:tk + 1], None, ALU.mult, None, [y1b[j3], b_rw], [acb[i]])
                        stt(ac[i][:], y2[j3][:], w2s[:, tk:tk + 1], ac[i][:], ALU.mult, ALU.add, [y2b[j3], b_rw, acb[i]], [acb[i]])
                        for half in range(2):
                            ps = 2 * (tk % 2) + half

                            def f(e, half=half, ps=ps):
                                for c4 in range(4):
                                    c = half * 4 + c4
                                    ins = e.transpose(PS[ps][:, c4 * 128:(c4 + 1) * 128], ac[i][:, c * 128:(c + 1) * 128], ident[:])
                                return ins
                            OP("pe", f, [acb[i], b_ident], [PSB[ps]])
                            for c4 in range(4):
                                c = half * 4 + c4
                                stt(xm[j3][:, c, :], PS[ps][:, c4 * 128:(c4 + 1) * 128], G2[:, c:c + 1], xm[j3][:, c, :], ALU.mult, ALU.add,
                                    [PSB[ps], b_mod, xmb[j3]], [xmb[j3]])
                        if not last:
                            fw.dma("sp", xT_d[:, :, tk * 128:(tk + 1) * 128].rearrange("c p t -> p c t"), xm[j3][:], reads=[xmb[j3]])
                        else:
                            for half in range(2):
                                ps = 4 + 2 * (tk % 2) + half

                                def f(e, half=half, ps=ps):
                                    for c4 in range(4):
                                        c = half * 4 + c4
                                        ins = e.transpose(PS[ps][:, c4 * 128:(c4 + 1) * 128], xm[j3][:, c, :], ident[:])
                                    return ins
                                OP("pe", f, [xmb[j3], b_ident], [PSB[ps]])
                                cp(otile[i][:, half * 512:(half + 1) * 512], PS[ps][:, :], [PSB[ps]], [otb[i]], eng=("act" if half == 0 else "dve"))
                            fw.dma("sp", out_d[tk * 128:(tk + 1) * 128, :], otile[i][:], reads=[otb[i]])
                    fw.barrier()
    fw.barrier()
    return nc, fw, dbg


def host_inputs(inp, b):
    f = np.float32
    A = np.ascontiguousarray
    m = {}
    m["x"] = A(inp["x"][b])
    m["c_fm"] = A(inp["c"][b].reshape(8, 128).T)
    m["ada_w"] = inp["ada_w"]
    m["ada_b_fm"] = A(inp["ada_b"].reshape(2, 48, 128).transpose(0, 2, 1))
    m["n1g"] = A(inp["norm1_g"].reshape(2, 8, 128).transpose(0, 2, 1))
    m["n2g"] = A(inp["norm2_g"].reshape(2, 8, 128).transpose(0, 2, 1))
    m["w_in"] = inp["w_in"]
    m["fb"] = A(inp["forget_b"].reshape(2, 8, 1))
    def gp_lay(a):
        return A(a.reshape(2, 8, 2, 64).transpose(0, 2, 3, 1).reshape(2, 128, 8))
    m["lam_re"] = gp_lay(inp["lam_re"])
    m["lam_im"] = gp_lay(inp["lam_im"])
    m["log_dt"] = gp_lay(np.broadcast_to(inp["log_dt"][:, :, None], (2, 16, 64)))
    m["sb_re"] = A(inp["ssm_b_re"].reshape(2, 8, 2, 64, 16).transpose(0, 2, 3, 1, 4).reshape(2, 128, 8, 16))
    m["sb_im"] = A(inp["ssm_b_im"].reshape(2, 8, 2, 64, 16).transpose(0, 2, 3, 1, 4).reshape(2, 128, 8, 16))
    m["sc_re"] = A(inp["ssm_c_re"].reshape(2, 8, 2, 16, 64).transpose(0, 2, 4, 1, 3).reshape(2, 128, 8, 16))
    m["sc_im"] = A(inp["ssm_c_im"].reshape(2, 8, 2, 16, 64).transpose(0, 2, 4, 1, 3).reshape(2, 128, 8, 16))
    m["ssm_d"] = A(inp["ssm_d"].reshape(2, 2, 128).transpose(0, 2, 1))
    m["glu_w"] = inp["glu_w"]
    m["glu_b"] = A(inp["glu_b"].reshape(2, 2, 128).transpose(0, 2, 1))
    m["qg"] = A(np.tile(inp["q_norm_g"], (1, 2)).reshape(2, 128, 1))
    m["kg"] = A(np.tile(inp["k_norm_g"], (1, 2)).reshape(2, 128, 1))
    m["conv_w"] = A(inp["conv_w"].reshape(2, 3, 2, 128).transpose(0, 3, 2, 1))
    m["og_fm"] = A(inp["out_norm_g"].reshape(2, 8, 128).transpose(0, 2, 1))
    m["og_at"] = A(inp["out_norm_g"][:, 256:768].reshape(2, 8, 64).transpose(0, 2, 1))
    m["w_out"] = inp["w_out"]
    m["w_router"] = inp["w_router"]
    m["rbias"] = A(np.broadcast_to(inp["router_bias"][None, :], (128, 16)))
    m["w_gate"] = inp["w_gate"]
    m["w_up"] = inp["w_up"]
    m["w_down"] = inp["w_down"]
    m["ident"] = np.eye(128, dtype=f)
    e127 = np.zeros((128, 128), f); e127[127, :] = 1
    m["e127"] = e127
    blk = np.zeros((128, 128), f); blk[:64, :64] = 1; blk[64:, 64:] = 1
    m["blk64"] = blk
    m["tri"] = np.triu(np.ones((128, 128), f))
    m["iota"] = A(np.broadcast_to(np.arange(TT + 1, dtype=f)[None, :], (128, TT + 1)))
    sel = np.zeros((16, 16, 128), f)
    for e in range(16):
        sel[e, e, :] = 1
    m["sel16"] = sel
    return {k: np.asarray(v, dtype=f) for k, v in m.items()}


_CACHE = {}


def kernel(**inputs):
    inp = {k: np.asarray(v) for k, v in inputs.items()}
    if "nc" not in _CACHE:
        _CACHE["nc"] = build_program()[0]
    nc = _CACHE["nc"]
    in_maps = [host_inputs(inp, b) for b in range(8)]
    res = run_bass_kernel_spmd(nc, in_maps, core_ids=list(range(8)))
    out = np.stack([np.asarray(r["out"]) for r in res.results], axis=0)
    return out.astype(np.float32)
```

```python
import numpy as np
from contextlib import ExitStack
import concourse.bass as bass
import concourse.mybir as mybir
from concourse.bass_utils import run_bass_kernel_spmd

F32 = mybir.dt.float32
BF16 = mybir.dt.bfloat16
I32 = mybir.dt.int32
AF = mybir.ActivationFunctionType
ALU = mybir.AluOpType

S = 4096
D = 1024
TT = 512
NT = S // TT
DIN = 2568
NE = 16
DE = 512
EPS = 1e-6
TWO_PI = 6.283185307179586
import os as _os
NOCONV = bool(_os.environ.get('NOCONV'))
POOLENG = _os.environ.get('POOLENG', 'pool')
OFF_U, OFF_Q, OFF_K, OFF_V, OFF_F, OFF_HC, OFF_BG, OFF_CG = 0, 256, 768, 1280, 1792, 1800, 2056, 2312


class Buf:
    __slots__ = ("name", "w", "r")

    def __init__(self, name=""):
        self.name = name
        self.w = {}
        self.r = {}


class Fw:
    ENG = ("pe", "act", "dve", "pool", "sp")

    def __init__(self, nc, ndma=20):
        self.nc = nc
        self.eng = dict(pe=nc.tensor, act=nc.scalar, dve=nc.vector, pool=nc.gpsimd, sp=nc.sync)
        self.sem = {e: nc.alloc_semaphore("sem_" + e) for e in self.ENG}
        self.cnt = {e: 0 for e in self.ENG}
        self.known = {e: {} for e in self.ENG}
        self.dq = {}
        for q in ("sp", "pool"):
            self.dq[q] = dict(sems=[nc.alloc_semaphore(f"dq_{q}_{i}") for i in range(ndma)],
                              uses=[0] * ndma, nxt=0)
        self.allsems = {}
        self.nwaits = 0
        self.bg_on = False
        self.bg_sems = {s_.num for s_ in self.dq["pool"]["sems"]}

    def _wait(self, e, sem, val):
        k = self.known[e]
        if k.get(sem.num, 0) >= val:
            return
        self.eng[e].wait_ge(sem, val)
        self.nwaits += 1
        k[sem.num] = val

    def _deps(self, e, reads, writes):
        need = {}
        mysem = self.sem[e].num

        def add(tok, same_ok):
            sem, val = tok
            if same_ok and sem.num == mysem and e == "pe":
                return
            if need.get(sem.num, (None, 0))[1] < val:
                need[sem.num] = (sem, val)

        for b in reads:
            for tok in b.w.values():
                add(tok, False)
        for b in writes:
            for tok in b.w.values():
                add(tok, True)
            for tok in b.r.values():
                add(tok, True)
        for sem, val in need.values():
            self._wait(e, sem, val)

    def op(self, e, fn, reads=(), writes=()):
        self._deps(e, reads, writes)
        ins = fn(self.eng[e])
        self.cnt[e] += 1
        sem = self.sem[e]
        ins.then_inc(sem, 1)
        tok = (sem, self.cnt[e])
        for b in reads:
            b.r[sem.num] = tok
        for b in writes:
            b.w = {sem.num: tok}
            b.r = {}
        self.allsems[sem.num] = tok
        return tok

    def dma(self, q, out, in_, reads=(), writes=()):
        d = self.dq[q]
        i = d["nxt"]
        d["nxt"] = (i + 1) % len(d["sems"])
        sem = d["sems"][i]
        if d["uses"][i] > 0:
            self._wait(q, sem, 16 * d["uses"][i])
        self._deps(q, reads, writes)
        ins = self.eng[q].dma_start(out=out, in_=in_)
        d["uses"][i] += 1
        tok = (sem, 16 * d["uses"][i])
        ins.then_inc(sem, 16)
        for b in reads:
            b.r[sem.num] = tok
        for b in writes:
            b.w = {sem.num: tok}
            b.r = {}
        self.allsems[sem.num] = tok
        return tok

    def dma_ind(self, out, out_off, in_, in_off, reads=(), writes=(), bounds_check=None):
        q = "pool"
        d = self.dq[q]
        i = d["nxt"]
        d["nxt"] = (i + 1) % len(d["sems"])
        sem = d["sems"][i]
        if d["uses"][i] > 0:
            self._wait(q, sem, 16 * d["uses"][i])
        self._deps(q, reads, writes)
        if bounds_check is None:
            ins = self.eng[q].indirect_dma_start(out=out, out_offset=out_off, in_=in_, in_offset=in_off)
        else:
            ins = self.eng[q].indirect_dma_start(out=out, out_offset=out_off, in_=in_, in_offset=in_off,
                                                 bounds_check=bounds_check, oob_is_err=False)
        d["uses"][i] += 1
        tok = (sem, 16 * d["uses"][i])
        ins.then_inc(sem, 16)
        for b in reads:
            b.r[sem.num] = tok
        for b in writes:
            b.w = {sem.num: tok}
            b.r = {}
        self.allsems[sem.num] = tok
        return tok

    def barrier(self):
        for e in self.ENG:
            for sem, val in list(self.allsems.values()):
                if self.bg_on and sem.num in self.bg_sems:
                    continue
                self._wait(e, sem, val)


def build_program(nlayers=2, debug=False, stop_after=None):
    nc = bass.Bass("TRN2", target_bir_lowering=False)
    fw = Fw(nc)
    dbg = {}

    def din(name, shape, dt=F32):
        return nc.dram_tensor(name, list(shape), dt, kind="ExternalInput").ap()

    def dscr(name, shape, dt=F32):
        if debug:
            return nc.dram_tensor(name, list(shape), dt, kind="ExternalOutput").ap()
        return nc.dram_tensor(name, list(shape), dt).ap()

    x_in = din("x", [S, D])
    c_fm = din("c_fm", [128, 8])
    ada_w = din("ada_w", [2, D, 6 * D])
    ada_b_fm = din("ada_b_fm", [2, 128, 48])
    n1g = din("n1g", [2, 128, 8])
    n2g = din("n2g", [2, 128, 8])
    w_in = din("w_in", [2, D, DIN])
    fb = din("fb", [2, 8, 1])
    lam_re = din("lam_re", [2, 128, 8])
    lam_im = din("lam_im", [2, 128, 8])
    log_dt = din("log_dt", [2, 128, 8])
    sb_re = din("sb_re", [2, 128, 8, 16])
    sb_im = din("sb_im", [2, 128, 8, 16])
    sc_re = din("sc_re", [2, 128, 8, 16])
    sc_im = din("sc_im", [2, 128, 8, 16])
    ssm_d = din("ssm_d", [2, 128, 2])
    glu_w = din("glu_w", [2, 256, 256])
    glu_b = din("glu_b", [2, 128, 2])
    qg = din("qg", [2, 128, 1])
    kg = din("kg", [2, 128, 1])
    conv_w = din("conv_w", [2, 128, 2, 3])
    og_fm = din("og_fm", [2, 128, 8])
    og_at = din("og_at", [2, 64, 8])
    w_out = din("w_out", [2, D, D])
    w_router = din("w_router", [D, NE])
    rbias = din("rbias", [128, NE])
    w_gate = din("w_gate", [2, NE, D, DE])
    w_up = din("w_up", [2, NE, D, DE])
    w_down = din("w_down", [2, NE, DE, D])
    ident_in = din("ident", [128, 128])
    e127_in = din("e127", [128, 128])
    blk64_in = din("blk64", [128, 128])
    tri_in = din("tri", [128, 128])
    iota_in = din("iota", [128, TT + 1])
    sel_in = din("sel16", [16, NE, 128])
    out_d = nc.dram_tensor("out", [S, D], F32, kind="ExternalOutput").ap()

    xT_d = dscr("xT_d", [8, 128, S])
    wg_d = dscr("wg_d", [2 * NE, 128, 8 * DE], BF16)
    wu_d = dscr("wu_d", [2 * NE, 128, 8 * DE], BF16)
    wd_d = dscr("wd_d", [2 * NE, 128, 4 * D], BF16)
    uT_d = dscr("uT_d", [2, 128, S])
    qT_d = dscr("qT_d", [8, 65, S], BF16)
    kT_d = dscr("kT_d", [8, 65, S], BF16)
    yh_d = dscr("yh_d", [4, 128, S], BF16)
    ya_d = dscr("ya_d", [8, 64, S], BF16)
    h2_d = dscr("h2_d", [S, D])
    Xs_d = dscr("Xs_d", [80 * 128, D])
    Ys_d = dscr("Ys_d", [80 * 128, D])
    if debug:
        for nm, shp, dt in (("dbg_h", [8, 128, S], BF16), ("dbg_cum", [8, S], F32), ("dbg_mod", [128, 48], F32),
                            ("dbg_v", [128, 32, 8, 65], BF16), ("dbg_ssmpre", [2, 128, S], F32),
                            ("dbg_comb", [S, NE], F32), ("dbg_xmid", [8, 128, S], F32)):
            dbg[nm] = nc.dram_tensor(nm, shp, dt, kind="ExternalOutput").ap()

    es = ExitStack()

    uid = [0]

    def sb(name, shape, dt=F32, stack=None):
        uid[0] += 1
        return (stack or es).enter_context(nc.sbuf_tensor(f"s{uid[0]}_{name}", list(shape), dt))

    PS = [es.enter_context(nc.psum_tensor(f"ps{i}", [128, 512], F32)) for i in range(8)]
    PSB = [Buf(f"ps{i}") for i in range(8)]

    ident = sb("ident", [128, 128]); b_ident = Buf()
    e127 = sb("e127", [128, 128]); b_e127 = Buf()
    tri_f = sb("tri_f", [128, 128]); b_tri = Buf()
    blk_f = sb("blk_f", [128, 128]); blk = sb("blk", [128, 128], BF16); b_blk = Buf()
    ones_b = sb("ones_b", [128, 128], BF16); b_ones = Buf()
    ones_f = sb("ones_f", [128, 128]); b_onesf = Buf()
    iota = sb("iota", [128, TT + 1]); b_iota = Buf()
    sel16_f = sb("sel16_f", [16, NE, 128]); b_sel = Buf()
    cfm = sb("cfm", [128, 8]); b_cfm = Buf()
    cact = sb("cact", [128, 8]); b_cact = Buf()
    rb_sb = sb("rb_sb", [128, NE]); b_rb = Buf()
    wr_sb = sb("wr_sb", [128, 8, NE]); b_wr = Buf()

    fw.dma("sp", ident[:], ident_in, writes=[b_ident])
    fw.dma("sp", e127[:], e127_in, writes=[b_e127])
    fw.dma("sp", tri_f[:], tri_in, writes=[b_tri])
    fw.dma("sp", blk_f[:], blk64_in, writes=[b_blk])
    fw.dma("sp", iota[:], iota_in, writes=[b_iota])
    fw.dma("sp", sel16_f[:], sel_in, writes=[b_sel])
    fw.dma("sp", cfm[:], c_fm, writes=[b_cfm])
    fw.dma("sp", rb_sb[:], rbias, writes=[b_rb])
    fw.dma("sp", wr_sb[:], w_router.rearrange("(kc p) n -> p kc n", p=128), writes=[b_wr])
    fw.op("dve", lambda e: e.tensor_copy(out=blk[:], in_=blk_f[:]), reads=[b_blk], writes=[b_blk])
    fw.op("dve", lambda e: e.memset(ones_b[:], 1.0), writes=[b_ones])
    fw.op("dve", lambda e: e.memset(ones_f[:], 1.0), writes=[b_onesf])
    fw.op("act", lambda e: e.activation(out=cact[:], in_=cfm[:], func=AF.Silu), reads=[b_cfm], writes=[b_cact])

    def OP(e, fn, reads=(), writes=()):
        return fw.op(e, fn, reads, writes)

    def act(out, in_, func, reads, writes, scale=1.0, bias=0.0):
        return fw.op("act", lambda e: e.activation(out=out, in_=in_, func=func, bias=bias, scale=scale), reads, writes)

    def tt(out, in0, in1, op, reads, writes, eng="dve"):
        return fw.op(eng, lambda e: e.tensor_tensor(out=out, in0=in0, in1=in1, op=op), reads, writes)

    def ts(out, in0, s1, s2, op0, op1, reads, writes, eng="dve"):
        if op1 is None:
            return fw.op(eng, lambda e: e.tensor_scalar(out=out, in0=in0, scalar1=s1, scalar2=None, op0=op0), reads, writes)
        return fw.op(eng, lambda e: e.tensor_scalar(out=out, in0=in0, scalar1=s1, scalar2=s2, op0=op0, op1=op1), reads, writes)

    def stt(out, in0, scalar, in1, op0, op1, reads, writes):
        return fw.op("dve", lambda e: e.scalar_tensor_tensor(out=out, in0=in0, scalar=scalar, in1=in1, op0=op0, op1=op1),
                     reads, writes)

    def cp(out, in_, reads, writes, eng="dve"):
        if eng == "act":
            return fw.op("act", lambda e: e.copy(out=out, in_=in_), reads, writes)
        return fw.op(eng, lambda e: e.tensor_copy(out=out, in_=in_), reads, writes)

    ntri = sb("ntri", [128, 128], BF16); ident_b = sb("ident_b", [128, 128], BF16); b_ntri = Buf()
    tt(tri_f[:], tri_f[:], ident[:], ALU.subtract, [b_tri, b_ident], [b_tri])
    ts(ntri[:], tri_f[:], -30000.0, None, ALU.mult, None, [b_tri], [b_ntri])
    cp(ident_b[:], ident[:], [b_ident], [b_ntri])
    eps_c = sb("eps_c", [128, 1]); b_eps = Buf()
    OP("dve", lambda e: e.memset(eps_c[:], EPS), writes=[b_eps])

    hn_sq = [sb(f"hn_sq{i}", [128, TT], BF16) for i in range(2)]; hn_sqb = [Buf() for _ in range(2)]
    hn_rt = [sb(f"hn_rt{i}", [128, TT]) for i in range(2)]; hn_rtb = [Buf() for _ in range(2)]
    hn_ctr = [0]
    HN_PS = 7

    def hnorm(src, srcb, g_ap, gb, out, outb, P=128, n=TT):
        i = hn_ctr[0] % 2
        hn_ctr[0] += 1
        act(hn_sq[i][0:P, 0:n], src, AF.Square, [srcb], [hn_sqb[i]])
        OP("pe", lambda e: e.matmul(PS[HN_PS][0:P, 0:n], blk[0:P, 0:P], hn_sq[i][0:P, 0:n], start=True, stop=True),
           [hn_sqb[i], b_blk], [PSB[HN_PS]])
        act(hn_rt[i][0:P, 0:n], PS[HN_PS][0:P, 0:n], AF.Ln, [PSB[HN_PS], b_eps], [hn_rtb[i]], scale=1.0 / 64, bias=eps_c[0:P, :])
        act(hn_rt[i][0:P, 0:n], hn_rt[i][0:P, 0:n], AF.Exp, [hn_rtb[i]], [hn_rtb[i]], scale=-0.5)
        stt(out, src, g_ap, hn_rt[i][0:P, 0:n], ALU.mult, ALU.mult, [srcb, gb, hn_rtb[i]], [outb])

    mods, A1s, A2s, b_mods = [], [], [], []
    for l_ in range(nlayers):
        mods.append(sb(f"mod{l_}", [128, 48])); A1s.append(sb(f"A1_{l_}", [128, 8])); A2s.append(sb(f"A2_{l_}", [128, 8])); b_mods.append(Buf())
    with ExitStack() as st:
        xin = [sb(f"xin{i}", [128, D], F32, st) for i in range(4)]
        xinb = [Buf() for _ in range(4)]
        xo = [sb(f"xo{i}", [128, 8, 128], F32, st) for i in range(4)]
        xob = [Buf() for _ in range(4)]
        n1g_sb = sb("n1g_sb", [128, nlayers, 8], F32, st); n2g_sb = sb("n2g_sb", [128, nlayers, 8], F32, st); b_ng = Buf()
        adab = sb("adab", [128, nlayers, 48], F32, st); b_adab = Buf()
        cact2 = sb("cact2", [128, 8, 2], F32, st); b_cact2 = Buf()
        adw = [sb(f"adw{i}", [128, 8, D], F32, st) for i in range(2)]
        adwb = [Buf() for _ in range(2)]
        for l_ in range(nlayers):
            fw.dma("sp", n1g_sb[:, l_, :], n1g[l_], writes=[b_ng])
            fw.dma("sp", n2g_sb[:, l_, :], n2g[l_], writes=[b_ng])
            fw.dma("sp", adab[:, l_, :], ada_b_fm[l_], writes=[b_adab])
        cp(cact2[:], cact[:].unsqueeze(2).to_broadcast([128, 8, 2]), [b_cact], [b_cact2])

        def ada_units():
            for l_ in range(nlayers):
                for j in range(6):
                    i = (l_ * 6 + j) % 2
                    fw.dma("sp", adw[i][:], ada_w[l_, :, j * D:(j + 1) * D].rearrange("(kc p) n -> p kc n", p=128), writes=[adwb[i]])
                    for c in range(8):
                        def f(e, i=i, j=j, c=c, l_=l_):
                            for kc in range(8):
                                col = 2 * (j * 8 + c)
                                ins = e.matmul(PS[l_][:, col:col + 2], adw[i][:, kc, c * 128:(c + 1) * 128], cact2[:, kc, :],
                                               start=(kc == 0), stop=(kc == 7))
                            return ins
                        OP("pe", f, [adwb[i], b_cact2], [PSB[l_]])
                        yield
        au = ada_units()
        per = (nlayers * 48 + 31) // 32
        for tk in range(S // 128):
            i = tk % 4
            fw.dma("sp", xin[i][:], x_in[tk * 128:(tk + 1) * 128, :], writes=[xinb[i]])
            for _ in range(per):
                next(au, None)
            for half in range(2):
                pb = 2 + (2 * tk + half) % 6

                def f(e, i=i, half=half, pb=pb):
                    for c4 in range(4):
                        c = half * 4 + c4
                        ins = e.transpose(PS[pb][:, c4 * 128:(c4 + 1) * 128], xin[i][:, c * 128:(c + 1) * 128], ident[:])
                    return ins
                OP("pe", f, [xinb[i], b_ident], [PSB[pb]])
                cp(xo[i][:, half * 4:(half + 1) * 4, :], PS[pb][:].rearrange("p (c t) -> p c t", c=4),
                   [PSB[pb]], [xob[i]], eng=("act" if half == 0 else "dve"))
            fw.dma("sp", xT_d[:, :, tk * 128:(tk + 1) * 128].rearrange("c p t -> p c t"), xo[i][:], reads=[xob[i]])
        for _ in au:
            pass
        for l_ in range(nlayers):
            tt(mods[l_][:], PS[l_][:, 0:96].rearrange("p (n two) -> p n two", two=2)[:, :, 0], adab[:, l_, :], ALU.add,
               [PSB[l_], b_adab], [b_mods[l_]])
            stt(A1s[l_][:], mods[l_][:, 8:16], 1.0, n1g_sb[:, l_, :], ALU.add, ALU.mult, [b_mods[l_], b_ng], [b_mods[l_]])
            stt(A2s[l_][:], mods[l_][:, 32:40], 1.0, n2g_sb[:, l_, :], ALU.add, ALU.mult, [b_mods[l_], b_ng], [b_mods[l_]])
        if debug:
            fw.dma("sp", dbg["dbg_mod"], mods[0][:], reads=[b_mods[0]])
        fw.barrier()

    def norm_mod(st_, xt, xtb, A, B, ABb, hb, hbb, xn, xnb, sqb, sqbb, rt, rtb, psn):
        act(sqb[:], xt[:], AF.Square, [xtb], [sqbb])

        def f(e):
            for c in range(8):
                ins = e.matmul(PS[psn][:, :], ones_b[:], sqb[:, c, :], start=(c == 0), stop=(c == 7))
            return ins
        OP("pe", f, [sqbb, b_ones], [PSB[psn]])
        act(rt[:], PS[psn][:, :], AF.Ln, [PSB[psn], b_eps], [rtb], scale=1.0 / D, bias=eps_c[:])
        act(rt[:], rt[:], AF.Exp, [rtb], [rtb], scale=-0.5)
        tt(xn[:], xt[:], rt[:].unsqueeze(1).to_broadcast([128, 8, TT]), ALU.mult, [xtb, rtb], [xnb])
        for c in range(8):
            if c % 2 == 0:
                ts(xn[:, c, :], xn[:, c, :], A[:, c:c + 1], B[:, c:c + 1], ALU.mult, ALU.add, [xnb, ABb], [xnb])
            else:
                act(xn[:, c, :], xn[:, c, :], AF.Identity, [xnb, ABb], [xnb], scale=A[:, c:c + 1], bias=B[:, c:c + 1])
        cp(hb[:, 0:4, :], xn[:, 0:4, :], [xnb], [hbb], eng="dve")
        cp(hb[:, 4:8, :], xn[:, 4:8, :], [xnb], [hbb], eng="act")

    bc_val = {}
    for l in range(nlayers if stop_after != "0" else 0):
        last = (l == nlayers - 1)
        with ExitStack() as lay:
            mod = mods[l]; A1 = A1s[l]; A2 = A2s[l]; b_mod = b_mods[l]
            B1 = mod[:, 0:8]; G1 = mod[:, 16:24]; B2 = mod[:, 24:32]; G2 = mod[:, 40:48]

            with ExitStack() as mix:
                V_sb = sb("V_sb", [128, 32, 8, 65], BF16, mix); b_V = Buf()
                cum = sb("cum", [8, S], F32, mix); b_cum = Buf()
                og_fm_sb = sb("og_fm_sb", [128, 8], F32, mix); og_at_sb = sb("og_at_sb", [64, 8], F32, mix); b_og = Buf()
                fw.dma("sp", og_fm_sb[:], og_fm[l], writes=[b_og])
                fw.dma("sp", og_at_sb[:], og_at[l], writes=[b_og])
                OP("dve", lambda e: e.memset(V_sb[:, :, :, 64:65], 1.0), writes=[b_V])
                if l == 0:
                    cv = [sb(f"cv{i}", [128, 2048], BF16, mix) for i in range(2)]; cvb = [Buf() for _ in range(2)]; cvk = [0]
                fw.barrier()
                with ExitStack() as st:
                    win = sb("win", [128, 8, DIN], BF16, st); b_win = Buf()
                    for hh in range(2):
                        fw.dma("pool", win[:, :, hh * 1284:(hh + 1) * 1284],
                               w_in[l, :, hh * 1284:(hh + 1) * 1284].rearrange("(kc p) n -> p kc n", p=128), writes=[Buf()])
                    fw.barrier()
                    if l == 0 and not NOCONV:
                        for l2 in range(nlayers):
                            for e_ in range(NE):
                                for (src, dst, pat) in ((w_gate, wg_d, 8), (w_up, wu_d, 8), (w_down, wd_d, 4)):
                                    for hf in range(2):
                                        i = cvk[0] % 2
                                        cvk[0] += 1
                                        kcs = pat // 2
                                        srcap = src[l2, e_].rearrange("(kc p) n -> p kc n", p=128)[:, hf * kcs:(hf + 1) * kcs, :]
                                        dstv = cv[i][:].rearrange("p (kc n) -> p kc n", kc=kcs)
                                        fw.dma("pool", dstv, srcap, writes=[cvb[i]])
                                        fw.dma("pool", dst[l2 * NE + e_][:, hf * 2048:(hf + 1) * 2048], cv[i][:], reads=[cvb[i]])
                        fw.bg_on = True
                    fbs = sb("fbs", [8, 1], F32, st); b_fb = Buf()
                    qgs = sb("qgs", [128, 1], F32, st); kgs = sb("kgs", [128, 1], F32, st); b_qk = Buf()
                    cws = sb("cws", [128, 2, 3], F32, st); b_cw = Buf()
                    fw.dma("sp", fbs[:], fb[l], writes=[b_fb])
                    fw.dma("sp", qgs[:], qg[l], writes=[b_qk])
                    fw.dma("sp", kgs[:], kg[l], writes=[b_qk])
                    fw.dma("sp", cws[:], conv_w[l], writes=[b_cw])
                    ts(fbs[:], fbs[:], -1.0, None, ALU.mult, None, [b_fb], [b_fb])
                    ts(qgs[:], qgs[:], 0.125, None, ALU.mult, None, [b_qk], [b_qk])
                    if l == 0:
                        xt = [sb("xt0", [128, 8, TT], F32, st)] * 2; xtb = [Buf()] * 2
                    else:
                        xt = [sb(f"xt{i}", [128, 8, TT], F32, st) for i in range(2)]; xtb = [Buf() for _ in range(2)]
                    sqb = sb("sqb", [128, 8, TT], BF16, st); sqbb = Buf()
                    rt = sb("rt", [128, TT], F32, st); rtb = Buf()
                    xn = sb("xn", [128, 8, TT], F32, st); xnb = Buf()
                    hb = [sb(f"hb{i}", [128, 8, TT], BF16, st) for i in range(2)]; hbb = [Buf() for _ in range(2)]
                    ev = [sb(f"ev{i}", [128, TT], F32, st) for i in range(4)]; evb = [Buf() for _ in range(4)]
                    evo = [sb(f"evo{i}", [128, TT], BF16, st) for i in range(4)]; evob = [Buf() for _ in range(4)]
                    zt = [[sb(f"zt{cc}{i}", [128, TT + 2], F32, st) for i in range(2)] for cc in range(2)]
                    ztb = [[Buf() for _ in range(2)] for _ in range(2)]
                    cy = sb("cy", [128, TT], F32, st); cyb = Buf()
                    fe = sb("fe", [8, TT], F32, st); feb = Buf()
                    evc = [0]
                    gen = [0]
                    for cc in range(2):
                        OP("dve", lambda e, cc=cc: e.memset(zt[cc][1][:, TT:TT + 2], 0.0), writes=[ztb[cc][1]])

                    def proj(ps, off, M, hbt, hbtb):
                        def f(e):
                            for kc in range(8):
                                ins = e.matmul(PS[ps][0:M, :], win[:, kc, off:off + M], hbt[:, kc, :], start=(kc == 0), stop=(kc == 7))
                            return ins
                        OP("pe", f, [hbtb], [PSB[ps]])

                    for it in range(NT):
                        t0 = it * TT
                        i = it % 2
                        fw.dma("sp", xt[i][:], xT_d[:, :, t0:t0 + TT].rearrange("c p t -> p c t"), writes=[xtb[i]])
                        norm_mod(st, xt[i], xtb[i], A1, B1, b_mod, hb[i], hbb[i], xn, xnb, sqb, sqbb, rt, rtb, 0)
                        if debug and l == 0:
                            fw.dma("sp", dbg["dbg_h"][:, :, t0:t0 + TT].rearrange("c p t -> p c t"), hb[i][:], reads=[hbb[i]])
                        for c in range(2):
                            ps = 1 + gen[0] % 2; gen[0] += 1
                            proj(ps, OFF_U + c * 128, 128, hb[i], hbb[i])
                            k = evc[0] % 4; evc[0] += 1
                            cp(ev[k][:], PS[ps][:, :], [PSB[ps]], [evb[k]], eng="act")
                            fw.dma("sp", uT_d[c, :, t0:t0 + TT], ev[k][:], reads=[evb[k]])
                        for (off, gsb, dst) in ((OFF_Q, qgs, qT_d), (OFF_K, kgs, kT_d)):
                            for c in range(4):
                                ps = 1 + gen[0] % 2; gen[0] += 1
                                proj(ps, off + c * 128, 128, hb[i], hbb[i])
                                k = evc[0] % 4; evc[0] += 1
                                cp(ev[k][:], PS[ps][:, :], [PSB[ps]], [evb[k]], eng="act")
                                hnorm(ev[k][:], evb[k], gsb[:, 0:1], b_qk, evo[k][:], evob[k])
                                fw.dma("sp", dst[2 * c, 0:64, t0:t0 + TT], evo[k][0:64, :], reads=[evob[k]])
                                fw.dma("sp", dst[2 * c + 1, 0:64, t0:t0 + TT], evo[k][64:128, :], reads=[evob[k]])
                        ps = 1 + gen[0] % 2; gen[0] += 1
                        proj(ps, OFF_F, 8, hb[i], hbb[i])
                        act(fe[:], PS[ps][0:8, :], AF.Exp, [PSB[ps], b_fb], [feb], scale=-1.0, bias=fbs[:])
                        act(fe[:], fe[:], AF.Ln, [feb], [feb], bias=1.0)
                        init = 0.0 if it == 0 else cum[:, t0 - 1:t0]
                        OP("dve", lambda e, t0=t0, init=init: e.tensor_tensor_scan(
                            out=cum[:, t0:t0 + TT], data0=ones_f[0:8, 0:1].to_broadcast([8, TT]), data1=fe[:], initial=init,
                            op0=ALU.mult, op1=ALU.subtract), [feb, b_onesf, b_cum], [b_cum])
                        for sub in range(4):
                            def f(e, sub=sub, i=i):
                                for kc in range(8):
                                    ins = e.matmul(PS[3][:, :], hb[i][:, kc, sub * 128:(sub + 1) * 128], win[:, kc, OFF_V:OFF_V + 512],
                                                   start=(kc == 0), stop=(kc == 7))
                                return ins
                            OP("pe", f, [hbb[i]], [PSB[3]])
                            cp(V_sb[:, 4 * it + sub, :, 0:64], PS[3][:, :].rearrange("p (h d) -> p h d", h=8), [PSB[3]], [b_V],
                               eng=("act" if sub % 2 else "dve"))
                        for cc in range(2):
                            proj(4, OFF_HC + cc * 128, 128, hb[i], hbb[i])
                            proj(5, OFF_CG + cc * 128, 128, hb[i], hbb[i])
                            proj(6, OFF_BG + cc * 128, 128, hb[i], hbb[i])
                            k = evc[0] % 4; evc[0] += 1
                            z, zb = zt[cc][i], ztb[cc][i]
                            zp, zpb = zt[cc][1 - i], ztb[cc][1 - i]
                            cp(ev[k][:], PS[5][:, :], [PSB[5]], [evb[k]], eng="act")
                            cp(z[:, 0:2], zp[:, TT:TT + 2], [zpb], [zb])
                            tt(z[:, 2:TT + 2], PS[4][:, :], ev[k][:], ALU.mult, [PSB[4], evb[k]], [zb])
                            ts(cy[:], z[:, 2:TT + 2], cws[:, cc, 2:3], None, ALU.mult, None, [zb, b_cw], [cyb])
                            stt(cy[:], z[:, 1:TT + 1], cws[:, cc, 1:2], cy[:], ALU.mult, ALU.add, [zb, b_cw, cyb], [cyb])
                            stt(cy[:], z[:, 0:TT], cws[:, cc, 0:1], cy[:], ALU.mult, ALU.add, [zb, b_cw, cyb], [cyb])
                            tt(ev[k][:], PS[6][:, :], cy[:], ALU.mult, [PSB[6], cyb], [evb[k]])
                            hnorm(ev[k][:], evb[k], og_fm_sb[:, 6 + cc:7 + cc], b_og, evo[k][:], evob[k])
                            fw.dma("sp", yh_d[2 + cc, :, t0:t0 + TT], evo[k][:], reads=[evob[k]])
                    if debug and l == 0:
                        fw.dma("sp", dbg["dbg_cum"], cum[:], reads=[b_cum])
                        fw.dma("sp", dbg["dbg_v"], V_sb[:], reads=[b_V])
                    fw.barrier()
                if stop_after == "A":
                    break
                with ExitStack() as st:
                    def t8(name):
                        return sb(name, [128, 8], F32, st)
                    lre, lim, ldt = t8("lre"), t8("lim"), t8("ldt"); b_p = Buf()
                    fw.dma("sp", lre[:], lam_re[l], writes=[b_p])
                    fw.dma("sp", lim[:], lam_im[l], writes=[b_p])
                    fw.dma("sp", ldt[:], log_dt[l], writes=[b_p])
                    bre = sb("bre", [128, 8, 16], F32, st); bim = sb("bim", [128, 8, 16], F32, st)
                    cre = sb("cre", [128, 8, 16], F32, st); cim = sb("cim", [128, 8, 16], F32, st); b_bc = Buf()
                    fw.dma("sp", bre[:], sb_re[l], writes=[b_bc]); fw.dma("sp", bim[:], sb_im[l], writes=[b_bc])
                    fw.dma("sp", cre[:], sc_re[l], writes=[b_bc]); fw.dma("sp", cim[:], sc_im[l], writes=[b_bc])
                    dsk = sb("dsk", [128, 2], F32, st); glb = sb("glb", [128, 2], F32, st); b_dg = Buf()
                    fw.dma("sp", dsk[:], ssm_d[l], writes=[b_dg]); fw.dma("sp", glb[:], glu_b[l], writes=[b_dg])
                    gluw = sb("gluw", [128, 2, 256], BF16, st); gluwf = sb("gluwf", [128, 2, 256], F32, st); b_gw = Buf()
                    fw.dma("sp", gluwf[:], glu_w[l].rearrange("(kc p) n -> p kc n", p=128), writes=[b_gw])
                    cp(gluw[:], gluwf[:], [b_gw], [b_gw], eng="act")
                    r_sb, th = t8("r_sb"), t8("th")
                    dtv, a_, cs, sn, t1_, t2_, zre, zim = t8("dtv"), t8("a_"), t8("cs"), t8("sn"), t8("t1_"), t8("t2_"), t8("zre"), t8("zim")
                    ti = sb("ti", [128, 8], I32, st)
                    C1 = 6.28125
                    C2 = TWO_PI - C1

                    def sincos(out, ang, shape, tmpf, tmpi, bq, shift):
                        ts(tmpf, ang, 1.0 / TWO_PI, shift / TWO_PI, ALU.mult, ALU.add, [bq], [bq])
                        cp(tmpi, tmpf, [bq], [bq])
                        cp(tmpf, tmpi, [bq], [bq])
                        if shift != 0.0:
                            ts(out, ang, shift, None, ALU.add, None, [bq], [bq])
                            stt(out, tmpf, -C1, out, ALU.mult, ALU.add, [bq], [bq])
                        else:
                            stt(out, tmpf, -C1, ang, ALU.mult, ALU.add, [bq], [bq])
                        stt(out, tmpf, -C2, out, ALU.mult, ALU.add, [bq], [bq])
                        ts(out, out, 3.1415925, -3.1415925, ALU.min, ALU.max, [bq], [bq])
                        act(out, out, AF.Sin, [bq], [bq])

                    ts(lre[:], lre[:], -1e-4, None, ALU.min, None, [b_p], [b_p])
                    act(dtv[:], ldt[:], AF.Exp, [b_p], [b_p])
                    tt(a_[:], lre[:], dtv[:], ALU.mult, [b_p], [b_p])
                    act(r_sb[:], a_[:], AF.Exp, [b_p], [b_p])
                    tt(th[:], lim[:], dtv[:], ALU.mult, [b_p], [b_p])
                    sincos(sn[:], th[:], None, t1_[:], ti[:], b_p, 0.0)
                    sincos(cs[:], th[:], None, t1_[:], ti[:], b_p, 1.5707963267948966)
                    tt(cs[:], cs[:], r_sb[:], ALU.mult, [b_p], [b_p])
                    tt(sn[:], sn[:], r_sb[:], ALU.mult, [b_p], [b_p])
                    ts(cs[:], cs[:], -1.0, None, ALU.add, None, [b_p], [b_p])
                    tt(t1_[:], lre[:], lre[:], ALU.mult, [b_p], [b_p])
                    tt(t2_[:], lim[:], lim[:], ALU.mult, [b_p], [b_p])
                    tt(t1_[:], t1_[:], t2_[:], ALU.add, [b_p], [b_p])
                    OP("dve", lambda e: e.reciprocal(out=t1_[:], in_=t1_[:]), [b_p], [b_p])
                    tt(zre[:], cs[:], lre[:], ALU.mult, [b_p], [b_p])
                    tt(t2_[:], sn[:], lim[:], ALU.mult, [b_p], [b_p])
                    tt(zre[:], zre[:], t2_[:], ALU.add, [b_p], [b_p])
                    tt(zre[:], zre[:], t1_[:], ALU.mult, [b_p], [b_p])
                    tt(zim[:], sn[:], lre[:], ALU.mult, [b_p], [b_p])
                    tt(t2_[:], cs[:], lim[:], ALU.mult, [b_p], [b_p])
                    tt(zim[:], zim[:], t2_[:], ALU.subtract, [b_p], [b_p])
                    tt(zim[:], zim[:], t1_[:], ALU.mult, [b_p], [b_p])
                    bbr = sb("bbr", [128, 8, 16], F32, st); bbi = sb("bbi", [128, 8, 16], F32, st); tb = sb("tb", [128, 8, 16], F32, st)
                    zre_b = zre[:].unsqueeze(2).to_broadcast([128, 8, 16]); zim_b = zim[:].unsqueeze(2).to_broadcast([128, 8, 16])
                    tt(bbr[:], bre[:], zre_b, ALU.mult, [b_p, b_bc], [b_bc])
                    tt(tb[:], bim[:], zim_b, ALU.mult, [b_p, b_bc], [b_bc])
                    tt(bbr[:], bbr[:], tb[:], ALU.subtract, [b_bc], [b_bc])
                    tt(bbi[:], bim[:], zre_b, ALU.mult, [b_p, b_bc], [b_bc])
                    tt(tb[:], bre[:], zim_b, ALU.mult, [b_p, b_bc], [b_bc])
                    tt(bbi[:], bbi[:], tb[:], ALU.add, [b_bc], [b_bc])
                    WT = []
                    for nm, src in (("re", bbr), ("im", bbi)):
                        w1 = sb("w1" + nm, [128, 8, 2, 16], F32, st); bw1 = Buf()
                        OP("dve", lambda e, w1=w1: e.memset(w1[:], 0.0), writes=[bw1])
                        cp(w1[0:64, :, 0, :], src[0:64], [b_bc], [bw1])
                        cp(w1[64:128, :, 1, :], src[64:128], [b_bc], [bw1])
                        wt = sb("wt" + nm, [128, 2, 128], BF16, st); bwt = Buf()
                        w1v = w1[:].rearrange("p g a c -> p (g a c)")
                        for ch in range(2):
                            OP("pe", lambda e, ch=ch, w1v=w1v: e.transpose(PS[0][:, 0:128], w1v[:, ch * 128:(ch + 1) * 128], ident[:]),
                               [bw1, b_ident], [PSB[0]])
                            cp(wt[:, ch, :], PS[0][:, 0:128], [PSB[0]], [bwt])
                        WT.append((wt, bwt))
                    CT = []
                    for nm, src, sgn in (("re", cre, 1.0), ("im", cim, -1.0)):
                        ct = sb("ct" + nm, [128, 8, 2, 16], BF16, st); bct = Buf()
                        OP("dve", lambda e, ct=ct: e.memset(ct[:], 0.0), writes=[bct])
                        ts(ct[0:64, :, 0, :], src[0:64], sgn, None, ALU.mult, None, [b_bc], [bct])
                        ts(ct[64:128, :, 1, :], src[64:128], sgn, None, ALU.mult, None, [b_bc], [bct])
                        CT.append((ct, bct))
                    cosT = sb("cosT", [128, 8, TT + 1], F32, st); sinT = sb("sinT", [128, 8, TT + 1], F32, st); b_tab = Buf()
                    with ExitStack() as st2:
                        ang = sb("ang", [128, 8, TT + 1], F32, st2); tf = sb("tf", [128, 8, TT + 1], F32, st2)
                        tii = sb("tii", [128, 8, TT + 1], I32, st2); b_ang = Buf()
                        for gp in range(8):
                            ts(ang[:, gp, :], iota[:], th[:, gp:gp + 1], None, ALU.mult, None, [b_iota, b_p], [b_ang])
                        sincos(sinT[:], ang[:], None, tf[:], tii[:], b_ang, 0.0)
                        sincos(cosT[:], ang[:], None, tf[:], tii[:], b_ang, 1.5707963267948966)
                        fw.barrier()
                    uf = [sb(f"uf{i}", [128, 2, TT], F32, st) for i in range(2)]; ufb = [Buf() for _ in range(2)]
                    ub = [sb(f"ub{i}", [128, 2, TT], BF16, st) for i in range(2)]; ubb = [Buf() for _ in range(2)]
                    ta = [sb(f"ta{i}", [128, TT], F32, st) for i in range(4)]; tab_ = [Buf() for _ in range(4)]
                    wre = [sb(f"wre{i}", [128, TT], F32, st) for i in range(2)]; wim = [sb(f"wim{i}", [128, TT], F32, st) for i in range(2)]
                    wb_ = [Buf() for _ in range(2)]
                    zr = [sb(f"zr{i}", [128, TT], BF16, st) for i in range(2)]; zi = [sb(f"zi{i}", [128, TT], BF16, st) for i in range(2)]
                    zb_ = [Buf() for _ in range(2)]
                    ini = sb("ini", [128, 8, 2], F32, st); b_ini = [Buf() for _ in range(8)]
                    tiny = sb("tiny", [128, 2], F32, st)
                    yp = sb("yp", [128, 2, TT], F32, st); ypb = [Buf() for _ in range(2)]
                    yg = sb("yg", [128, 2, TT], F32, st); ygb_f = [Buf() for _ in range(2)]
                    ygb = sb("ygb", [128, 2, TT], BF16, st); ygbb = Buf()
                    g1t = sb("g1t", [128, TT], F32, st); g1b = Buf(); g2t = sb("g2t", [128, TT], F32, st); g2b = Buf()
                    yo = [sb(f"yo{i}", [128, TT], F32, st) for i in range(2)]; yob = [Buf() for _ in range(2)]
                    yob16 = [sb(f"yob16{i}", [128, TT], BF16, st) for i in range(2)]; yob16b = [Buf() for _ in range(2)]
                    OP("dve", lambda e: e.memset(ini[:], 0.0), writes=b_ini)
                    k = 0
                    def gen_B():
                        k = 0
                        pend = []

                        def run_due(force=False):
                            keep = []
                            for item in list(pend):
                                item[0] -= 1
                                if force or item[0] <= 0:
                                    nxt = item[1]()
                                    while force and nxt is not None:
                                        nxt = nxt()
                                    if nxt is not None:
                                        keep.append([1, nxt])
                                else:
                                    keep.append(item)
                            pend[:] = keep
                        for it in range(NT):
                            t0 = it * TT
                            i = it % 2
                            fw.dma("sp", uf[i][:], uT_d[:, :, t0:t0 + TT].rearrange("c p t -> p c t"), writes=[ufb[i]])
                            cp(ub[i][:], uf[i][:], [ufb[i]], [ubb[i]], eng="act")
                            for gp in range(8):
                                ch, j = gp // 4, gp % 4
                                pa, pb = 0, 1
                                for (pp, (wt, bwt)) in ((pa, WT[0]), (pb, WT[1])):
                                    OP("pe", lambda e, pp=pp, wt=wt, ch=ch, j=j, i=i: e.matmul(
                                        PS[pp][:, :], wt[32 * j:32 * j + 32, ch, :], ub[i][32 * j:32 * j + 32, ch, :],
                                        start=True, stop=True, tile_position=(32 * j, 0)), [bwt, ubb[i]], [PSB[pp]])
                                run_due()
                                cT = cosT[:, gp, 0:TT]; sT = sinT[:, gp, 0:TT]
                                kk = k % 2; k += 1
                                tt(ta[0][:], PS[pa][:, :], cT, ALU.mult, [PSB[pa], b_tab], [tab_[0]])
                                tt(ta[1][:], PS[pb][:, :], sT, ALU.mult, [PSB[pb], b_tab], [tab_[1]])
                                tt(ta[0][:], ta[0][:], ta[1][:], ALU.add, [tab_[0], tab_[1]], [tab_[0]])
                                tt(ta[2][:], PS[pb][:, :], cT, ALU.mult, [PSB[pb], b_tab], [tab_[2]])
                                tt(ta[3][:], PS[pa][:, :], sT, ALU.mult, [PSB[pa], b_tab], [tab_[3]])
                                tt(ta[2][:], ta[2][:], ta[3][:], ALU.subtract, [tab_[2], tab_[3]], [tab_[2]])
                                rb = r_sb[:, gp:gp + 1].to_broadcast([128, TT])
                                OP("dve", lambda e, kk=kk, rb=rb, gp=gp: e.tensor_tensor_scan(
                                    out=wre[kk][:], data0=rb, data1=ta[0][:], initial=ini[:, gp, 0:1], op0=ALU.mult, op1=ALU.add),
                                    [tab_[0], b_p, b_ini[gp]], [wb_[kk]])
                                OP("dve", lambda e, kk=kk, rb=rb, gp=gp: e.tensor_tensor_scan(
                                    out=wim[kk][:], data0=rb, data1=ta[2][:], initial=ini[:, gp, 1:2], op0=ALU.mult, op1=ALU.add),
                                    [tab_[2], b_p, b_ini[gp]], [wb_[kk]])
                                tt(ta[0][:], wre[kk][:], cT, ALU.mult, [wb_[kk], b_tab], [tab_[0]])
                                tt(ta[1][:], wim[kk][:], sT, ALU.mult, [wb_[kk], b_tab], [tab_[1]])
                                tt(zr[kk][:], ta[0][:], ta[1][:], ALU.subtract, [tab_[0], tab_[1]], [zb_[kk]])
                                tt(ta[2][:], wre[kk][:], sT, ALU.mult, [wb_[kk], b_tab], [tab_[2]])
                                tt(ta[3][:], wim[kk][:], cT, ALU.mult, [wb_[kk], b_tab], [tab_[3]])
                                tt(zi[kk][:], ta[2][:], ta[3][:], ALU.add, [tab_[2], tab_[3]], [zb_[kk]])
                                cL = cosT[:, gp, TT:TT + 1]; sL = sinT[:, gp, TT:TT + 1]
                                ts(tiny[:, 0:1], wim[kk][:, TT - 1:TT], sL, None, ALU.mult, None, [wb_[kk], b_tab], [b_ini[gp]])
                                ts(tiny[:, 1:2], wim[kk][:, TT - 1:TT], cL, None, ALU.mult, None, [wb_[kk], b_tab], [b_ini[gp]])
                                stt(ini[:, gp, 0:1], wre[kk][:, TT - 1:TT], cL, tiny[:, 0:1], ALU.mult, ALU.subtract, [wb_[kk], b_tab, b_ini[gp]], [b_ini[gp]])
                                stt(ini[:, gp, 1:2], wre[kk][:, TT - 1:TT], sL, tiny[:, 1:2], ALU.mult, ALU.add, [wb_[kk], b_tab, b_ini[gp]], [b_ini[gp]])
                                py = 2

                                def tail(gp=gp, j=j, kk=kk, py=py, ch=ch, i=i, t0=t0):
                                  def f(e):
                                    e.matmul(PS[py][32 * j:32 * j + 32, :], CT[0][0][:, gp, :, :].rearrange("p a c -> p (a c)"), zr[kk][:],
                                             start=True, stop=False, tile_position=(0, 32 * j))
                                    return e.matmul(PS[py][32 * j:32 * j + 32, :], CT[1][0][:, gp, :, :].rearrange("p a c -> p (a c)"), zi[kk][:],
                                                    start=False, stop=True, tile_position=(0, 32 * j))
                                  OP("pe", f, [zb_[kk], CT[0][1], CT[1][1]], [PSB[py]])
                                  if j == 3:
                                    stt(yp[:, ch, :], uf[i][:, ch, :], dsk[:, ch:ch + 1], PS[py][:, :], ALU.mult, ALU.add,
                                        [ufb[i], b_dg, PSB[py]], [ypb[ch]])
                                    if debug and l == 0:
                                        fw.dma("sp", dbg["dbg_ssmpre"][ch, :, t0:t0 + TT], yp[:, ch, :], reads=[ypb[ch]])
                                    tt(g1t[:], yp[:, ch, :], yp[:, ch, :], ALU.mult, [ypb[ch]], [g1b])
                                    ts(g1t[:], g1t[:], 0.044715, 1.0, ALU.mult, ALU.add, [g1b], [g1b])
                                    tt(g1t[:], g1t[:], yp[:, ch, :], ALU.mult, [g1b, ypb[ch]], [g1b])

                                    def tailB():
                                        act(g1t[:], g1t[:], AF.Sigmoid, [g1b], [g1b], scale=1.5957691216057308)

                                        def tailC():
                                            tt(yg[:, ch, :], yp[:, ch, :], g1t[:], ALU.mult, [g1b, ypb[ch]], [ygb_f[ch]])
                                            cp(ygb[:, ch, :], yg[:, ch, :], [ygb_f[ch]], [ygbb])
                                            return None
                                        return tailC
                                    return tailB
                                  return None
                                pend.append([1, tail])
                                yield
                            def glu_block(t0=t0):
                                for mc in range(2):
                                    def f(e, mc=mc):
                                        e.matmul(PS[7][:, :], gluw[:, 0, mc * 128:(mc + 1) * 128], ygb[:, 0, :], start=True, stop=False)
                                        return e.matmul(PS[7][:, :], gluw[:, 1, mc * 128:(mc + 1) * 128], ygb[:, 1, :], start=False, stop=True)
                                    OP("pe", f, [ygbb, b_gw], [PSB[7]])
                                    act(g2t[:], PS[7][:, :], AF.Sigmoid, [PSB[7], b_dg], [g2b], bias=glb[:, mc:mc + 1])
                                    tt(yo[mc][:], yg[:, mc, :], g2t[:], ALU.mult, [g2b, ygb_f[mc]], [yob[mc]])
                                    hnorm(yo[mc][:], yob[mc], og_fm_sb[:, mc:mc + 1], b_og, yob16[mc][:], yob16b[mc])
                                    fw.dma("sp", yh_d[mc, :, t0:t0 + TT], yob16[mc][:], reads=[yob16b[mc]])
                                return None
                            pend.append([4, glu_block])
                            yield
                        run_due(force=True)
                        yield
                    ckT = sb("ckT", [128, 32, 8], F32, st); cref = sb("cref", [128, 32, 8], F32, st); b_ck = Buf()
                    st3 = ExitStack()
                    ce = sb("ce", [8, 32], F32, st3); dq = sb("dq", [8, 8, 4], F32, st3); b_ce = Buf()
                    dqrow = sb("dqrow", [8, 32, 128], BF16, st3); onesrow = sb("onesrow", [8, S], BF16, st3); b_row = Buf()
                    cp(ce[:], cum[:].rearrange("h (s j) -> h s j", j=128)[:, :, 127], [b_cum], [b_ce])
                    cev = ce[:].rearrange("h (q s) -> h q s", s=4)
                    tt(dq[:], cev, cev[:, :, 3:4].to_broadcast([8, 8, 4]), ALU.subtract, [b_ce], [b_ce])
                    cp(dqrow[:], dq[:].rearrange("h q s -> h (q s)").unsqueeze(2).to_broadcast([8, 32, 128]), [b_ce], [b_row])
                    OP("dve", lambda e: e.memset(onesrow[:], 1.0), writes=[b_row])
                    fw.dma("sp", qT_d[:, 64, :], dqrow[:].rearrange("h s j -> h (s j)"), reads=[b_row])
                    fw.dma("sp", kT_d[:, 64, :], onesrow[:], reads=[b_row])

                    def f(e):
                        for kt in range(32):
                            ins = e.transpose(PS[0][:, kt * 8:(kt + 1) * 8], cum[0:8, kt * 128:(kt + 1) * 128], ident[0:8, 0:8])
                        return ins
                    OP("pe", f, [b_cum, b_ident], [PSB[0]])
                    cp(ckT[:].rearrange("p k h -> p (k h)"), PS[0][:, 0:256], [PSB[0]], [b_ck])
                    OP("pe", lambda e: e.matmul(PS[1][:, 0:256], e127[:], ckT[:].rearrange("p k h -> p (k h)"), start=True, stop=True),
                       [b_ck, b_e127], [PSB[1]])
                    cp(cref[:].rearrange("p k h -> p (k h)"), PS[1][:, 0:256], [PSB[1]], [b_ck])
                    fw.barrier()
                    st3.close()
                    qa = [sb("qa0", [65, S], BF16, st)]; ka = [sb("ka0", [65, S], BF16, st)]
                    qab = [Buf()]; kab = [Buf()]
                    NP = 6
                    pT = [sb(f"pT{i}", [128, TT], BF16, st) for i in range(NP)]; pTb = [Buf() for _ in range(NP)]
                    biasT = [sb(f"biasT{i}", [128, 32], F32, st) for i in range(2)]; biasb = [Buf() for _ in range(2)]
                    osb = [sb(f"osb{i}", [65, TT], F32, st) for i in range(2)]; osbb = [Buf() for _ in range(2)]
                    yat = [sb(f"yat{i}", [64, TT], F32, st) for i in range(2)]; yatb = [Buf() for _ in range(2)]
                    yab = [sb(f"yab{i}", [64, TT], BF16, st) for i in range(2)] ; yabb = [Buf() for _ in range(2)]
                    def emit_bias(u_):
                        h_, qt_ = u_ // 8, u_ % 8
                        n_ = 4 * qt_ + 4
                        ts(biasT[u_ % 2][:, 0:n_], ckT[:, 0:n_, h_], cref[:, 4 * qt_ + 3, h_:h_ + 1], -1.0, ALU.subtract, ALU.mult,
                           [b_ck], [biasb[u_ % 2]])

                    def gen_C():
                        blkctr = 0
                        pend2 = pend3 = None
                        for h in range(8):
                            hi = 0
                            fw.dma("sp", qa[hi][:], qT_d[h], writes=[qab[hi]])
                            fw.dma("sp", ka[hi][:], kT_d[h], writes=[kab[hi]])
                            for qt in range(8):
                                nkt = 4 * qt + 4
                                bi = (h * 8 + qt) % 2
                                oi = bi
                                po = 6
                                if h * 8 + qt == 0:
                                    emit_bias(0)
                                if h * 8 + qt + 1 < 64:
                                    emit_bias(h * 8 + qt + 1)

                                SL = (3, 4, 5)
                                LA = 2

                                def s_mm(kt):
                                    slot = SL[(blkctr + kt) % 3]
                                    m = kt - 4 * qt
                                    c0 = 128 * m if m > 0 else 0
                                    def f(e):
                                        ins = e.matmul(PS[slot][:, c0:TT], ka[hi][:, kt * 128:(kt + 1) * 128],
                                                       qa[hi][:, qt * TT + c0:(qt + 1) * TT], start=True, stop=(m < 0))
                                        if m >= 0:
                                            ins = e.matmul(PS[slot][:, c0:c0 + 128], ntri[:], ident_b[:], start=False, stop=True)
                                        return ins
                                    OP("pe", f, [kab[hi], qab[hi], b_ntri], [PSB[slot]])
                                for kt in range(min(LA, nkt)):
                                    s_mm(kt)
                                for kt in range(nkt):
                                    slot = SL[(blkctr + kt) % 3]
                                    if kt + LA < nkt:
                                        s_mm(kt + LA)
                                    m = kt - 4 * qt
                                    c0 = 128 * m if m > 0 else 0
                                    pi = (blkctr + kt) % NP
                                    act(pT[pi][:, c0:TT], PS[slot][:, c0:TT], AF.Exp, [PSB[slot], biasb[bi]], [pTb[pi]],
                                        bias=biasT[bi][:, kt:kt + 1])
                                    OP("pe", lambda e, kt=kt, c0=c0, pi=pi: e.matmul(
                                        PS[po][0:65, c0:TT], V_sb[:, kt, h, :], pT[pi][:, c0:TT], start=(kt == 0), stop=(kt == nkt - 1)),
                                        [pTb[pi], b_V], [PSB[po]])
                                    if kt % 8 == 7 and kt + 1 < nkt:
                                        yield 8
                                blkctr += nkt
                                cp(osb[oi][:], PS[po][0:65, :], [PSB[po]], [osbb[oi]], eng="act")
                                OP("dve", lambda e, oi=oi: e.reciprocal(out=osb[oi][64:65, :], in_=osb[oi][64:65, :]), [osbb[oi]], [osbb[oi]])

                                def phase2(oi=oi, h=h, qt=qt):
                                    OP("pe", lambda e: e.matmul(PS[7][0:64, :], ones_f[64:65, 0:64], osb[oi][64:65, :], start=True, stop=True),
                                       [osbb[oi], b_onesf], [PSB[7]])
                                    tt(yat[oi][:], osb[oi][0:64, :], PS[7][0:64, :], ALU.mult, [osbb[oi], PSB[7]], [yatb[oi]])

                                    def phase3():
                                        hnorm(yat[oi][:], yatb[oi], og_at_sb[:, h:h + 1], b_og, yab[oi][:], yabb[oi], P=64)
                                        fw.dma("sp", ya_d[h, :, qt * TT:(qt + 1) * TT], yab[oi][:], reads=[yabb[oi]])
                                    return phase3
                                if pend3 is not None:
                                    pend3()
                                pend3 = pend2() if pend2 is not None else None
                                pend2 = phase2
                                yield ((nkt - 1) % 8) + 1
                        if pend3 is not None:
                            pend3()
                        if pend2 is not None:
                            pend2()()
                    gB, gC = gen_B(), gen_C()
                    aliveB = aliveC = True
                    cdone, bdone = 0, 0
                    CTOT, BTOT = 8 * sum(4 * q_ + 4 for q_ in range(8)), NT * 9
                    while aliveB or aliveC:
                        if aliveC:
                            try:
                                cdone += next(gC)
                            except StopIteration:
                                aliveC = False
                        while aliveB and (not aliveC or bdone * CTOT <= cdone * BTOT):
                            try:
                                next(gB)
                                bdone += 1
                            except StopIteration:
                                aliveB = False
                    fw.bg_on = False
                    fw.barrier()
            if stop_after == "C":
                break
            NSLOT = 80
            RS = 128
            SUB = RS // 128
            BIG = 1.0e4
            with ExitStack() as dl:
                msk_all = sb("msk_all", [128, 32, 16], F32, dl); eq1_all = sb("eq1_all", [128, 32, 16], F32, dl)
                comb_all = sb("comb_all", [128, 32, 16], F32, dl); b_all = Buf()
                r1i = sb("r1i", [128, 32], I32, dl); r2i = sb("r2i", [128, 32], I32, dl)
                w1s = sb("w1s", [128, 32], F32, dl); w2s = sb("w2s", [128, 32], F32, dl); b_rw = Buf()
                widx = sb("widx", [128, NSLOT], I32, dl); b_slot = Buf()
                with ExitStack() as st:
                    maskT = sb("maskT", [16, S], F32, dl); b_mT = Buf()
                    woa = sb("woa", [128, 4, D], BF16, st); wob = sb("wob", [64, 8, D], BF16, st)
                    fw.dma("pool", woa[:, 0:2, :], w_out[l, 0:256, :].rearrange("(kc p) n -> p kc n", p=128), writes=[Buf()])
                    fw.dma("pool", woa[:, 2:4, :], w_out[l, 768:1024, :].rearrange("(kc p) n -> p kc n", p=128), writes=[Buf()])
                    fw.dma("pool", wob[:], w_out[l, 256:768, :].rearrange("(h p) n -> p h n", p=64), writes=[Buf()])
                    fw.barrier()
                    zrow = sb("zrow", [128, 2048], F32, st); b_z = Buf()
                    OP("dve", lambda e: e.memset(zrow[:], 0.0), writes=[b_z])
                    for c_ in range(NSLOT * RS // 256):
                        fw.dma("pool", Xs_d[c_ * 256:(c_ + 1) * 256, :].rearrange("(p two) n -> p (two n)", two=2), zrow[:], reads=[b_z])
                    xt = sb("xtD", [128, 8, TT], F32, st); xtb = Buf()
                    ys = sb("ys", [128, 4, TT], BF16, st); ysb = Buf()
                    yatt = sb("yatt", [64, 8, TT], BF16, st); yattb = Buf()
                    sqb = sb("sqbD", [128, 8, TT], BF16, st); sqbb = Buf()
                    rt = sb("rtD", [128, TT], F32, st); rtb = Buf()
                    h2f = sb("h2f", [128, 8, TT], F32, st); h2fb = Buf()
                    h2 = sb("h2", [128, 8, TT], BF16, st); h2b = Buf()
                    htok = [sb(f"htok{i}", [128, D], F32, st) for i in range(2)]; htokb = [Buf() for _ in range(2)]
                    aff = sb("aff", [128, 4, 16], F32, st); selv = sb("selv", [128, 4, 16], F32, st); rtmp = sb("rtmp", [128, 4, 16], F32, st)
                    m1 = sb("m1", [128, 16], F32, st); m2 = sb("m2", [128, 16], F32, st); gm = sb("gm", [128, 4], F32, st)
                    b_r = Buf()
                    for it in range(NT):
                        t0 = it * TT
                        msk = msk_all[:, 4 * it:4 * it + 4, :]; comb = comb_all[:, 4 * it:4 * it + 4, :]; eq1 = eq1_all[:, 4 * it:4 * it + 4, :]
                        fw.dma("sp", xt[:], xT_d[:, :, t0:t0 + TT].rearrange("c p t -> p c t"), writes=[xtb])
                        fw.dma("sp", ys[:], yh_d[:, :, t0:t0 + TT].rearrange("c p t -> p c t"), writes=[ysb])
                        fw.dma("sp", yatt[:], ya_d[:, :, t0:t0 + TT].rearrange("h p t -> p h t"), writes=[yattb])
                        for mc in range(8):
                            ps = 4 + mc % 2

                            def f(e, mc=mc, ps=ps):
                                for kc in range(4):
                                    e.matmul(PS[ps][:, :], woa[:, kc, mc * 128:(mc + 1) * 128], ys[:, kc, :], start=(kc == 0), stop=False)
                                for hh in range(8):
                                    ins = e.matmul(PS[ps][:, :], wob[:, hh, mc * 128:(mc + 1) * 128], yatt[:, hh, :], start=False, stop=(hh == 7))
                                return ins
                            OP("pe", f, [ysb, yattb], [PSB[ps]])
                            stt(xt[:, mc, :], PS[ps][:, :], G1[:, mc:mc + 1], xt[:, mc, :], ALU.mult, ALU.add, [PSB[ps], b_mod, xtb], [xtb])
                        if debug and l == 0:
                            fw.dma("sp", dbg["dbg_xmid"][:, :, t0:t0 + TT].rearrange("c p t -> p c t"), xt[:], reads=[xtb])
                        fw.dma("sp", xT_d[:, :, t0:t0 + TT].rearrange("c p t -> p c t"), xt[:], reads=[xtb])
                        norm_mod(st, xt, xtb, A2, B2, b_mod, h2, h2b, h2f, h2fb, sqb, sqbb, rt, rtb, 7)
                        for sub in range(4):
                            hi_ = sub % 2
                            for half in range(2):
                                ps = half

                                def f(e, sub=sub, half=half, ps=ps):
                                    for c4 in range(4):
                                        c = half * 4 + c4
                                        ins = e.transpose(PS[ps][:, c4 * 128:(c4 + 1) * 128], h2f[:, c, sub * 128:(sub + 1) * 128], ident[:])
                                    return ins
                                OP("pe", f, [h2fb, b_ident], [PSB[ps]])
                                cp(htok[hi_][:, half * 512:(half + 1) * 512], PS[ps][:, :], [PSB[ps]], [htokb[hi_]],
                                   eng=("act" if half == 0 else "dve"))
                            fw.dma("sp", h2_d[t0 + sub * 128:t0 + (sub + 1) * 128, :], htok[hi_][:], reads=[htokb[hi_]])
                        for sub in range(4):
                            def f(e, sub=sub):
                                for kc in range(8):
                                    ins = e.matmul(PS[6][:, sub * 16:(sub + 1) * 16], h2f[:, kc, sub * 128:(sub + 1) * 128], wr_sb[:, kc, :],
                                                   start=(kc == 0), stop=(kc == 7))
                                return ins
                            OP("pe", f, [h2fb, b_wr], [PSB[6]])
                        act(aff[:].rearrange("p s e -> p (s e)"), PS[6][:, 0:64], AF.Sigmoid, [PSB[6]], [b_r])
                        tt(selv[:], aff[:], rb_sb[:].unsqueeze(1).to_broadcast([128, 4, 16]), ALU.add, [b_r, b_rb], [b_r])
                        s44 = selv[:].rearrange("p s (g e) -> p (s g) e", e=4)
                        r44 = rtmp[:].rearrange("p s (g e) -> p (s g) e", e=4)
                        RD = lambda o, i_, op: OP("dve", lambda e: e.tensor_reduce(out=o, in_=i_, axis=mybir.AxisListType.X, op=op), [b_r, b_all], [b_r, b_all])
                        RD(m1[:], s44, ALU.max)
                        tt(r44, s44, m1[:].unsqueeze(2).to_broadcast([128, 16, 4]), ALU.is_equal, [b_r], [b_r])
                        stt(r44, r44, -BIG, s44, ALU.mult, ALU.add, [b_r], [b_r])
                        RD(m2[:], r44, ALU.max)
                        tt(m1[:], m1[:], m2[:], ALU.add, [b_r], [b_r])
                        gs = m1[:].rearrange("p (s g) -> p s g", g=4)
                        RD(gm[:], gs, ALU.max)
                        m2v = m2[:].rearrange("p (s g) -> p s g", g=4)
                        tt(m2v, gs, gm[:].unsqueeze(2).to_broadcast([128, 4, 4]), ALU.is_equal, [b_r], [b_r])
                        ts(m2[:], m2[:], BIG, -BIG, ALU.mult, ALU.add, [b_r], [b_r])
                        tt(r44, s44, m2[:].unsqueeze(2).to_broadcast([128, 16, 4]), ALU.add, [b_r], [b_r])
                        RD(gm[:], rtmp[:], ALU.max)
                        tt(eq1, rtmp[:], gm[:].unsqueeze(2).to_broadcast([128, 4, 16]), ALU.is_equal, [b_r, b_all], [b_r, b_all])
                        stt(msk, eq1, -BIG, rtmp[:], ALU.mult, ALU.add, [b_r, b_all], [b_r, b_all])
                        RD(gm[:], msk, ALU.max)
                        tt(msk, rtmp[:], gm[:].unsqueeze(2).to_broadcast([128, 4, 16]), ALU.is_ge, [b_r, b_all], [b_r, b_all])
                        tt(comb, aff[:], msk, ALU.mult, [b_r, b_all], [b_r, b_all])
                        RD(gm[:], comb, ALU.add)
                        OP("dve", lambda e: e.reciprocal(out=gm[:], in_=gm[:]), [b_r], [b_r])
                        tt(comb, comb, gm[:].unsqueeze(2).to_broadcast([128, 4, 16]), ALU.mult, [b_r, b_all], [b_r, b_all])
                        if debug and l == 0:
                            fw.dma("sp", dbg["dbg_comb"][t0:t0 + TT, :].rearrange("(s p) e -> p s e", p=128), comb, reads=[b_all])

                        def f(e, it=it):
                            for sub in range(4):
                                ins = e.transpose(PS[6][0:16, sub * 128:(sub + 1) * 128], msk_all[:, 4 * it + sub, :], ident[:])
                            return ins
                        OP("pe", f, [b_all, b_ident], [PSB[6]])
                        cp(maskT[:, t0:t0 + TT], PS[6][0:16, :], [PSB[6]], [b_mT])
                    fw.barrier()
                with ExitStack() as st:
                    inc = sb("inc", [16, S], F32, st); b_s = Buf()
                    cntf = sb("cntf", [16, 2], F32, st); slf = sb("slf", [16, 2], F32, st); offf = sb("offf", [16, 1], F32, st)
                    endf = sb("endf", [16, 1], F32, st); cnti = sb("cnti", [16, 2], I32, st)
                    cmpt = sb("cmpt", [16, NSLOT], F32, st); sef = sb("sef", [128, NSLOT], F32, st); pidx = sb("pidx", [128, 1], F32, st); pit = sb("pit", [128, 128], F32, st)
                    pos_all = sb("pos_all", [128, 32, 16], F32, st); tmp3 = sb("tmp3", [128, 32, 16], F32, st)
                    rf = sb("rf", [128, 32], F32, st)
                    OP("dve", lambda e: e.tensor_tensor_scan(out=inc[:], data0=ones_f[0:16, 0:1].to_broadcast([16, S]), data1=maskT[:],
                                                             initial=0.0, op0=ALU.mult, op1=ALU.add), [b_mT, b_onesf], [b_s])
                    ts(cntf[:], inc[:, S - 1:S].to_broadcast([16, 2]), 1.0 / RS, (RS - 1.0) / RS - (RS - 1.0) / (2 * RS), ALU.mult, ALU.add, [b_s], [b_s])
                    cp(cnti[:], cntf[:], [b_s], [b_s])
                    cp(slf[:], cnti[:], [b_s], [b_s])
                    OP("pe", lambda e: e.matmul(PS[0][0:16, 0:2], tri_f[0:16, 0:16], slf[:], start=True, stop=True), [b_s, b_tri], [PSB[0]])
                    cp(offf[:], PS[0][0:16, 0:1], [PSB[0]], [b_s])
                    tt(endf[:], offf[:], slf[:, 0:1], ALU.add, [b_s], [b_s])
                    ts(offf[:], offf[:], float(RS), None, ALU.mult, None, [b_s], [b_s])
                    tt(inc[:], inc[:], maskT[:], ALU.subtract, [b_s, b_mT], [b_s])
                    ts(inc[:], inc[:], offf[:, 0:1], None, ALU.add, None, [b_s], [b_s])

                    def f(e):
                        for tk in range(32):
                            ins = e.transpose(PS[1][:, tk * 16:(tk + 1) * 16], inc[:, tk * 128:(tk + 1) * 128], ident[0:16, 0:16])
                        return ins
                    OP("pe", f, [b_s, b_ident], [PSB[1]])
                    cp(pos_all[:].rearrange("p k e -> p (k e)"), PS[1][:, :], [PSB[1]], [b_s])
                    RD2 = lambda o, i_: OP("dve", lambda e: e.tensor_reduce(out=o, in_=i_, axis=mybir.AxisListType.X, op=ALU.add), [b_s, b_all], [b_s, b_rw])
                    tt(tmp3[:], eq1_all[:], pos_all[:], ALU.mult, [b_s, b_all], [b_s])
                    RD2(rf[:], tmp3[:])
                    cp(r1i[:], rf[:], [b_s], [b_rw])
                    tt(tmp3[:], eq1_all[:], comb_all[:], ALU.mult, [b_s, b_all], [b_s])
                    RD2(w1s[:], tmp3[:])
                    tt(eq1_all[:], msk_all[:], eq1_all[:], ALU.subtract, [b_all], [b_all])
                    tt(tmp3[:], eq1_all[:], pos_all[:], ALU.mult, [b_s, b_all], [b_s])
                    RD2(rf[:], tmp3[:])
                    cp(r2i[:], rf[:], [b_s], [b_rw])
                    tt(tmp3[:], eq1_all[:], comb_all[:], ALU.mult, [b_s, b_all], [b_s])
                    RD2(w2s[:], tmp3[:])
                    ts(cmpt[:], iota[0:16, 0:NSLOT], endf[:, 0:1], None, ALU.is_ge, None, [b_iota, b_s], [b_s])
                    OP("pe", lambda e: e.matmul(PS[2][:, 0:NSLOT], ones_f[0:16, :], cmpt[:], start=True, stop=True), [b_s, b_onesf], [PSB[2]])
                    ts(sef[:], PS[2][:, 0:NSLOT], 15.0, float(l * NE), ALU.min, ALU.add, [PSB[2]], [b_s])
                    tt(pit[:], ident[:], iota[:, 0:128], ALU.mult, [b_ident, b_iota], [b_s])
                    OP("dve", lambda e: e.tensor_reduce(out=pidx[:], in_=pit[:], axis=mybir.AxisListType.X, op=ALU.add), [b_s], [b_s])
                    sk = sb("sk", [128, NSLOT], F32, st)
                    OP("dve", lambda e: e.memset(sk[:, 0:2], 0.0), writes=[b_s])
                    tt(sk[:, 2:NSLOT], sef[:, 2:NSLOT], sef[:, 0:NSLOT - 2], ALU.is_equal, [b_s], [b_s])
                    stt(sef[:], sef[:], 128.0, pidx[:, 0:1].to_broadcast([128, NSLOT]), ALU.mult, ALU.add, [b_s], [b_s])
                    stt(sef[:], sk[:], 1.0e6, sef[:], ALU.mult, ALU.add, [b_s], [b_s])
                    cp(widx[:], sef[:], [b_s], [b_slot])
                    fw.barrier()
                with ExitStack() as st:
                    hrow = [sb(f"hrow{i}", [128, D], F32, st) for i in range(3)]; hrowb = [Buf() for _ in range(3)]
                    for tk in range(32):
                        i = tk % 3
                        fw.dma("sp", hrow[i][:], h2_d[tk * 128:(tk + 1) * 128, :], writes=[hrowb[i]])
                        for ri in (r1i, r2i):
                            fw.dma_ind(Xs_d[:, :], bass.IndirectOffsetOnAxis(ap=ri[:, tk:tk + 1], axis=0), hrow[i][:], None,
                                       reads=[hrowb[i], b_rw])
                    fw.barrier()
                with ExitStack() as st:
                    wg = [sb(f"wg{i}", [128, 8, DE], BF16, st) for i in range(2)]; wgb = [Buf() for _ in range(2)]
                    wu = [sb(f"wu{i}", [128, 8, DE], BF16, st) for i in range(2)]; wub = [Buf() for _ in range(2)]
                    wd = [sb(f"wd{i}", [128, 4, D], BF16, st) for i in range(2)]; wdb = [Buf() for _ in range(2)]
                    xs = [sb(f"xs{i}", [128, D], F32, st) for i in range(3)]; xsb = [Buf() for _ in range(3)]
                    xsT = [sb(f"xsT{i}", [128, 8, 128], BF16, st) for i in range(2)]; xsTb = [Buf() for _ in range(2)]
                    sg = [sb(f"sg{i}", [128, DE], F32, st) for i in range(2)]; sgb = [Buf() for _ in range(2)]
                    hd = [sb(f"hd{i}", [128, DE], F32, st) for i in range(2)]; hdb = [Buf() for _ in range(2)]
                    hdT = [sb(f"hdT{i}", [128, 4, 128], BF16, st) for i in range(2)]; hdTb = [Buf() for _ in range(2)]
                    yt = [sb(f"yt{i}", [128, D], F32, st) for i in range(2)]; ytb = [Buf() for _ in range(2)]

                    if "bc" not in bc_val:
                        bc_reg = nc.gpsimd.alloc_register("bc_reg")
                        nc.gpsimd.reg_mov(bc_reg, 2 * NE * 128 - 1)
                        bc_val["bc"] = nc.gpsimd.snap(bc_reg, donate=True)
                    wg_rows = wg_d.rearrange("e p n -> (e p) n"); wu_rows = wu_d.rearrange("e p n -> (e p) n"); wd_rows = wd_d.rearrange("e p n -> (e p) n")

                    def load_gu(s_):
                        i = s_ % 2
                        off = bass.IndirectOffsetOnAxis(ap=widx[:, s_:s_ + 1], axis=0)
                        fw.dma_ind(wg[i][:].rearrange("p k n -> p (k n)"), None, wg_rows, off, reads=[b_slot], writes=[wgb[i]], bounds_check=bc_val["bc"])
                        fw.dma_ind(wu[i][:].rearrange("p k n -> p (k n)"), None, wu_rows, off, reads=[b_slot], writes=[wub[i]], bounds_check=bc_val["bc"])

                    def load_d(s_):
                        i = s_ % 2
                        off = bass.IndirectOffsetOnAxis(ap=widx[:, s_:s_ + 1], axis=0)
                        fw.dma_ind(wd[i][:].rearrange("p k n -> p (k n)"), None, wd_rows, off, reads=[b_slot], writes=[wdb[i]], bounds_check=bc_val["bc"])

                    def load_x(u_):
                        fw.dma("sp", xs[u_ % 3][:], Xs_d[u_ * 128:(u_ + 1) * 128, :], writes=[xsb[u_ % 3]])

                    def st_T(u_):
                        i = u_ % 2
                        x3 = u_ % 3
                        for half in range(2):
                            def f(e, half=half):
                                for c4 in range(4):
                                    c = half * 4 + c4
                                    ins = e.transpose(PS[half][:, c4 * 128:(c4 + 1) * 128], xs[x3][:, c * 128:(c + 1) * 128], ident[:])
                                return ins
                            OP("pe", f, [xsb[x3], b_ident], [PSB[half]])
                            cp(xsT[i][:, half * 4:(half + 1) * 4, :], PS[half][:, :].rearrange("p (c t) -> p c t", c=4), [PSB[half]], [xsTb[i]],
                               eng=("act" if half == 0 else "dve"))

                    def st_GU(u_):
                        i = u_ % 2
                        wi = (u_ // SUB) % 2
                        for (pp, w_, wb__) in ((2, wg[wi], wgb[wi]), (3, wu[wi], wub[wi])):
                            def f(e, pp=pp, w_=w_):
                                for kc in range(8):
                                    ins = e.matmul(PS[pp][:, :], xsT[i][:, kc, :], w_[:, kc, :], start=(kc == 0), stop=(kc == 7))
                                return ins
                            OP("pe", f, [xsTb[i], wb__], [PSB[pp]])
                        act(sg[i][:], PS[2][:, :], AF.Silu, [PSB[2]], [sgb[i]])
                        tt(hd[i][:], PS[3][:, :], sg[i][:], ALU.mult, [PSB[3], sgb[i]], [hdb[i]])

                    def st_HT(u_):
                        i = u_ % 2

                        def f(e):
                            for c4 in range(4):
                                ins = e.transpose(PS[4][:, c4 * 128:(c4 + 1) * 128], hd[i][:, c4 * 128:(c4 + 1) * 128], ident[:])
                            return ins
                        OP("pe", f, [hdb[i], b_ident], [PSB[4]])
                        cp(hdT[i][:], PS[4][:, :].rearrange("p (c t) -> p c t", c=4), [PSB[4]], [hdTb[i]], eng="act")

                    def st_D(u_):
                        i = u_ % 2
                        wi = (u_ // SUB) % 2
                        for half in range(2):
                            ps = 5 + half

                            def f(e, half=half, ps=ps):
                                for kc in range(4):
                                    ins = e.matmul(PS[ps][:, :], hdT[i][:, kc, :], wd[wi][:, kc, half * 512:(half + 1) * 512], start=(kc == 0), stop=(kc == 3))
                                return ins
                            OP("pe", f, [hdTb[i], wdb[wi]], [PSB[ps]])
                            cp(yt[i][:, half * 512:(half + 1) * 512], PS[ps][:, :], [PSB[ps]], [ytb[i]], eng=("dve" if half == 0 else "act"))
                        fw.dma("sp", Ys_d[u_ * 128:(u_ + 1) * 128, :], yt[i][:], reads=[ytb[i]])

                    NU = SUB * NSLOT
                    for s_ in range(2):
                        load_gu(s_)
                        load_d(s_)
                    for u_ in range(3):
                        load_x(u_)
                    for step in range(NU + 3):
                        if step < NU:
                            st_T(step)
                            if step + 3 < NU:
                                load_x(step + 3)
                        if 0 <= step - 1 < NU:
                            u_ = step - 1
                            st_GU(u_)
                            if u_ % SUB == SUB - 1 and u_ // SUB + 2 < NSLOT:
                                load_gu(u_ // SUB + 2)
                        if 0 <= step - 2 < NU:
                            st_HT(step - 2)
                        if 0 <= step - 3 < NU:
                            u_ = step - 3
                            st_D(u_)
                            if u_ % SUB == SUB - 1 and u_ // SUB + 2 < NSLOT:
                                load_d(u_ // SUB + 2)
                    fw.barrier()
                with ExitStack() as st:
                    y1 = [sb(f"y1_{i}", [128, D], F32, st) for i in range(3)]; y2 = [sb(f"y2_{i}", [128, D], F32, st) for i in range(3)]
                    y1b = [Buf() for _ in range(3)]; y2b = [Buf() for _ in range(3)]
                    ac = [sb(f"ac{i}", [128, D], F32, st) for i in range(2)]; acb = [Buf() for _ in range(2)]
                    xm = [sb(f"xm{i}", [128, 8, 128], F32, st) for i in range(3)]; xmb = [Buf() for _ in range(3)]
                    otile = [sb(f"otile{i}", [128, D], F32, st) for i in range(2)]; otb = [Buf() for _ in range(2)]
                    def issue5(tk):
                        i = tk % 3
                        fw.dma_ind(y1[i][:], None, Ys_d[:, :], bass.IndirectOffsetOnAxis(ap=r1i[:, tk:tk + 1], axis=0), reads=[b_rw], writes=[y1b[i]])
                        fw.dma_ind(y2[i][:], None, Ys_d[:, :], bass.IndirectOffsetOnAxis(ap=r2i[:, tk:tk + 1], axis=0), reads=[b_rw], writes=[y2b[i]])
                        fw.dma("sp", xm[i][:], xT_d[:, :, tk * 128:(tk + 1) * 128].rearrange("c p t -> p c t"), writes=[xmb[i]])
                    issue5(0)
                    issue5(1)
                    for tk in range(32):
                        i = tk % 2
                        j3 = tk % 3
                        if tk + 2 < 32:
                            issue5(tk + 2)
                        ts(ac[i][:], y1[j3][:], w1s[:, tk:tk + 1], None, ALU.mult, None, [y1b[j3], b_rw], [acb[i]])
                        stt(ac[i][:], y2[j3][:], w2s[:, tk:tk + 1], ac[i][:], ALU.mult, ALU.add, [y2b[j3], b_rw, acb[i]], [acb[i]])
                        for half in range(2):
                            ps = 2 * (tk % 2) + half

                            def f(e, half=half, ps=ps):
                                for c4 in range(4):
                                    c = half * 4 + c4
                                    ins = e.transpose(PS[ps][:, c4 * 128:(c4 + 1) * 128], ac[i][:, c * 128:(c + 1) * 128], ident[:])
                                return ins
                            OP("pe", f, [acb[i], b_ident], [PSB[ps]])
                            for c4 in range(4):
                                c = half * 4 + c4
                                stt(xm[j3][:, c, :], PS[ps][:, c4 * 128:(c4 + 1) * 128], G2[:, c:c + 1], xm[j3][:, c, :], ALU.mult, ALU.add,
                                    [PSB[ps], b_mod, xmb[j3]], [xmb[j3]])
                        if not last:
                            fw.dma("sp", xT_d[:, :, tk * 128:(tk + 1) * 128].rearrange("c p t -> p c t"), xm[j3][:], reads=[xmb[j3]])
                        else:
                            for half in range(2):
                                ps = 4 + 2 * (tk % 2) + half

                                def f(e, half=half, ps=ps):
                                    for c4 in range(4):
                                        c = half * 4 + c4
                                        ins = e.transpose(PS[ps][:, c4 * 128:(c4 + 1) * 128], xm[j3][:, c, :], ident[:])
                                    return ins
                                OP("pe", f, [xmb[j3], b_ident], [PSB[ps]])
                                cp(otile[i][:, half * 512:(half + 1) * 512], PS[ps][:, :], [PSB[ps]], [otb[i]], eng=("act" if half == 0 else "dve"))
                            fw.dma("sp", out_d[tk * 128:(tk + 1) * 128, :], otile[i][:], reads=[otb[i]])
                    fw.barrier()
    fw.barrier()
    return nc, fw, dbg


def host_inputs(inp, b):
    f = np.float32
    A = np.ascontiguousarray
    m = {}
    m["x"] = A(inp["x"][b])
    m["c_fm"] = A(inp["c"][b].reshape(8, 128).T)
    m["ada_w"] = inp["ada_w"]
    m["ada_b_fm"] = A(inp["ada_b"].reshape(2, 48, 128).transpose(0, 2, 1))
    m["n1g"] = A(inp["norm1_g"].reshape(2, 8, 128).transpose(0, 2, 1))
    m["n2g"] = A(inp["norm2_g"].reshape(2, 8, 128).transpose(0, 2, 1))
    m["w_in"] = inp["w_in"]
    m["fb"] = A(inp["forget_b"].reshape(2, 8, 1))
    def gp_lay(a):
        return A(a.reshape(2, 8, 2, 64).transpose(0, 2, 3, 1).reshape(2, 128, 8))
    m["lam_re"] = gp_lay(inp["lam_re"])
    m["lam_im"] = gp_lay(inp["lam_im"])
    m["log_dt"] = gp_lay(np.broadcast_to(inp["log_dt"][:, :, None], (2, 16, 64)))
    m["sb_re"] = A(inp["ssm_b_re"].reshape(2, 8, 2, 64, 16).transpose(0, 2, 3, 1, 4).reshape(2, 128, 8, 16))
    m["sb_im"] = A(inp["ssm_b_im"].reshape(2, 8, 2, 64, 16).transpose(0, 2, 3, 1, 4).reshape(2, 128, 8, 16))
    m["sc_re"] = A(inp["ssm_c_re"].reshape(2, 8, 2, 16, 64).transpose(0, 2, 4, 1, 3).reshape(2, 128, 8, 16))
    m["sc_im"] = A(inp["ssm_c_im"].reshape(2, 8, 2, 16, 64).transpose(0, 2, 4, 1, 3).reshape(2, 128, 8, 16))
    m["ssm_d"] = A(inp["ssm_d"].reshape(2, 2, 128).transpose(0, 2, 1))
    m["glu_w"] = inp["glu_w"]
    m["glu_b"] = A(inp["glu_b"].reshape(2, 2, 128).transpose(0, 2, 1))
    m["qg"] = A(np.tile(inp["q_norm_g"], (1, 2)).reshape(2, 128, 1))
    m["kg"] = A(np.tile(inp["k_norm_g"], (1, 2)).reshape(2, 128, 1))
    m["conv_w"] = A(inp["conv_w"].reshape(2, 3, 2, 128).transpose(0, 3, 2, 1))
    m["og_fm"] = A(inp["out_norm_g"].reshape(2, 8, 128).transpose(0, 2, 1))
    m["og_at"] = A(inp["out_norm_g"][:, 256:768].reshape(2, 8, 64).transpose(0, 2, 1))
    m["w_out"] = inp["w_out"]
    m["w_router"] = inp["w_router"]
    m["rbias"] = A(np.broadcast_to(inp["router_bias"][None, :], (128, 16)))
    m["w_gate"] = inp["w_gate"]
    m["w_up"] = inp["w_up"]
    m["w_down"] = inp["w_down"]
    m["ident"] = np.eye(128, dtype=f)
    e127 = np.zeros((128, 128), f); e127[127, :] = 1
    m["e127"] = e127
    blk = np.zeros((128, 128), f); blk[:64, :64] = 1; blk[64:, 64:] = 1
    m["blk64"] = blk
    m["tri"] = np.triu(np.ones((128, 128), f))
    m["iota"] = A(np.broadcast_to(np.arange(TT + 1, dtype=f)[None, :], (128, TT + 1)))
    sel = np.zeros((16, 16, 128), f)
    for e in range(16):
        sel[e, e, :] = 1
    m["sel16"] = sel
    return {k: np.asarray(v, dtype=f) for k, v in m.items()}


_CACHE = {}


def kernel(**inputs):
    inp = {k: np.asarray(v) for k, v in inputs.items()}
    if "nc" not in _CACHE:
        _CACHE["nc"] = build_program()[0]
    nc = _CACHE["nc"]
    in_maps = [host_inputs(inp, b) for b in range(8)]
    res = run_bass_kernel_spmd(nc, in_maps, core_ids=list(range(8)))
    out = np.stack([np.asarray(r["out"]) for r in res.results], axis=0)
    return out.astype(np.float32)
```

```python
import numpy as np
from contextlib import ExitStack
import concourse.bass as bass
import concourse.mybir as mybir
from concourse.bass_utils import run_bass_kernel_spmd

F32 = mybir.dt.float32
BF16 = mybir.dt.bfloat16
I32 = mybir.dt.int32
AF = mybir.ActivationFunctionType
ALU = mybir.AluOpType

S = 4096
D = 1024
TT = 512
NT = S // TT
DIN = 2568
NE = 16
DE = 512
EPS = 1e-6
TWO_PI = 6.283185307179586
import os as _os
NOCONV = bool(_os.environ.get('NOCONV'))
POOLENG = _os.environ.get('POOLENG', 'pool')
OFF_U, OFF_Q, OFF_K, OFF_V, OFF_F, OFF_HC, OFF_BG, OFF_CG = 0, 256, 768, 1280, 1792, 1800, 2056, 2312


class Buf:
    __slots__ = ("name", "w", "r")

    def __init__(self, name=""):
        self.name = name
        self.w = {}
        self.r = {}


class Fw:
    ENG = ("pe", "act", "dve", "pool", "sp")

    def __init__(self, nc, ndma=20):
        self.nc = nc
        self.eng = dict(pe=nc.tensor, act=nc.scalar, dve=nc.vector, pool=nc.gpsimd, sp=nc.sync)
        self.sem = {e: nc.alloc_semaphore("sem_" + e) for e in self.ENG}
        self.cnt = {e: 0 for e in self.ENG}
        self.known = {e: {} for e in self.ENG}
        self.dq = {}
        for q in ("sp", "pool"):
            self.dq[q] = dict(sems=[nc.alloc_semaphore(f"dq_{q}_{i}") for i in range(ndma)],
                              uses=[0] * ndma, nxt=0)
        self.allsems = {}
        self.nwaits = 0
        self.bg_on = False
        self.bg_sems = {s_.num for s_ in self.dq["pool"]["sems"]}

    def _wait(self, e, sem, val):
        k = self.known[e]
        if k.get(sem.num, 0) >= val:
            return
        self.eng[e].wait_ge(sem, val)
        self.nwaits += 1
        k[sem.num] = val

    def _deps(self, e, reads, writes):
        need = {}
        mysem = self.sem[e].num

        def add(tok, same_ok):
            sem, val = tok
            if same_ok and sem.num == mysem and e == "pe":
                return
            if need.get(sem.num, (None, 0))[1] < val:
                need[sem.num] = (sem, val)

        for b in reads:
            for tok in b.w.values():
                add(tok, False)
        for b in writes:
            for tok in b.w.values():
                add(tok, True)
            for tok in b.r.values():
                add(tok, True)
        for sem, val in need.values():
            self._wait(e, sem, val)

    def op(self, e, fn, reads=(), writes=()):
        self._deps(e, reads, writes)
        ins = fn(self.eng[e])
        self.cnt[e] += 1
        sem = self.sem[e]
        ins.then_inc(sem, 1)
        tok = (sem, self.cnt[e])
        for b in reads:
            b.r[sem.num] = tok
        for b in writes:
            b.w = {sem.num: tok}
            b.r = {}
        self.allsems[sem.num] = tok
        return tok

    def dma(self, q, out, in_, reads=(), writes=()):
        d = self.dq[q]
        i = d["nxt"]
        d["nxt"] = (i + 1) % len(d["sems"])
        sem = d["sems"][i]
        if d["uses"][i] > 0:
            self._wait(q, sem, 16 * d["uses"][i])
        self._deps(q, reads, writes)
        ins = self.eng[q].dma_start(out=out, in_=in_)
        d["uses"][i] += 1
        tok = (sem, 16 * d["uses"][i])
        ins.then_inc(sem, 16)
        for b in reads:
            b.r[sem.num] = tok
        for b in writes:
            b.w = {sem.num: tok}
            b.r = {}
        self.allsems[sem.num] = tok
        return tok

    def dma_ind(self, out, out_off, in_, in_off, reads=(), writes=(), bounds_check=None):
        q = "pool"
        d = self.dq[q]
        i = d["nxt"]
        d["nxt"] = (i + 1) % len(d["sems"])
        sem = d["sems"][i]
        if d["uses"][i] > 0:
            self._wait(q, sem, 16 * d["uses"][i])
        self._deps(q, reads, writes)
        if bounds_check is None:
            ins = self.eng[q].indirect_dma_start(out=out, out_offset=out_off, in_=in_, in_offset=in_off)
        else:
            ins = self.eng[q].indirect_dma_start(out=out, out_offset=out_off, in_=in_, in_offset=in_off,
                                                 bounds_check=bounds_check, oob_is_err=False)
        d["uses"][i] += 1
        tok = (sem, 16 * d["uses"][i])
        ins.then_inc(sem, 16)
        for b in reads:
            b.r[sem.num] = tok
        for b in writes:
            b.w = {sem.num: tok}
            b.r = {}
        self.allsems[sem.num] = tok
        return tok

    def barrier(self):
        for e in self.ENG:
            for sem, val in list(self.allsems.values()):
                if self.bg_on and sem.num in self.bg_sems:
                    continue
                self._wait(e, sem, val)


def build_program(nlayers=2, debug=False, stop_after=None):
    nc = bass.Bass("TRN2", target_bir_lowering=False)
    fw = Fw(nc)
    dbg = {}

    def din(name, shape, dt=F32):
        return nc.dram_tensor(name, list(shape), dt, kind="ExternalInput").ap()

    def dscr(name, shape, dt=F32):
        if debug:
            return nc.dram_tensor(name, list(shape), dt, kind="ExternalOutput").ap()
        return nc.dram_tensor(name, list(shape), dt).ap()

    x_in = din("x", [S, D])
    c_fm = din("c_fm", [128, 8])
    ada_w = din("ada_w", [2, D, 6 * D])
    ada_b_fm = din("ada_b_fm", [2, 128, 48])
    n1g = din("n1g", [2, 128, 8])
    n2g = din("n2g", [2, 128, 8])
    w_in = din("w_in", [2, D, DIN])
    fb = din("fb", [2, 8, 1])
    lam_re = din("lam_re", [2, 128, 8])
    lam_im = din("lam_im", [2, 128, 8])
    log_dt = din("log_dt", [2, 128, 8])
    sb_re = din("sb_re", [2, 128, 8, 16])
    sb_im = din("sb_im", [2, 128, 8, 16])
    sc_re = din("sc_re", [2, 128, 8, 16])
    sc_im = din("sc_im", [2, 128, 8, 16])
    ssm_d = din("ssm_d", [2, 128, 2])
    glu_w = din("glu_w", [2, 256, 256])
    glu_b = din("glu_b", [2, 128, 2])
    qg = din("qg", [2, 128, 1])
    kg = din("kg", [2, 128, 1])
    conv_w = din("conv_w", [2, 128, 2, 3])
    og_fm = din("og_fm", [2, 128, 8])
    og_at = din("og_at", [2, 64, 8])
    w_out = din("w_out", [2, D, D])
    w_router = din("w_router", [D, NE])
    rbias = din("rbias", [128, NE])
    w_gate = din("w_gate", [2, NE, D, DE])
    w_up = din("w_up", [2, NE, D, DE])
    w_down = din("w_down", [2, NE, DE, D])
    ident_in = din("ident", [128, 128])
    e127_in = din("e127", [128, 128])
    blk64_in = din("blk64", [128, 128])
    tri_in = din("tri", [128, 128])
    iota_in = din("iota", [128, TT + 1])
    sel_in = din("sel16", [16, NE, 128])
    out_d = nc.dram_tensor("out", [S, D], F32, kind="ExternalOutput").ap()

    xT_d = dscr("xT_d", [8, 128, S])
    wg_d = dscr("wg_d", [2 * NE, 128, 8 * DE], BF16)
    wu_d = dscr("wu_d", [2 * NE, 128, 8 * DE], BF16)
    wd_d = dscr("wd_d", [2 * NE, 128, 4 * D], BF16)
    uT_d = dscr("uT_d", [2, 128, S])
    qT_d = dscr("qT_d", [8, 65, S], BF16)
    kT_d = dscr("kT_d", [8, 65, S], BF16)
    yh_d = dscr("yh_d", [4, 128, S], BF16)
    ya_d = dscr("ya_d", [4, 128, S], BF16)
    h2_d = dscr("h2_d", [S, D])
    Xs_d = dscr("Xs_d", [80 * 128, D])
    Ys_d = dscr("Ys_d", [80 * 128, D])
    if debug:
        for nm, shp, dt in (("dbg_h", [8, 128, S], BF16), ("dbg_cum", [8, S], F32), ("dbg_mod", [128, 48], F32),
                            ("dbg_v", [128, 32, 8, 65], BF16), ("dbg_ssmpre", [2, 128, S], F32),
                            ("dbg_comb", [S, NE], F32), ("dbg_xmid", [8, 128, S], F32)):
            dbg[nm] = nc.dram_tensor(nm, shp, dt, kind="ExternalOutput").ap()

    es = ExitStack()

    uid = [0]

    def sb(name, shape, dt=F32, stack=None):
        uid[0] += 1
        return (stack or es).enter_context(nc.sbuf_tensor(f"s{uid[0]}_{name}", list(shape), dt))

    PS = [es.enter_context(nc.psum_tensor(f"ps{i}", [128, 512], F32)) for i in range(8)]
    PSB = [Buf(f"ps{i}") for i in range(8)]

    ident = sb("ident", [128, 128]); b_ident = Buf()
    e127 = sb("e127", [128, 128]); b_e127 = Buf()
    tri_f = sb("tri_f", [128, 128]); b_tri = Buf()
    blk_f = sb("blk_f", [128, 128]); blk = sb("blk", [128, 128], BF16); b_blk = Buf()
    ones_b = sb("ones_b", [128, 128], BF16); b_ones = Buf()
    ones_f = sb("ones_f", [128, 128]); b_onesf = Buf()
    iota = sb("iota", [128, TT + 1]); b_iota = Buf()
    sel16_f = sb("sel16_f", [16, NE, 128]); b_sel = Buf()
    cfm = sb("cfm", [128, 8]); b_cfm = Buf()
    cact = sb("cact", [128, 8]); b_cact = Buf()
    rb_sb = sb("rb_sb", [128, NE]); b_rb = Buf()
    wr_sb = sb("wr_sb", [128, 8, NE]); b_wr = Buf()

    fw.dma("sp", ident[:], ident_in, writes=[b_ident])
    fw.dma("sp", e127[:], e127_in, writes=[b_e127])
    fw.dma("sp", tri_f[:], tri_in, writes=[b_tri])
    fw.dma("sp", blk_f[:], blk64_in, writes=[b_blk])
    fw.dma("sp", iota[:], iota_in, writes=[b_iota])
    fw.dma("sp", sel16_f[:], sel_in, writes=[b_sel])
    fw.dma("sp", cfm[:], c_fm, writes=[b_cfm])
    fw.dma("sp", rb_sb[:], rbias, writes=[b_rb])
    fw.dma("sp", wr_sb[:], w_router.rearrange("(kc p) n -> p kc n", p=128), writes=[b_wr])
    fw.op("dve", lambda e: e.tensor_copy(out=blk[:], in_=blk_f[:]), reads=[b_blk], writes=[b_blk])
    fw.op("dve", lambda e: e.memset(ones_b[:], 1.0), writes=[b_ones])
    fw.op("dve", lambda e: e.memset(ones_f[:], 1.0), writes=[b_onesf])
    fw.op("act", lambda e: e.activation(out=cact[:], in_=cfm[:], func=AF.Silu), reads=[b_cfm], writes=[b_cact])

    def OP(e, fn, reads=(), writes=()):
        return fw.op(e, fn, reads, writes)

    def act(out, in_, func, reads, writes, scale=1.0, bias=0.0):
        return fw.op("act", lambda e: e.activation(out=out, in_=in_, func=func, bias=bias, scale=scale), reads, writes)

    def tt(out, in0, in1, op, reads, writes, eng="dve"):
        return fw.op(eng, lambda e: e.tensor_tensor(out=out, in0=in0, in1=in1, op=op), reads, writes)

    def ts(out, in0, s1, s2, op0, op1, reads, writes, eng="dve"):
        if op1 is None:
            return fw.op(eng, lambda e: e.tensor_scalar(out=out, in0=in0, scalar1=s1, scalar2=None, op0=op0), reads, writes)
        return fw.op(eng, lambda e: e.tensor_scalar(out=out, in0=in0, scalar1=s1, scalar2=s2, op0=op0, op1=op1), reads, writes)

    def stt(out, in0, scalar, in1, op0, op1, reads, writes):
        return fw.op("dve", lambda e: e.scalar_tensor_tensor(out=out, in0=in0, scalar=scalar, in1=in1, op0=op0, op1=op1),
                     reads, writes)

    def cp(out, in_, reads, writes, eng="dve"):
        if eng == "act":
            return fw.op("act", lambda e: e.copy(out=out, in_=in_), reads, writes)
        return fw.op(eng, lambda e: e.tensor_copy(out=out, in_=in_), reads, writes)

    ntri = sb("ntri", [128, 128], BF16); ident_b = sb("ident_b", [128, 128], BF16); b_ntri = Buf()
    tt(tri_f[:], tri_f[:], ident[:], ALU.subtract, [b_tri, b_ident], [b_tri])
    ts(ntri[:], tri_f[:], -30000.0, None, ALU.mult, None, [b_tri], [b_ntri])
    cp(ident_b[:], ident[:], [b_ident], [b_ntri])
    eps_c = sb("eps_c", [128, 1]); b_eps = Buf()
    OP("dve", lambda e: e.memset(eps_c[:], EPS), writes=[b_eps])

    hn_sq = [sb(f"hn_sq{i}", [128, TT], BF16) for i in range(2)]; hn_sqb = [Buf() for _ in range(2)]
    hn_rt = [sb(f"hn_rt{i}", [128, TT]) for i in range(2)]; hn_rtb = [Buf() for _ in range(2)]
    hn_ctr = [0]
    HN_PS = 7

    def hnorm(src, srcb, g_ap, gb, out, outb, P=128, n=TT):
        i = hn_ctr[0] % 2
        hn_ctr[0] += 1
        act(hn_sq[i][0:P, 0:n], src, AF.Square, [srcb], [hn_sqb[i]])
        OP("pe", lambda e: e.matmul(PS[HN_PS][0:P, 0:n], blk[0:P, 0:P], hn_sq[i][0:P, 0:n], start=True, stop=True),
           [hn_sqb[i], b_blk], [PSB[HN_PS]])
        act(hn_rt[i][0:P, 0:n], PS[HN_PS][0:P, 0:n], AF.Ln, [PSB[HN_PS], b_eps], [hn_rtb[i]], scale=1.0 / 64, bias=eps_c[0:P, :])
        act(hn_rt[i][0:P, 0:n], hn_rt[i][0:P, 0:n], AF.Exp, [hn_rtb[i]], [hn_rtb[i]], scale=-0.5)
        stt(out, src, g_ap, hn_rt[i][0:P, 0:n], ALU.mult, ALU.mult, [srcb, gb, hn_rtb[i]], [outb])

    mods, A1s, A2s, b_mods = [], [], [], []
    for l_ in range(nlayers):
        mods.append(sb(f"mod{l_}", [128, 48])); A1s.append(sb(f"A1_{l_}", [128, 8])); A2s.append(sb(f"A2_{l_}", [128, 8])); b_mods.append(Buf())
    with ExitStack() as st:
        xin = [sb(f"xin{i}", [128, D], F32, st) for i in range(4)]
        xinb = [Buf() for _ in range(4)]
        xo = [sb(f"xo{i}", [128, 8, 128], F32, st) for i in range(4)]
        xob = [Buf() for _ in range(4)]
        n1g_sb = sb("n1g_sb", [128, nlayers, 8], F32, st); n2g_sb = sb("n2g_sb", [128, nlayers, 8], F32, st); b_ng = Buf()
        adab = sb("adab", [128, nlayers, 48], F32, st); b_adab = Buf()
        cact2 = sb("cact2", [128, 8, 2], F32, st); b_cact2 = Buf()
        adw = [sb(f"adw{i}", [128, 8, D], F32, st) for i in range(2)]
        adwb = [Buf() for _ in range(2)]
        for l_ in range(nlayers):
            fw.dma("sp", n1g_sb[:, l_, :], n1g[l_], writes=[b_ng])
            fw.dma("sp", n2g_sb[:, l_, :], n2g[l_], writes=[b_ng])
            fw.dma("sp", adab[:, l_, :], ada_b_fm[l_], writes=[b_adab])
        cp(cact2[:], cact[:].unsqueeze(2).to_broadcast([128, 8, 2]), [b_cact], [b_cact2])

        def ada_units():
            for l_ in range(nlayers):
                for j in range(6):
                    i = (l_ * 6 + j) % 2
                    fw.dma("sp", adw[i][:], ada_w[l_, :, j * D:(j + 1) * D].rearrange("(kc p) n -> p kc n", p=128), writes=[adwb[i]])
                    for c in range(8):
                        def f(e, i=i, j=j, c=c, l_=l_):
                            for kc in range(8):
                                col = 2 * (j * 8 + c)
                                ins = e.matmul(PS[l_][:, col:col + 2], adw[i][:, kc, c * 128:(c + 1) * 128], cact2[:, kc, :],
                                               start=(kc == 0), stop=(kc == 7))
                            return ins
                        OP("pe", f, [adwb[i], b_cact2], [PSB[l_]])
                        yield
        au = ada_units()
        per = (nlayers * 48 + 31) // 32
        for tk in range(S // 128):
            i = tk % 4
            fw.dma("sp", xin[i][:], x_in[tk * 128:(tk + 1) * 128, :], writes=[xinb[i]])
            for _ in range(per):
                next(au, None)
            for half in range(2):
                pb = 2 + (2 * tk + half) % 6

                def f(e, i=i, half=half, pb=pb):
                    for c4 in range(4):
                        c = half * 4 + c4
                        ins = e.transpose(PS[pb][:, c4 * 128:(c4 + 1) * 128], xin[i][:, c * 128:(c + 1) * 128], ident[:])
                    return ins
                OP("pe", f, [xinb[i], b_ident], [PSB[pb]])
                cp(xo[i][:, half * 4:(half + 1) * 4, :], PS[pb][:].rearrange("p (c t) -> p c t", c=4),
                   [PSB[pb]], [xob[i]], eng=("act" if half == 0 else "dve"))
            fw.dma("sp", xT_d[:, :, tk * 128:(tk + 1) * 128].rearrange("c p t -> p c t"), xo[i][:], reads=[xob[i]])
        for _ in au:
            pass
        for l_ in range(nlayers):
            tt(mods[l_][:], PS[l_][:, 0:96].rearrange("p (n two) -> p n two", two=2)[:, :, 0], adab[:, l_, :], ALU.add,
               [PSB[l_], b_adab], [b_mods[l_]])
            stt(A1s[l_][:], mods[l_][:, 8:16], 1.0, n1g_sb[:, l_, :], ALU.add, ALU.mult, [b_mods[l_], b_ng], [b_mods[l_]])
            stt(A2s[l_][:], mods[l_][:, 32:40], 1.0, n2g_sb[:, l_, :], ALU.add, ALU.mult, [b_mods[l_], b_ng], [b_mods[l_]])
        if debug:
            fw.dma("sp", dbg["dbg_mod"], mods[0][:], reads=[b_mods[0]])
        fw.barrier()

    def norm_mod(st_, xt, xtb, A, B, ABb, hb, hbb, xn, xnb, sqb, sqbb, rt, rtb, psn):
        act(sqb[:], xt[:], AF.Square, [xtb], [sqbb])

        def f(e):
            for c in range(8):
                ins = e.matmul(PS[psn][:, :], ones_b[:], sqb[:, c, :], start=(c == 0), stop=(c == 7))
            return ins
        OP("pe", f, [sqbb, b_ones], [PSB[psn]])
        act(rt[:], PS[psn][:, :], AF.Ln, [PSB[psn], b_eps], [rtb], scale=1.0 / D, bias=eps_c[:])
        act(rt[:], rt[:], AF.Exp, [rtb], [rtb], scale=-0.5)
        tt(xn[:], xt[:], rt[:].unsqueeze(1).to_broadcast([128, 8, TT]), ALU.mult, [xtb, rtb], [xnb])
        for c in range(8):
            if c % 2 == 0:
                ts(xn[:, c, :], xn[:, c, :], A[:, c:c + 1], B[:, c:c + 1], ALU.mult, ALU.add, [xnb, ABb], [xnb])
            else:
                act(xn[:, c, :], xn[:, c, :], AF.Identity, [xnb, ABb], [xnb], scale=A[:, c:c + 1], bias=B[:, c:c + 1])
        cp(hb[:, 0:4, :], xn[:, 0:4, :], [xnb], [hbb], eng="dve")
        cp(hb[:, 4:8, :], xn[:, 4:8, :], [xnb], [hbb], eng="act")

    bc_val = {}
    for l in range(nlayers if stop_after != "0" else 0):
        last = (l == nlayers - 1)
        with ExitStack() as lay:
            mod = mods[l]; A1 = A1s[l]; A2 = A2s[l]; b_mod = b_mods[l]
            B1 = mod[:, 0:8]; G1 = mod[:, 16:24]; B2 = mod[:, 24:32]; G2 = mod[:, 40:48]

            with ExitStack() as mix:
                V_sb = sb("V_sb", [128, 32, 8, 65], BF16, mix); b_V = Buf()
                cum = sb("cum", [8, S], F32, mix); b_cum = Buf()
                og_fm_sb = sb("og_fm_sb", [128, 8], F32, mix); og_at_sb = sb("og_at_sb", [64, 8], F32, mix); b_og = Buf()
                fw.dma("sp", og_fm_sb[:], og_fm[l], writes=[b_og])
                fw.dma("sp", og_at_sb[:], og_at[l], writes=[b_og])
                OP("dve", lambda e: e.memset(V_sb[:, :, :, 64:65], 1.0), writes=[b_V])
                if l == 0:
                    cv = [sb(f"cv{i}", [128, 2048], BF16, mix) for i in range(2)]; cvb = [Buf() for _ in range(2)]; cvk = [0]
                fw.barrier()
                with ExitStack() as st:
                    win = sb("win", [128, 8, DIN], BF16, st); b_win = Buf()
                    for hh in range(2):
                        fw.dma("pool", win[:, :, hh * 1284:(hh + 1) * 1284],
                               w_in[l, :, hh * 1284:(hh + 1) * 1284].rearrange("(kc p) n -> p kc n", p=128), writes=[Buf()])
                    fw.barrier()
                    if l == 0 and not NOCONV:
                        for l2 in range(nlayers):
                            for e_ in range(NE):
                                for (src, dst, pat) in ((w_gate, wg_d, 8), (w_up, wu_d, 8), (w_down, wd_d, 4)):
                                    for hf in range(2):
                                        i = cvk[0] % 2
                                        cvk[0] += 1
                                        kcs = pat // 2
                                        srcap = src[l2, e_].rearrange("(kc p) n -> p kc n", p=128)[:, hf * kcs:(hf + 1) * kcs, :]
                                        dstv = cv[i][:].rearrange("p (kc n) -> p kc n", kc=kcs)
                                        fw.dma("pool", dstv, srcap, writes=[cvb[i]])
                                        fw.dma("pool", dst[l2 * NE + e_][:, hf * 2048:(hf + 1) * 2048], cv[i][:], reads=[cvb[i]])
                        fw.bg_on = True
                    fbs = sb("fbs", [8, 1], F32, st); b_fb = Buf()
                    qgs = sb("qgs", [128, 1], F32, st); kgs = sb("kgs", [128, 1], F32, st); b_qk = Buf()
                    cws = sb("cws", [128, 2, 3], F32, st); b_cw = Buf()
                    fw.dma("sp", fbs[:], fb[l], writes=[b_fb])
                    fw.dma("sp", qgs[:], qg[l], writes=[b_qk])
                    fw.dma("sp", kgs[:], kg[l], writes=[b_qk])
                    fw.dma("sp", cws[:], conv_w[l], writes=[b_cw])
                    ts(fbs[:], fbs[:], -1.0, None, ALU.mult, None, [b_fb], [b_fb])
                    ts(qgs[:], qgs[:], 0.125, None, ALU.mult, None, [b_qk], [b_qk])
                    if l == 0:
                        xt = [sb("xt0", [128, 8, TT], F32, st)] * 2; xtb = [Buf()] * 2
                    else:
                        xt = [sb(f"xt{i}", [128, 8, TT], F32, st) for i in range(2)]; xtb = [Buf() for _ in range(2)]
                    sqb = sb("sqb", [128, 8, TT], BF16, st); sqbb = Buf()
                    rt = sb("rt", [128, TT], F32, st); rtb = Buf()
                    xn = sb("xn", [128, 8, TT], F32, st); xnb = Buf()
                    hb = [sb(f"hb{i}", [128, 8, TT], BF16, st) for i in range(2)]; hbb = [Buf() for _ in range(2)]
                    ev = [sb(f"ev{i}", [128, TT], F32, st) for i in range(4)]; evb = [Buf() for _ in range(4)]
                    evo = [sb(f"evo{i}", [128, TT], BF16, st) for i in range(4)]; evob = [Buf() for _ in range(4)]
                    zt = [[sb(f"zt{cc}{i}", [128, TT + 2], F32, st) for i in range(2)] for cc in range(2)]
                    ztb = [[Buf() for _ in range(2)] for _ in range(2)]
                    cy = sb("cy", [128, TT], F32, st); cyb = Buf()
                    fe = sb("fe", [8, TT], F32, st); feb = Buf()
                    evc = [0]
                    gen = [0]
                    for cc in range(2):
                        OP("dve", lambda e, cc=cc: e.memset(zt[cc][1][:, TT:TT + 2], 0.0), writes=[ztb[cc][1]])

                    def proj(ps, off, M, hbt, hbtb):
                        def f(e):
                            for kc in range(8):
                                ins = e.matmul(PS[ps][0:M, :], win[:, kc, off:off + M], hbt[:, kc, :], start=(kc == 0), stop=(kc == 7))
                            return ins
                        OP("pe", f, [hbtb], [PSB[ps]])

                    for it in range(NT):
                        t0 = it * TT
                        i = it % 2
                        fw.dma("sp", xt[i][:], xT_d[:, :, t0:t0 + TT].rearrange("c p t -> p c t"), writes=[xtb[i]])
                        norm_mod(st, xt[i], xtb[i], A1, B1, b_mod, hb[i], hbb[i], xn, xnb, sqb, sqbb, rt, rtb, 0)
                        if debug and l == 0:
                            fw.dma("sp", dbg["dbg_h"][:, :, t0:t0 + TT].rearrange("c p t -> p c t"), hb[i][:], reads=[hbb[i]])
                        for c in range(2):
                            ps = 1 + gen[0] % 2; gen[0] += 1
                            proj(ps, OFF_U + c * 128, 128, hb[i], hbb[i])
                            k = evc[0] % 4; evc[0] += 1
                            cp(ev[k][:], PS[ps][:, :], [PSB[ps]], [evb[k]], eng="act")
                            fw.dma("sp", uT_d[c, :, t0:t0 + TT], ev[k][:], reads=[evb[k]])
                        for (off, gsb, dst) in ((OFF_Q, qgs, qT_d), (OFF_K, kgs, kT_d)):
                            for c in range(4):
                                ps = 1 + gen[0] % 2; gen[0] += 1
                                proj(ps, off + c * 128, 128, hb[i], hbb[i])
                                k = evc[0] % 4; evc[0] += 1
                                cp(ev[k][:], PS[ps][:, :], [PSB[ps]], [evb[k]], eng="act")
                                hnorm(ev[k][:], evb[k], gsb[:, 0:1], b_qk, evo[k][:], evob[k])
                                fw.dma("sp", dst[2 * c, 0:64, t0:t0 + TT], evo[k][0:64, :], reads=[evob[k]])
                                fw.dma("sp", dst[2 * c + 1, 0:64, t0:t0 + TT], evo[k][64:128, :], reads=[evob[k]])
                        ps = 1 + gen[0] % 2; gen[0] += 1
                        proj(ps, OFF_F, 8, hb[i], hbb[i])
                        act(fe[:], PS[ps][0:8, :], AF.Exp, [PSB[ps], b_fb], [feb], scale=-1.0, bias=fbs[:])
                        act(fe[:], fe[:], AF.Ln, [feb], [feb], bias=1.0)
                        init = 0.0 if it == 0 else cum[:, t0 - 1:t0]
                        OP("dve", lambda e, t0=t0, init=init: e.tensor_tensor_scan(
                            out=cum[:, t0:t0 + TT], data0=ones_f[0:8, 0:1].to_broadcast([8, TT]), data1=fe[:], initial=init,
                            op0=ALU.mult, op1=ALU.subtract), [feb, b_onesf, b_cum], [b_cum])
                        for sub in range(4):
                            def f(e, sub=sub, i=i):
                                for kc in range(8):
                                    ins = e.matmul(PS[3][:, :], hb[i][:, kc, sub * 128:(sub + 1) * 128], win[:, kc, OFF_V:OFF_V + 512],
                                                   start=(kc == 0), stop=(kc == 7))
                                return ins
                            OP("pe", f, [hbb[i]], [PSB[3]])
                            cp(V_sb[:, 4 * it + sub, :, 0:64], PS[3][:, :].rearrange("p (h d) -> p h d", h=8), [PSB[3]], [b_V],
                               eng=("act" if sub % 2 else "dve"))
                        for cc in range(2):
                            proj(4, OFF_HC + cc * 128, 128, hb[i], hbb[i])
                            proj(5, OFF_CG + cc * 128, 128, hb[i], hbb[i])
                            proj(6, OFF_BG + cc * 128, 128, hb[i], hbb[i])
                            k = evc[0] % 4; evc[0] += 1
                            z, zb = zt[cc][i], ztb[cc][i]
                            zp, zpb = zt[cc][1 - i], ztb[cc][1 - i]
                            cp(ev[k][:], PS[5][:, :], [PSB[5]], [evb[k]], eng="act")
                            cp(z[:, 0:2], zp[:, TT:TT + 2], [zpb], [zb])
                            tt(z[:, 2:TT + 2], PS[4][:, :], ev[k][:], ALU.mult, [PSB[4], evb[k]], [zb])
                            ts(cy[:], z[:, 2:TT + 2], cws[:, cc, 2:3], None, ALU.mult, None, [zb, b_cw], [cyb])
                            stt(cy[:], z[:, 1:TT + 1], cws[:, cc, 1:2], cy[:], ALU.mult, ALU.add, [zb, b_cw, cyb], [cyb])
                            stt(cy[:], z[:, 0:TT], cws[:, cc, 0:1], cy[:], ALU.mult, ALU.add, [zb, b_cw, cyb], [cyb])
                            tt(ev[k][:], PS[6][:, :], cy[:], ALU.mult, [PSB[6], cyb], [evb[k]])
                            hnorm(ev[k][:], evb[k], og_fm_sb[:, 6 + cc:7 + cc], b_og, evo[k][:], evob[k])
                            fw.dma("sp", yh_d[2 + cc, :, t0:t0 + TT], evo[k][:], reads=[evob[k]])
                    if debug and l == 0:
                        fw.dma("sp", dbg["dbg_cum"], cum[:], reads=[b_cum])
                        fw.dma("sp", dbg["dbg_v"], V_sb[:], reads=[b_V])
                    fw.barrier()
                if stop_after == "A":
                    break
                with ExitStack() as st:
                    def t8(name):
                        return sb(name, [128, 8], F32, st)
                    lre, lim, ldt = t8("lre"), t8("lim"), t8("ldt"); b_p = Buf()
                    fw.dma("sp", lre[:], lam_re[l], writes=[b_p])
                    fw.dma("sp", lim[:], lam_im[l], writes=[b_p])
                    fw.dma("sp", ldt[:], log_dt[l], writes=[b_p])
                    bre = sb("bre", [128, 8, 16], F32, st); bim = sb("bim", [128, 8, 16], F32, st)
                    cre = sb("cre", [128, 8, 16], F32, st); cim = sb("cim", [128, 8, 16], F32, st); b_bc = Buf()
                    fw.dma("sp", bre[:], sb_re[l], writes=[b_bc]); fw.dma("sp", bim[:], sb_im[l], writes=[b_bc])
                    fw.dma("sp", cre[:], sc_re[l], writes=[b_bc]); fw.dma("sp", cim[:], sc_im[l], writes=[b_bc])
                    dsk = sb("dsk", [128, 2], F32, st); glb = sb("glb", [128, 2], F32, st); b_dg = Buf()
                    fw.dma("sp", dsk[:], ssm_d[l], writes=[b_dg]); fw.dma("sp", glb[:], glu_b[l], writes=[b_dg])
                    gluw = sb("gluw", [128, 2, 256], BF16, st); gluwf = sb("gluwf", [128, 2, 256], F32, st); b_gw = Buf()
                    fw.dma("sp", gluwf[:], glu_w[l].rearrange("(kc p) n -> p kc n", p=128), writes=[b_gw])
                    cp(gluw[:], gluwf[:], [b_gw], [b_gw], eng="act")
                    r_sb, th = t8("r_sb"), t8("th")
                    dtv, a_, cs, sn, t1_, t2_, zre, zim = t8("dtv"), t8("a_"), t8("cs"), t8("sn"), t8("t1_"), t8("t2_"), t8("zre"), t8("zim")
                    ti = sb("ti", [128, 8], I32, st)
                    C1 = 6.28125
                    C2 = TWO_PI - C1

                    def sincos(out, ang, shape, tmpf, tmpi, bq, shift):
                        ts(tmpf, ang, 1.0 / TWO_PI, shift / TWO_PI, ALU.mult, ALU.add, [bq], [bq])
                        cp(tmpi, tmpf, [bq], [bq])
                        cp(tmpf, tmpi, [bq], [bq])
                        if shift != 0.0:
                            ts(out, ang, shift, None, ALU.add, None, [bq], [bq])
                            stt(out, tmpf, -C1, out, ALU.mult, ALU.add, [bq], [bq])
                        else:
                            stt(out, tmpf, -C1, ang, ALU.mult, ALU.add, [bq], [bq])
                        stt(out, tmpf, -C2, out, ALU.mult, ALU.add, [bq], [bq])
                        ts(out, out, 3.1415925, -3.1415925, ALU.min, ALU.max, [bq], [bq])
                        act(out, out, AF.Sin, [bq], [bq])

                    ts(lre[:], lre[:], -1e-4, None, ALU.min, None, [b_p], [b_p])
                    act(dtv[:], ldt[:], AF.Exp, [b_p], [b_p])
                    tt(a_[:], lre[:], dtv[:], ALU.mult, [b_p], [b_p])
                    act(r_sb[:], a_[:], AF.Exp, [b_p], [b_p])
                    tt(th[:], lim[:], dtv[:], ALU.mult, [b_p], [b_p])
                    sincos(sn[:], th[:], None, t1_[:], ti[:], b_p, 0.0)
                    sincos(cs[:], th[:], None, t1_[:], ti[:], b_p, 1.5707963267948966)
                    tt(cs[:], cs[:], r_sb[:], ALU.mult, [b_p], [b_p])
                    tt(sn[:], sn[:], r_sb[:], ALU.mult, [b_p], [b_p])
                    ts(cs[:], cs[:], -1.0, None, ALU.add, None, [b_p], [b_p])
                    tt(t1_[:], lre[:], lre[:], ALU.mult, [b_p], [b_p])
                    tt(t2_[:], lim[:], lim[:], ALU.mult, [b_p], [b_p])
                    tt(t1_[:], t1_[:], t2_[:], ALU.add, [b_p], [b_p])
                    OP("dve", lambda e: e.reciprocal(out=t1_[:], in_=t1_[:]), [b_p], [b_p])
                    tt(zre[:], cs[:], lre[:], ALU.mult, [b_p], [b_p])
                    tt(t2_[:], sn[:], lim[:], ALU.mult, [b_p], [b_p])
                    tt(zre[:], zre[:], t2_[:], ALU.add, [b_p], [b_p])
                    tt(zre[:], zre[:], t1_[:], ALU.mult, [b_p], [b_p])
                    tt(zim[:], sn[:], lre[:], ALU.mult, [b_p], [b_p])
                    tt(t2_[:], cs[:], lim[:], ALU.mult, [b_p], [b_p])
                    tt(zim[:], zim[:], t2_[:], ALU.subtract, [b_p], [b_p])
                    tt(zim[:], zim[:], t1_[:], ALU.mult, [b_p], [b_p])
                    bbr = sb("bbr", [128, 8, 16], F32, st); bbi = sb("bbi", [128, 8, 16], F32, st); tb = sb("tb", [128, 8, 16], F32, st)
                    zre_b = zre[:].unsqueeze(2).to_broadcast([128, 8, 16]); zim_b = zim[:].unsqueeze(2).to_broadcast([128, 8, 16])
                    tt(bbr[:], bre[:], zre_b, ALU.mult, [b_p, b_bc], [b_bc])
                    tt(tb[:], bim[:], zim_b, ALU.mult, [b_p, b_bc], [b_bc])
                    tt(bbr[:], bbr[:], tb[:], ALU.subtract, [b_bc], [b_bc])
                    tt(bbi[:], bim[:], zre_b, ALU.mult, [b_p, b_bc], [b_bc])
                    tt(tb[:], bre[:], zim_b, ALU.mult, [b_p, b_bc], [b_bc])
                    tt(bbi[:], bbi[:], tb[:], ALU.add, [b_bc], [b_bc])
                    WT = []
                    for nm, src in (("re", bbr), ("im", bbi)):
                        w1 = sb("w1" + nm, [128, 8, 2, 16], F32, st); bw1 = Buf()
                        OP("dve", lambda e, w1=w1: e.memset(w1[:], 0.0), writes=[bw1])
                        cp(w1[0:64, :, 0, :], src[0:64], [b_bc], [bw1])
                        cp(w1[64:128, :, 1, :], src[64:128], [b_bc], [bw1])
                        wt = sb("wt" + nm, [128, 2, 128], BF16, st); bwt = Buf()
                        w1v = w1[:].rearrange("p g a c -> p (g a c)")
                        for ch in range(2):
                            OP("pe", lambda e, ch=ch, w1v=w1v: e.transpose(PS[0][:, 0:128], w1v[:, ch * 128:(ch + 1) * 128], ident[:]),
                               [bw1, b_ident], [PSB[0]])
                            cp(wt[:, ch, :], PS[0][:, 0:128], [PSB[0]], [bwt])
                        WT.append((wt, bwt))
                    CT = []
                    for nm, src, sgn in (("re", cre, 1.0), ("im", cim, -1.0)):
                        ct = sb("ct" + nm, [128, 8, 2, 16], BF16, st); bct = Buf()
                        OP("dve", lambda e, ct=ct: e.memset(ct[:], 0.0), writes=[bct])
                        ts(ct[0:64, :, 0, :], src[0:64], sgn, None, ALU.mult, None, [b_bc], [bct])
                        ts(ct[64:128, :, 1, :], src[64:128], sgn, None, ALU.mult, None, [b_bc], [bct])
                        CT.append((ct, bct))
                    cosT = sb("cosT", [128, 8, TT + 1], F32, st); sinT = sb("sinT", [128, 8, TT + 1], F32, st); b_tab = Buf()
                    with ExitStack() as st2:
                        ang = sb("ang", [128, 8, TT + 1], F32, st2); tf = sb("tf", [128, 8, TT + 1], F32, st2)
                        tii = sb("tii", [128, 8, TT + 1], I32, st2); b_ang = Buf()
                        for gp in range(8):
                            ts(ang[:, gp, :], iota[:], th[:, gp:gp + 1], None, ALU.mult, None, [b_iota, b_p], [b_ang])
                        sincos(sinT[:], ang[:], None, tf[:], tii[:], b_ang, 0.0)
                        sincos(cosT[:], ang[:], None, tf[:], tii[:], b_ang, 1.5707963267948966)
                        fw.barrier()
                    uf = [sb(f"uf{i}", [128, 2, TT], F32, st) for i in range(2)]; ufb = [Buf() for _ in range(2)]
                    ub = [sb(f"ub{i}", [128, 2, TT], BF16, st) for i in range(2)]; ubb = [Buf() for _ in range(2)]
                    ta = [sb(f"ta{i}", [128, TT], F32, st) for i in range(4)]; tab_ = [Buf() for _ in range(4)]
                    wre = [sb(f"wre{i}", [128, TT], F32, st) for i in range(2)]; wim = [sb(f"wim{i}", [128, TT], F32, st) for i in range(2)]
                    wb_ = [Buf() for _ in range(2)]
                    zr = [sb(f"zr{i}", [128, TT], BF16, st) for i in range(2)]; zi = [sb(f"zi{i}", [128, TT], BF16, st) for i in range(2)]
                    zb_ = [Buf() for _ in range(2)]
                    ini = sb("ini", [128, 8, 2], F32, st); b_ini = [Buf() for _ in range(8)]
                    tiny = sb("tiny", [128, 2], F32, st)
                    yp = sb("yp", [128, 2, TT], F32, st); ypb = [Buf() for _ in range(2)]
                    yg = sb("yg", [128, 2, TT], F32, st); ygb_f = [Buf() for _ in range(2)]
                    ygb = sb("ygb", [128, 2, TT], BF16, st); ygbb = Buf()
                    g1t = sb("g1t", [128, TT], F32, st); g1b = Buf(); g2t = sb("g2t", [128, TT], F32, st); g2b = Buf()
                    yo = [sb(f"yo{i}", [128, TT], F32, st) for i in range(2)]; yob = [Buf() for _ in range(2)]
                    yob16 = [sb(f"yob16{i}", [128, TT], BF16, st) for i in range(2)]; yob16b = [Buf() for _ in range(2)]
                    OP("dve", lambda e: e.memset(ini[:], 0.0), writes=b_ini)
                    k = 0
                    def gen_B():
                        k = 0
                        pend = []

                        def run_due(force=False):
                            keep = []
                            for item in list(pend):
                                item[0] -= 1
                                if force or item[0] <= 0:
                                    nxt = item[1]()
                                    while force and nxt is not None:
                                        nxt = nxt()
                                    if nxt is not None:
                                        keep.append([1, nxt])
                                else:
                                    keep.append(item)
                            pend[:] = keep
                        for it in range(NT):
                            t0 = it * TT
                            i = it % 2
                            fw.dma("sp", uf[i][:], uT_d[:, :, t0:t0 + TT].rearrange("c p t -> p c t"), writes=[ufb[i]])
                            cp(ub[i][:], uf[i][:], [ufb[i]], [ubb[i]], eng="act")
                            for gp in range(8):
                                ch, j = gp // 4, gp % 4
                                pa, pb = 0, 1
                                for (pp, (wt, bwt)) in ((pa, WT[0]), (pb, WT[1])):
                                    OP("pe", lambda e, pp=pp, wt=wt, ch=ch, j=j, i=i: e.matmul(
                                        PS[pp][:, :], wt[32 * j:32 * j + 32, ch, :], ub[i][32 * j:32 * j + 32, ch, :],
                                        start=True, stop=True, tile_position=(32 * j, 0)), [bwt, ubb[i]], [PSB[pp]])
                                run_due()
                                cT = cosT[:, gp, 0:TT]; sT = sinT[:, gp, 0:TT]
                                kk = k % 2; k += 1
                                tt(ta[0][:], PS[pa][:, :], cT, ALU.mult, [PSB[pa], b_tab], [tab_[0]])
                                tt(ta[1][:], PS[pb][:, :], sT, ALU.mult, [PSB[pb], b_tab], [tab_[1]])
                                tt(ta[0][:], ta[0][:], ta[1][:], ALU.add, [tab_[0], tab_[1]], [tab_[0]])
                                tt(ta[2][:], PS[pb][:, :], cT, ALU.mult, [PSB[pb], b_tab], [tab_[2]])
                                tt(ta[3][:], PS[pa][:, :], sT, ALU.mult, [PSB[pa], b_tab], [tab_[3]])
                                tt(ta[2][:], ta[2][:], ta[3][:], ALU.subtract, [tab_[2], tab_[3]], [tab_[2]])
                                rb = r_sb[:, gp:gp + 1].to_broadcast([128, TT])
                                OP("dve", lambda e, kk=kk, rb=rb, gp=gp: e.tensor_tensor_scan(
                                    out=wre[kk][:], data0=rb, data1=ta[0][:], initial=ini[:, gp, 0:1], op0=ALU.mult, op1=ALU.add),
                                    [tab_[0], b_p, b_ini[gp]], [wb_[kk]])
                                OP("dve", lambda e, kk=kk, rb=rb, gp=gp: e.tensor_tensor_scan(
                                    out=wim[kk][:], data0=rb, data1=ta[2][:], initial=ini[:, gp, 1:2], op0=ALU.mult, op1=ALU.add),
                                    [tab_[2], b_p, b_ini[gp]], [wb_[kk]])
                                tt(ta[0][:], wre[kk][:], cT, ALU.mult, [wb_[kk], b_tab], [tab_[0]])
                                tt(ta[1][:], wim[kk][:], sT, ALU.mult, [wb_[kk], b_tab], [tab_[1]])
                                tt(zr[kk][:], ta[0][:], ta[1][:], ALU.subtract, [tab_[0], tab_[1]], [zb_[kk]])
                                tt(ta[2][:], wre[kk][:], sT, ALU.mult, [wb_[kk], b_tab], [tab_[2]])
                                tt(ta[3][:], wim[kk][:], cT, ALU.mult, [wb_[kk], b_tab], [tab_[3]])
                                tt(zi[kk][:], ta[2][:], ta[3][:], ALU.add, [tab_[2], tab_[3]], [zb_[kk]])
                                cL = cosT[:, gp, TT:TT + 1]; sL = sinT[:, gp, TT:TT + 1]
                                ts(tiny[:, 0:1], wim[kk][:, TT - 1:TT], sL, None, ALU.mult, None, [wb_[kk], b_tab], [b_ini[gp]])
                                ts(tiny[:, 1:2], wim[kk][:, TT - 1:TT], cL, None, ALU.mult, None, [wb_[kk], b_tab], [b_ini[gp]])
                                stt(ini[:, gp, 0:1], wre[kk][:, TT - 1:TT], cL, tiny[:, 0:1], ALU.mult, ALU.subtract, [wb_[kk], b_tab, b_ini[gp]], [b_ini[gp]])
                                stt(ini[:, gp, 1:2], wre[kk][:, TT - 1:TT], sL, tiny[:, 1:2], ALU.mult, ALU.add, [wb_[kk], b_tab, b_ini[gp]], [b_ini[gp]])
                                py = 2

                                def tail(gp=gp, j=j, kk=kk, py=py, ch=ch, i=i, t0=t0):
                                  def f(e):
                                    e.matmul(PS[py][32 * j:32 * j + 32, :], CT[0][0][:, gp, :, :].rearrange("p a c -> p (a c)"), zr[kk][:],
                                             start=True, stop=False, tile_position=(0, 32 * j))
                                    return e.matmul(PS[py][32 * j:32 * j + 32, :], CT[1][0][:, gp, :, :].rearrange("p a c -> p (a c)"), zi[kk][:],
                                                    start=False, stop=True, tile_position=(0, 32 * j))
                                  OP("pe", f, [zb_[kk], CT[0][1], CT[1][1]], [PSB[py]])
                                  if j == 3:
                                    stt(yp[:, ch, :], uf[i][:, ch, :], dsk[:, ch:ch + 1], PS[py][:, :], ALU.mult, ALU.add,
                                        [ufb[i], b_dg, PSB[py]], [ypb[ch]])
                                    if debug and l == 0:
                                        fw.dma("sp", dbg["dbg_ssmpre"][ch, :, t0:t0 + TT], yp[:, ch, :], reads=[ypb[ch]])
                                    tt(g1t[:], yp[:, ch, :], yp[:, ch, :], ALU.mult, [ypb[ch]], [g1b])
                                    ts(g1t[:], g1t[:], 0.044715, 1.0, ALU.mult, ALU.add, [g1b], [g1b])
                                    tt(g1t[:], g1t[:], yp[:, ch, :], ALU.mult, [g1b, ypb[ch]], [g1b])

                                    def tailB():
                                        act(g1t[:], g1t[:], AF.Sigmoid, [g1b], [g1b], scale=1.5957691216057308)

                                        def tailC():
                                            tt(yg[:, ch, :], yp[:, ch, :], g1t[:], ALU.mult, [g1b, ypb[ch]], [ygb_f[ch]])
                                            cp(ygb[:, ch, :], yg[:, ch, :], [ygb_f[ch]], [ygbb])
                                            return None
                                        return tailC
                                    return tailB
                                  return None
                                pend.append([1, tail])
                                yield
                            def glu_block(t0=t0):
                                for mc in range(2):
                                    def f(e, mc=mc):
                                        e.matmul(PS[7][:, :], gluw[:, 0, mc * 128:(mc + 1) * 128], ygb[:, 0, :], start=True, stop=False)
                                        return e.matmul(PS[7][:, :], gluw[:, 1, mc * 128:(mc + 1) * 128], ygb[:, 1, :], start=False, stop=True)
                                    OP("pe", f, [ygbb, b_gw], [PSB[7]])
                                    act(g2t[:], PS[7][:, :], AF.Sigmoid, [PSB[7], b_dg], [g2b], bias=glb[:, mc:mc + 1])
                                    tt(yo[mc][:], yg[:, mc, :], g2t[:], ALU.mult, [g2b, ygb_f[mc]], [yob[mc]])
                                    hnorm(yo[mc][:], yob[mc], og_fm_sb[:, mc:mc + 1], b_og, yob16[mc][:], yob16b[mc])
                                    fw.dma("sp", yh_d[mc, :, t0:t0 + TT], yob16[mc][:], reads=[yob16b[mc]])
                                return None
                            pend.append([4, glu_block])
                            yield
                        run_due(force=True)
                        yield
                    ckT = sb("ckT", [128, 32, 8], F32, st); cref = sb("cref", [128, 32, 8], F32, st); b_ck = Buf()
                    st3 = ExitStack()
                    ce = sb("ce", [8, 32], F32, st3); dq = sb("dq", [8, 8, 4], F32, st3); b_ce = Buf()
                    dqrow = sb("dqrow", [8, 32, 128], BF16, st3); onesrow = sb("onesrow", [8, S], BF16, st3); b_row = Buf()
                    cp(ce[:], cum[:].rearrange("h (s j) -> h s j", j=128)[:, :, 127], [b_cum], [b_ce])
                    cev = ce[:].rearrange("h (q s) -> h q s", s=4)
                    tt(dq[:], cev, cev[:, :, 3:4].to_broadcast([8, 8, 4]), ALU.subtract, [b_ce], [b_ce])
                    cp(dqrow[:], dq[:].rearrange("h q s -> h (q s)").unsqueeze(2).to_broadcast([8, 32, 128]), [b_ce], [b_row])
                    OP("dve", lambda e: e.memset(onesrow[:], 1.0), writes=[b_row])
                    fw.dma("sp", qT_d[:, 64, :], dqrow[:].rearrange("h s j -> h (s j)"), reads=[b_row])
                    fw.dma("sp", kT_d[:, 64, :], onesrow[:], reads=[b_row])

                    def f(e):
                        for kt in range(32):
                            ins = e.transpose(PS[0][:, kt * 8:(kt + 1) * 8], cum[0:8, kt * 128:(kt + 1) * 128], ident[0:8, 0:8])
                        return ins
                    OP("pe", f, [b_cum, b_ident], [PSB[0]])
                    cp(ckT[:].rearrange("p k h -> p (k h)"), PS[0][:, 0:256], [PSB[0]], [b_ck])
                    OP("pe", lambda e: e.matmul(PS[1][:, 0:256], e127[:], ckT[:].rearrange("p k h -> p (k h)"), start=True, stop=True),
                       [b_ck, b_e127], [PSB[1]])
                    cp(cref[:].rearrange("p k h -> p (k h)"), PS[1][:, 0:256], [PSB[1]], [b_ck])
                    fw.barrier()
                    st3.close()
                    qa = [sb("qa0", [65, S], BF16, st)]; ka = [sb("ka0", [65, S], BF16, st)]
                    qab = [Buf()]; kab = [Buf()]
                    NP = 6
                    pT = [sb(f"pT{i}", [128, TT], BF16, st) for i in range(NP)]; pTb = [Buf() for _ in range(NP)]
                    biasT = [sb(f"biasT{i}", [128, 32], F32, st) for i in range(2)]; biasb = [Buf() for _ in range(2)]
                    osb = [sb(f"osb{i}", [65, TT], F32, st) for i in range(2)]; osbb = [Buf() for _ in range(2)]
                    yat = [sb(f"yat{i}", [64, TT], F32, st) for i in range(2)]; yatb = [Buf() for _ in range(2)]
                    yab = [sb(f"yab{i}", [64, TT], BF16, st) for i in range(2)] ; yabb = [Buf() for _ in range(2)]
                    def emit_bias(u_):
                        h_, qt_ = u_ // 8, u_ % 8
                        n_ = 4 * qt_ + 4
                        ts(biasT[u_ % 2][:, 0:n_], ckT[:, 0:n_, h_], cref[:, 4 * qt_ + 3, h_:h_ + 1], -1.0, ALU.subtract, ALU.mult,
                           [b_ck], [biasb[u_ % 2]])

                    def gen_C():
                        blkctr = 0
                        pend2 = pend3 = None
                        for h in range(8):
                            hi = 0
                            fw.dma("sp", qa[hi][:], qT_d[h], writes=[qab[hi]])
                            fw.dma("sp", ka[hi][:], kT_d[h], writes=[kab[hi]])
                            for qt in range(8):
                                nkt = 4 * qt + 4
                                bi = (h * 8 + qt) % 2
                                oi = bi
                                po = 6
                                if h * 8 + qt == 0:
                                    emit_bias(0)
                                if h * 8 + qt + 1 < 64:
                                    emit_bias(h * 8 + qt + 1)

                                SL = (3, 4, 5)
                                LA = 2

                                def s_mm(kt):
                                    slot = SL[(blkctr + kt) % 3]
                                    m = kt - 4 * qt
                                    c0 = 128 * m if m > 0 else 0
                                    def f(e):
                                        ins = e.matmul(PS[slot][:, c0:TT], ka[hi][:, kt * 128:(kt + 1) * 128],
                                                       qa[hi][:, qt * TT + c0:(qt + 1) * TT], start=True, stop=(m < 0))
                                        if m >= 0:
                                            ins = e.matmul(PS[slot][:, c0:c0 + 128], ntri[:], ident_b[:], start=False, stop=True)
                                        return ins
                                    OP("pe", f, [kab[hi], qab[hi], b_ntri], [PSB[slot]])
                                for kt in range(min(LA, nkt)):
                                    s_mm(kt)
                                for kt in range(nkt):
                                    slot = SL[(blkctr + kt) % 3]
                                    if kt + LA < nkt:
                                        s_mm(kt + LA)
                                    m = kt - 4 * qt
                                    c0 = 128 * m if m > 0 else 0
                                    pi = (blkctr + kt) % NP
                                    act(pT[pi][:, c0:TT], PS[slot][:, c0:TT], AF.Exp, [PSB[slot], biasb[bi]], [pTb[pi]],
                                        bias=biasT[bi][:, kt:kt + 1])
                                    OP("pe", lambda e, kt=kt, c0=c0, pi=pi: e.matmul(
                                        PS[po][0:65, c0:TT], V_sb[:, kt, h, :], pT[pi][:, c0:TT], start=(kt == 0), stop=(kt == nkt - 1)),
                                        [pTb[pi], b_V], [PSB[po]])
                                    if kt % 8 == 7 and kt + 1 < nkt:
                                        yield 8
                                blkctr += nkt
                                cp(osb[oi][:], PS[po][0:65, :], [PSB[po]], [osbb[oi]], eng="act")
                                OP("dve", lambda e, oi=oi: e.reciprocal(out=osb[oi][64:65, :], in_=osb[oi][64:65, :]), [osbb[oi]], [osbb[oi]])

                                def phase2(oi=oi, h=h, qt=qt):
                                    OP("pe", lambda e: e.matmul(PS[7][0:64, :], ones_f[64:65, 0:64], osb[oi][64:65, :], start=True, stop=True),
                                       [osbb[oi], b_onesf], [PSB[7]])
                                    tt(yat[oi][:], osb[oi][0:64, :], PS[7][0:64, :], ALU.mult, [osbb[oi], PSB[7]], [yatb[oi]])

                                    def phase3():
                                        hnorm(yat[oi][:], yatb[oi], og_at_sb[:, h:h + 1], b_og, yab[oi][:], yabb[oi], P=64)
                                        fw.dma("sp", ya_d[h // 2, (h % 2) * 64:(h % 2) * 64 + 64, qt * TT:(qt + 1) * TT], yab[oi][:], reads=[yabb[oi]])
                                    return phase3
                                if pend3 is not None:
                                    pend3()
                                pend3 = pend2() if pend2 is not None else None
                                pend2 = phase2
                                yield ((nkt - 1) % 8) + 1
                        if pend3 is not None:
                            pend3()
                        if pend2 is not None:
                            pend2()()
                    gB, gC = gen_B(), gen_C()
                    aliveB = aliveC = True
                    cdone, bdone = 0, 0
                    CTOT, BTOT = 8 * sum(4 * q_ + 4 for q_ in range(8)), NT * 9
                    while aliveB or aliveC:
                        if aliveC:
                            try:
                                cdone += next(gC)
                            except StopIteration:
                                aliveC = False
                        while aliveB and (not aliveC or bdone * CTOT <= cdone * BTOT):
                            try:
                                next(gB)
                                bdone += 1
                            except StopIteration:
                                aliveB = False
                    fw.bg_on = False
                    fw.barrier()
            if stop_after == "C":
                break
            NSLOT = 80
            RS = 128
            SUB = RS // 128
            BIG = 1.0e4
            with ExitStack() as dl:
                msk_all = sb("msk_all", [128, 32, 16], F32, dl); eq1_all = sb("eq1_all", [128, 32, 16], F32, dl)
                comb_all = sb("comb_all", [128, 32, 16], F32, dl); b_all = Buf()
                r1i = sb("r1i", [128, 32], I32, dl); r2i = sb("r2i", [128, 32], I32, dl)
                w1s = sb("w1s", [128, 32], F32, dl); w2s = sb("w2s", [128, 32], F32, dl); b_rw = Buf()
                widx = sb("widx", [128, NSLOT], I32, dl); b_slot = Buf()
                with ExitStack() as st:
                    maskT = sb("maskT", [16, S], F32, dl); b_mT = Buf()
                    woa = sb("woa", [128, 4, D], BF16, st); wob = sb("wob", [128, 4, D], BF16, st)
                    fw.dma("pool", woa[:, 0:2, :], w_out[l, 0:256, :].rearrange("(kc p) n -> p kc n", p=128), writes=[Buf()])
                    fw.dma("pool", woa[:, 2:4, :], w_out[l, 768:1024, :].rearrange("(kc p) n -> p kc n", p=128), writes=[Buf()])
                    fw.dma("pool", wob[:, 0:2, :], w_out[l, 256:512, :].rearrange("(kc p) n -> p kc n", p=128), writes=[Buf()])
                    fw.dma("pool", wob[:, 2:4, :], w_out[l, 512:768, :].rearrange("(kc p) n -> p kc n", p=128), writes=[Buf()])
                    fw.barrier()
                    zrow = sb("zrow", [128, 2048], F32, st); b_z = Buf()
                    OP("dve", lambda e: e.memset(zrow[:], 0.0), writes=[b_z])
                    for c_ in range(NSLOT * RS // 256):
                        fw.dma("pool", Xs_d[c_ * 256:(c_ + 1) * 256, :].rearrange("(p two) n -> p (two n)", two=2), zrow[:], reads=[b_z])
                    xt2 = [sb(f"xtD{i}", [128, 8, TT], F32, st) for i in range(2)]; xtb2 = [Buf() for _ in range(2)]
                    ys2 = [sb(f"ys{i}", [128, 4, TT], BF16, st) for i in range(2)]; ysb2 = [Buf() for _ in range(2)]
                    yatt2 = [sb(f"yatt{i}", [128, 4, TT], BF16, st) for i in range(2)]; yattb2 = [Buf() for _ in range(2)]

                    def load_in(it_):
                        i_ = it_ % 2
                        fw.dma("sp", xt2[i_][:], xT_d[:, :, it_ * TT:(it_ + 1) * TT].rearrange("c p t -> p c t"), writes=[xtb2[i_]])
                        fw.dma("sp", ys2[i_][:], yh_d[:, :, it_ * TT:(it_ + 1) * TT].rearrange("c p t -> p c t"), writes=[ysb2[i_]])
                        fw.dma("sp", yatt2[i_][:], ya_d[:, :, it_ * TT:(it_ + 1) * TT].rearrange("c p t -> p c t"), writes=[yattb2[i_]])
                    load_in(0)
                    sqb = sb("sqbD", [128, 8, TT], BF16, st); sqbb = Buf()
                    rt = sb("rtD", [128, TT], F32, st); rtb = Buf()
                    h2f = sb("h2f", [128, 8, TT], F32, st); h2fb = Buf()
                    h2 = sb("h2", [128, 8, TT], BF16, st); h2b = Buf()
                    htok = [sb(f"htok{i}", [128, D], F32, st) for i in range(2)]; htokb = [Buf() for _ in range(2)]
                    aff = sb("aff", [128, 4, 16], F32, st); selv = sb("selv", [128, 4, 16], F32, st); rtmp = sb("rtmp", [128, 4, 16], F32, st)
                    m1 = sb("m1", [128, 16], F32, st); m2 = sb("m2", [128, 16], F32, st); gm = sb("gm", [128, 4], F32, st)
                    b_r = Buf()
                    for it in range(NT):
                        t0 = it * TT
                        msk = msk_all[:, 4 * it:4 * it + 4, :]; comb = comb_all[:, 4 * it:4 * it + 4, :]; eq1 = eq1_all[:, 4 * it:4 * it + 4, :]
                        xt, xtb, ys, ysb, yatt, yattb = xt2[it % 2], xtb2[it % 2], ys2[it % 2], ysb2[it % 2], yatt2[it % 2], yattb2[it % 2]
                        if it + 1 < NT:
                            load_in(it + 1)
                        for mc in range(8):
                            ps = 4 + mc % 2

                            def f(e, mc=mc, ps=ps):
                                for kc in range(4):
                                    e.matmul(PS[ps][:, :], woa[:, kc, mc * 128:(mc + 1) * 128], ys[:, kc, :], start=(kc == 0), stop=False)
                                for hh in range(4):
                                    ins = e.matmul(PS[ps][:, :], wob[:, hh, mc * 128:(mc + 1) * 128], yatt[:, hh, :], start=False, stop=(hh == 3))
                                return ins
                            OP("pe", f, [ysb, yattb], [PSB[ps]])
                            stt(xt[:, mc, :], PS[ps][:, :], G1[:, mc:mc + 1], xt[:, mc, :], ALU.mult, ALU.add, [PSB[ps], b_mod, xtb], [xtb])
                        if debug and l == 0:
                            fw.dma("sp", dbg["dbg_xmid"][:, :, t0:t0 + TT].rearrange("c p t -> p c t"), xt[:], reads=[xtb])
                        fw.dma("sp", xT_d[:, :, t0:t0 + TT].rearrange("c p t -> p c t"), xt[:], reads=[xtb])
                        norm_mod(st, xt, xtb, A2, B2, b_mod, h2, h2b, h2f, h2fb, sqb, sqbb, rt, rtb, 7)
                        for sub in range(4):
                            hi_ = sub % 2
                            for half in range(2):
                                ps = half

                                def f(e, sub=sub, half=half, ps=ps):
                                    for c4 in range(4):
                                        c = half * 4 + c4
                                        ins = e.transpose(PS[ps][:, c4 * 128:(c4 + 1) * 128], h2f[:, c, sub * 128:(sub + 1) * 128], ident[:])
                                    return ins
                                OP("pe", f, [h2fb, b_ident], [PSB[ps]])
                                cp(htok[hi_][:, half * 512:(half + 1) * 512], PS[ps][:, :], [PSB[ps]], [htokb[hi_]],
                                   eng=("act" if half == 0 else "dve"))
                            fw.dma("sp", h2_d[t0 + sub * 128:t0 + (sub + 1) * 128, :], htok[hi_][:], reads=[htokb[hi_]])
                        for sub in range(4):
                            def f(e, sub=sub):
                                for kc in range(8):
                                    ins = e.matmul(PS[6][:, sub * 16:(sub + 1) * 16], h2f[:, kc, sub * 128:(sub + 1) * 128], wr_sb[:, kc, :],
                                                   start=(kc == 0), stop=(kc == 7))
                                return ins
                            OP("pe", f, [h2fb, b_wr], [PSB[6]])
                        act(aff[:].rearrange("p s e -> p (s e)"), PS[6][:, 0:64], AF.Sigmoid, [PSB[6]], [b_r])
                        tt(selv[:], aff[:], rb_sb[:].unsqueeze(1).to_broadcast([128, 4, 16]), ALU.add, [b_r, b_rb], [b_r])
                        s44 = selv[:].rearrange("p s (g e) -> p (s g) e", e=4)
                        r44 = rtmp[:].rearrange("p s (g e) -> p (s g) e", e=4)
                        RD = lambda o, i_, op: OP("dve", lambda e: e.tensor_reduce(out=o, in_=i_, axis=mybir.AxisListType.X, op=op), [b_r, b_all], [b_r, b_all])
                        RD(m1[:], s44, ALU.max)
                        tt(r44, s44, m1[:].unsqueeze(2).to_broadcast([128, 16, 4]), ALU.is_equal, [b_r], [b_r])
                        stt(r44, r44, -BIG, s44, ALU.mult, ALU.add, [b_r], [b_r])
                        RD(m2[:], r44, ALU.max)
                        tt(m1[:], m1[:], m2[:], ALU.add, [b_r], [b_r])
                        gs = m1[:].rearrange("p (s g) -> p s g", g=4)
                        RD(gm[:], gs, ALU.max)
                        m2v = m2[:].rearrange("p (s g) -> p s g", g=4)
                        tt(m2v, gs, gm[:].unsqueeze(2).to_broadcast([128, 4, 4]), ALU.is_equal, [b_r], [b_r])
                        ts(m2[:], m2[:], BIG, -BIG, ALU.mult, ALU.add, [b_r], [b_r])
                        tt(r44, s44, m2[:].unsqueeze(2).to_broadcast([128, 16, 4]), ALU.add, [b_r], [b_r])
                        RD(gm[:], rtmp[:], ALU.max)
                        tt(eq1, rtmp[:], gm[:].unsqueeze(2).to_broadcast([128, 4, 16]), ALU.is_equal, [b_r, b_all], [b_r, b_all])
                        stt(msk, eq1, -BIG, rtmp[:], ALU.mult, ALU.add, [b_r, b_all], [b_r, b_all])
                        RD(gm[:], msk, ALU.max)
                        tt(msk, rtmp[:], gm[:].unsqueeze(2).to_broadcast([128, 4, 16]), ALU.is_ge, [b_r, b_all], [b_r, b_all])
                        tt(comb, aff[:], msk, ALU.mult, [b_r, b_all], [b_r, b_all])
                        RD(gm[:], comb, ALU.add)
                        OP("dve", lambda e: e.reciprocal(out=gm[:], in_=gm[:]), [b_r], [b_r])
                        tt(comb, comb, gm[:].unsqueeze(2).to_broadcast([128, 4, 16]), ALU.mult, [b_r, b_all], [b_r, b_all])
                        if debug and l == 0:
                            fw.dma("sp", dbg["dbg_comb"][t0:t0 + TT, :].rearrange("(s p) e -> p s e", p=128), comb, reads=[b_all])

                        def f(e, it=it):
                            for sub in range(4):
                                ins = e.transpose(PS[6][0:16, sub * 128:(sub + 1) * 128], msk_all[:, 4 * it + sub, :], ident[:])
                            return ins
                        OP("pe", f, [b_all, b_ident], [PSB[6]])
                        cp(maskT[:, t0:t0 + TT], PS[6][0:16, :], [PSB[6]], [b_mT])
                    fw.barrier()
                with ExitStack() as st:
                    inc = sb("inc", [16, S], F32, st); b_s = Buf()
                    cntf = sb("cntf", [16, 2], F32, st); slf = sb("slf", [16, 2], F32, st); offf = sb("offf", [16, 1], F32, st)
                    endf = sb("endf", [16, 1], F32, st); cnti = sb("cnti", [16, 2], I32, st)
                    cmpt = sb("cmpt", [16, NSLOT], F32, st); sef = sb("sef", [128, NSLOT], F32, st); pidx = sb("pidx", [128, 1], F32, st); pit = sb("pit", [128, 128], F32, st)
                    pos_all = sb("pos_all", [128, 32, 16], F32, st); tmp3 = sb("tmp3", [128, 32, 16], F32, st)
                    rf = sb("rf", [128, 32], F32, st)
                    OP("dve", lambda e: e.tensor_tensor_scan(out=inc[:], data0=ones_f[0:16, 0:1].to_broadcast([16, S]), data1=maskT[:],
                                                             initial=0.0, op0=ALU.mult, op1=ALU.add), [b_mT, b_onesf], [b_s])
                    ts(cntf[:], inc[:, S - 1:S].to_broadcast([16, 2]), 1.0 / RS, (RS - 1.0) / RS - (RS - 1.0) / (2 * RS), ALU.mult, ALU.add, [b_s], [b_s])
                    cp(cnti[:], cntf[:], [b_s], [b_s])
                    cp(slf[:], cnti[:], [b_s], [b_s])
                    OP("pe", lambda e: e.matmul(PS[0][0:16, 0:2], tri_f[0:16, 0:16], slf[:], start=True, stop=True), [b_s, b_tri], [PSB[0]])
                    cp(offf[:], PS[0][0:16, 0:1], [PSB[0]], [b_s])
                    tt(endf[:], offf[:], slf[:, 0:1], ALU.add, [b_s], [b_s])
                    ts(offf[:], offf[:], float(RS), None, ALU.mult, None, [b_s], [b_s])
                    tt(inc[:], inc[:], maskT[:], ALU.subtract, [b_s, b_mT], [b_s])
                    ts(inc[:], inc[:], offf[:, 0:1], None, ALU.add, None, [b_s], [b_s])

                    def f(e):
                        for tk in range(32):
                            ins = e.transpose(PS[1][:, tk * 16:(tk + 1) * 16], inc[:, tk * 128:(tk + 1) * 128], ident[0:16, 0:16])
                        return ins
                    OP("pe", f, [b_s, b_ident], [PSB[1]])
                    cp(pos_all[:].rearrange("p k e -> p (k e)"), PS[1][:, :], [PSB[1]], [b_s])
                    RD2 = lambda o, i_: OP("dve", lambda e: e.tensor_reduce(out=o, in_=i_, axis=mybir.AxisListType.X, op=ALU.add), [b_s, b_all], [b_s, b_rw])
                    tt(tmp3[:], eq1_all[:], pos_all[:], ALU.mult, [b_s, b_all], [b_s])
                    RD2(rf[:], tmp3[:])
                    cp(r1i[:], rf[:], [b_s], [b_rw])
                    tt(tmp3[:], eq1_all[:], comb_all[:], ALU.mult, [b_s, b_all], [b_s])
                    RD2(w1s[:], tmp3[:])
                    tt(eq1_all[:], msk_all[:], eq1_all[:], ALU.subtract, [b_all], [b_all])
                    tt(tmp3[:], eq1_all[:], pos_all[:], ALU.mult, [b_s, b_all], [b_s])
                    RD2(rf[:], tmp3[:])
                    cp(r2i[:], rf[:], [b_s], [b_rw])
                    tt(tmp3[:], eq1_all[:], comb_all[:], ALU.mult, [b_s, b_all], [b_s])
                    RD2(w2s[:], tmp3[:])
                    ts(cmpt[:], iota[0:16, 0:NSLOT], endf[:, 0:1], None, ALU.is_ge, None, [b_iota, b_s], [b_s])
                    OP("pe", lambda e: e.matmul(PS[2][:, 0:NSLOT], ones_f[0:16, :], cmpt[:], start=True, stop=True), [b_s, b_onesf], [PSB[2]])
                    ts(sef[:], PS[2][:, 0:NSLOT], 15.0, float(l * NE), ALU.min, ALU.add, [PSB[2]], [b_s])
                    tt(pit[:], ident[:], iota[:, 0:128], ALU.mult, [b_ident, b_iota], [b_s])
                    OP("dve", lambda e: e.tensor_reduce(out=pidx[:], in_=pit[:], axis=mybir.AxisListType.X, op=ALU.add), [b_s], [b_s])
                    sk = sb("sk", [128, NSLOT], F32, st)
                    OP("dve", lambda e: e.memset(sk[:, 0:2], 0.0), writes=[b_s])
                    tt(sk[:, 2:NSLOT], sef[:, 2:NSLOT], sef[:, 0:NSLOT - 2], ALU.is_equal, [b_s], [b_s])
                    stt(sef[:], sef[:], 128.0, pidx[:, 0:1].to_broadcast([128, NSLOT]), ALU.mult, ALU.add, [b_s], [b_s])
                    stt(sef[:], sk[:], 1.0e6, sef[:], ALU.mult, ALU.add, [b_s], [b_s])
                    cp(widx[:], sef[:], [b_s], [b_slot])
                    fw.barrier()
                with ExitStack() as st:
                    hrow = [sb(f"hrow{i}", [128, D], F32, st) for i in range(3)]; hrowb = [Buf() for _ in range(3)]
                    for tk in range(32):
                        i = tk % 3
                        fw.dma("sp", hrow[i][:], h2_d[tk * 128:(tk + 1) * 128, :], writes=[hrowb[i]])
                        for ri in (r1i, r2i):
                            fw.dma_ind(Xs_d[:, :], bass.IndirectOffsetOnAxis(ap=ri[:, tk:tk + 1], axis=0), hrow[i][:], None,
                                       reads=[hrowb[i], b_rw])
                    fw.barrier()
                with ExitStack() as st:
                    wg = [sb(f"wg{i}", [128, 8, DE], BF16, st) for i in range(2)]; wgb = [Buf() for _ in range(2)]
                    wu = [sb(f"wu{i}", [128, 8, DE], BF16, st) for i in range(2)]; wub = [Buf() for _ in range(2)]
                    wd = [sb(f"wd{i}", [128, 4, D], BF16, st) for i in range(2)]; wdb = [Buf() for _ in range(2)]
                    xs = [sb(f"xs{i}", [128, D], F32, st) for i in range(3)]; xsb = [Buf() for _ in range(3)]
                    xsT = [sb(f"xsT{i}", [128, 8, 128], BF16, st) for i in range(2)]; xsTb = [Buf() for _ in range(2)]
                    sg = [sb(f"sg{i}", [128, DE], F32, st) for i in range(2)]; sgb = [Buf() for _ in range(2)]
                    hd = [sb(f"hd{i}", [128, DE], F32, st) for i in range(2)]; hdb = [Buf() for _ in range(2)]
                    hdT = [sb(f"hdT{i}", [128, 4, 128], BF16, st) for i in range(2)]; hdTb = [Buf() for _ in range(2)]
                    yt = [sb(f"yt{i}", [128, D], F32, st) for i in range(2)]; ytb = [Buf() for _ in range(2)]

                    if "bc" not in bc_val:
                        bc_reg = nc.gpsimd.alloc_register("bc_reg")
                        nc.gpsimd.reg_mov(bc_reg, 2 * NE * 128 - 1)
                        bc_val["bc"] = nc.gpsimd.snap(bc_reg, donate=True)
                    wg_rows = wg_d.rearrange("e p n -> (e p) n"); wu_rows = wu_d.rearrange("e p n -> (e p) n"); wd_rows = wd_d.rearrange("e p n -> (e p) n")

                    def load_gu(s_):
                        i = s_ % 2
                        off = bass.IndirectOffsetOnAxis(ap=widx[:, s_:s_ + 1], axis=0)
                        fw.dma_ind(wg[i][:].rearrange("p k n -> p (k n)"), None, wg_rows, off, reads=[b_slot], writes=[wgb[i]], bounds_check=bc_val["bc"])
                        fw.dma_ind(wu[i][:].rearrange("p k n -> p (k n)"), None, wu_rows, off, reads=[b_slot], writes=[wub[i]], bounds_check=bc_val["bc"])

                    def load_d(s_):
                        i = s_ % 2
                        off = bass.IndirectOffsetOnAxis(ap=widx[:, s_:s_ + 1], axis=0)
                        fw.dma_ind(wd[i][:].rearrange("p k n -> p (k n)"), None, wd_rows, off, reads=[b_slot], writes=[wdb[i]], bounds_check=bc_val["bc"])

                    def load_x(u_):
                        fw.dma("sp", xs[u_ % 3][:], Xs_d[u_ * 128:(u_ + 1) * 128, :], writes=[xsb[u_ % 3]])

                    def st_T(u_):
                        i = u_ % 2
                        x3 = u_ % 3
                        for half in range(2):
                            def f(e, half=half):
                                for c4 in range(4):
                                    c = half * 4 + c4
                                    ins = e.transpose(PS[half][:, c4 * 128:(c4 + 1) * 128], xs[x3][:, c * 128:(c + 1) * 128], ident[:])
                                return ins
                            OP("pe", f, [xsb[x3], b_ident], [PSB[half]])
                            cp(xsT[i][:, half * 4:(half + 1) * 4, :], PS[half][:, :].rearrange("p (c t) -> p c t", c=4), [PSB[half]], [xsTb[i]],
                               eng=("act" if half == 0 else "dve"))

                    def st_GU(u_):
                        i = u_ % 2
                        wi = (u_ // SUB) % 2
                        for (pp, w_, wb__) in ((2, wg[wi], wgb[wi]), (3, wu[wi], wub[wi])):
                            def f(e, pp=pp, w_=w_):
                                for kc in range(8):
                                    ins = e.matmul(PS[pp][:, :], xsT[i][:, kc, :], w_[:, kc, :], start=(kc == 0), stop=(kc == 7))
                                return ins
                            OP("pe", f, [xsTb[i], wb__], [PSB[pp]])
                        act(sg[i][:], PS[2][:, :], AF.Silu, [PSB[2]], [sgb[i]])
                        tt(hd[i][:], PS[3][:, :], sg[i][:], ALU.mult, [PSB[3], sgb[i]], [hdb[i]])

                    def st_HT(u_):
                        i = u_ % 2

                        def f(e):
                            for c4 in range(4):
                                ins = e.transpose(PS[4][:, c4 * 128:(c4 + 1) * 128], hd[i][:, c4 * 128:(c4 + 1) * 128], ident[:])
                            return ins
                        OP("pe", f, [hdb[i], b_ident], [PSB[4]])
                        cp(hdT[i][:], PS[4][:, :].rearrange("p (c t) -> p c t", c=4), [PSB[4]], [hdTb[i]], eng="act")

                    def st_D(u_):
                        i = u_ % 2
                        wi = (u_ // SUB) % 2
                        for half in range(2):
                            ps = 5 + half

                            def f(e, half=half, ps=ps):
                                for kc in range(4):
                                    ins = e.matmul(PS[ps][:, :], hdT[i][:, kc, :], wd[wi][:, kc, half * 512:(half + 1) * 512], start=(kc == 0), stop=(kc == 3))
                                return ins
                            OP("pe", f, [hdTb[i], wdb[wi]], [PSB[ps]])
                            cp(yt[i][:, half * 512:(half + 1) * 512], PS[ps][:, :], [PSB[ps]], [ytb[i]], eng=("dve" if half == 0 else "act"))
                        fw.dma("sp", Ys_d[u_ * 128:(u_ + 1) * 128, :], yt[i][:], reads=[ytb[i]])

                    NU = SUB * NSLOT
                    for s_ in range(2):
                        load_gu(s_)
                        load_d(s_)
                    for u_ in range(3):
                        load_x(u_)
                    for step in range(NU + 3):
                        if step < NU:
                            st_T(step)
                            if step + 3 < NU:
                                load_x(step + 3)
                        if 0 <= step - 1 < NU:
                            u_ = step - 1
                            st_GU(u_)
                            if u_ % SUB == SUB - 1 and u_ // SUB + 2 < NSLOT:
                                load_gu(u_ // SUB + 2)
                        if 0 <= step - 2 < NU:
                            st_HT(step - 2)
                        if 0 <= step - 3 < NU:
                            u_ = step - 3
                            st_D(u_)
                            if u_ % SUB == SUB - 1 and u_ // SUB + 2 < NSLOT:
                                load_d(u_ // SUB + 2)
                    fw.barrier()
                with ExitStack() as st:
                    y1 = [sb(f"y1_{i}", [128, D], F32, st) for i in range(3)]; y2 = [sb(f"y2_{i}", [128, D], F32, st) for i in range(3)]
                    y1b = [Buf() for _ in range(3)]; y2b = [Buf() for _ in range(3)]
                    ac = [sb(f"ac{i}", [128, D], F32, st) for i in range(2)]; acb = [Buf() for _ in range(2)]
                    xm = [sb(f"xm{i}", [128, 8, 128], F32, st) for i in range(3)]; xmb = [Buf() for _ in range(3)]
                    otile = [sb(f"otile{i}", [128, D], F32, st) for i in range(2)]; otb = [Buf() for _ in range(2)]
                    def issue5(tk):
                        i = tk % 3
                        fw.dma_ind(y1[i][:], None, Ys_d[:, :], bass.IndirectOffsetOnAxis(ap=r1i[:, tk:tk + 1], axis=0), reads=[b_rw], writes=[y1b[i]])
                        fw.dma_ind(y2[i][:], None, Ys_d[:, :], bass.IndirectOffsetOnAxis(ap=r2i[:, tk:tk + 1], axis=0), reads=[b_rw], writes=[y2b[i]])
                        fw.dma("sp", xm[i][:], xT_d[:, :, tk * 128:(tk + 1) * 128].rearrange("c p t -> p c t"), writes=[xmb[i]])
                    issue5(0)
                    issue5(1)
                    for tk in range(32):
                        i = tk % 2
                        j3 = tk % 3
                        if tk + 2 < 32:
                            issue5(tk + 2)
                        ts(ac[i][:], y1[j3][:], w1s[:, tk:tk + 1], None, ALU.mult, None, [y1b[j3], b_rw], [acb[i]])
                        stt(ac[i][:], y2[j3][:], w2s[:, tk:tk + 1], ac[i][:], ALU.mult, ALU.add, [y2b[j3], b_rw, acb[i]], [acb[i]])
                        for half in range(2):
                            ps = 2 * (tk % 2) + half

                            def f(e, half=half, ps=ps):
                                for c4 in range(4):
                                    c = half * 4 + c4
                                    ins = e.transpose(PS[ps][:, c4 * 128:(c4 + 1) * 128], ac[i][:, c * 128:(c + 1) * 128], ident[:])
                                return ins
                            OP("pe", f, [acb[i], b_ident], [PSB[ps]])
                            for c4 in range(4):
                                c = half * 4 + c4
                                stt(xm[j3][:, c, :], PS[ps][:, c4 * 128:(c4 + 1) * 128], G2[:, c:c + 1], xm[j3][:, c, :], ALU.mult, ALU.add,
                                    [PSB[ps], b_mod, xmb[j3]], [xmb[j3]])
                        if not last:
                            fw.dma("sp", xT_d[:, :, tk * 128:(tk + 1) * 128].rearrange("c p t -> p c t"), xm[j3][:], reads=[xmb[j3]])
                        else:
                            for half in range(2):
                                ps = 4 + 2 * (tk % 2) + half

                                def f(e, half=half, ps=ps):
                                    for c4 in range(4):
                                        c = half * 4 + c4
                                        ins = e.transpose(PS[ps][:, c4 * 128:(c4 + 1) * 128], xm[j3][:, c, :], ident[:])
                                    return ins
                                OP("pe", f, [xmb[j3], b_ident], [PSB[ps]])
                                cp(otile[i][:, half * 512:(half + 1) * 512], PS[ps][:, :], [PSB[ps]], [otb[i]], eng=("act" if half == 0 else "dve"))
                            fw.dma("sp", out_d[tk * 128:(tk + 1) * 128, :], otile[i][:], reads=[otb[i]])
                    fw.barrier()
    fw.barrier()
    return nc, fw, dbg


def host_inputs(inp, b):
    f = np.float32
    A = np.ascontiguousarray
    m = {}
    m["x"] = A(inp["x"][b])
    m["c_fm"] = A(inp["c"][b].reshape(8, 128).T)
    m["ada_w"] = inp["ada_w"]
    m["ada_b_fm"] = A(inp["ada_b"].reshape(2, 48, 128).transpose(0, 2, 1))
    m["n1g"] = A(inp["norm1_g"].reshape(2, 8, 128).transpose(0, 2, 1))
    m["n2g"] = A(inp["norm2_g"].reshape(2, 8, 128).transpose(0, 2, 1))
    m["w_in"] = inp["w_in"]
    m["fb"] = A(inp["forget_b"].reshape(2, 8, 1))
    def gp_lay(a):
        return A(a.reshape(2, 8, 2, 64).transpose(0, 2, 3, 1).reshape(2, 128, 8))
    m["lam_re"] = gp_lay(inp["lam_re"])
    m["lam_im"] = gp_lay(inp["lam_im"])
    m["log_dt"] = gp_lay(np.broadcast_to(inp["log_dt"][:, :, None], (2, 16, 64)))
    m["sb_re"] = A(inp["ssm_b_re"].reshape(2, 8, 2, 64, 16).transpose(0, 2, 3, 1, 4).reshape(2, 128, 8, 16))
    m["sb_im"] = A(inp["ssm_b_im"].reshape(2, 8, 2, 64, 16).transpose(0, 2, 3, 1, 4).reshape(2, 128, 8, 16))
    m["sc_re"] = A(inp["ssm_c_re"].reshape(2, 8, 2, 16, 64).transpose(0, 2, 4, 1, 3).reshape(2, 128, 8, 16))
    m["sc_im"] = A(inp["ssm_c_im"].reshape(2, 8, 2, 16, 64).transpose(0, 2, 4, 1, 3).reshape(2, 128, 8, 16))
    m["ssm_d"] = A(inp["ssm_d"].reshape(2, 2, 128).transpose(0, 2, 1))
    m["glu_w"] = inp["glu_w"]
    m["glu_b"] = A(inp["glu_b"].reshape(2, 2, 128).transpose(0, 2, 1))
    m["qg"] = A(np.tile(inp["q_norm_g"], (1, 2)).reshape(2, 128, 1))
    m["kg"] = A(np.tile(inp["k_norm_g"], (1, 2)).reshape(2, 128, 1))
    m["conv_w"] = A(inp["conv_w"].reshape(2, 3, 2, 128).transpose(0, 3, 2, 1))
    m["og_fm"] = A(inp["out_norm_g"].reshape(2, 8, 128).transpose(0, 2, 1))
    m["og_at"] = A(inp["out_norm_g"][:, 256:768].reshape(2, 8, 64).transpose(0, 2, 1))
    m["w_out"] = inp["w_out"]
    m["w_router"] = inp["w_router"]
    m["rbias"] = A(np.broadcast_to(inp["router_bias"][None, :], (128, 16)))
    m["w_gate"] = inp["w_gate"]
    m["w_up"] = inp["w_up"]
    m["w_down"] = inp["w_down"]
    m["ident"] = np.eye(128, dtype=f)
    e127 = np.zeros((128, 128), f); e127[127, :] = 1
    m["e127"] = e127
    blk = np.zeros((128, 128), f); blk[:64, :64] = 1; blk[64:, 64:] = 1
    m["blk64"] = blk
    m["tri"] = np.triu(np.ones((128, 128), f))
    m["iota"] = A(np.broadcast_to(np.arange(TT + 1, dtype=f)[None, :], (128, TT + 1)))
    sel = np.zeros((16, 16, 128), f)
    for e in range(16):
        sel[e, e, :] = 1
    m["sel16"] = sel
    return {k: np.asarray(v, dtype=f) for k, v in m.items()}


_CACHE = {}


def kernel(**inputs):
    inp = {k: np.asarray(v) for k, v in inputs.items()}
    if "nc" not in _CACHE:
        _CACHE["nc"] = build_program()[0]
    nc = _CACHE["nc"]
    in_maps = [host_inputs(inp, b) for b in range(8)]
    res = run_bass_kernel_spmd(nc, in_maps, core_ids=list(range(8)))
    out = np.stack([np.asarray(r["out"]) for r in res.results], axis=0)
    return out.astype(np.float32)
```

```python
import numpy as np
from contextlib import ExitStack
import concourse.bass as bass
import concourse.mybir as mybir
from concourse.bass_utils import run_bass_kernel_spmd

F32 = mybir.dt.float32
BF16 = mybir.dt.bfloat16
I32 = mybir.dt.int32
AF = mybir.ActivationFunctionType
ALU = mybir.AluOpType

S = 4096
D = 1024
TT = 512
NT = S // TT
DIN = 2568
NE = 16
DE = 512
EPS = 1e-6
TWO_PI = 6.283185307179586
import os as _os
NOCONV = bool(_os.environ.get('NOCONV'))
POOLENG = _os.environ.get('POOLENG', 'pool')
OFF_U, OFF_Q, OFF_K, OFF_V, OFF_F, OFF_HC, OFF_BG, OFF_CG = 0, 256, 768, 1280, 1792, 1800, 2056, 2312


class Buf:
    __slots__ = ("name", "w", "r")

    def __init__(self, name=""):
        self.name = name
        self.w = {}
        self.r = {}


class Fw:
    ENG = ("pe", "act", "dve", "pool", "sp")

    def __init__(self, nc, ndma=20):
        self.nc = nc
        self.eng = dict(pe=nc.tensor, act=nc.scalar, dve=nc.vector, pool=nc.gpsimd, sp=nc.sync)
        self.sem = {e: nc.alloc_semaphore("sem_" + e) for e in self.ENG}
        self.cnt = {e: 0 for e in self.ENG}
        self.known = {e: {} for e in self.ENG}
        self.dq = {}
        for q in ("sp", "pool"):
            self.dq[q] = dict(sems=[nc.alloc_semaphore(f"dq_{q}_{i}") for i in range(ndma)],
                              uses=[0] * ndma, nxt=0)
        self.allsems = {}
        self.nwaits = 0
        self.bg_on = False
        self.bg_sems = {s_.num for s_ in self.dq["pool"]["sems"]}

    def _wait(self, e, sem, val):
        k = self.known[e]
        if k.get(sem.num, 0) >= val:
            return
        self.eng[e].wait_ge(sem, val)
        self.nwaits += 1
        k[sem.num] = val

    def _deps(self, e, reads, writes):
        need = {}
        mysem = self.sem[e].num

        def add(tok, same_ok):
            sem, val = tok
            if same_ok and sem.num == mysem and e == "pe":
                return
            if need.get(sem.num, (None, 0))[1] < val:
                need[sem.num] = (sem, val)

        for b in reads:
            for tok in b.w.values():
                add(tok, False)
        for b in writes:
            for tok in b.w.values():
                add(tok, True)
            for tok in b.r.values():
                add(tok, True)
        for sem, val in need.values():
            self._wait(e, sem, val)

    def op(self, e, fn, reads=(), writes=()):
        self._deps(e, reads, writes)
        ins = fn(self.eng[e])
        self.cnt[e] += 1
        sem = self.sem[e]
        ins.then_inc(sem, 1)
        tok = (sem, self.cnt[e])
        for b in reads:
            b.r[sem.num] = tok
        for b in writes:
            b.w = {sem.num: tok}
            b.r = {}
        self.allsems[sem.num] = tok
        return tok

    def dma(self, q, out, in_, reads=(), writes=()):
        d = self.dq[q]
        i = d["nxt"]
        d["nxt"] = (i + 1) % len(d["sems"])
        sem = d["sems"][i]
        if d["uses"][i] > 0:
            self._wait(q, sem, 16 * d["uses"][i])
        self._deps(q, reads, writes)
        ins = self.eng[q].dma_start(out=out, in_=in_)
        d["uses"][i] += 1
        tok = (sem, 16 * d["uses"][i])
        ins.then_inc(sem, 16)
        for b in reads:
            b.r[sem.num] = tok
        for b in writes:
            b.w = {sem.num: tok}
            b.r = {}
        self.allsems[sem.num] = tok
        return tok

    def dma_ind(self, out, out_off, in_, in_off, reads=(), writes=(), bounds_check=None):
        q = "pool"
        d = self.dq[q]
        i = d["nxt"]
        d["nxt"] = (i + 1) % len(d["sems"])
        sem = d["sems"][i]
        if d["uses"][i] > 0:
            self._wait(q, sem, 16 * d["uses"][i])
        self._deps(q, reads, writes)
        if bounds_check is None:
            ins = self.eng[q].indirect_dma_start(out=out, out_offset=out_off, in_=in_, in_offset=in_off)
        else:
            ins = self.eng[q].indirect_dma_start(out=out, out_offset=out_off, in_=in_, in_offset=in_off,
                                                 bounds_check=bounds_check, oob_is_err=False)
        d["uses"][i] += 1
        tok = (sem, 16 * d["uses"][i])
        ins.then_inc(sem, 16)
        for b in reads:
            b.r[sem.num] = tok
        for b in writes:
            b.w = {sem.num: tok}
            b.r = {}
        self.allsems[sem.num] = tok
        return tok

    def barrier(self):
        for e in self.ENG:
            for sem, val in list(self.allsems.values()):
                if self.bg_on and sem.num in self.bg_sems:
                    continue
                self._wait(e, sem, val)


def build_program(nlayers=2, debug=False, stop_after=None):
    nc = bass.Bass("TRN2", target_bir_lowering=False)
    fw = Fw(nc)
    dbg = {}

    def din(name, shape, dt=F32):
        return nc.dram_tensor(name, list(shape), dt, kind="ExternalInput").ap()

    def dscr(name, shape, dt=F32):
        if debug:
            return nc.dram_tensor(name, list(shape), dt, kind="ExternalOutput").ap()
        return nc.dram_tensor(name, list(shape), dt).ap()

    x_in = din("x", [S, D])
    c_fm = din("c_fm", [128, 8])
    ada_w = din("ada_w", [2, D, 6 * D])
    ada_b_fm = din("ada_b_fm", [2, 128, 48])
    n1g = din("n1g", [2, 128, 8])
    n2g = din("n2g", [2, 128, 8])
    w_in = din("w_in", [2, D, DIN])
    fb = din("fb", [2, 8, 1])
    lam_re = din("lam_re", [2, 128, 8])
    lam_im = din("lam_im", [2, 128, 8])
    log_dt = din("log_dt", [2, 128, 8])
    sb_re = din("sb_re", [2, 128, 8, 16])
    sb_im = din("sb_im", [2, 128, 8, 16])
    sc_re = din("sc_re", [2, 128, 8, 16])
    sc_im = din("sc_im", [2, 128, 8, 16])
    ssm_d = din("ssm_d", [2, 128, 2])
    glu_w = din("glu_w", [2, 256, 256])
    glu_b = din("glu_b", [2, 128, 2])
    qg = din("qg", [2, 128, 1])
    kg = din("kg", [2, 128, 1])
    conv_w = din("conv_w", [2, 128, 2, 3])
    og_fm = din("og_fm", [2, 128, 8])
    og_at = din("og_at", [2, 64, 8])
    w_out = din("w_out", [2, D, D])
    w_router = din("w_router", [D, NE])
    rbias = din("rbias", [128, NE])
    w_gate = din("w_gate", [2, NE, D, DE])
    w_up = din("w_up", [2, NE, D, DE])
    w_down = din("w_down", [2, NE, DE, D])
    ident_in = din("ident", [128, 128])
    e127_in = din("e127", [128, 128])
    blk64_in = din("blk64", [128, 128])
    tri_in = din("tri", [128, 128])
    iota_in = din("iota", [128, TT + 1])
    sel_in = din("sel16", [16, NE, 128])
    out_d = nc.dram_tensor("out", [S, D], F32, kind="ExternalOutput").ap()

    xT_d = dscr("xT_d", [8, 128, S])
    wg_d = dscr("wg_d", [2 * NE, 128, 8 * DE], BF16)
    wu_d = dscr("wu_d", [2 * NE, 128, 8 * DE], BF16)
    wd_d = dscr("wd_d", [2 * NE, 128, 4 * D], BF16)
    uT_d = dscr("uT_d", [2, 128, S])
    qT_d = dscr("qT_d", [8, 65, S], BF16)
    kT_d = dscr("kT_d", [8, 65, S], BF16)
    yh_d = dscr("yh_d", [4, 128, S], BF16)
    ya_d = dscr("ya_d", [4, 128, S], BF16)
    h2_d = dscr("h2_d", [S, D])
    Xs_d = dscr("Xs_d", [80 * 128, D])
    Ys_d = dscr("Ys_d", [80 * 128, D])
    if debug:
        for nm, shp, dt in (("dbg_h", [8, 128, S], BF16), ("dbg_cum", [8, S], F32), ("dbg_mod", [128, 48], F32),
                            ("dbg_v", [128, 32, 8, 65], BF16), ("dbg_ssmpre", [2, 128, S], F32),
                            ("dbg_comb", [S, NE], F32), ("dbg_xmid", [8, 128, S], F32)):
            dbg[nm] = nc.dram_tensor(nm, shp, dt, kind="ExternalOutput").ap()

    es = ExitStack()

    uid = [0]

    def sb(name, shape, dt=F32, stack=None):
        uid[0] += 1
        return (stack or es).enter_context(nc.sbuf_tensor(f"s{uid[0]}_{name}", list(shape), dt))

    PS = [es.enter_context(nc.psum_tensor(f"ps{i}", [128, 512], F32)) for i in range(8)]
    PSB = [Buf(f"ps{i}") for i in range(8)]

    ident = sb("ident", [128, 128]); b_ident = Buf()
    e127 = sb("e127", [128, 128]); b_e127 = Buf()
    tri_f = sb("tri_f", [128, 128]); b_tri = Buf()
    blk_f = sb("blk_f", [128, 128]); blk = sb("blk", [128, 128], BF16); b_blk = Buf()
    ones_b = sb("ones_b", [128, 128], BF16); b_ones = Buf()
    ones_f = sb("ones_f", [128, 128]); b_onesf = Buf()
    iota = sb("iota", [128, TT + 1]); b_iota = Buf()
    sel16_f = sb("sel16_f", [16, NE, 128]); b_sel = Buf()
    cfm = sb("cfm", [128, 8]); b_cfm = Buf()
    cact = sb("cact", [128, 8]); b_cact = Buf()
    rb_sb = sb("rb_sb", [128, NE]); b_rb = Buf()
    wr_sb = sb("wr_sb", [128, 8, NE]); b_wr = Buf()

    fw.dma("sp", ident[:], ident_in, writes=[b_ident])
    fw.dma("sp", e127[:], e127_in, writes=[b_e127])
    fw.dma("sp", tri_f[:], tri_in, writes=[b_tri])
    fw.dma("sp", blk_f[:], blk64_in, writes=[b_blk])
    fw.dma("sp", iota[:], iota_in, writes=[b_iota])
    fw.dma("sp", sel16_f[:], sel_in, writes=[b_sel])
    fw.dma("sp", cfm[:], c_fm, writes=[b_cfm])
    fw.dma("sp", rb_sb[:], rbias, writes=[b_rb])
    fw.dma("sp", wr_sb[:], w_router.rearrange("(kc p) n -> p kc n", p=128), writes=[b_wr])
    fw.op("dve", lambda e: e.tensor_copy(out=blk[:], in_=blk_f[:]), reads=[b_blk], writes=[b_blk])
    fw.op("dve", lambda e: e.memset(ones_b[:], 1.0), writes=[b_ones])
    fw.op("dve", lambda e: e.memset(ones_f[:], 1.0), writes=[b_onesf])
    fw.op("act", lambda e: e.activation(out=cact[:], in_=cfm[:], func=AF.Silu), reads=[b_cfm], writes=[b_cact])

    def OP(e, fn, reads=(), writes=()):
        return fw.op(e, fn, reads, writes)

    def act(out, in_, func, reads, writes, scale=1.0, bias=0.0):
        return fw.op("act", lambda e: e.activation(out=out, in_=in_, func=func, bias=bias, scale=scale), reads, writes)

    def tt(out, in0, in1, op, reads, writes, eng="dve"):
        return fw.op(eng, lambda e: e.tensor_tensor(out=out, in0=in0, in1=in1, op=op), reads, writes)

    def ts(out, in0, s1, s2, op0, op1, reads, writes, eng="dve"):
        if op1 is None:
            return fw.op(eng, lambda e: e.tensor_scalar(out=out, in0=in0, scalar1=s1, scalar2=None, op0=op0), reads, writes)
        return fw.op(eng, lambda e: e.tensor_scalar(out=out, in0=in0, scalar1=s1, scalar2=s2, op0=op0, op1=op1), reads, writes)

    def stt(out, in0, scalar, in1, op0, op1, reads, writes):
        return fw.op("dve", lambda e: e.scalar_tensor_tensor(out=out, in0=in0, scalar=scalar, in1=in1, op0=op0, op1=op1),
                     reads, writes)

    def cp(out, in_, reads, writes, eng="dve"):
        if eng == "act":
            return fw.op("act", lambda e: e.copy(out=out, in_=in_), reads, writes)
        return fw.op(eng, lambda e: e.tensor_copy(out=out, in_=in_), reads, writes)

    ntri = sb("ntri", [128, 128], BF16); ident_b = sb("ident_b", [128, 128], BF16); b_ntri = Buf()
    tt(tri_f[:], tri_f[:], ident[:], ALU.subtract, [b_tri, b_ident], [b_tri])
    ts(ntri[:], tri_f[:], -30000.0, None, ALU.mult, None, [b_tri], [b_ntri])
    cp(ident_b[:], ident[:], [b_ident], [b_ntri])
    eps_c = sb("eps_c", [128, 1]); b_eps = Buf()
    OP("dve", lambda e: e.memset(eps_c[:], EPS), writes=[b_eps])

    hn_sq = [sb(f"hn_sq{i}", [128, TT], BF16) for i in range(2)]; hn_sqb = [Buf() for _ in range(2)]
    hn_rt = [sb(f"hn_rt{i}", [128, TT]) for i in range(2)]; hn_rtb = [Buf() for _ in range(2)]
    hn_ctr = [0]
    HN_PS = 7

    def hnorm(src, srcb, g_ap, gb, out, outb, P=128, n=TT):
        i = hn_ctr[0] % 2
        hn_ctr[0] += 1
        act(hn_sq[i][0:P, 0:n], src, AF.Square, [srcb], [hn_sqb[i]])
        OP("pe", lambda e: e.matmul(PS[HN_PS][0:P, 0:n], blk[0:P, 0:P], hn_sq[i][0:P, 0:n], start=True, stop=True),
           [hn_sqb[i], b_blk], [PSB[HN_PS]])
        act(hn_rt[i][0:P, 0:n], PS[HN_PS][0:P, 0:n], AF.Ln, [PSB[HN_PS], b_eps], [hn_rtb[i]], scale=1.0 / 64, bias=eps_c[0:P, :])
        act(hn_rt[i][0:P, 0:n], hn_rt[i][0:P, 0:n], AF.Exp, [hn_rtb[i]], [hn_rtb[i]], scale=-0.5)
        stt(out, src, g_ap, hn_rt[i][0:P, 0:n], ALU.mult, ALU.mult, [srcb, gb, hn_rtb[i]], [outb])

    mods, A1s, A2s, b_mods = [], [], [], []
    for l_ in range(nlayers):
        mods.append(sb(f"mod{l_}", [128, 48])); A1s.append(sb(f"A1_{l_}", [128, 8])); A2s.append(sb(f"A2_{l_}", [128, 8])); b_mods.append(Buf())
    with ExitStack() as st:
        xin = [sb(f"xin{i}", [128, D], F32, st) for i in range(4)]
        xinb = [Buf() for _ in range(4)]
        xo = [sb(f"xo{i}", [128, 8, 128], F32, st) for i in range(4)]
        xob = [Buf() for _ in range(4)]
        n1g_sb = sb("n1g_sb", [128, nlayers, 8], F32, st); n2g_sb = sb("n2g_sb", [128, nlayers, 8], F32, st); b_ng = Buf()
        adab = sb("adab", [128, nlayers, 48], F32, st); b_adab = Buf()
        cact2 = sb("cact2", [128, 8, 2], F32, st); b_cact2 = Buf()
        adw = [sb(f"adw{i}", [128, 8, D], F32, st) for i in range(2)]
        adwb = [Buf() for _ in range(2)]
        for l_ in range(nlayers):
            fw.dma("sp", n1g_sb[:, l_, :], n1g[l_], writes=[b_ng])
            fw.dma("sp", n2g_sb[:, l_, :], n2g[l_], writes=[b_ng])
            fw.dma("sp", adab[:, l_, :], ada_b_fm[l_], writes=[b_adab])
        cp(cact2[:], cact[:].unsqueeze(2).to_broadcast([128, 8, 2]), [b_cact], [b_cact2])

        def ada_units():
            for l_ in range(nlayers):
                for j in range(6):
                    i = (l_ * 6 + j) % 2
                    fw.dma("sp", adw[i][:], ada_w[l_, :, j * D:(j + 1) * D].rearrange("(kc p) n -> p kc n", p=128), writes=[adwb[i]])
                    for c in range(8):
                        def f(e, i=i, j=j, c=c, l_=l_):
                            for kc in range(8):
                                col = 2 * (j * 8 + c)
                                ins = e.matmul(PS[l_][:, col:col + 2], adw[i][:, kc, c * 128:(c + 1) * 128], cact2[:, kc, :],
                                               start=(kc == 0), stop=(kc == 7))
                            return ins
                        OP("pe", f, [adwb[i], b_cact2], [PSB[l_]])
                        yield
        au = ada_units()
        per = (nlayers * 48 + 31) // 32
        for tk in range(S // 128):
            i = tk % 4
            fw.dma("sp", xin[i][:], x_in[tk * 128:(tk + 1) * 128, :], writes=[xinb[i]])
            for _ in range(per):
                next(au, None)
            for half in range(2):
                pb = 2 + (2 * tk + half) % 6

                def f(e, i=i, half=half, pb=pb):
                    for c4 in range(4):
                        c = half * 4 + c4
                        ins = e.transpose(PS[pb][:, c4 * 128:(c4 + 1) * 128], xin[i][:, c * 128:(c + 1) * 128], ident[:])
                    return ins
                OP("pe", f, [xinb[i], b_ident], [PSB[pb]])
                cp(xo[i][:, half * 4:(half + 1) * 4, :], PS[pb][:].rearrange("p (c t) -> p c t", c=4),
                   [PSB[pb]], [xob[i]], eng=("act" if half == 0 else "dve"))
            fw.dma("sp", xT_d[:, :, tk * 128:(tk + 1) * 128].rearrange("c p t -> p c t"), xo[i][:], reads=[xob[i]])
        for _ in au:
            pass
        for l_ in range(nlayers):
            tt(mods[l_][:], PS[l_][:, 0:96].rearrange("p (n two) -> p n two", two=2)[:, :, 0], adab[:, l_, :], ALU.add,
               [PSB[l_], b_adab], [b_mods[l_]])
            stt(A1s[l_][:], mods[l_][:, 8:16], 1.0, n1g_sb[:, l_, :], ALU.add, ALU.mult, [b_mods[l_], b_ng], [b_mods[l_]])
            stt(A2s[l_][:], mods[l_][:, 32:40], 1.0, n2g_sb[:, l_, :], ALU.add, ALU.mult, [b_mods[l_], b_ng], [b_mods[l_]])
        if debug:
            fw.dma("sp", dbg["dbg_mod"], mods[0][:], reads=[b_mods[0]])
        fw.barrier()

    def norm_mod(st_, xt, xtb, A, B, ABb, hb, hbb, xn, xnb, sqb, sqbb, rt, rtb, psn):
        act(sqb[:], xt[:], AF.Square, [xtb], [sqbb])

        def f(e):
            for c in range(8):
                ins = e.matmul(PS[psn][:, :], ones_b[:], sqb[:, c, :], start=(c == 0), stop=(c == 7))
            return ins
        OP("pe", f, [sqbb, b_ones], [PSB[psn]])
        act(rt[:], PS[psn][:, :], AF.Ln, [PSB[psn], b_eps], [rtb], scale=1.0 / D, bias=eps_c[:])
        act(rt[:], rt[:], AF.Exp, [rtb], [rtb], scale=-0.5)
        tt(xn[:], xt[:], rt[:].unsqueeze(1).to_broadcast([128, 8, TT]), ALU.mult, [xtb, rtb], [xnb])
        for c in range(8):
            if c % 2 == 0:
                ts(xn[:, c, :], xn[:, c, :], A[:, c:c + 1], B[:, c:c + 1], ALU.mult, ALU.add, [xnb, ABb], [xnb])
            else:
                act(xn[:, c, :], xn[:, c, :], AF.Identity, [xnb, ABb], [xnb], scale=A[:, c:c + 1], bias=B[:, c:c + 1])
        cp(hb[:, 0:4, :], xn[:, 0:4, :], [xnb], [hbb], eng="dve")
        cp(hb[:, 4:8, :], xn[:, 4:8, :], [xnb], [hbb], eng="act")

    bc_val = {}
    for l in range(nlayers if stop_after != "0" else 0):
        last = (l == nlayers - 1)
        with ExitStack() as lay:
            mod = mods[l]; A1 = A1s[l]; A2 = A2s[l]; b_mod = b_mods[l]
            B1 = mod[:, 0:8]; G1 = mod[:, 16:24]; B2 = mod[:, 24:32]; G2 = mod[:, 40:48]

            with ExitStack() as mix:
                V_sb = sb("V_sb", [128, 32, 8, 65], BF16, mix); b_V = Buf()
                cum = sb("cum", [8, S], F32, mix); b_cum = Buf()
                og_fm_sb = sb("og_fm_sb", [128, 8], F32, mix); og_at_sb = sb("og_at_sb", [64, 8], F32, mix); b_og = Buf()
                fw.dma("sp", og_fm_sb[:], og_fm[l], writes=[b_og])
                fw.dma("sp", og_at_sb[:], og_at[l], writes=[b_og])
                OP("dve", lambda e: e.memset(V_sb[:, :, :, 64:65], 1.0), writes=[b_V])
                if l == 0:
                    cv = [sb(f"cv{i}", [128, 2048], BF16, mix) for i in range(2)]; cvb = [Buf() for _ in range(2)]; cvk = [0]
                fw.barrier()
                with ExitStack() as st:
                    win = sb("win", [128, 8, DIN], BF16, st); b_win = Buf()
                    for hh in range(2):
                        fw.dma("pool", win[:, :, hh * 1284:(hh + 1) * 1284],
                               w_in[l, :, hh * 1284:(hh + 1) * 1284].rearrange("(kc p) n -> p kc n", p=128), writes=[Buf()])
                    fw.barrier()
                    if l == 0 and not NOCONV:
                        for l2 in range(nlayers):
                            for e_ in range(NE):
                                for (src, dst, pat) in ((w_gate, wg_d, 8), (w_up, wu_d, 8), (w_down, wd_d, 4)):
                                    for hf in range(2):
                                        i = cvk[0] % 2
                                        cvk[0] += 1
                                        kcs = pat // 2
                                        srcap = src[l2, e_].rearrange("(kc p) n -> p kc n", p=128)[:, hf * kcs:(hf + 1) * kcs, :]
                                        dstv = cv[i][:].rearrange("p (kc n) -> p kc n", kc=kcs)
                                        fw.dma("pool", dstv, srcap, writes=[cvb[i]])
                                        fw.dma("pool", dst[l2 * NE + e_][:, hf * 2048:(hf + 1) * 2048], cv[i][:], reads=[cvb[i]])
                        fw.bg_on = True
                    fbs = sb("fbs", [8, 1], F32, st); b_fb = Buf()
                    qgs = sb("qgs", [128, 1], F32, st); kgs = sb("kgs", [128, 1], F32, st); b_qk = Buf()
                    cws = sb("cws", [128, 2, 3], F32, st); b_cw = Buf()
                    fw.dma("sp", fbs[:], fb[l], writes=[b_fb])
                    fw.dma("sp", qgs[:], qg[l], writes=[b_qk])
                    fw.dma("sp", kgs[:], kg[l], writes=[b_qk])
                    fw.dma("sp", cws[:], conv_w[l], writes=[b_cw])
                    ts(fbs[:], fbs[:], -1.0, None, ALU.mult, None, [b_fb], [b_fb])
                    ts(qgs[:], qgs[:], 0.125, None, ALU.mult, None, [b_qk], [b_qk])
                    if l == 0:
                        xt = [sb("xt0", [128, 8, TT], F32, st)] * 2; xtb = [Buf()] * 2
                    else:
                        xt = [sb(f"xt{i}", [128, 8, TT], F32, st) for i in range(2)]; xtb = [Buf() for _ in range(2)]
                    sqb = sb("sqb", [128, 8, TT], BF16, st); sqbb = Buf()
                    rt = sb("rt", [128, TT], F32, st); rtb = Buf()
                    xn = sb("xn", [128, 8, TT], F32, st); xnb = Buf()
                    hb = [sb(f"hb{i}", [128, 8, TT], BF16, st) for i in range(2)]; hbb = [Buf() for _ in range(2)]
                    ev = [sb(f"ev{i}", [128, TT], F32, st) for i in range(4)]; evb = [Buf() for _ in range(4)]
                    evo = [sb(f"evo{i}", [128, TT], BF16, st) for i in range(4)]; evob = [Buf() for _ in range(4)]
                    zt = [[sb(f"zt{cc}{i}", [128, TT + 2], F32, st) for i in range(2)] for cc in range(2)]
                    ztb = [[Buf() for _ in range(2)] for _ in range(2)]
                    cy = sb("cy", [128, TT], F32, st); cyb = Buf()
                    fe = sb("fe", [8, TT], F32, st); feb = Buf()
                    evc = [0]
                    gen = [0]
                    for cc in range(2):
                        OP("dve", lambda e, cc=cc: e.memset(zt[cc][1][:, TT:TT + 2], 0.0), writes=[ztb[cc][1]])

                    def proj(ps, off, M, hbt, hbtb):
                        def f(e):
                            for kc in range(8):
                                ins = e.matmul(PS[ps][0:M, :], win[:, kc, off:off + M], hbt[:, kc, :], start=(kc == 0), stop=(kc == 7))
                            return ins
                        OP("pe", f, [hbtb], [PSB[ps]])

                    for it in range(NT):
                        t0 = it * TT
                        i = it % 2
                        fw.dma("sp", xt[i][:], xT_d[:, :, t0:t0 + TT].rearrange("c p t -> p c t"), writes=[xtb[i]])
                        norm_mod(st, xt[i], xtb[i], A1, B1, b_mod, hb[i], hbb[i], xn, xnb, sqb, sqbb, rt, rtb, 0)
                        if debug and l == 0:
                            fw.dma("sp", dbg["dbg_h"][:, :, t0:t0 + TT].rearrange("c p t -> p c t"), hb[i][:], reads=[hbb[i]])
                        def do_u(c):
                            ps = 1 + gen[0] % 2; gen[0] += 1
                            proj(ps, OFF_U + c * 128, 128, hb[i], hbb[i])
                            k = evc[0] % 4; evc[0] += 1
                            cp(ev[k][:], PS[ps][:, :], [PSB[ps]], [evb[k]], eng="act")
                            fw.dma("sp", uT_d[c, :, t0:t0 + TT], ev[k][:], reads=[evb[k]])

                        def do_qk(which, c):
                            off, gsb, dst = ((OFF_Q, qgs, qT_d), (OFF_K, kgs, kT_d))[which]
                            ps = 1 + gen[0] % 2; gen[0] += 1
                            proj(ps, off + c * 128, 128, hb[i], hbb[i])
                            k = evc[0] % 4; evc[0] += 1
                            cp(ev[k][:], PS[ps][:, :], [PSB[ps]], [evb[k]], eng="act")
                            hnorm(ev[k][:], evb[k], gsb[:, 0:1], b_qk, evo[k][:], evob[k])
                            fw.dma("sp", dst[2 * c, 0:64, t0:t0 + TT], evo[k][0:64, :], reads=[evob[k]])
                            fw.dma("sp", dst[2 * c + 1, 0:64, t0:t0 + TT], evo[k][64:128, :], reads=[evob[k]])

                        def do_f():
                            ps = 1 + gen[0] % 2; gen[0] += 1
                            proj(ps, OFF_F, 8, hb[i], hbb[i])
                            act(fe[:], PS[ps][0:8, :], AF.Exp, [PSB[ps], b_fb], [feb], scale=-1.0, bias=fbs[:])
                            act(fe[:], fe[:], AF.Ln, [feb], [feb], bias=1.0)
                            init = 0.0 if it == 0 else cum[:, t0 - 1:t0]
                            OP("dve", lambda e: e.tensor_tensor_scan(
                                out=cum[:, t0:t0 + TT], data0=ones_f[0:8, 0:1].to_broadcast([8, TT]), data1=fe[:], initial=init,
                                op0=ALU.mult, op1=ALU.subtract), [feb, b_onesf, b_cum], [b_cum])

                        def do_v(sub):
                            def f(e):
                                for kc in range(8):
                                    ins = e.matmul(PS[3][:, :], hb[i][:, kc, sub * 128:(sub + 1) * 128], win[:, kc, OFF_V:OFF_V + 512],
                                                   start=(kc == 0), stop=(kc == 7))
                                return ins
                            OP("pe", f, [hbb[i]], [PSB[3]])
                            cp(V_sb[:, 4 * it + sub, :, 0:64], PS[3][:, :].rearrange("p (h d) -> p h d", h=8), [PSB[3]], [b_V],
                               eng=("act" if sub % 2 else "dve"))

                        def do_conv(cc):
                            proj(4, OFF_HC + cc * 128, 128, hb[i], hbb[i])
                            proj(5, OFF_CG + cc * 128, 128, hb[i], hbb[i])
                            proj(6, OFF_BG + cc * 128, 128, hb[i], hbb[i])
                            k = evc[0] % 4; evc[0] += 1
                            z, zb = zt[cc][i], ztb[cc][i]
                            zp, zpb = zt[cc][1 - i], ztb[cc][1 - i]
                            cp(ev[k][:], PS[5][:, :], [PSB[5]], [evb[k]], eng="act")
                            cp(z[:, 0:2], zp[:, TT:TT + 2], [zpb], [zb])
                            tt(z[:, 2:TT + 2], PS[4][:, :], ev[k][:], ALU.mult, [PSB[4], evb[k]], [zb])
                            ts(cy[:], z[:, 2:TT + 2], cws[:, cc, 2:3], None, ALU.mult, None, [zb, b_cw], [cyb])
                            stt(cy[:], z[:, 1:TT + 1], cws[:, cc, 1:2], cy[:], ALU.mult, ALU.add, [zb, b_cw, cyb], [cyb])
                            stt(cy[:], z[:, 0:TT], cws[:, cc, 0:1], cy[:], ALU.mult, ALU.add, [zb, b_cw, cyb], [cyb])
                            tt(ev[k][:], PS[6][:, :], cy[:], ALU.mult, [PSB[6], cyb], [evb[k]])
                            hnorm(ev[k][:], evb[k], og_fm_sb[:, 6 + cc:7 + cc], b_og, evo[k][:], evob[k])
                            fw.dma("sp", yh_d[2 + cc, :, t0:t0 + TT], evo[k][:], reads=[evob[k]])

                        do_u(0); do_qk(0, 0); do_v(0); do_qk(0, 1); do_u(1); do_qk(0, 2); do_v(1); do_qk(0, 3)
                        do_conv(0)
                        do_qk(1, 0); do_v(2); do_qk(1, 1); do_f(); do_qk(1, 2); do_v(3); do_qk(1, 3)
                        do_conv(1)
                    if debug and l == 0:
                        fw.dma("sp", dbg["dbg_cum"], cum[:], reads=[b_cum])
                        fw.dma("sp", dbg["dbg_v"], V_sb[:], reads=[b_V])
                    fw.barrier()
                if stop_after == "A":
                    break
                with ExitStack() as st:
                    def t8(name):
                        return sb(name, [128, 8], F32, st)
                    lre, lim, ldt = t8("lre"), t8("lim"), t8("ldt"); b_p = Buf()
                    fw.dma("sp", lre[:], lam_re[l], writes=[b_p])
                    fw.dma("sp", lim[:], lam_im[l], writes=[b_p])
                    fw.dma("sp", ldt[:], log_dt[l], writes=[b_p])
                    bre = sb("bre", [128, 8, 16], F32, st); bim = sb("bim", [128, 8, 16], F32, st)
                    cre = sb("cre", [128, 8, 16], F32, st); cim = sb("cim", [128, 8, 16], F32, st); b_bc = Buf()
                    fw.dma("sp", bre[:], sb_re[l], writes=[b_bc]); fw.dma("sp", bim[:], sb_im[l], writes=[b_bc])
                    fw.dma("sp", cre[:], sc_re[l], writes=[b_bc]); fw.dma("sp", cim[:], sc_im[l], writes=[b_bc])
                    dsk = sb("dsk", [128, 2], F32, st); glb = sb("glb", [128, 2], F32, st); b_dg = Buf()
                    fw.dma("sp", dsk[:], ssm_d[l], writes=[b_dg]); fw.dma("sp", glb[:], glu_b[l], writes=[b_dg])
                    gluw = sb("gluw", [128, 2, 256], BF16, st); gluwf = sb("gluwf", [128, 2, 256], F32, st); b_gw = Buf()
                    fw.dma("sp", gluwf[:], glu_w[l].rearrange("(kc p) n -> p kc n", p=128), writes=[b_gw])
                    cp(gluw[:], gluwf[:], [b_gw], [b_gw], eng="act")
                    r_sb, th = t8("r_sb"), t8("th")
                    dtv, a_, cs, sn, t1_, t2_, zre, zim = t8("dtv"), t8("a_"), t8("cs"), t8("sn"), t8("t1_"), t8("t2_"), t8("zre"), t8("zim")
                    ti = sb("ti", [128, 8], I32, st)
                    C1 = 6.28125
                    C2 = TWO_PI - C1

                    def sincos(out, ang, shape, tmpf, tmpi, bq, shift):
                        ts(tmpf, ang, 1.0 / TWO_PI, shift / TWO_PI, ALU.mult, ALU.add, [bq], [bq])
                        cp(tmpi, tmpf, [bq], [bq])
                        cp(tmpf, tmpi, [bq], [bq])
                        if shift != 0.0:
                            ts(out, ang, shift, None, ALU.add, None, [bq], [bq])
                            stt(out, tmpf, -C1, out, ALU.mult, ALU.add, [bq], [bq])
                        else:
                            stt(out, tmpf, -C1, ang, ALU.mult, ALU.add, [bq], [bq])
                        stt(out, tmpf, -C2, out, ALU.mult, ALU.add, [bq], [bq])
                        ts(out, out, 3.1415925, -3.1415925, ALU.min, ALU.max, [bq], [bq])
                        act(out, out, AF.Sin, [bq], [bq])

                    ts(lre[:], lre[:], -1e-4, None, ALU.min, None, [b_p], [b_p])
                    act(dtv[:], ldt[:], AF.Exp, [b_p], [b_p])
                    tt(a_[:], lre[:], dtv[:], ALU.mult, [b_p], [b_p])
                    act(r_sb[:], a_[:], AF.Exp, [b_p], [b_p])
                    tt(th[:], lim[:], dtv[:], ALU.mult, [b_p], [b_p])
                    sincos(sn[:], th[:], None, t1_[:], ti[:], b_p, 0.0)
                    sincos(cs[:], th[:], None, t1_[:], ti[:], b_p, 1.5707963267948966)
                    tt(cs[:], cs[:], r_sb[:], ALU.mult, [b_p], [b_p])
                    tt(sn[:], sn[:], r_sb[:], ALU.mult, [b_p], [b_p])
                    ts(cs[:], cs[:], -1.0, None, ALU.add, None, [b_p], [b_p])
                    tt(t1_[:], lre[:], lre[:], ALU.mult, [b_p], [b_p])
                    tt(t2_[:], lim[:], lim[:], ALU.mult, [b_p], [b_p])
                    tt(t1_[:], t1_[:], t2_[:], ALU.add, [b_p], [b_p])
                    OP("dve", lambda e: e.reciprocal(out=t1_[:], in_=t1_[:]), [b_p], [b_p])
                    tt(zre[:], cs[:], lre[:], ALU.mult, [b_p], [b_p])
                    tt(t2_[:], sn[:], lim[:], ALU.mult, [b_p], [b_p])
                    tt(zre[:], zre[:], t2_[:], ALU.add, [b_p], [b_p])
                    tt(zre[:], zre[:], t1_[:], ALU.mult, [b_p], [b_p])
                    tt(zim[:], sn[:], lre[:], ALU.mult, [b_p], [b_p])
                    tt(t2_[:], cs[:], lim[:], ALU.mult, [b_p], [b_p])
                    tt(zim[:], zim[:], t2_[:], ALU.subtract, [b_p], [b_p])
                    tt(zim[:], zim[:], t1_[:], ALU.mult, [b_p], [b_p])
                    bbr = sb("bbr", [128, 8, 16], F32, st); bbi = sb("bbi", [128, 8, 16], F32, st); tb = sb("tb", [128, 8, 16], F32, st)
                    zre_b = zre[:].unsqueeze(2).to_broadcast([128, 8, 16]); zim_b = zim[:].unsqueeze(2).to_broadcast([128, 8, 16])
                    tt(bbr[:], bre[:], zre_b, ALU.mult, [b_p, b_bc], [b_bc])
                    tt(tb[:], bim[:], zim_b, ALU.mult, [b_p, b_bc], [b_bc])
                    tt(bbr[:], bbr[:], tb[:], ALU.subtract, [b_bc], [b_bc])
                    tt(bbi[:], bim[:], zre_b, ALU.mult, [b_p, b_bc], [b_bc])
                    tt(tb[:], bre[:], zim_b, ALU.mult, [b_p, b_bc], [b_bc])
                    tt(bbi[:], bbi[:], tb[:], ALU.add, [b_bc], [b_bc])
                    WT = []
                    for nm, src in (("re", bbr), ("im", bbi)):
                        w1 = sb("w1" + nm, [128, 8, 2, 16], F32, st); bw1 = Buf()
                        OP("dve", lambda e, w1=w1: e.memset(w1[:], 0.0), writes=[bw1])
                        cp(w1[0:64, :, 0, :], src[0:64], [b_bc], [bw1])
                        cp(w1[64:128, :, 1, :], src[64:128], [b_bc], [bw1])
                        wt = sb("wt" + nm, [128, 2, 128], BF16, st); bwt = Buf()
                        w1v = w1[:].rearrange("p g a c -> p (g a c)")
                        for ch in range(2):
                            OP("pe", lambda e, ch=ch, w1v=w1v: e.transpose(PS[0][:, 0:128], w1v[:, ch * 128:(ch + 1) * 128], ident[:]),
                               [bw1, b_ident], [PSB[0]])
                            cp(wt[:, ch, :], PS[0][:, 0:128], [PSB[0]], [bwt])
                        WT.append((wt, bwt))
                    CT = []
                    for nm, src, sgn in (("re", cre, 1.0), ("im", cim, -1.0)):
                        ct = sb("ct" + nm, [128, 8, 2, 16], BF16, st); bct = Buf()
                        OP("dve", lambda e, ct=ct: e.memset(ct[:], 0.0), writes=[bct])
                        ts(ct[0:64, :, 0, :], src[0:64], sgn, None, ALU.mult, None, [b_bc], [bct])
                        ts(ct[64:128, :, 1, :], src[64:128], sgn, None, ALU.mult, None, [b_bc], [bct])
                        CT.append((ct, bct))
                    cosT = sb("cosT", [128, 8, TT + 1], F32, st); sinT = sb("sinT", [128, 8, TT + 1], F32, st); b_tab = Buf()
                    with ExitStack() as st2:
                        ang = sb("ang", [128, 8, TT + 1], F32, st2); tf = sb("tf", [128, 8, TT + 1], F32, st2)
                        tii = sb("tii", [128, 8, TT + 1], I32, st2); b_ang = Buf()
                        for gp in range(8):
                            ts(ang[:, gp, :], iota[:], th[:, gp:gp + 1], None, ALU.mult, None, [b_iota, b_p], [b_ang])
                        sincos(sinT[:], ang[:], None, tf[:], tii[:], b_ang, 0.0)
                        sincos(cosT[:], ang[:], None, tf[:], tii[:], b_ang, 1.5707963267948966)
                        fw.barrier()
                    uf = [sb(f"uf{i}", [128, 2, TT], F32, st) for i in range(2)]; ufb = [Buf() for _ in range(2)]
                    ub = [sb(f"ub{i}", [128, 2, TT], BF16, st) for i in range(2)]; ubb = [Buf() for _ in range(2)]
                    ta = [sb(f"ta{i}", [128, TT], F32, st) for i in range(4)]; tab_ = [Buf() for _ in range(4)]
                    wre = [sb(f"wre{i}", [128, TT], F32, st) for i in range(2)]; wim = [sb(f"wim{i}", [128, TT], F32, st) for i in range(2)]
                    wb_ = [Buf() for _ in range(2)]
                    zr = [sb(f"zr{i}", [128, TT], BF16, st) for i in range(2)]; zi = [sb(f"zi{i}", [128, TT], BF16, st) for i in range(2)]
                    zb_ = [Buf() for _ in range(2)]
                    ini = sb("ini", [128, 8, 2], F32, st); b_ini = [Buf() for _ in range(8)]
                    tiny = sb("tiny", [128, 2], F32, st)
                    yp = sb("yp", [128, 2, TT], F32, st); ypb = [Buf() for _ in range(2)]
                    yg = sb("yg", [128, 2, TT], F32, st); ygb_f = [Buf() for _ in range(2)]
                    ygb = sb("ygb", [128, 2, TT], BF16, st); ygbb = Buf()
                    g1t = sb("g1t", [128, TT], F32, st); g1b = Buf(); g2t = sb("g2t", [128, TT], F32, st); g2b = Buf()
                    yo = [sb(f"yo{i}", [128, TT], F32, st) for i in range(2)]; yob = [Buf() for _ in range(2)]
                    yob16 = [sb(f"yob16{i}", [128, TT], BF16, st) for i in range(2)]; yob16b = [Buf() for _ in range(2)]
                    OP("dve", lambda e: e.memset(ini[:], 0.0), writes=b_ini)
                    k = 0
                    def gen_B():
                        k = 0
                        pend = []

                        def run_due(force=False):
                            keep = []
                            for item in list(pend):
                                item[0] -= 1
                                if force or item[0] <= 0:
                                    nxt = item[1]()
                                    while force and nxt is not None:
                                        nxt = nxt()
                                    if nxt is not None:
                                        keep.append([1, nxt])
                                else:
                                    keep.append(item)
                            pend[:] = keep
                        for it in range(NT):
                            t0 = it * TT
                            i = it % 2
                            fw.dma("sp", uf[i][:], uT_d[:, :, t0:t0 + TT].rearrange("c p t -> p c t"), writes=[ufb[i]])
                            cp(ub[i][:], uf[i][:], [ufb[i]], [ubb[i]], eng="act")
                            for gp in range(8):
                                ch, j = gp // 4, gp % 4
                                pa, pb = 0, 1
                                for (pp, (wt, bwt)) in ((pa, WT[0]), (pb, WT[1])):
                                    OP("pe", lambda e, pp=pp, wt=wt, ch=ch, j=j, i=i: e.matmul(
                                        PS[pp][:, :], wt[32 * j:32 * j + 32, ch, :], ub[i][32 * j:32 * j + 32, ch, :],
                                        start=True, stop=True, tile_position=(32 * j, 0)), [bwt, ubb[i]], [PSB[pp]])
                                run_due()
                                cT = cosT[:, gp, 0:TT]; sT = sinT[:, gp, 0:TT]
                                kk = k % 2; k += 1
                                tt(ta[0][:], PS[pa][:, :], cT, ALU.mult, [PSB[pa], b_tab], [tab_[0]])
                                tt(ta[1][:], PS[pb][:, :], sT, ALU.mult, [PSB[pb], b_tab], [tab_[1]])
                                tt(ta[0][:], ta[0][:], ta[1][:], ALU.add, [tab_[0], tab_[1]], [tab_[0]])
                                tt(ta[2][:], PS[pb][:, :], cT, ALU.mult, [PSB[pb], b_tab], [tab_[2]])
                                tt(ta[3][:], PS[pa][:, :], sT, ALU.mult, [PSB[pa], b_tab], [tab_[3]])
                                tt(ta[2][:], ta[2][:], ta[3][:], ALU.subtract, [tab_[2], tab_[3]], [tab_[2]])
                                rb = r_sb[:, gp:gp + 1].to_broadcast([128, TT])
                                OP("dve", lambda e, kk=kk, rb=rb, gp=gp: e.tensor_tensor_scan(
                                    out=wre[kk][:], data0=rb, data1=ta[0][:], initial=ini[:, gp, 0:1], op0=ALU.mult, op1=ALU.add),
                                    [tab_[0], b_p, b_ini[gp]], [wb_[kk]])
                                OP("dve", lambda e, kk=kk, rb=rb, gp=gp: e.tensor_tensor_scan(
                                    out=wim[kk][:], data0=rb, data1=ta[2][:], initial=ini[:, gp, 1:2], op0=ALU.mult, op1=ALU.add),
                                    [tab_[2], b_p, b_ini[gp]], [wb_[kk]])
                                tt(ta[0][:], wre[kk][:], cT, ALU.mult, [wb_[kk], b_tab], [tab_[0]])
                                tt(ta[1][:], wim[kk][:], sT, ALU.mult, [wb_[kk], b_tab], [tab_[1]])
                                tt(zr[kk][:], ta[0][:], ta[1][:], ALU.subtract, [tab_[0], tab_[1]], [zb_[kk]])
                                tt(ta[2][:], wre[kk][:], sT, ALU.mult, [wb_[kk], b_tab], [tab_[2]])
                                tt(ta[3][:], wim[kk][:], cT, ALU.mult, [wb_[kk], b_tab], [tab_[3]])
                                tt(zi[kk][:], ta[2][:], ta[3][:], ALU.add, [tab_[2], tab_[3]], [zb_[kk]])
                                cL = cosT[:, gp, TT:TT + 1]; sL = sinT[:, gp, TT:TT + 1]
                                ts(tiny[:, 0:1], wim[kk][:, TT - 1:TT], sL, None, ALU.mult, None, [wb_[kk], b_tab], [b_ini[gp]])
                                ts(tiny[:, 1:2], wim[kk][:, TT - 1:TT], cL, None, ALU.mult, None, [wb_[kk], b_tab], [b_ini[gp]])
                                stt(ini[:, gp, 0:1], wre[kk][:, TT - 1:TT], cL, tiny[:, 0:1], ALU.mult, ALU.subtract, [wb_[kk], b_tab, b_ini[gp]], [b_ini[gp]])
                                stt(ini[:, gp, 1:2], wre[kk][:, TT - 1:TT], sL, tiny[:, 1:2], ALU.mult, ALU.add, [wb_[kk], b_tab, b_ini[gp]], [b_ini[gp]])
                                py = 2

                                def tail(gp=gp, j=j, kk=kk, py=py, ch=ch, i=i, t0=t0):
                                  def f(e):
                                    e.matmul(PS[py][32 * j:32 * j + 32, :], CT[0][0][:, gp, :, :].rearrange("p a c -> p (a c)"), zr[kk][:],
                                             start=True, stop=False, tile_position=(0, 32 * j))
                                    return e.matmul(PS[py][32 * j:32 * j + 32, :], CT[1][0][:, gp, :, :].rearrange("p a c -> p (a c)"), zi[kk][:],
                                                    start=False, stop=True, tile_position=(0, 32 * j))
                                  OP("pe", f, [zb_[kk], CT[0][1], CT[1][1]], [PSB[py]])
                                  if j == 3:
                                    stt(yp[:, ch, :], uf[i][:, ch, :], dsk[:, ch:ch + 1], PS[py][:, :], ALU.mult, ALU.add,
                                        [ufb[i], b_dg, PSB[py]], [ypb[ch]])
                                    if debug and l == 0:
                                        fw.dma("sp", dbg["dbg_ssmpre"][ch, :, t0:t0 + TT], yp[:, ch, :], reads=[ypb[ch]])
                                    tt(g1t[:], yp[:, ch, :], yp[:, ch, :], ALU.mult, [ypb[ch]], [g1b])
                                    ts(g1t[:], g1t[:], 0.044715, 1.0, ALU.mult, ALU.add, [g1b], [g1b])
                                    tt(g1t[:], g1t[:], yp[:, ch, :], ALU.mult, [g1b, ypb[ch]], [g1b])

                                    def tailB():
                                        act(g1t[:], g1t[:], AF.Sigmoid, [g1b], [g1b], scale=1.5957691216057308)

                                        def tailC():
                                            tt(yg[:, ch, :], yp[:, ch, :], g1t[:], ALU.mult, [g1b, ypb[ch]], [ygb_f[ch]])
                                            cp(ygb[:, ch, :], yg[:, ch, :], [ygb_f[ch]], [ygbb])
                                            return None
                                        return tailC
                                    return tailB
                                  return None
                                pend.append([1, tail])
                                yield
                            def glu_block(t0=t0):
                                for mc in range(2):
                                    def f(e, mc=mc):
                                        e.matmul(PS[7][:, :], gluw[:, 0, mc * 128:(mc + 1) * 128], ygb[:, 0, :], start=True, stop=False)
                                        return e.matmul(PS[7][:, :], gluw[:, 1, mc * 128:(mc + 1) * 128], ygb[:, 1, :], start=False, stop=True)
                                    OP("pe", f, [ygbb, b_gw], [PSB[7]])
                                    act(g2t[:], PS[7][:, :], AF.Sigmoid, [PSB[7], b_dg], [g2b], bias=glb[:, mc:mc + 1])
                                    tt(yo[mc][:], yg[:, mc, :], g2t[:], ALU.mult, [g2b, ygb_f[mc]], [yob[mc]])
                                    hnorm(yo[mc][:], yob[mc], og_fm_sb[:, mc:mc + 1], b_og, yob16[mc][:], yob16b[mc])
                                    fw.dma("sp", yh_d[mc, :, t0:t0 + TT], yob16[mc][:], reads=[yob16b[mc]])
                                return None
                            pend.append([4, glu_block])
                            yield
                        run_due(force=True)
                        yield
                    ckT = sb("ckT", [128, 32, 8], F32, st); cref = sb("cref", [128, 32, 8], F32, st); b_ck = Buf()
                    st3 = ExitStack()
                    ce = sb("ce", [8, 32], F32, st3); dq = sb("dq", [8, 8, 4], F32, st3); b_ce = Buf()
                    dqrow = sb("dqrow", [8, 32, 128], BF16, st3); onesrow = sb("onesrow", [8, S], BF16, st3); b_row = Buf()
                    cp(ce[:], cum[:].rearrange("h (s j) -> h s j", j=128)[:, :, 127], [b_cum], [b_ce])
                    cev = ce[:].rearrange("h (q s) -> h q s", s=4)
                    tt(dq[:], cev, cev[:, :, 3:4].to_broadcast([8, 8, 4]), ALU.subtract, [b_ce], [b_ce])
                    cp(dqrow[:], dq[:].rearrange("h q s -> h (q s)").unsqueeze(2).to_broadcast([8, 32, 128]), [b_ce], [b_row])
                    OP("dve", lambda e: e.memset(onesrow[:], 1.0), writes=[b_row])
                    fw.dma("sp", qT_d[:, 64, :], dqrow[:].rearrange("h s j -> h (s j)"), reads=[b_row])
                    fw.dma("sp", kT_d[:, 64, :], onesrow[:], reads=[b_row])

                    def f(e):
                        for kt in range(32):
                            ins = e.transpose(PS[0][:, kt * 8:(kt + 1) * 8], cum[0:8, kt * 128:(kt + 1) * 128], ident[0:8, 0:8])
                        return ins
                    OP("pe", f, [b_cum, b_ident], [PSB[0]])
                    cp(ckT[:].rearrange("p k h -> p (k h)"), PS[0][:, 0:256], [PSB[0]], [b_ck])
                    OP("pe", lambda e: e.matmul(PS[1][:, 0:256], e127[:], ckT[:].rearrange("p k h -> p (k h)"), start=True, stop=True),
                       [b_ck, b_e127], [PSB[1]])
                    cp(cref[:].rearrange("p k h -> p (k h)"), PS[1][:, 0:256], [PSB[1]], [b_ck])
                    fw.barrier()
                    st3.close()
                    qa = [sb("qa0", [65, S], BF16, st)]; ka = [sb("ka0", [65, S], BF16, st)]
                    qab = [Buf()]; kab = [Buf()]
                    NP = 6
                    pT = [sb(f"pT{i}", [128, TT], BF16, st) for i in range(NP)]; pTb = [Buf() for _ in range(NP)]
                    biasT = [sb(f"biasT{i}", [128, 32], F32, st) for i in range(2)]; biasb = [Buf() for _ in range(2)]
                    osb = [sb(f"osb{i}", [65, TT], F32, st) for i in range(2)]; osbb = [Buf() for _ in range(2)]
                    yat = [sb(f"yat{i}", [64, TT], F32, st) for i in range(2)]; yatb = [Buf() for _ in range(2)]
                    yab = [sb(f"yab{i}", [64, TT], BF16, st) for i in range(2)] ; yabb = [Buf() for _ in range(2)]
                    def emit_bias(u_):
                        h_, qt_ = u_ // 8, u_ % 8
                        n_ = 4 * qt_ + 4
                        ts(biasT[u_ % 2][:, 0:n_], ckT[:, 0:n_, h_], cref[:, 4 * qt_ + 3, h_:h_ + 1], -1.0, ALU.subtract, ALU.mult,
                           [b_ck], [biasb[u_ % 2]])

                    def gen_C():
                        blkctr = 0
                        pend2 = pend3 = None
                        for h in range(8):
                            hi = 0
                            fw.dma("sp", qa[hi][:], qT_d[h], writes=[qab[hi]])
                            fw.dma("sp", ka[hi][:], kT_d[h], writes=[kab[hi]])
                            for qt in range(8):
                                nkt = 4 * qt + 4
                                bi = (h * 8 + qt) % 2
                                oi = bi
                                po = 6
                                if h * 8 + qt == 0:
                                    emit_bias(0)
                                if h * 8 + qt + 1 < 64:
                                    emit_bias(h * 8 + qt + 1)

                                SL = (3, 4, 5)
                                LA = 2

                                def s_mm(kt):
                                    slot = SL[(blkctr + kt) % 3]
                                    m = kt - 4 * qt
                                    c0 = 128 * m if m > 0 else 0
                                    def f(e):
                                        ins = e.matmul(PS[slot][:, c0:TT], ka[hi][:, kt * 128:(kt + 1) * 128],
                                                       qa[hi][:, qt * TT + c0:(qt + 1) * TT], start=True, stop=(m < 0))
                                        if m >= 0:
                                            ins = e.matmul(PS[slot][:, c0:c0 + 128], ntri[:], ident_b[:], start=False, stop=True)
                                        return ins
                                    OP("pe", f, [kab[hi], qab[hi], b_ntri], [PSB[slot]])
                                for kt in range(min(LA, nkt)):
                                    s_mm(kt)
                                for kt in range(nkt):
                                    slot = SL[(blkctr + kt) % 3]
                                    if kt + LA < nkt:
                                        s_mm(kt + LA)
                                    m = kt - 4 * qt
                                    c0 = 128 * m if m > 0 else 0
                                    pi = (blkctr + kt) % NP
                                    act(pT[pi][:, c0:TT], PS[slot][:, c0:TT], AF.Exp, [PSB[slot], biasb[bi]], [pTb[pi]],
                                        bias=biasT[bi][:, kt:kt + 1])
                                    OP("pe", lambda e, kt=kt, c0=c0, pi=pi: e.matmul(
                                        PS[po][0:65, c0:TT], V_sb[:, kt, h, :], pT[pi][:, c0:TT], start=(kt == 0), stop=(kt == nkt - 1)),
                                        [pTb[pi], b_V], [PSB[po]])
                                    if kt % 8 == 7 and kt + 1 < nkt:
                                        yield 8
                                blkctr += nkt
                                cp(osb[oi][:], PS[po][0:65, :], [PSB[po]], [osbb[oi]], eng="act")
                                OP("dve", lambda e, oi=oi: e.reciprocal(out=osb[oi][64:65, :], in_=osb[oi][64:65, :]), [osbb[oi]], [osbb[oi]])

                                def phase2(oi=oi, h=h, qt=qt):
                                    OP("pe", lambda e: e.matmul(PS[7][0:64, :], ones_f[64:65, 0:64], osb[oi][64:65, :], start=True, stop=True),
                                       [osbb[oi], b_onesf], [PSB[7]])
                                    tt(yat[oi][:], osb[oi][0:64, :], PS[7][0:64, :], ALU.mult, [osbb[oi], PSB[7]], [yatb[oi]])

                                    def phase3():
                                        hnorm(yat[oi][:], yatb[oi], og_at_sb[:, h:h + 1], b_og, yab[oi][:], yabb[oi], P=64)
                                        fw.dma("sp", ya_d[h // 2, (h % 2) * 64:(h % 2) * 64 + 64, qt * TT:(qt + 1) * TT], yab[oi][:], reads=[yabb[oi]])
                                    return phase3
                                if pend3 is not None:
                                    pend3()
                                pend3 = pend2() if pend2 is not None else None
                                pend2 = phase2
                                yield ((nkt - 1) % 8) + 1
                        if pend3 is not None:
                            pend3()
                        if pend2 is not None:
                            pend2()()
                    gB, gC = gen_B(), gen_C()
                    aliveB = aliveC = True
                    cdone, bdone = 0, 0
                    CTOT, BTOT = 8 * sum(4 * q_ + 4 for q_ in range(8)), NT * 9
                    while aliveB or aliveC:
                        if aliveC:
                            try:
                                cdone += next(gC)
                            except StopIteration:
                                aliveC = False
                        while aliveB and (not aliveC or bdone * CTOT <= cdone * BTOT):
                            try:
                                next(gB)
                                bdone += 1
                            except StopIteration:
                                aliveB = False
                    fw.bg_on = False
                    fw.barrier()
            if stop_after == "C":
                break
            NSLOT = 80
            RS = 128
            SUB = RS // 128
            BIG = 1.0e4
            with ExitStack() as dl:
                msk_all = sb("msk_all", [128, 32, 16], F32, dl); eq1_all = sb("eq1_all", [128, 32, 16], F32, dl)
                comb_all = sb("comb_all", [128, 32, 16], F32, dl); b_all = Buf()
                r1i = sb("r1i", [128, 32], I32, dl); r2i = sb("r2i", [128, 32], I32, dl)
                w1s = sb("w1s", [128, 32], F32, dl); w2s = sb("w2s", [128, 32], F32, dl); b_rw = Buf()
                widx = sb("widx", [128, NSLOT], I32, dl); b_slot = Buf()
                with ExitStack() as st:
                    maskT = sb("maskT", [16, S], F32, dl); b_mT = Buf()
                    woa = sb("woa", [128, 4, D], BF16, st); wob = sb("wob", [128, 4, D], BF16, st)
                    fw.dma("pool", woa[:, 0:2, :], w_out[l, 0:256, :].rearrange("(kc p) n -> p kc n", p=128), writes=[Buf()])
                    fw.dma("pool", woa[:, 2:4, :], w_out[l, 768:1024, :].rearrange("(kc p) n -> p kc n", p=128), writes=[Buf()])
                    fw.dma("pool", wob[:, 0:2, :], w_out[l, 256:512, :].rearrange("(kc p) n -> p kc n", p=128), writes=[Buf()])
                    fw.dma("pool", wob[:, 2:4, :], w_out[l, 512:768, :].rearrange("(kc p) n -> p kc n", p=128), writes=[Buf()])
                    fw.barrier()
                    zrow = sb("zrow", [128, 2048], F32, st); b_z = Buf()
                    OP("dve", lambda e: e.memset(zrow[:], 0.0), writes=[b_z])
                    for c_ in range(NSLOT * RS // 256):
                        fw.dma("pool", Xs_d[c_ * 256:(c_ + 1) * 256, :].rearrange("(p two) n -> p (two n)", two=2), zrow[:], reads=[b_z])
                    xt2 = [sb(f"xtD{i}", [128, 8, TT], F32, st) for i in range(2)]; xtb2 = [Buf() for _ in range(2)]
                    ys2 = [sb(f"ys{i}", [128, 4, TT], BF16, st) for i in range(2)]; ysb2 = [Buf() for _ in range(2)]
                    yatt2 = [sb(f"yatt{i}", [128, 4, TT], BF16, st) for i in range(2)]; yattb2 = [Buf() for _ in range(2)]

                    def load_in(it_):
                        i_ = it_ % 2
                        fw.dma("sp", xt2[i_][:], xT_d[:, :, it_ * TT:(it_ + 1) * TT].rearrange("c p t -> p c t"), writes=[xtb2[i_]])
                        fw.dma("sp", ys2[i_][:], yh_d[:, :, it_ * TT:(it_ + 1) * TT].rearrange("c p t -> p c t"), writes=[ysb2[i_]])
                        fw.dma("sp", yatt2[i_][:], ya_d[:, :, it_ * TT:(it_ + 1) * TT].rearrange("c p t -> p c t"), writes=[yattb2[i_]])
                    load_in(0)
                    sqb = sb("sqbD", [128, 8, TT], BF16, st); sqbb = Buf()
                    rt = sb("rtD", [128, TT], F32, st); rtb = Buf()
                    h2f = sb("h2f", [128, 8, TT], F32, st); h2fb = Buf()
                    h2 = sb("h2", [128, 8, TT], BF16, st); h2b = Buf()
                    htok = [sb(f"htok{i}", [128, D], F32, st) for i in range(2)]; htokb = [Buf() for _ in range(2)]
                    aff = sb("aff", [128, 4, 16], F32, st); selv = sb("selv", [128, 4, 16], F32, st); rtmp = sb("rtmp", [128, 4, 16], F32, st)
                    m1 = sb("m1", [128, 16], F32, st); m2 = sb("m2", [128, 16], F32, st); gm = sb("gm", [128, 4], F32, st)
                    b_r = Buf()
                    for it in range(NT):
                        t0 = it * TT
                        msk = msk_all[:, 4 * it:4 * it + 4, :]; comb = comb_all[:, 4 * it:4 * it + 4, :]; eq1 = eq1_all[:, 4 * it:4 * it + 4, :]
                        xt, xtb, ys, ysb, yatt, yattb = xt2[it % 2], xtb2[it % 2], ys2[it % 2], ysb2[it % 2], yatt2[it % 2], yattb2[it % 2]
                        if it + 1 < NT:
                            load_in(it + 1)
                        for mc in range(8):
                            ps = 4 + mc % 2

                            def f(e, mc=mc, ps=ps):
                                for kc in range(4):
                                    e.matmul(PS[ps][:, :], woa[:, kc, mc * 128:(mc + 1) * 128], ys[:, kc, :], start=(kc == 0), stop=False)
                                for hh in range(4):
                                    ins = e.matmul(PS[ps][:, :], wob[:, hh, mc * 128:(mc + 1) * 128], yatt[:, hh, :], start=False, stop=(hh == 3))
                                return ins
                            OP("pe", f, [ysb, yattb], [PSB[ps]])
                            stt(xt[:, mc, :], PS[ps][:, :], G1[:, mc:mc + 1], xt[:, mc, :], ALU.mult, ALU.add, [PSB[ps], b_mod, xtb], [xtb])
                        if debug and l == 0:
                            fw.dma("sp", dbg["dbg_xmid"][:, :, t0:t0 + TT].rearrange("c p t -> p c t"), xt[:], reads=[xtb])
                        fw.dma("sp", xT_d[:, :, t0:t0 + TT].rearrange("c p t -> p c t"), xt[:], reads=[xtb])
                        norm_mod(st, xt, xtb, A2, B2, b_mod, h2, h2b, h2f, h2fb, sqb, sqbb, rt, rtb, 7)
                        for sub in range(4):
                            hi_ = sub % 2
                            for half in range(2):
                                ps = half

                                def f(e, sub=sub, half=half, ps=ps):
                                    for c4 in range(4):
                                        c = half * 4 + c4
                                        ins = e.transpose(PS[ps][:, c4 * 128:(c4 + 1) * 128], h2f[:, c, sub * 128:(sub + 1) * 128], ident[:])
                                    return ins
                                OP("pe", f, [h2fb, b_ident], [PSB[ps]])
                                cp(htok[hi_][:, half * 512:(half + 1) * 512], PS[ps][:, :], [PSB[ps]], [htokb[hi_]],
                                   eng=("act" if half == 0 else "dve"))
                            fw.dma("sp", h2_d[t0 + sub * 128:t0 + (sub + 1) * 128, :], htok[hi_][:], reads=[htokb[hi_]])
                        for sub in range(4):
                            def f(e, sub=sub):
                                for kc in range(8):
                                    ins = e.matmul(PS[6][:, sub * 16:(sub + 1) * 16], h2f[:, kc, sub * 128:(sub + 1) * 128], wr_sb[:, kc, :],
                                                   start=(kc == 0), stop=(kc == 7))
                                return ins
                            OP("pe", f, [h2fb, b_wr], [PSB[6]])
                        act(aff[:].rearrange("p s e -> p (s e)"), PS[6][:, 0:64], AF.Sigmoid, [PSB[6]], [b_r])
                        tt(selv[:], aff[:], rb_sb[:].unsqueeze(1).to_broadcast([128, 4, 16]), ALU.add, [b_r, b_rb], [b_r])
                        s44 = selv[:].rearrange("p s (g e) -> p (s g) e", e=4)
                        r44 = rtmp[:].rearrange("p s (g e) -> p (s g) e", e=4)
                        RD = lambda o, i_, op: OP("dve", lambda e: e.tensor_reduce(out=o, in_=i_, axis=mybir.AxisListType.X, op=op), [b_r, b_all], [b_r, b_all])
                        RD(m1[:], s44, ALU.max)
                        tt(r44, s44, m1[:].unsqueeze(2).to_broadcast([128, 16, 4]), ALU.is_equal, [b_r], [b_r])
                        stt(r44, r44, -BIG, s44, ALU.mult, ALU.add, [b_r], [b_r])
                        RD(m2[:], r44, ALU.max)
                        tt(m1[:], m1[:], m2[:], ALU.add, [b_r], [b_r])
                        gs = m1[:].rearrange("p (s g) -> p s g", g=4)
                        RD(gm[:], gs, ALU.max)
                        m2v = m2[:].rearrange("p (s g) -> p s g", g=4)
                        tt(m2v, gs, gm[:].unsqueeze(2).to_broadcast([128, 4, 4]), ALU.is_equal, [b_r], [b_r])
                        ts(m2[:], m2[:], BIG, -BIG, ALU.mult, ALU.add, [b_r], [b_r])
                        tt(r44, s44, m2[:].unsqueeze(2).to_broadcast([128, 16, 4]), ALU.add, [b_r], [b_r])
                        RD(gm[:], rtmp[:], ALU.max)
                        tt(eq1, rtmp[:], gm[:].unsqueeze(2).to_broadcast([128, 4, 16]), ALU.is_equal, [b_r, b_all], [b_r, b_all])
                        stt(msk, eq1, -BIG, rtmp[:], ALU.mult, ALU.add, [b_r, b_all], [b_r, b_all])
                        RD(gm[:], msk, ALU.max)
                        tt(msk, rtmp[:], gm[:].unsqueeze(2).to_broadcast([128, 4, 16]), ALU.is_ge, [b_r, b_all], [b_r, b_all])
                        tt(comb, aff[:], msk, ALU.mult, [b_r, b_all], [b_r, b_all])
                        RD(gm[:], comb, ALU.add)
                        OP("dve", lambda e: e.reciprocal(out=gm[:], in_=gm[:]), [b_r], [b_r])
                        tt(comb, comb, gm[:].unsqueeze(2).to_broadcast([128, 4, 16]), ALU.mult, [b_r, b_all], [b_r, b_all])
                        if debug and l == 0:
                            fw.dma("sp", dbg["dbg_comb"][t0:t0 + TT, :].rearrange("(s p) e -> p s e", p=128), comb, reads=[b_all])

                        def f(e, it=it):
                            for sub in range(4):
                                ins = e.transpose(PS[6][0:16, sub * 128:(sub + 1) * 128], msk_all[:, 4 * it + sub, :], ident[:])
                            return ins
                        OP("pe", f, [b_all, b_ident], [PSB[6]])
                        cp(maskT[:, t0:t0 + TT], PS[6][0:16, :], [PSB[6]], [b_mT])
                    fw.barrier()
                with ExitStack() as st:
                    inc = sb("inc", [16, S], F32, st); b_s = Buf()
                    cntf = sb("cntf", [16, 2], F32, st); slf = sb("slf", [16, 2], F32, st); offf = sb("offf", [16, 1], F32, st)
                    endf = sb("endf", [16, 1], F32, st); cnti = sb("cnti", [16, 2], I32, st)
                    cmpt = sb("cmpt", [16, NSLOT], F32, st); sef = sb("sef", [128, NSLOT], F32, st); pidx = sb("pidx", [128, 1], F32, st); pit = sb("pit", [128, 128], F32, st)
                    pos_all = sb("pos_all", [128, 32, 16], F32, st); tmp3 = sb("tmp3", [128, 32, 16], F32, st)
                    rf = sb("rf", [128, 32], F32, st)
                    OP("dve", lambda e: e.tensor_tensor_scan(out=inc[:], data0=ones_f[0:16, 0:1].to_broadcast([16, S]), data1=maskT[:],
                                                             initial=0.0, op0=ALU.mult, op1=ALU.add), [b_mT, b_onesf], [b_s])
                    ts(cntf[:], inc[:, S - 1:S].to_broadcast([16, 2]), 1.0 / RS, (RS - 1.0) / RS - (RS - 1.0) / (2 * RS), ALU.mult, ALU.add, [b_s], [b_s])
                    cp(cnti[:], cntf[:], [b_s], [b_s])
                    cp(slf[:], cnti[:], [b_s], [b_s])
                    OP("pe", lambda e: e.matmul(PS[0][0:16, 0:2], tri_f[0:16, 0:16], slf[:], start=True, stop=True), [b_s, b_tri], [PSB[0]])
                    cp(offf[:], PS[0][0:16, 0:1], [PSB[0]], [b_s])
                    tt(endf[:], offf[:], slf[:, 0:1], ALU.add, [b_s], [b_s])
                    ts(offf[:], offf[:], float(RS), None, ALU.mult, None, [b_s], [b_s])
                    tt(inc[:], inc[:], maskT[:], ALU.subtract, [b_s, b_mT], [b_s])
                    ts(inc[:], inc[:], offf[:, 0:1], None, ALU.add, None, [b_s], [b_s])

                    def f(e):
                        for tk in range(32):
                            ins = e.transpose(PS[1][:, tk * 16:(tk + 1) * 16], inc[:, tk * 128:(tk + 1) * 128], ident[0:16, 0:16])
                        return ins
                    OP("pe", f, [b_s, b_ident], [PSB[1]])
                    cp(pos_all[:].rearrange("p k e -> p (k e)"), PS[1][:, :], [PSB[1]], [b_s])
                    RD2 = lambda o, i_: OP("dve", lambda e: e.tensor_reduce(out=o, in_=i_, axis=mybir.AxisListType.X, op=ALU.add), [b_s, b_all], [b_s, b_rw])
                    tt(tmp3[:], eq1_all[:], pos_all[:], ALU.mult, [b_s, b_all], [b_s])
                    RD2(rf[:], tmp3[:])
                    cp(r1i[:], rf[:], [b_s], [b_rw])
                    tt(tmp3[:], eq1_all[:], comb_all[:], ALU.mult, [b_s, b_all], [b_s])
                    RD2(w1s[:], tmp3[:])
                    tt(eq1_all[:], msk_all[:], eq1_all[:], ALU.subtract, [b_all], [b_all])
                    tt(tmp3[:], eq1_all[:], pos_all[:], ALU.mult, [b_s, b_all], [b_s])
                    RD2(rf[:], tmp3[:])
                    cp(r2i[:], rf[:], [b_s], [b_rw])
                    tt(tmp3[:], eq1_all[:], comb_all[:], ALU.mult, [b_s, b_all], [b_s])
                    RD2(w2s[:], tmp3[:])
                    ts(cmpt[:], iota[0:16, 0:NSLOT], endf[:, 0:1], None, ALU.is_ge, None, [b_iota, b_s], [b_s])
                    OP("pe", lambda e: e.matmul(PS[2][:, 0:NSLOT], ones_f[0:16, :], cmpt[:], start=True, stop=True), [b_s, b_onesf], [PSB[2]])
                    ts(sef[:], PS[2][:, 0:NSLOT], 15.0, float(l * NE), ALU.min, ALU.add, [PSB[2]], [b_s])
                    tt(pit[:], ident[:], iota[:, 0:128], ALU.mult, [b_ident, b_iota], [b_s])
                    OP("dve", lambda e: e.tensor_reduce(out=pidx[:], in_=pit[:], axis=mybir.AxisListType.X, op=ALU.add), [b_s], [b_s])
                    sk = sb("sk", [128, NSLOT], F32, st)
                    OP("dve", lambda e: e.memset(sk[:, 0:2], 0.0), writes=[b_s])
                    tt(sk[:, 2:NSLOT], sef[:, 2:NSLOT], sef[:, 0:NSLOT - 2], ALU.is_equal, [b_s], [b_s])
                    stt(sef[:], sef[:], 128.0, pidx[:, 0:1].to_broadcast([128, NSLOT]), ALU.mult, ALU.add, [b_s], [b_s])
                    stt(sef[:], sk[:], 1.0e6, sef[:], ALU.mult, ALU.add, [b_s], [b_s])
                    cp(widx[:], sef[:], [b_s], [b_slot])
                    fw.barrier()
                with ExitStack() as st:
                    hrow = [sb(f"hrow{i}", [128, D], F32, st) for i in range(3)]; hrowb = [Buf() for _ in range(3)]
                    for tk in range(32):
                        i = tk % 3
                        fw.dma("sp", hrow[i][:], h2_d[tk * 128:(tk + 1) * 128, :], writes=[hrowb[i]])
                        for ri in (r1i, r2i):
                            fw.dma_ind(Xs_d[:, :], bass.IndirectOffsetOnAxis(ap=ri[:, tk:tk + 1], axis=0), hrow[i][:], None,
                                       reads=[hrowb[i], b_rw])
                    fw.barrier()
                with ExitStack() as st:
                    wg = [sb(f"wg{i}", [128, 8, DE], BF16, st) for i in range(2)]; wgb = [Buf() for _ in range(2)]
                    wu = [sb(f"wu{i}", [128, 8, DE], BF16, st) for i in range(2)]; wub = [Buf() for _ in range(2)]
                    wd = [sb(f"wd{i}", [128, 4, D], BF16, st) for i in range(2)]; wdb = [Buf() for _ in range(2)]
                    xs = [sb(f"xs{i}", [128, D], F32, st) for i in range(3)]; xsb = [Buf() for _ in range(3)]
                    xsT = [sb(f"xsT{i}", [128, 8, 128], BF16, st) for i in range(2)]; xsTb = [Buf() for _ in range(2)]
                    sg = [sb(f"sg{i}", [128, DE], F32, st) for i in range(2)]; sgb = [Buf() for _ in range(2)]
                    hd = [sb(f"hd{i}", [128, DE], F32, st) for i in range(2)]; hdb = [Buf() for _ in range(2)]
                    hdT = [sb(f"hdT{i}", [128, 4, 128], BF16, st) for i in range(2)]; hdTb = [Buf() for _ in range(2)]
                    yt = [sb(f"yt{i}", [128, D], F32, st) for i in range(2)]; ytb = [Buf() for _ in range(2)]

                    if "bc" not in bc_val:
                        bc_reg = nc.gpsimd.alloc_register("bc_reg")
                        nc.gpsimd.reg_mov(bc_reg, 2 * NE * 128 - 1)
                        bc_val["bc"] = nc.gpsimd.snap(bc_reg, donate=True)
                    wg_rows = wg_d.rearrange("e p n -> (e p) n"); wu_rows = wu_d.rearrange("e p n -> (e p) n"); wd_rows = wd_d.rearrange("e p n -> (e p) n")

                    def load_gu(s_):
                        i = s_ % 2
                        off = bass.IndirectOffsetOnAxis(ap=widx[:, s_:s_ + 1], axis=0)
                        fw.dma_ind(wg[i][:].rearrange("p k n -> p (k n)"), None, wg_rows, off, reads=[b_slot], writes=[wgb[i]], bounds_check=bc_val["bc"])
                        fw.dma_ind(wu[i][:].rearrange("p k n -> p (k n)"), None, wu_rows, off, reads=[b_slot], writes=[wub[i]], bounds_check=bc_val["bc"])

                    def load_d(s_):
                        i = s_ % 2
                        off = bass.IndirectOffsetOnAxis(ap=widx[:, s_:s_ + 1], axis=0)
                        fw.dma_ind(wd[i][:].rearrange("p k n -> p (k n)"), None, wd_rows, off, reads=[b_slot], writes=[wdb[i]], bounds_check=bc_val["bc"])

                    def load_x(u_):
                        fw.dma("sp", xs[u_ % 3][:], Xs_d[u_ * 128:(u_ + 1) * 128, :], writes=[xsb[u_ % 3]])

                    def st_T(u_):
                        i = u_ % 2
                        x3 = u_ % 3
                        for half in range(2):
                            def f(e, half=half):
                                for c4 in range(4):
                                    c = half * 4 + c4
                                    ins = e.transpose(PS[half][:, c4 * 128:(c4 + 1) * 128], xs[x3][:, c * 128:(c + 1) * 128], ident[:])
                                return ins
                            OP("pe", f, [xsb[x3], b_ident], [PSB[half]])
                            cp(xsT[i][:, half * 4:(half + 1) * 4, :], PS[half][:, :].rearrange("p (c t) -> p c t", c=4), [PSB[half]], [xsTb[i]],
                               eng=("act" if half == 0 else "dve"))

                    def st_GU(u_):
                        i = u_ % 2
                        wi = (u_ // SUB) % 2
                        for (pp, w_, wb__) in ((2, wg[wi], wgb[wi]), (3, wu[wi], wub[wi])):
                            def f(e, pp=pp, w_=w_):
                                for kc in range(8):
                                    ins = e.matmul(PS[pp][:, :], xsT[i][:, kc, :], w_[:, kc, :], start=(kc == 0), stop=(kc == 7))
                                return ins
                            OP("pe", f, [xsTb[i], wb__], [PSB[pp]])
                        act(sg[i][:], PS[2][:, :], AF.Silu, [PSB[2]], [sgb[i]])
                        tt(hd[i][:], PS[3][:, :], sg[i][:], ALU.mult, [PSB[3], sgb[i]], [hdb[i]])

                    def st_HT(u_):
                        i = u_ % 2

                        def f(e):
                            for c4 in range(4):
                                ins = e.transpose(PS[4][:, c4 * 128:(c4 + 1) * 128], hd[i][:, c4 * 128:(c4 + 1) * 128], ident[:])
                            return ins
                        OP("pe", f, [hdb[i], b_ident], [PSB[4]])
                        cp(hdT[i][:], PS[4][:, :].rearrange("p (c t) -> p c t", c=4), [PSB[4]], [hdTb[i]], eng="act")

                    def st_D(u_):
                        i = u_ % 2
                        wi = (u_ // SUB) % 2
                        for half in range(2):
                            ps = 5 + half

                            def f(e, half=half, ps=ps):
                                for kc in range(4):
                                    ins = e.matmul(PS[ps][:, :], hdT[i][:, kc, :], wd[wi][:, kc, half * 512:(half + 1) * 512], start=(kc == 0), stop=(kc == 3))
                                return ins
                            OP("pe", f, [hdTb[i], wdb[wi]], [PSB[ps]])
                            cp(yt[i][:, half * 512:(half + 1) * 512], PS[ps][:, :], [PSB[ps]], [ytb[i]], eng=("dve" if half == 0 else "act"))
                        fw.dma("sp", Ys_d[u_ * 128:(u_ + 1) * 128, :], yt[i][:], reads=[ytb[i]])

                    NU = SUB * NSLOT
                    for s_ in range(2):
                        load_gu(s_)
                        load_d(s_)
                    for u_ in range(3):
                        load_x(u_)
                    for step in range(NU + 3):
                        if step < NU:
                            st_T(step)
                            if step + 3 < NU:
                                load_x(step + 3)
                        if 0 <= step - 1 < NU:
                            u_ = step - 1
                            st_GU(u_)
                            if u_ % SUB == SUB - 1 and u_ // SUB + 2 < NSLOT:
                                load_gu(u_ // SUB + 2)
                        if 0 <= step - 2 < NU:
                            st_HT(step - 2)
                        if 0 <= step - 3 < NU:
                            u_ = step - 3
                            st_D(u_)
                            if u_ % SUB == SUB - 1 and u_ // SUB + 2 < NSLOT:
                                load_d(u_ // SUB + 2)
                    fw.barrier()
                with ExitStack() as st:
                    y1 = [sb(f"y1_{i}", [128, D], F32, st) for i in range(3)]; y2 = [sb(f"y2_{i}", [128, D], F32, st) for i in range(3)]
                    y1b = [Buf() for _ in range(3)]; y2b = [Buf() for _ in range(3)]
                    ac = [sb(f"ac{i}", [128, D], F32, st) for i in range(2)]; acb = [Buf() for _ in range(2)]
                    xm = [sb(f"xm{i}", [128, 8, 128], F32, st) for i in range(3)]; xmb = [Buf() for _ in range(3)]
                    otile = [sb(f"otile{i}", [128, D], F32, st) for i in range(2)]; otb = [Buf() for _ in range(2)]
                    def issue5(tk):
                        i = tk % 3
                        fw.dma_ind(y1[i][:], None, Ys_d[:, :], bass.IndirectOffsetOnAxis(ap=r1i[:, tk:tk + 1], axis=0), reads=[b_rw], writes=[y1b[i]])
                        fw.dma_ind(y2[i][:], None, Ys_d[:, :], bass.IndirectOffsetOnAxis(ap=r2i[:, tk:tk + 1], axis=0), reads=[b_rw], writes=[y2b[i]])
                        fw.dma("sp", xm[i][:], xT_d[:, :, tk * 128:(tk + 1) * 128].rearrange("c p t -> p c t"), writes=[xmb[i]])
                    issue5(0)
                    issue5(1)
                    for tk in range(32):
                        i = tk % 2
                        j3 = tk % 3
                        if tk + 2 < 32:
                            issue5(tk + 2)
                        ts(ac[i][:], y1[j3][:], w1s[:, tk:tk + 1], None, ALU.mult, None, [y1b[j3], b_rw], [acb[i]])
                        stt(ac[i][:], y2[j3][:], w2s[:, tk:tk + 1], ac[i][:], ALU.mult, ALU.add, [y2b[j3], b_rw, acb[i]], [acb[i]])
                        for half in range(2):
                            ps = 2 * (tk % 2) + half

                            def f(e, half=half, ps=ps):
                                for c4 in range(4):
                                    c = half * 4 + c4
                                    ins = e.transpose(PS[ps][:, c4 * 128:(c4 + 1) * 128], ac[i][:, c * 128:(c + 1) * 128], ident[:])
                                return ins
                            OP("pe", f, [acb[i], b_ident], [PSB[ps]])
                            for c4 in range(4):
                                c = half * 4 + c4
                                stt(xm[j3][:, c, :], PS[ps][:, c4 * 128:(c4 + 1) * 128], G2[:, c:c + 1], xm[j3][:, c, :], ALU.mult, ALU.add,
                                    [PSB[ps], b_mod, xmb[j3]], [xmb[j3]])
                        if not last:
                            fw.dma("sp", xT_d[:, :, tk * 128:(tk + 1) * 128].rearrange("c p t -> p c t"), xm[j3][:], reads=[xmb[j3]])
                        else:
                            for half in range(2):
                                ps = 4 + 2 * (tk % 2) + half

                                def f(e, half=half, ps=ps):
                                    for c4 in range(4):
                                        c = half * 4 + c4
                                        ins = e.transpose(PS[ps][:, c4 * 128:(c4 + 1) * 128], xm[j3][:, c, :], ident[:])
                                    return ins
                                OP("pe", f, [xmb[j3], b_ident], [PSB[ps]])
                                cp(otile[i][:, half * 512:(half + 1) * 512], PS[ps][:, :], [PSB[ps]], [otb[i]], eng=("act" if half == 0 else "dve"))
                            fw.dma("sp", out_d[tk * 128:(tk + 1) * 128, :], otile[i][:], reads=[otb[i]])
                    fw.barrier()
    fw.barrier()
    return nc, fw, dbg


def host_inputs(inp, b):
    f = np.float32
    A = np.ascontiguousarray
    m = {}
    m["x"] = A(inp["x"][b])
    m["c_fm"] = A(inp["c"][b].reshape(8, 128).T)
    m["ada_w"] = inp["ada_w"]
    m["ada_b_fm"] = A(inp["ada_b"].reshape(2, 48, 128).transpose(0, 2, 1))
    m["n1g"] = A(inp["norm1_g"].reshape(2, 8, 128).transpose(0, 2, 1))
    m["n2g"] = A(inp["norm2_g"].reshape(2, 8, 128).transpose(0, 2, 1))
    m["w_in"] = inp["w_in"]
    m["fb"] = A(inp["forget_b"].reshape(2, 8, 1))
    def gp_lay(a):
        return A(a.reshape(2, 8, 2, 64).transpose(0, 2, 3, 1).reshape(2, 128, 8))
    m["lam_re"] = gp_lay(inp["lam_re"])
    m["lam_im"] = gp_lay(inp["lam_im"])
    m["log_dt"] = gp_lay(np.broadcast_to(inp["log_dt"][:, :, None], (2, 16, 64)))
    m["sb_re"] = A(inp["ssm_b_re"].reshape(2, 8, 2, 64, 16).transpose(0, 2, 3, 1, 4).reshape(2, 128, 8, 16))
    m["sb_im"] = A(inp["ssm_b_im"].reshape(2, 8, 2, 64, 16).transpose(0, 2, 3, 1, 4).reshape(2, 128, 8, 16))
    m["sc_re"] = A(inp["ssm_c_re"].reshape(2, 8, 2, 16, 64).transpose(0, 2, 4, 1, 3).reshape(2, 128, 8, 16))
    m["sc_im"] = A(inp["ssm_c_im"].reshape(2, 8, 2, 16, 64).transpose(0, 2, 4, 1, 3).reshape(2, 128, 8, 16))
    m["ssm_d"] = A(inp["ssm_d"].reshape(2, 2, 128).transpose(0, 2, 1))
    m["glu_w"] = inp["glu_w"]
    m["glu_b"] = A(inp["glu_b"].reshape(2, 2, 128).transpose(0, 2, 1))
    m["qg"] = A(np.tile(inp["q_norm_g"], (1, 2)).reshape(2, 128, 1))
    m["kg"] = A(np.tile(inp["k_norm_g"], (1, 2)).reshape(2, 128, 1))
    m["conv_w"] = A(inp["conv_w"].reshape(2, 3, 2, 128).transpose(0, 3, 2, 1))
    m["og_fm"] = A(inp["out_norm_g"].reshape(2, 8, 128).transpose(0, 2, 1))
    m["og_at"] = A(inp["out_norm_g"][:, 256:768].reshape(2, 8, 64).transpose(0, 2, 1))
    m["w_out"] = inp["w_out"]
    m["w_router"] = inp["w_router"]
    m["rbias"] = A(np.broadcast_to(inp["router_bias"][None, :], (128, 16)))
    m["w_gate"] = inp["w_gate"]
    m["w_up"] = inp["w_up"]
    m["w_down"] = inp["w_down"]
    m["ident"] = np.eye(128, dtype=f)
    e127 = np.zeros((128, 128), f); e127[127, :] = 1
    m["e127"] = e127
    blk = np.zeros((128, 128), f); blk[:64, :64] = 1; blk[64:, 64:] = 1
    m["blk64"] = blk
    m["tri"] = np.triu(np.ones((128, 128), f))
    m["iota"] = A(np.broadcast_to(np.arange(TT + 1, dtype=f)[None, :], (128, TT + 1)))
    sel = np.zeros((16, 16, 128), f)
    for e in range(16):
        sel[e, e, :] = 1
    m["sel16"] = sel
    return {k: np.asarray(v, dtype=f) for k, v in m.items()}


_CACHE = {}


def kernel(**inputs):
    inp = {k: np.asarray(v) for k, v in inputs.items()}
    if "nc" not in _CACHE:
        _CACHE["nc"] = build_program()[0]
    nc = _CACHE["nc"]
    in_maps = [host_inputs(inp, b) for b in range(8)]
    res = run_bass_kernel_spmd(nc, in_maps, core_ids=list(range(8)))
    out = np.stack([np.asarray(r["out"]) for r in res.results], axis=0)
    return out.astype(np.float32)
```
